# Optimizing a Trainium2 kernel written in Bass

```python
import numpy as np
import jax
import jax.numpy as jnp
from jax import lax

D_MODEL = 1024
BATCH = 8
SEQ = 4096
DEPTH = 2

HEAD_DIM = 64
N_MIX_HEADS = D_MODEL // HEAD_DIM
MIX_WIDTH = N_MIX_HEADS * HEAD_DIM
NSA_HEADS = N_MIX_HEADS // 2
FOX_HEADS = N_MIX_HEADS // 4
SB_HEADS = N_MIX_HEADS - NSA_HEADS - FOX_HEADS
NSA_KV_GROUPS = 2
NSA_HEADS_PER_GROUP = NSA_HEADS // NSA_KV_GROUPS
NSA_CMP_LEN = 32
NSA_CMP_STRIDE = 16
NSA_CMP_HIDDEN = 128
NSA_SLC_LEN = 64
NSA_TOP_N = 16
NSA_WINDOW = 512
NSA_N_BRANCHES = 3
NSA_Q_BLOCK = 32
Q_BLOCK = 128
ROPE_THETA = 10000.0
RMS_EPS = 1e-6
PLE_DIM = 256
D_FF = -(-(8 * D_MODEL) // (3 * 256)) * 256
NEG_INF = -1e30
FORCE = 1e9
TINY = 1e-20

_NSA_KV = NSA_KV_GROUPS * HEAD_DIM
IN_SPLITS = (
    NSA_HEADS * HEAD_DIM,
    _NSA_KV, _NSA_KV,
    _NSA_KV, _NSA_KV,
    _NSA_KV, _NSA_KV,
    NSA_HEADS * NSA_N_BRANCHES,
    FOX_HEADS * HEAD_DIM, FOX_HEADS * HEAD_DIM, FOX_HEADS * HEAD_DIM,
    FOX_HEADS,
    SB_HEADS * HEAD_DIM, SB_HEADS * HEAD_DIM, SB_HEADS * HEAD_DIM,
)
IN_WIDTH = sum(IN_SPLITS)

kernel_name = "hybrid_nsa_fox_stickbreaking_parallel_heads"


def rms_norm(x, g):
    xf = x.astype(jnp.float32)
    y = xf * lax.rsqrt(jnp.mean(xf * xf, axis=-1, keepdims=True) + RMS_EPS)
    return (y * g.astype(jnp.float32)).astype(x.dtype)


def rope(x, positions):
    half = x.shape[-1] // 2
    inv_freq = ROPE_THETA ** (-jnp.arange(half, dtype=jnp.float32) / half)
    ang = positions.astype(jnp.float32)[..., None] * inv_freq
    cos = jnp.cos(ang)[:, :, None, :]
    sin = jnp.sin(ang)[:, :, None, :]
    xf = x.astype(jnp.float32)
    x1, x2 = xf[..., :half], xf[..., half:]
    return jnp.concatenate([x1 * cos - x2 * sin, x2 * cos + x1 * sin], axis=-1).astype(x.dtype)


def masked_softmax(logits, mask):
    l = jnp.where(mask, logits, NEG_INF)
    m = jnp.max(l, axis=-1, keepdims=True)
    e = jnp.where(mask, jnp.exp(l - m), 0.0)
    return e / jnp.maximum(jnp.sum(e, axis=-1, keepdims=True), TINY)


def compress_blocks(kv, pos_emb, w1, w2):
    b, s, g, d = kv.shape
    n_cmp = (s - NSA_CMP_LEN) // NSA_CMP_STRIDE + 1
    idx = np.arange(n_cmp)[:, None] * NSA_CMP_STRIDE + np.arange(NSA_CMP_LEN)[None, :]
    blocks = kv[:, idx] + pos_emb[:, None, :]
    blocks = blocks.transpose(0, 1, 3, 2, 4).reshape(b, n_cmp, g, NSA_CMP_LEN * d)
    return jax.nn.gelu(blocks @ w1) @ w2


def nsa_attention(q, k_cmp, v_cmp, k_slc, v_slc, k_win, v_win, gates,
                  pos_k, w1_k, w2_k, pos_v, w1_v, w2_v):
    b, s, h, d = q.shape
    g_, hpg, tq, win = NSA_KV_GROUPS, NSA_HEADS_PER_GROUP, NSA_Q_BLOCK, NSA_WINDOW
    scale = d ** -0.5
    kc = compress_blocks(k_cmp, pos_k, w1_k, w2_k)
    vc = compress_blocks(v_cmp, pos_v, w1_v, w2_v)
    n_cmp = kc.shape[1]
    cmp_start = np.arange(n_cmp) * NSA_CMP_STRIDE
    cmp_end = jnp.asarray(cmp_start + NSA_CMP_LEN - 1)
    n_blk = s // NSA_SLC_LEN
    n_sel = min(NSA_TOP_N, n_blk)
    blk_start = np.arange(n_blk) * NSA_SLC_LEN
    overlap = jnp.asarray(((cmp_start[:, None] < blk_start[None, :] + NSA_SLC_LEN)
                           & (cmp_start[:, None] + NSA_CMP_LEN > blk_start[None, :])).astype(np.float32))
    ks_blocks = k_slc.reshape(b, n_blk, NSA_SLC_LEN, g_, d).transpose(0, 3, 1, 2, 4)
    vs_blocks = v_slc.reshape(b, n_blk, NSA_SLC_LEN, g_, d).transpose(0, 3, 1, 2, 4)
    pad = ((0, 0), (win, 0), (0, 0), (0, 0))
    kw_pad = jnp.pad(k_win, pad)
    vw_pad = jnp.pad(v_win, pad)
    b_idx = jnp.arange(b)[:, None, None, None]
    g_idx = jnp.arange(g_)[None, :, None, None]
    blk_ids = jnp.arange(n_blk)
    win_off = jnp.arange(win + tq)
    slc_off = jnp.arange(NSA_SLC_LEN)

    def step(args):
        q_blk, g_blk, q0 = args
        t = q0 + jnp.arange(tq)
        qg = q_blk.reshape(b, tq, g_, hpg, d)
        s_c = jnp.einsum('btghd,bngd->bghtn', qg, kc).astype(jnp.float32) * scale
        p_c = masked_softmax(s_c, cmp_end[None, :] <= t[:, None])
        o_c = jnp.einsum('bghtn,bngd->btghd', p_c.astype(vc.dtype), vc)
        imp = jnp.einsum('bghtn,nj->bgtj', p_c, overlap)
        cur = t // NSA_SLC_LEN
        valid = blk_ids[None, :] <= cur[:, None]
        forced = ((blk_ids[None, :] == 0) | (blk_ids[None, :] == cur[:, None])
                  | (blk_ids[None, :] == cur[:, None] - 1))
        score = jnp.where(forced, FORCE, jnp.where(valid, imp, -FORCE))
        _, sel = lax.top_k(score, n_sel)
        ks = ks_blocks[b_idx, g_idx, sel]
        vs = vs_blocks[b_idx, g_idx, sel]
        s_s = jnp.einsum('btghd,bgtnkd->bghtnk', qg, ks).astype(jnp.float32) * scale
        kpos = sel[..., None] * NSA_SLC_LEN + slc_off
        mask_s = (kpos <= t[:, None, None])[:, :, None].reshape(b, g_, 1, tq, n_sel * NSA_SLC_LEN)
        p_s = masked_softmax(s_s.reshape(b, g_, hpg, tq, n_sel * NSA_SLC_LEN), mask_s)
        p_s = p_s.reshape(b, g_, hpg, tq, n_sel, NSA_SLC_LEN)
        o_s = jnp.einsum('bghtnk,bgtnkd->btghd', p_s.astype(vs.dtype), vs)
        kw = lax.dynamic_slice_in_dim(kw_pad, q0, win + tq, axis=1)
        vw = lax.dynamic_slice_in_dim(vw_pad, q0, win + tq, axis=1)
        s_pos = q0 - win + win_off
        diff = t[:, None] - s_pos[None, :]
        mask_w = (diff >= 0) & (diff < win) & (s_pos[None, :] >= 0)
        s_w = jnp.einsum('btghd,bkgd->bghtk', qg, kw).astype(jnp.float32) * scale
        p_w = masked_softmax(s_w, mask_w)
        o_w = jnp.einsum('bghtk,bkgd->btghd', p_w.astype(vw.dtype), vw)
        gt = g_blk.reshape(b, tq, g_, hpg, NSA_N_BRANCHES)
        o = gt[..., 0:1] * o_c + gt[..., 1:2] * o_s + gt[..., 2:3] * o_w
        return o.reshape(b, tq, h, d)

    nqb = s // tq
    q_b = q.reshape(b, nqb, tq, h, d).swapaxes(0, 1)
    g_b = gates.reshape(b, nqb, tq, h, NSA_N_BRANCHES).swapaxes(0, 1)
    starts = jnp.arange(nqb, dtype=jnp.int32) * tq
    out = lax.map(step, (q_b, g_b, starts))
    return out.swapaxes(0, 1).reshape(b, s, h, d)


def forgetting_attention(q, k, v, log_f):
    b, s, h, d = q.shape
    scale = d ** -0.5
    c = jnp.cumsum(log_f.astype(jnp.float32), axis=1).transpose(0, 2, 1)
    s_idx = jnp.arange(s)

    def step(args):
        q_blk, c_blk, q0 = args
        t = q0 + jnp.arange(Q_BLOCK)
        logits = (jnp.einsum('bthd,bshd->bhts', q_blk, k).astype(jnp.float32) * scale
                  + c_blk[..., None] - c[:, :, None, :])
        p = masked_softmax(logits, s_idx[None, :] <= t[:, None])
        return jnp.einsum('bhts,bshd->bthd', p.astype(v.dtype), v)

    nqb = s // Q_BLOCK
    q_b = q.reshape(b, nqb, Q_BLOCK, h, d).swapaxes(0, 1)
    c_b = c.reshape(b, h, nqb, Q_BLOCK).transpose(2, 0, 1, 3)
    starts = jnp.arange(nqb, dtype=jnp.int32) * Q_BLOCK
    out = lax.map(step, (q_b, c_b, starts))
    return out.swapaxes(0, 1).reshape(b, s, h, d)


def stick_breaking_attention(q, k, v):
    b, s, h, d = q.shape
    scale = d ** -0.5
    s_idx = jnp.arange(s)

    def step(args):
        q_blk, q0 = args
        t = q0 + jnp.arange(Q_BLOCK)
        z = jnp.einsum('bthd,bshd->bhts', q_blk, k).astype(jnp.float32) * scale
        mask = s_idx[None, :] < t[:, None]
        log_rest = jnp.where(mask, jax.nn.log_sigmoid(-z), 0.0)
        after = lax.cumsum(log_rest, axis=3, reverse=True) - log_rest
        a = jnp.where(mask, jnp.exp(jax.nn.log_sigmoid(z) + after), 0.0)
        return jnp.einsum('bhts,bshd->bthd', a.astype(v.dtype), v)

    nqb = s // Q_BLOCK
    q_b = q.reshape(b, nqb, Q_BLOCK, h, d).swapaxes(0, 1)
    starts = jnp.arange(nqb, dtype=jnp.int32) * Q_BLOCK
    out = lax.map(step, (q_b, starts))
    return out.swapaxes(0, 1).reshape(b, s, h, d)


def setup_inputs(seed: int = 0) -> dict:
    key = jax.random.key(seed)
    ks = jax.random.split(key, 24)
    f32 = jnp.float32
    L = DEPTH

    def nrm(k, shape, scale):
        return jax.random.normal(k, shape, f32) * scale

    cmp_in = NSA_CMP_LEN * HEAD_DIM
    return {
        "x": nrm(ks[0], (BATCH, SEQ, D_MODEL), 1.0),
        "p": nrm(ks[1], (DEPTH, BATCH, SEQ, PLE_DIM), 1.0),
        "positions": jnp.tile(jnp.arange(SEQ, dtype=jnp.int32)[None, :], (BATCH, 1)),
        "norm_mix": 1.0 + nrm(ks[2], (L, D_MODEL), 0.02),
        "w_in": nrm(ks[3], (L, D_MODEL, IN_WIDTH), D_MODEL ** -0.5),
        "b_nsa_gate": nrm(ks[4], (L, NSA_HEADS * NSA_N_BRANCHES), 0.1),
        "b_forget": 2.0 + nrm(ks[5], (L, FOX_HEADS), 0.5),
        "nsa_cmp_pos_k": nrm(ks[6], (L, NSA_CMP_LEN, HEAD_DIM), 0.1),
        "nsa_cmp_w1_k": nrm(ks[7], (L, cmp_in, NSA_CMP_HIDDEN), cmp_in ** -0.5),
        "nsa_cmp_w2_k": nrm(ks[8], (L, NSA_CMP_HIDDEN, HEAD_DIM), NSA_CMP_HIDDEN ** -0.5),
        "nsa_cmp_pos_v": nrm(ks[9], (L, NSA_CMP_LEN, HEAD_DIM), 0.1),
        "nsa_cmp_w1_v": nrm(ks[10], (L, cmp_in, NSA_CMP_HIDDEN), cmp_in ** -0.5),
        "nsa_cmp_w2_v": nrm(ks[11], (L, NSA_CMP_HIDDEN, HEAD_DIM), NSA_CMP_HIDDEN ** -0.5),
        "head_norm": 1.0 + nrm(ks[12], (L, MIX_WIDTH), 0.02),
        "w_out": nrm(ks[13], (L, MIX_WIDTH, D_MODEL), MIX_WIDTH ** -0.5),
        "norm_ffn": 1.0 + nrm(ks[14], (L, D_MODEL), 0.02),
        "w_ffn_gate": nrm(ks[15], (L, D_MODEL, D_FF), D_MODEL ** -0.5),
        "w_ffn_up": nrm(ks[16], (L, D_MODEL, D_FF), D_MODEL ** -0.5),
        "w_ffn_down": nrm(ks[17], (L, D_FF, D_MODEL), D_FF ** -0.5),
        "norm_ple": 1.0 + nrm(ks[18], (L, D_MODEL), 0.02),
        "w_ple_proj": nrm(ks[19], (L, PLE_DIM, D_MODEL), PLE_DIM ** -0.5),
        "w_ple_gate": nrm(ks[20], (L, D_MODEL, D_MODEL), D_MODEL ** -0.5),
        "norm_final": 1.0 + nrm(ks[21], (D_MODEL,), 0.02),
    }


def reference(x, p, positions, norm_mix, w_in, b_nsa_gate, b_forget,
              nsa_cmp_pos_k, nsa_cmp_w1_k, nsa_cmp_w2_k,
              nsa_cmp_pos_v, nsa_cmp_w1_v, nsa_cmp_w2_v,
              head_norm, w_out, norm_ffn, w_ffn_gate, w_ffn_up, w_ffn_down,
              norm_ple, w_ple_proj, w_ple_gate, norm_final):
    b, s, _ = x.shape
    offsets = [int(o) for o in np.cumsum(IN_SPLITS)[:-1]]

    def heads(a, n):
        return a.reshape(b, s, n, HEAD_DIM)

    h = x
    for i in range(DEPTH):
        hn = rms_norm(h, norm_mix[i])
        proj = hn @ w_in[i]
        (nq, nkc, nvc, nks, nvs, nkw, nvw, ngate,
         fq, fk, fv, ff, sq, sk, sv) = jnp.split(proj, offsets, axis=-1)
        nq = rope(heads(nq, NSA_HEADS), positions)
        nkc = rope(heads(nkc, NSA_KV_GROUPS), positions)
        nks = rope(heads(nks, NSA_KV_GROUPS), positions)
        nkw = rope(heads(nkw, NSA_KV_GROUPS), positions)
        gates = jax.nn.sigmoid(ngate.reshape(b, s, NSA_HEADS, NSA_N_BRANCHES)
                               + b_nsa_gate[i].reshape(NSA_HEADS, NSA_N_BRANCHES))
        o_nsa = nsa_attention(nq, nkc, heads(nvc, NSA_KV_GROUPS), nks, heads(nvs, NSA_KV_GROUPS),
                              nkw, heads(nvw, NSA_KV_GROUPS), gates,
                              nsa_cmp_pos_k[i], nsa_cmp_w1_k[i], nsa_cmp_w2_k[i],
                              nsa_cmp_pos_v[i], nsa_cmp_w1_v[i], nsa_cmp_w2_v[i])
        log_f = jax.nn.log_sigmoid((ff + b_forget[i]).astype(jnp.float32))
        o_fox = forgetting_attention(heads(fq, FOX_HEADS), heads(fk, FOX_HEADS),
                                     heads(fv, FOX_HEADS), log_f)
        o_sb = stick_breaking_attention(heads(sq, SB_HEADS), heads(sk, SB_HEADS), heads(sv, SB_HEADS))
        o = jnp.concatenate([o_nsa, o_fox, o_sb], axis=2)
        o = rms_norm(o, head_norm[i].reshape(N_MIX_HEADS, HEAD_DIM))
        h = h + o.reshape(b, s, MIX_WIDTH) @ w_out[i]
        hn = rms_norm(h, norm_ffn[i])
        h = h + (jax.nn.silu(hn @ w_ffn_gate[i]) * (hn @ w_ffn_up[i])) @ w_ffn_down[i]
        gate = jax.nn.sigmoid(rms_norm(h, norm_ple[i]) @ w_ple_gate[i])
        h = h + (p[i] @ w_ple_proj[i]) * gate
    return rms_norm(h, norm_final)
```

```python
import contextlib
import numpy as np
import ml_dtypes
import concourse.bass as bass
import concourse.mybir as mybir
from concourse.bass_utils import run_bass_kernel_spmd

F32 = mybir.dt.float32
BF16 = mybir.dt.bfloat16
I32 = mybir.dt.int32
AF = mybir.ActivationFunctionType
ALU = mybir.AluOpType
AX = mybir.AxisListType

S = 4096
D = 1024
L = 2
NTILE = 32
CH = 512
NCH = 8
DFF = 2816
NF = 22
BIG = 30000.0
WCOLS = 3740
COMPUTE = ("pe", "act", "dve", "pool", "sp")
import os as _os
NSA_STOP = int(_os.environ.get("NSA_STOP", "0"))
DMAQ = ("sp", "pool", "act")


def I(m, *a, **k):
    return lambda e: getattr(e, m)(*a, **k)


class Op:
    __slots__ = ("eng", "fn", "waits", "signal", "cnt", "dma_sem", "dma_val", "is_dma", "idx")

    def __init__(self, eng, fn, is_dma):
        self.eng = eng
        self.fn = fn
        self.waits = []
        self.signal = False
        self.cnt = None
        self.dma_sem = None
        self.dma_val = None
        self.is_dma = is_dma
        self.idx = None


class Prog:
    def __init__(self, nc, st, n_dma_sems=8):
        self.nc = nc
        self.lists = {e: [] for e in COMPUTE}
        self.last_w = {}
        self.readers = {}
        self.n_dma_sems = n_dma_sems
        self.dma_count = {q: 0 for q in DMAQ}
        self.csem = {e: st.enter_context(nc.semaphore("c_" + e)) for e in COMPUTE}
        self.dsem = {(q, j): st.enter_context(nc.semaphore("d_%s%d" % (q, j)))
                     for q in DMAQ for j in range(n_dma_sems)}
        self.cbase = {e: 0 for e in COMPUTE}
        self.gidx = {e: 0 for e in COMPUTE}
        self.barrier = {}
        self.dma_last = {}

    def _deps(self, reads, writes):
        deps = []
        for k in reads:
            w = self.last_w.get(k)
            if w is not None:
                deps.append(w)
        for k in writes:
            w = self.last_w.get(k)
            if w is not None:
                deps.append(w)
            deps.extend(self.readers.get(k, ()))
        return deps

    def _record(self, h, reads, writes):
        for k in reads:
            self.readers.setdefault(k, []).append(h)
        for k in writes:
            self.last_w[k] = h
            self.readers[k] = []

    def _attach(self, h, deps):
        best = {}
        for d in deps:
            if d is h or d.fn is None:
                continue
            if d.is_dma:
                key = ("d",) + d.dma_sem
                cur = best.get(key)
                if cur is None or d.dma_val > cur.dma_val:
                    best[key] = d
            else:
                if d.eng == "pe" and h.eng == "pe" and not h.is_dma:
                    continue
                cur = best.get(d.eng)
                if cur is None or d.idx > cur.idx:
                    best[d.eng] = d
        for d in best.values():
            d.signal = True
            h.waits.append(d)

    def op(self, eng, fn, reads=(), writes=(), extra=()):
        h = Op(eng, fn, False)
        self._attach(h, self._deps(reads, writes) + list(extra))
        h.idx = self.gidx[eng]
        self.gidx[eng] += 1
        self.lists[eng].append(h)
        self._record(h, reads, writes)
        return h

    def dma(self, q, fn, reads=(), writes=(), extra=()):
        h = Op(q, fn, True)
        self._attach(h, self._deps(reads, writes) + list(extra))
        i = self.dma_count[q]
        self.dma_count[q] += 1
        h.dma_sem = (q, i % self.n_dma_sems)
        h.dma_val = 16 * (i // self.n_dma_sems + 1)
        h.idx = self.gidx[q]
        self.gidx[q] += 1
        self.lists[q].append(h)
        self._record(h, reads, writes)
        self.dma_last[h.dma_sem] = h.dma_val
        return h

    def flush(self, final=False):
        nc = self.nc
        for e in COMPUTE:
            c = self.cbase[e]
            for h in self.lists[e]:
                if not h.is_dma and h.signal:
                    c += 1
                    h.cnt = c
        if final:
            pass
        barrier = dict(self.barrier)
        with nc.Block() as block:
            engs = {"pe": block.tensor, "act": block.scalar, "dve": block.vector,
                    "pool": block.gpsimd, "sp": block.sync}

            def make(ename):
                lst = self.lists[ename]

                def body(eng):
                    waited = {}
                    for key, val in barrier.items():
                        if val <= 0:
                            continue
                        sem = self.csem[key[1]] if key[0] == "c" else self.dsem[key[1:]]
                        eng.wait_ge(sem, val)
                        waited[key] = val
                    for h in lst:
                        for d in h.waits:
                            if d.is_dma:
                                key = ("d",) + d.dma_sem
                                sem = self.dsem[d.dma_sem]
                                val = d.dma_val
                            else:
                                key = ("c", d.eng)
                                sem = self.csem[d.eng]
                                val = d.cnt
                            if waited.get(key, 0) >= val:
                                continue
                            waited[key] = val
                            eng.wait_ge(sem, val)
                        if h.is_dma:
                            prev = h.dma_val - 16
                            key = ("d",) + h.dma_sem
                            if prev > 0 and waited.get(key, 0) < prev:
                                eng.wait_ge(self.dsem[h.dma_sem], prev)
                                waited[key] = prev
                            ins = h.fn(eng)
                            ins.then_inc(self.dsem[h.dma_sem], 16)
                        else:
                            ins = h.fn(eng)
                            if h.signal:
                                ins.then_inc(self.csem[ename], 1)
                    if final and ename == "sp":
                        for key, val in self._barrier_now().items():
                            if val > 0 and waited.get(key, 0) < val and key != ("c", "sp"):
                                sem = self.csem[key[1]] if key[0] == "c" else self.dsem[key[1:]]
                                eng.wait_ge(sem, val)
                return body

            for ename in ("sp", "pool", "act", "dve", "pe"):
                if self.lists[ename] or barrier or final:
                    engs[ename](make(ename))
        self.barrier = self._barrier_now()
        for e in COMPUTE:
            for h in self.lists[e]:
                h.fn = None
            self.lists[e] = []
        self.last_w = {}
        self.readers = {}

    def _barrier_now(self):
        b = {}
        for e in COMPUTE:
            c = self.cbase[e]
            for h in self.lists[e]:
                if h.cnt is not None and h.cnt > c:
                    c = h.cnt
            b[("c", e)] = c
        for k, v in self.dma_last.items():
            b[("d",) + k] = v
        return b

    def end_phase(self, final=False):
        for e in COMPUTE:
            for h in reversed(self.lists[e]):
                if not h.is_dma:
                    h.signal = True
                    break
        self.flush(final=final)
        for e in COMPUTE:
            self.cbase[e] = self.barrier[("c", e)]


def _win_cols():
    o = {}
    names = ["nq", "nkc", "nvc", "nks", "nvs", "nkw", "nvw", "ngate", "fq", "fk", "fv", "ff", "sq", "sk", "sv"]
    sizes = [512, 128, 128, 128, 128, 128, 128, 24, 256, 256, 256, 4, 256, 256, 256]
    off = 0
    for n, s in zip(names, sizes):
        o[n] = np.arange(off, off + s)
        off += s
    assert off == 2844

    def rot(c):
        c = c.reshape(-1, 2, 32)
        return c[:, ::-1, :].reshape(-1)

    ft = []
    for j in range(4):
        ft.append(np.concatenate([o["nq"][64 * j:64 * j + 64], o["nq"][64 * (4 + j):64 * (4 + j) + 64]]))
    ft += [o["nkc"], o["nks"], o["nkw"]]
    ft += [rot(c) for c in ft[:7]]
    ft.append(o["nvc"])
    for n in ("fq", "fk", "sq", "sk"):
        ft += [o[n][:128], o[n][128:]]
    cols = np.concatenate(ft + [o["ff"], o["nvs"], o["nvw"], o["fv"], o["sv"], o["ngate"]])
    assert cols.shape[0] == WCOLS
    return cols


def _consts():
    bf = ml_dtypes.bfloat16
    c = {}
    c["ident"] = np.eye(128, dtype=np.float32).astype(bf)
    s = np.arange(128)[:, None, None]
    r = np.arange(4)[None, :, None]
    t = np.arange(512)[None, None, :]
    sa = 128 * r + s
    c["m_gt"] = np.where(sa > t, -BIG, 0.0).astype(bf)
    c["m_ge"] = np.where(sa >= t, -BIG, 0.0).astype(bf)
    c["m_le"] = np.where(sa <= t, -BIG, 0.0).astype(bf)
    n = np.arange(128)[:, None, None] + 128 * np.arange(2)[None, :, None]
    tt = np.arange(S)[None, None, :]
    cm = np.where((16 * n + 31 > tt) | (n >= 255), -BIG, 0.0)
    c["m_cmp"] = cm.astype(bf)
    j = np.arange(64)[:, None, None]
    sb = np.arange(32)[None, :, None]
    ss = np.arange(128)[None, None, :]
    es = np.zeros((128, 32, 128), np.float32)
    es[0:64] = (j == 2 * sb + (ss >= 64))
    c["esel"] = es.astype(bf)
    nn = np.arange(256)
    cs = nn[:, None] * 16
    bs = np.arange(64)[None, :] * 64
    ov = ((cs < bs + 64) & (cs + 32 > bs) & (nn[:, None] < 255)).astype(np.float32)
    c["ovl"] = ov.reshape(2, 128, 64).transpose(1, 0, 2).astype(bf).copy()
    tq = np.arange(S)[:, None]
    jb = np.arange(64)[None, :]
    cur = tq // 64
    forced = (jb == 0) | (jb == cur) | (jb == cur - 1)
    valid = jb <= cur
    c["selbias"] = np.where(forced, 1e9, np.where(valid, 0.0, -1e9)).astype(np.float32)
    half = 32
    invf = (10000.0 ** (-np.arange(half, dtype=np.float32) / half)).astype(np.float32)
    rr = np.arange(128)
    c["invf"] = invf[rr % 32].reshape(128, 1).astype(np.float32)
    c["sgn"] = np.where((rr % 64) < 32, -1.0, 1.0).reshape(128, 1).astype(np.float32)
    jj = np.arange(128)
    c["ntri"] = np.where(jj[:, None] >= jj[None, :], -1.0, 0.0).astype(np.float32).astype(bf)
    c["nones"] = np.full((1, 128), -1.0, np.float32).astype(bf)
    c["onec"] = np.ones((128, 1), np.float32).astype(bf)
    return c


def _col8(v):
    return np.ascontiguousarray(v.reshape(8, 128).T)


def _prep_shared(inp):
    sh = {}
    cols = _win_cols()
    sh["w_in"] = np.ascontiguousarray(inp["w_in"][:, :, cols])
    for n in ("norm_mix", "norm_ffn", "norm_ple", "head_norm"):
        sh[n] = np.stack([_col8(inp[n][l]) for l in range(L)])
    sh["norm_final"] = inp["norm_final"].reshape(1, D)
    sh["b_gate"] = inp["b_nsa_gate"].reshape(L, 1, 24)
    sh["b_forget"] = inp["b_forget"].reshape(L, 4, 1)
    for kv in ("k", "v"):
        w1 = inp["nsa_cmp_w1_" + kv].reshape(L, 32, 64, 128).transpose(0, 2, 1, 3)
        sh["w1_" + kv] = np.ascontiguousarray(np.concatenate([w1, w1], axis=1).reshape(L, 128, 32 * 128))
        pt = inp["nsa_cmp_pos_" + kv].transpose(0, 2, 1)
        sh["pos_" + kv] = np.ascontiguousarray(np.concatenate([pt, pt], axis=1))
        sh["w2_" + kv] = inp["nsa_cmp_w2_" + kv]
    for n in ("w_out", "w_ffn_gate", "w_ffn_up", "w_ffn_down", "w_ple_proj", "w_ple_gate"):
        sh[n] = inp[n]
    sh.update(_consts())
    return sh


class Builder:
    def __init__(self, debug=(), nlayers=L, phases=None):
        self.debug = set(debug)
        self.nlayers = nlayers
        self.phases = phases
        self.nc = bass.Bass("TRN2", target_bir_lowering=False)
        self.dr = {}

    def din(self, name, shape, dt):
        self.dr[name] = self.nc.dram_tensor(name, list(shape), dt, kind="ExternalInput").ap()
        return self.dr[name]

    def dscr(self, name, shape, dt):
        kind = "ExternalOutput" if name in self.debug else "Internal"
        self.dr[name] = self.nc.dram_tensor(name, list(shape), dt, kind=kind).ap()
        return self.dr[name]

    def want(self, ph):
        return self.phases is None or ph in self.phases

    def build(self):
        nc = self.nc
        din, dscr = self.din, self.dscr
        din("x", [S, D], F32)
        din("p", [L, S, 256], F32)
        din("pos", [1, S], I32)
        din("w_in", [L, D, WCOLS], F32)
        for n in ("norm_mix", "norm_ffn", "norm_ple", "head_norm"):
            din(n, [L, 128, 8], F32)
        din("norm_final", [1, D], F32)
        din("b_gate", [L, 1, 24], F32)
        din("b_forget", [L, 4, 1], F32)
        for kv in ("k", "v"):
            din("w1_" + kv, [L, 128, 4096], F32)
            din("pos_" + kv, [L, 128, 32], F32)
            din("w2_" + kv, [L, 128, 64], F32)
        din("w_out", [L, D, D], F32)
        din("w_ffn_gate", [L, D, DFF], F32)
        din("w_ffn_up", [L, D, DFF], F32)
        din("w_ffn_down", [L, DFF, D], F32)
        din("w_ple_proj", [L, 256, D], F32)
        din("w_ple_gate", [L, D, D], F32)
        din("ident", [128, 128], BF16)
        for n in ("m_gt", "m_ge", "m_le"):
            din(n, [128, 4, 512], BF16)
        din("m_cmp", [128, 2, S], BF16)
        din("esel", [128, 32, 128], BF16)
        din("ovl", [128, 2, 64], BF16)
        din("selbias", [S, 64], F32)
        din("invf", [128, 1], F32)
        din("sgn", [128, 1], F32)
        din("ntri", [128, 128], BF16)
        din("nones", [1, 128], BF16)
        din("onec", [128, 1], BF16)
        self.out = nc.dram_tensor("out", [S, D], F32, kind="ExternalOutput").ap()
        dscr("hS", [S, D], F32)
        dscr("cosS", [128, S], F32)
        dscr("sinS", [128, S], F32)
        dscr("FT", [16, 128, S], BF16)
        dscr("cT", [4, S], F32)
        dscr("VA", [S, 8, 65], BF16)
        dscr("SV", [S, 256], BF16)
        dscr("GT", [S, 24], F32)
        dscr("kcT", [128, 256], BF16)
        dscr("VC", [128, 2, 2, 65], BF16)
        dscr("oS", [S, D], F32)
        dscr("WGU", [NF, 128, 2, 8, 128], BF16)
        if "hmix" in self.debug:
            dscr("hmix", [S, D], F32)

        with contextlib.ExitStack() as st:
            self.P = Prog(nc, st)
            self.ps = [st.enter_context(nc.psum_tensor("ps%d" % i, [128, 512], F32)) for i in range(8)]
            self.pt = self.ps[7][:, :].bitcast(BF16)
            self.ps_i = 0
            if self.want("T"):
                self.phase_tables()
            for l in range(self.nlayers):
                if self.want("A"):
                    self.phase_A(l)
                if self.want("B"):
                    self.phase_B(l)
                if self.want("N"):
                    self.phase_nsa(l)
                if self.want("F"):
                    self.phase_fox(l)
                if self.want("SB"):
                    self.phase_sb(l)
                if self.want("D"):
                    self.phase_D(l, last=(l == self.nlayers - 1))
            self.P.op("sp", I("nop"))
            self.P.end_phase(final=True)
        return nc

    def defer(self, fn, *a):
        if not hasattr(self, "_pend"):
            self._pend = []
        self._pend.append((fn, a))

    def run_deferred(self):
        pend = getattr(self, "_pend", [])
        self._pend = []
        for fn, a in pend:
            fn(*a)

    def uname(self, n):
        self._un = getattr(self, "_un", 0) + 1
        return "%s_u%d" % (n, self._un)

    def psn(self):
        i = self.ps_i
        self.ps_i = (i + 1) % 7
        return i

    def phase_tables(self):
        nc, P, dr = self.nc, self.P, self.dr
        with contextlib.ExitStack() as st:
            T = lambda n, sh, dt: st.enter_context(nc.sbuf_tensor(self.uname(n), sh, dt))
            posi = T("t_posi", [128, S], I32)
            ang = T("t_ang", [128, S], F32)
            kk = T("t_kk", [128, S], F32)
            rr = T("t_r", [128, S], F32)
            oo = T("t_o", [128, S], F32)
            invf = T("t_invf", [128, 1], F32)
            sgn = T("t_sgn", [128, 1], F32)
            hpi = T("t_hpi", [128, 1], F32)
            P.dma("sp", I("dma_start", out=posi[:], in_=dr["pos"].to_broadcast([128, S])), writes=["posi"])
            P.dma("sp", I("dma_start", out=invf[:], in_=dr["invf"]), writes=["invf"])
            P.dma("sp", I("dma_start", out=sgn[:], in_=dr["sgn"]), writes=["sgn"])
            P.op("pool", I("memset", hpi[:], float(np.pi / 2)), writes=["hpi"])
            P.op("dve", I("tensor_copy", out=ang[:], in_=posi[:]), reads=["posi"], writes=["ang"])
            P.op("dve", I("tensor_scalar", out=ang[:], in0=ang[:], scalar1=invf[:, 0:1], scalar2=None, op0=ALU.mult),
                 reads=["ang", "invf"], writes=["ang"])
            MAGIC = 12582912.0
            P.op("dve", I("tensor_scalar", out=kk[:], in0=ang[:], scalar1=float(1.0 / (2 * np.pi)), scalar2=MAGIC,
                                                   op0=ALU.mult, op1=ALU.add), reads=["ang"], writes=["kk"])
            P.op("dve", I("tensor_scalar", out=kk[:], in0=kk[:], scalar1=-MAGIC, scalar2=None, op0=ALU.add),
                 reads=["kk"], writes=["kk"])
            C1 = 6.28125
            C2 = float(np.float32(2 * np.pi - C1))
            P.op("dve", I("scalar_tensor_tensor", out=rr[:], in0=kk[:], scalar=-C1, in1=ang[:], op0=ALU.mult, op1=ALU.add),
                 reads=["kk", "ang"], writes=["rr"])
            P.op("dve", I("scalar_tensor_tensor", out=rr[:], in0=kk[:], scalar=-C2, in1=rr[:], op0=ALU.mult, op1=ALU.add),
                 reads=["kk", "rr"], writes=["rr"])
            PL = 3.1415925
            P.op("dve", I("tensor_scalar", out=rr[:], in0=rr[:], scalar1=-PL, scalar2=PL, op0=ALU.max, op1=ALU.min),
                 reads=["rr"], writes=["rr"])
            P.op("act", I("activation", out=oo[:], in_=rr[:], func=AF.Sin), reads=["rr"], writes=["oo"])
            P.op("dve", I("tensor_scalar", out=oo[:], in0=oo[:], scalar1=sgn[:, 0:1], scalar2=None, op0=ALU.mult),
                 reads=["oo", "sgn"], writes=["oo"])
            P.dma("sp", I("dma_start", out=dr["sinS"], in_=oo[:]), reads=["oo"], writes=["sinS"])
            P.op("dve", I("scalar_tensor_tensor", out=kk[:], in0=rr[:], scalar=-1.0, in1=rr[:], op0=ALU.mult, op1=ALU.max),
                 reads=["rr"], writes=["kk"])
            P.op("act", I("activation", out=ang[:], in_=kk[:], func=AF.Sin, bias=hpi[:, 0:1], scale=-1.0),
                 reads=["kk", "hpi", "ang"], writes=["ang"])
            P.dma("sp", I("dma_start", out=dr["cosS"], in_=ang[:]), reads=["ang"], writes=["cosS"])
            P.end_phase()

    def rms_tile(self, T4, i, hx, hxk, jk, ssk, rsk, hn, hnk):
        P = self.P
        junk, ss, rs = T4["junk"], T4["ss"], T4["rs"]
        P.op("act", I("activation", out=junk[:], in_=hx[:], func=AF.Square, accum_out=ss[:]),
             reads=[hxk], writes=[jk, ssk])
        P.op("dve", I("tensor_scalar", out=rs[:], in0=ss[:], scalar1=1.0 / D, scalar2=1e-6, op0=ALU.mult, op1=ALU.add),
             reads=[ssk], writes=[rsk])
        P.op("act", I("sqrt", out=rs[:], in_=rs[:]), reads=[rsk], writes=[rsk])
        P.op("dve", I("reciprocal", out=rs[:], in_=rs[:]), reads=[rsk], writes=[rsk])
        P.op("dve", I("tensor_scalar", out=hn[:], in0=hx[:], scalar1=rs[:, 0:1], scalar2=None, op0=ALU.mult),
             reads=[hxk, rsk], writes=[hnk])

    def transpose8(self, src, srck, dst_ap, dstk, ident, nblk=8, eng="dve"):
        P = self.P
        pt = self.pt
        for c in range(nblk):
            P.op("pe", I("transpose", out=pt[:, c * 128:(c + 1) * 128], in_=src[:, c * 128:(c + 1) * 128],
                                                  identity=ident[:]), reads=[srck, "ident"], writes=[("ps", 7)])
        view = pt[:, 0:nblk * 128].rearrange("p (c t) -> p c t", t=128)
        if eng == "dve":
            P.op("dve", I("tensor_copy", out=dst_ap, in_=view), reads=[("ps", 7)], writes=[dstk])
        else:
            P.op("act", I("copy", out=dst_ap, in_=view), reads=[("ps", 7)], writes=[dstk])

    def load_weight_bf(self, dst_ap, src_ap, stage, stagek, dstk, scale_ap=None, scalek=None, eng="dve", q="sp"):
        P = self.P
        P.dma(q, I("dma_start", out=stage, in_=src_ap), writes=[stagek])
        en = "dve" if eng == "dve" else "pool"
        if scale_ap is not None:
            P.op(en, I("tensor_scalar", out=dst_ap, in0=stage, scalar1=scale_ap, scalar2=None, op0=ALU.mult),
                 reads=[stagek, scalek], writes=[dstk])
        else:
            P.op(en, I("tensor_copy", out=dst_ap, in_=stage), reads=[stagek], writes=[dstk])

    def phase_A(self, l):
        nc, P, dr = self.nc, self.P, self.dr
        hsrc = dr["x"] if l == 0 else dr["hS"]
        with contextlib.ExitStack() as st:
            T = lambda n, sh, dt: st.enter_context(nc.sbuf_tensor(self.uname(n), sh, dt))
            W = T("a_W", [128, 8, WCOLS], BF16)
            stage = [T("a_stage%d" % i, [128, WCOLS], F32) for i in range(2)]
            cosT = T("a_cos", [128, S], F32)
            sinT = T("a_sin", [128, S], F32)
            gcol = T("a_gcol", [128, 8], F32)
            ident = T("a_ident", [128, 128], BF16)
            bgate = T("a_bgate", [128, 24], F32)
            negb = T("a_negb", [4, 1], F32)
            ones4 = T("a_ones4", [4, CH], F32)
            cc = T("a_cc", [4, S], F32)
            hx = [T("a_hx%d" % i, [128, D], F32) for i in range(2)]
            T4 = [dict(junk=T("a_junk%d" % i, [128, D], BF16), ss=T("a_ss%d" % i, [128, 1], F32),
                       rs=T("a_rs%d" % i, [128, 1], F32)) for i in range(2)]
            hn = [T("a_hn%d" % i, [128, D], BF16) for i in range(2)]
            hnT = [T("a_hnT%d" % i, [128, 8, CH], BF16) for i in range(2)]
            t1 = [T("a_t1%d" % i, [128, CH], F32) for i in range(2)]
            t2 = [T("a_t2%d" % i, [128, CH], F32) for i in range(2)]
            ob = [T("a_ob%d" % i, [128, CH], BF16) for i in range(4)]
            va = [T("a_va%d" % i, [128, 8, 65], BF16) for i in range(2)]
            svt = [T("a_sv%d" % i, [128, 256], BF16) for i in range(2)]
            gt = [T("a_gt%d" % i, [128, 24], F32) for i in range(2)]
            e4 = T("a_e4", [4, CH], F32)
            sp4 = T("a_sp4", [4, CH], F32)

            P.dma("sp", I("dma_start", out=gcol[:], in_=dr["norm_mix"][l]), writes=["gcol"])
            P.dma("sp", I("dma_start", out=ident[:], in_=dr["ident"]), writes=["ident"])
            P.dma("sp", I("dma_start", out=cosT[:], in_=dr["cosS"]), writes=["cosT"])
            P.dma("sp", I("dma_start", out=sinT[:], in_=dr["sinS"]), writes=["sinT"])
            P.dma("sp", I("dma_start", out=bgate[:], in_=dr["b_gate"][l].to_broadcast([128, 24])), writes=["bgate"])
            P.dma("sp", I("dma_start", out=negb[:], in_=dr["b_forget"][l]), writes=["negb"])
            P.op("dve", I("tensor_scalar", out=negb[:], in0=negb[:], scalar1=-1.0, scalar2=None, op0=ALU.mult),
                 reads=["negb"], writes=["negb"])
            P.op("pool", I("memset", ones4[:], 1.0), writes=["ones4"])
            for i in range(2):
                P.op("pool", I("memset", va[i][:], 1.0), writes=[("va", i)])
            for k in range(8):
                self.load_weight_bf(W[:, k, :], dr["w_in"][l, k * 128:(k + 1) * 128, :], stage[k % 2][:], ("stage", k % 2),
                                    ("W", k), scale_ap=gcol[:, k:k + 1], scalek="gcol", eng=("dve" if k % 2 == 0 else "pool"),
                                    q=("sp" if k % 2 == 0 else "act"))
            Wk = [("W", k) for k in range(8)]
            obi = 0
            for c in range(NCH):
                hb = c % 2
                csl = slice(c * CH, (c + 1) * CH)
                for i in range(4):
                    ti = c * 4 + i
                    b = ti % 2
                    P.dma("sp", I("dma_start", out=hx[b][:], in_=hsrc[ti * 128:(ti + 1) * 128, :]),
                          writes=[("hx", b)])
                    self.rms_tile(T4[b], b, hx[b], ("hx", b), ("junk", b), ("ss", b), ("rs", b), hn[b], ("hn", b))
                    self.transpose8(hn[b], ("hn", b), hnT[hb][:, :, i * 128:(i + 1) * 128], ("hnT", hb, i), ident,
                                    eng=("dve" if i % 2 == 0 else "act"))
                hk = [("hnT", hb, i) for i in range(4)]

                def fm_matmul(pi, col0, ncols=128):
                    for k in range(8):
                        P.op("pe", I("matmul", self.ps[pi][0:ncols, :], lhsT=W[:, k, col0:col0 + ncols],
                                                          rhs=hnT[hb][:, k, :], start=(k == 0), stop=(k == 7)),
                             reads=hk + Wk, writes=[("ps", pi)])
                for ft in range(7):
                    pa, pb = self.psn(), self.psn()
                    fm_matmul(pa, ft * 128)
                    fm_matmul(pb, (7 + ft) * 128)
                    tb = ft % 2
                    P.op("dve", I("tensor_tensor", out=t1[tb][:], in0=self.ps[pa][:], in1=cosT[:, csl], op=ALU.mult),
                         reads=[("ps", pa), "cosT"], writes=[("t1", tb)])
                    P.op("dve", I("tensor_tensor", out=t2[tb][:], in0=self.ps[pb][:], in1=sinT[:, csl], op=ALU.mult),
                         reads=[("ps", pb), "sinT"], writes=[("t2", tb)])
                    o = obi % 4
                    obi += 1
                    P.op("pool", I("tensor_tensor", out=ob[o][:], in0=t1[tb][:], in1=t2[tb][:], op=ALU.add),
                         reads=[("t1", tb), ("t2", tb)], writes=[("ob", o)])
                    P.dma("sp", I("dma_start", out=dr["FT"][ft, :, csl], in_=ob[o][:]),
                          reads=[("ob", o)], writes=[("FT", ft, c)])
                for ft in range(14, 23):
                    pa = self.psn()
                    fm_matmul(pa, ft * 128)
                    o = obi % 4
                    obi += 1
                    sc = 0.125 if ft in (15, 16, 19, 20) else 1.0
                    P.op("act", I("activation", out=ob[o][:], in_=self.ps[pa][:], func=AF.Copy, scale=sc),
                         reads=[("ps", pa)], writes=[("ob", o)])
                    P.dma("sp", I("dma_start", out=dr["FT"][ft - 7, :, csl], in_=ob[o][:]),
                          reads=[("ob", o)], writes=[("FT", ft, c)])
                pa = self.psn()
                fm_matmul(pa, 23 * 128, ncols=4)
                P.op("act", I("activation", out=e4[:], in_=self.ps[pa][0:4, :], func=AF.Exp, bias=negb[:, 0:1], scale=-1.0),
                     reads=[("ps", pa), "negb"], writes=["e4"])
                P.op("act", I("activation", out=sp4[:], in_=e4[:], func=AF.Ln, bias=1.0, scale=1.0), reads=["e4"], writes=["sp4"])
                if c == 0:
                    P.op("dve", I("tensor_tensor_scan", out=cc[:, csl], data0=ones4[:], data1=sp4[:], initial=0.0,
                                                                op0=ALU.mult, op1=ALU.subtract), reads=["sp4", "ones4"], writes=["cc"])
                else:
                    P.op("dve", I("tensor_tensor_scan", out=cc[:, csl], data0=ones4[:], data1=sp4[:],
                                                                     initial=cc[:, c * CH - 1:c * CH],
                                                                     op0=ALU.mult, op1=ALU.subtract), reads=["sp4", "ones4", "cc"], writes=["cc"])
                c0 = 23 * 128 + 4
                for i in range(4):
                    ti = c * 4 + i
                    b = ti % 2
                    pa, pb = self.psn(), self.psn()
                    for k in range(8):
                        P.op("pe", I("matmul", self.ps[pa][:, 0:512], lhsT=hnT[hb][:, k, i * 128:(i + 1) * 128],
                                                                    rhs=W[:, k, c0:c0 + 512], start=(k == 0), stop=(k == 7)),
                             reads=hk + Wk, writes=[("ps", pa)])
                    for k in range(8):
                        P.op("pe", I("matmul", self.ps[pb][:, 0:280], lhsT=hnT[hb][:, k, i * 128:(i + 1) * 128],
                                                                    rhs=W[:, k, c0 + 512:c0 + 792], start=(k == 0), stop=(k == 7)),
                             reads=hk + Wk, writes=[("ps", pb)])
                    P.op("act", I("copy", out=va[b][:, :, 0:64], in_=self.ps[pa][:, 0:512].rearrange("p (g d) -> p g d", d=64)),
                         reads=[("ps", pa)], writes=[("va", b)])
                    P.op("dve", I("tensor_copy", out=svt[b][:], in_=self.ps[pb][:, 0:256]),
                         reads=[("ps", pb)], writes=[("svt", b)])
                    P.op("dve", I("tensor_tensor", out=gt[b][:], in0=self.ps[pb][:, 256:280], in1=bgate[:], op=ALU.add),
                         reads=[("ps", pb), "bgate"], writes=[("gt", b)])
                    P.op("act", I("activation", out=gt[b][:], in_=gt[b][:], func=AF.Sigmoid), reads=[("gt", b)], writes=[("gt", b)])
                    rows = slice(ti * 128, (ti + 1) * 128)
                    P.dma("sp", I("dma_start", out=dr["VA"][rows], in_=va[b][:]), reads=[("va", b)], writes=[("VA", ti)])
                    P.dma("sp", I("dma_start", out=dr["SV"][rows], in_=svt[b][:]), reads=[("svt", b)], writes=[("SV", ti)])
                    P.dma("sp", I("dma_start", out=dr["GT"][rows], in_=gt[b][:]), reads=[("gt", b)], writes=[("GT", ti)])
            P.dma("sp", I("dma_start", out=dr["cT"], in_=cc[:]), reads=["cc"], writes=["cT"])
            P.end_phase()

    def phase_B(self, l):
        nc, P, dr = self.nc, self.P, self.dr
        with contextlib.ExitStack() as st:
            T = lambda n, sh, dt: st.enter_context(nc.sbuf_tensor(self.uname(n), sh, dt))
            kvT = {"k": T("b_kT", [128, S], BF16), "v": T("b_vT", [128, S], BF16)}
            stage = T("b_stage", [128, 4096], F32)
            W1 = {kv: T("b_w1" + kv, [128, 32, 128], BF16) for kv in "kv"}
            posT = {kv: T("b_pos" + kv, [128, 32], BF16) for kv in "kv"}
            W2 = {kv: T("b_w2" + kv, [128, 64], BF16) for kv in "kv"}
            st32 = T("b_st32", [128, 32], F32)
            st64 = T("b_st64", [128, 64], F32)
            bias = T("b_bias", [128, 1], F32)
            xs = T("b_xs", [128, 255], F32)
            x2 = T("b_x2", [128, 255], F32)
            sg = T("b_sg", [128, 255], F32)
            gl = T("b_gl", [128, 256], BF16)
            kc = T("b_kc", [128, 256], BF16)
            vc = T("b_vc", [128, 2, 2, 65], BF16)
            P.dma("sp", I("dma_start", out=kvT["k"][:], in_=dr["FT"][4]), writes=["kT"])
            P.dma("sp", I("dma_start", out=kvT["v"][:], in_=dr["FT"][7]), writes=["vT"])
            P.op("pool", I("memset", vc[:], 1.0), writes=["vc"])
            P.op("pool", I("memset", gl[:], 0.0), writes=["gl"])
            for kv in "kv":
                self.load_weight_bf(W1[kv][:].rearrange("p l h -> p (l h)"), dr["w1_" + kv][l], stage[:], "stage", "W1" + kv)
                self.load_weight_bf(posT[kv][:], dr["pos_" + kv][l], st32[:], "st32", "pos" + kv)
                self.load_weight_bf(W2[kv][:], dr["w2_" + kv][l], st64[:], "st64", "W2" + kv)
            for kv in "kv":
                pb = self.psn()
                for ll in range(32):
                    P.op("pe", I("matmul", self.ps[pb][:, 0:1], lhsT=W1[kv][0:64, ll, :], rhs=posT[kv][0:64, ll:ll + 1],
                                                                start=(ll == 0), stop=(ll == 31)),
                         reads=["W1" + kv, "pos" + kv], writes=[("ps", pb)])
                P.op("dve", I("tensor_copy", out=bias[:], in_=self.ps[pb][:, 0:1]), reads=[("ps", pb)], writes=["bias"])
                for g in range(2):
                    gs = slice(64 * g, 64 * g + 64)
                    pa = self.psn()
                    for ll in range(32):
                        P.op("pe", I("matmul",
                            self.ps[pa][:, 0:255], lhsT=W1[kv][gs, ll, :], rhs=kvT[kv][gs, ll:ll + 16 * 254 + 1:16],
                            start=(ll == 0), stop=(ll == 31)), reads=["W1" + kv, kv + "T"], writes=[("ps", pa)])
                    P.op("act", I("activation", out=xs[:], in_=self.ps[pa][:, 0:255], func=AF.Identity, bias=bias[:, 0:1], scale=1.0),
                         reads=[("ps", pa), "bias"], writes=["xs"])
                    P.op("dve", I("tensor_tensor", out=x2[:], in0=xs[:], in1=xs[:], op=ALU.mult), reads=["xs"], writes=["x2"])
                    P.op("dve", I("tensor_scalar", out=x2[:], in0=x2[:], scalar1=0.044715, scalar2=1.0, op0=ALU.mult, op1=ALU.add),
                         reads=["x2"], writes=["x2"])
                    P.op("dve", I("tensor_tensor", out=x2[:], in0=x2[:], in1=xs[:], op=ALU.mult), reads=["x2", "xs"], writes=["x2"])
                    P.op("act", I("activation", out=sg[:], in_=x2[:], func=AF.Sigmoid, scale=1.5957691216057308),
                         reads=["x2"], writes=["sg"])
                    P.op("dve", I("tensor_tensor", out=gl[:, 0:255], in0=xs[:], in1=sg[:], op=ALU.mult), reads=["xs", "sg"], writes=["gl"])
                    if kv == "k":
                        po = self.psn()
                        P.op("pe", I("matmul", self.ps[po][gs, 0:256], lhsT=W2["k"][:, :], rhs=gl[:, :], start=True, stop=True),
                             reads=["W2k", "gl"], writes=[("ps", po)])
                        P.op("dve", I("tensor_copy", out=kc[gs, :], in_=self.ps[po][gs, 0:256]),
                             reads=[("ps", po)], writes=[("kc", g)])
                    else:
                        for nb in range(2):
                            po = self.psn()
                            P.op("pe", I("matmul", self.ps[po][:, 0:64], lhsT=gl[:, nb * 128:(nb + 1) * 128], rhs=W2["v"][:, :],
                                                                     start=True, stop=True), reads=["W2v", "gl"], writes=[("ps", po)])
                            P.op("dve", I("tensor_copy", out=vc[:, g, nb, 0:64], in_=self.ps[po][:, 0:64]),
                                 reads=[("ps", po)], writes=["vc"])
            P.dma("sp", I("dma_start", out=dr["kcT"], in_=kc[:]), reads=[("kc", 0), ("kc", 1)], writes=["kcT"])
            P.dma("sp", I("dma_start", out=dr["VC"], in_=vc[:]), reads=["vc"], writes=["VC"])
            P.end_phase()

    def phase_fox(self, l):
        nc, P, dr, ps = self.nc, self.P, self.dr, self.ps
        if "cH" not in dr:
            self.dscr("cH", [4, 3, S], BF16)
        with contextlib.ExitStack() as st:
            T = lambda n, sh, dt: st.enter_context(nc.sbuf_tensor(self.uname(n), sh, dt))
            QaT = T("f_QaT", [70, 4, S], BF16)
            KaT = T("f_KaT", [70, 4, S], BF16)
            V = T("f_V", [128, 32, 4, 65], BF16)
            c4 = T("f_c4", [4, S], F32)
            rr = T("f_rr", [4, S], F32)
            H = T("f_H", [4, 3, S], BF16)
            mgt = T("f_mgt", [128, 4, 512], BF16)
            ident = T("f_ident", [128, 128], BF16)
            Pt = [T("f_P%d" % i, [128, CH], BF16) for i in range(3)]
            rs = [T("f_rs%d" % i, [128, 1], F32) for i in range(4)]
            of = [T("f_of%d" % i, [128, 4, 64], F32) for i in range(8)]
            P.dma("sp", I("dma_start", out=mgt[:], in_=dr["m_gt"]), writes=["mgt"])
            P.dma("sp", I("dma_start", out=ident[:], in_=dr["ident"]), writes=["ident"])
            P.dma("sp", I("dma_start", out=c4[:], in_=dr["cT"]), writes=["c4"])
            for q8 in range(8):
                P.dma("sp", I("dma_start", out=V[:, q8 * 4:(q8 + 1) * 4, :, :],
                              in_=dr["VA"][q8 * 512:(q8 + 1) * 512, 4:8, :].rearrange("(sb p) h c -> p sb h c", p=128)), writes=["V"])
            P.op("pool", I("memset", QaT[64:70, :, :], -1.0), writes=["Qaug"])
            P.op("pool", I("memset", KaT[64:70, :, :], 1.0), writes=["Kaug"])
            for hh in range(4):
                src_q = dr["FT"][8 + hh // 2, (hh % 2) * 64:(hh % 2) * 64 + 64, :]
                src_k = dr["FT"][10 + hh // 2, (hh % 2) * 64:(hh % 2) * 64 + 64, :]
                P.dma("sp", I("dma_start", out=QaT[0:64, hh, :], in_=src_q), writes=[("Q", hh)])
                P.dma("act", I("dma_start", out=KaT[0:64, hh, :], in_=src_k), writes=[("K", hh)])
            P.op("dve", I("tensor_copy", out=H[:, 0, :], in_=c4[:]), reads=["c4"], writes=["H0"])
            P.op("dve", I("tensor_tensor", out=rr[:], in0=c4[:], in1=H[:, 0, :], op=ALU.subtract), reads=["c4", "H0"], writes=["rr"])
            P.op("dve", I("tensor_copy", out=H[:, 1, :], in_=rr[:]), reads=["rr"], writes=["H1"])
            P.op("dve", I("tensor_tensor", out=rr[:], in0=rr[:], in1=H[:, 1, :], op=ALU.subtract), reads=["rr", "H1"], writes=["rr"])
            P.op("dve", I("tensor_copy", out=H[:, 2, :], in_=rr[:]), reads=["rr"], writes=["H2"])
            P.dma("sp", I("dma_start", out=dr["cH"], in_=H[:]), reads=["H0", "H1", "H2"], writes=["cH"])
            for hh in range(4):
                P.dma("sp", I("dma_start", out=QaT[64:67, hh, :], in_=dr["cH"][hh]), reads=["cH", "Qaug"], writes=[("Qa", hh)])
                P.dma("sp", I("dma_start", out=KaT[67:70, hh, :], in_=dr["cH"][hh]), reads=["cH", "Kaug"], writes=[("Ka", hh)])
            sti = 0
            pi = 0
            for qc in range(NCH):
                csl = slice(qc * CH, (qc + 1) * CH)
                for hh in range(4):
                    qk = [("Q", hh), ("Qa", hh), ("K", hh), ("Ka", hh), "Qaug", "Kaug"]
                    for sb in range(4 * qc + 4):
                        diag = sb >= 4 * qc
                        b = sti % 3
                        sti += 1
                        P.op("pe", I("matmul", ps[b][:, :], lhsT=KaT[0:70, hh, sb * 128:(sb + 1) * 128], rhs=QaT[0:70, hh, csl],
                                     start=True, stop=not diag), reads=qk, writes=[("ps", b)])
                        if diag:
                            P.op("pe", I("matmul", ps[b][:, :], lhsT=ident[:, :], rhs=mgt[:, sb - 4 * qc, :], start=False, stop=True),
                                 reads=["ident", "mgt"], writes=[("ps", b)])
                        pb = pi % 3
                        pi += 1
                        P.op("act", I("activation", out=Pt[pb][:], in_=ps[b][:, :], func=AF.Exp), reads=[("ps", b)], writes=[("P", pb)])
                        def back(sb, pb, hh, qc):
                            for ti in range(4):
                                if sb <= 4 * qc + ti:
                                    P.op("pe", I("matmul", ps[3 + ti][:, 0:65], lhsT=Pt[pb][:, ti * 128:(ti + 1) * 128], rhs=V[:, sb, hh, :],
                                                 start=(sb == 0), stop=(sb == 4 * qc + ti)), reads=[("P", pb), "V"], writes=[("ps", 3 + ti)])
                        self.run_deferred()
                        self.defer(back, sb, pb, hh, qc)

                    def fin(hh, qc):
                        for ti in range(4):
                            P.op("dve", I("tensor_scalar", out=rs[ti][:], in0=ps[3 + ti][:, 64:65], scalar1=1e-20, scalar2=None, op0=ALU.max),
                                 reads=[("ps", 3 + ti)], writes=[("rs", ti)])
                        for ti in range(4):
                            P.op("dve", I("reciprocal", out=rs[ti][:], in_=rs[ti][:]), reads=[("rs", ti)], writes=[("rs", ti)])
                        for ti in range(4):
                            o = (qc % 2) * 4 + ti
                            P.op("dve", I("tensor_scalar", out=of[o][:, hh, :], in0=ps[3 + ti][:, 0:64], scalar1=rs[ti][:, 0:1], scalar2=None, op0=ALU.mult),
                                 reads=[("ps", 3 + ti), ("rs", ti)], writes=[("of", o, hh)])
                    self.defer(fin, hh, qc)

                def store(qc):
                    for ti in range(4):
                        o = (qc % 2) * 4 + ti
                        rows = slice((qc * 4 + ti) * 128, (qc * 4 + ti + 1) * 128)
                        P.dma("sp", I("dma_start", out=dr["oS"][rows, 512:768], in_=of[o][:].rearrange("p h d -> p (h d)")),
                              reads=[("of", o, hh) for hh in range(4)], writes=[("oS", "f", qc, ti)])
                self.defer(store, qc)
            self.run_deferred()
            P.end_phase()

    def phase_sb(self, l):
        nc, P, dr, ps = self.nc, self.P, self.dr, self.ps
        with contextlib.ExitStack() as st:
            T = lambda n, sh, dt: st.enter_context(nc.sbuf_tensor(self.uname(n), sh, dt))
            QT = T("s_QT", [128, 2, S], BF16)
            KT = T("s_KT", [128, 2, S], BF16)
            V = T("s_V", [128, 32, 256], BF16)
            mge = T("s_mge", [128, 4, 512], BF16)
            ident = T("s_ident", [128, 128], BF16)
            ntri = T("s_ntri", [128, 128], BF16)
            nones = T("s_nones", [1, 128], BF16)
            onec = T("s_onec", [128, 1], BF16)
            et = [T("s_e%d" % i, [128, CH], F32) for i in range(2)]
            SP = [T("s_SP%d" % i, [128, CH], BF16) for i in range(3)]
            at = [T("s_a%d" % i, [128, CH], BF16) for i in range(2)]
            carry = T("s_carry", [1, CH], F32)
            ctmp = T("s_ctmp", [1, CH], F32)
            chi = [T("s_chi%d" % i, [1, CH], BF16) for i in range(3)]
            clo = [T("s_clo%d" % i, [1, CH], BF16) for i in range(3)]
            osb = [T("s_o%d" % i, [128, 4, 64], F32) for i in range(8)]
            for n, t_ in (("m_ge", mge), ("ident", ident), ("ntri", ntri), ("nones", nones), ("onec", onec)):
                P.dma("sp", I("dma_start", out=t_[:], in_=dr[n]), writes=[n])
            for j in range(2):
                P.dma("sp", I("dma_start", out=QT[:, j, :], in_=dr["FT"][12 + j]), writes=[("Q", j)])
                P.dma("act", I("dma_start", out=KT[:, j, :], in_=dr["FT"][14 + j]), writes=[("K", j)])
            for q8 in range(8):
                P.dma("sp", I("dma_start", out=V[:, q8 * 4:(q8 + 1) * 4, :],
                              in_=dr["SV"][q8 * 512:(q8 + 1) * 512, :].rearrange("(sb p) c -> p sb c", p=128)), writes=["V"])
            cnt = {"a": 0, "e": 0, "sp": 0, "at": 0, "c": 0}
            for qc in range(NCH):
                csl = slice(qc * CH, (qc + 1) * CH)
                for h in range(4):
                    hb = slice(64 * (h % 2), 64 * (h % 2) + 64)
                    j = h // 2
                    qk = [("Q", j), ("K", j)]
                    blocks = list(range(4 * qc + 3, -1, -1))
                    nblk = len(blocks)
                    info = {}

                    def stage1(idx):
                        sb = blocks[idx]
                        diag = sb >= 4 * qc
                        P.op("pe", I("matmul", ps[0][:, :], lhsT=KT[hb, j, sb * 128:(sb + 1) * 128], rhs=QT[hb, j, csl], start=True, stop=not diag),
                             reads=qk, writes=[("ps", 0)])
                        if diag:
                            P.op("pe", I("matmul", ps[0][:, :], lhsT=ident[:, :], rhs=mge[:, sb - 4 * qc, :], start=False, stop=True),
                                 reads=["ident", "m_ge"], writes=[("ps", 0)])
                        eb = cnt["e"] % 2
                        cnt["e"] += 1
                        P.op("act", I("activation", out=et[eb][:], in_=ps[0][:, :], func=AF.Exp), reads=[("ps", 0)], writes=[("e", eb)])
                        sb_i = cnt["sp"] % 3
                        cnt["sp"] += 1
                        P.op("act", I("activation", out=SP[sb_i][:], in_=et[eb][:], func=AF.Ln, bias=1.0, scale=1.0),
                             reads=[("e", eb)], writes=[("SP", sb_i)])
                        info[idx] = sb_i
                        if idx + 1 < nblk:
                            P.op("pe", I("matmul", ps[3][0:1, :], lhsT=onec[:, 0:1], rhs=SP[sb_i][:, :], start=(idx == 0), stop=(idx + 2 == nblk)),
                                 reads=["onec", ("SP", sb_i)], writes=[("ps", 3)])
                            cb = (idx + 1) % 3
                            P.op("dve", I("tensor_copy", out=chi[cb][:], in_=ps[3][0:1, :]), reads=[("ps", 3)], writes=[("chi", cb)])
                            P.op("dve", I("tensor_tensor", out=clo[cb][:], in0=ps[3][0:1, :], in1=chi[cb][:], op=ALU.subtract),
                                 reads=[("ps", 3), ("chi", cb)], writes=[("clo", cb)])

                    def stage2(idx):
                        sb = blocks[idx]
                        diag = sb >= 4 * qc
                        sb_i = info[idx]
                        bb = 1 + (cnt["a"] % 2)
                        cnt["a"] += 1
                        P.op("pe", I("matmul", ps[bb][:, :], lhsT=KT[hb, j, sb * 128:(sb + 1) * 128], rhs=QT[hb, j, csl], start=True, stop=False),
                             reads=qk, writes=[("ps", bb)])
                        if diag:
                            P.op("pe", I("matmul", ps[bb][:, :], lhsT=ident[:, :], rhs=mge[:, sb - 4 * qc, :], start=False, stop=False),
                                 reads=["ident", "m_ge"], writes=[("ps", bb)])
                        last = (idx == 0)
                        P.op("pe", I("matmul", ps[bb][:, :], lhsT=ntri[:, :], rhs=SP[sb_i][:, :], start=False, stop=last),
                             reads=["ntri", ("SP", sb_i)], writes=[("ps", bb)])
                        if idx > 0:
                            cb = idx % 3
                            P.op("pe", I("matmul", ps[bb][:, :], lhsT=nones[0:1, :], rhs=chi[cb][0:1, :], start=False, stop=False),
                                 reads=["nones", ("chi", cb)], writes=[("ps", bb)])
                            P.op("pe", I("matmul", ps[bb][:, :], lhsT=nones[0:1, :], rhs=clo[cb][0:1, :], start=False, stop=True),
                                 reads=["nones", ("clo", cb)], writes=[("ps", bb)])
                        ab = cnt["at"] % 2
                        cnt["at"] += 1
                        P.op("act", I("activation", out=at[ab][:], in_=ps[bb][:, :], func=AF.Exp), reads=[("ps", bb)], writes=[("at", ab)])
                        for ti in range(4):
                            if sb <= 4 * qc + ti:
                                P.op("pe", I("matmul", ps[4 + ti][:, 0:64], lhsT=at[ab][:, ti * 128:(ti + 1) * 128], rhs=V[:, sb, h * 64:(h + 1) * 64],
                                             start=(sb == 4 * qc + ti), stop=(sb == 0)), reads=[("at", ab), "V"], writes=[("ps", 4 + ti)])

                    stage1(0)
                    for idx in range(nblk):
                        if idx + 1 < nblk:
                            stage1(idx + 1)
                        stage2(idx)
                    for ti in range(4):
                        o = (qc % 2) * 4 + ti
                        P.op("dve", I("tensor_copy", out=osb[o][:, h, :], in_=ps[4 + ti][:, 0:64]), reads=[("ps", 4 + ti)], writes=[("osb", o, h)])
                for ti in range(4):
                    o = (qc % 2) * 4 + ti
                    rows = slice((qc * 4 + ti) * 128, (qc * 4 + ti + 1) * 128)
                    P.dma("sp", I("dma_start", out=dr["oS"][rows, 768:1024], in_=osb[o][:].rearrange("p h d -> p (h d)")),
                          reads=[("osb", o, h) for h in range(4)], writes=[("oS", "s", qc, ti)])
            P.end_phase()

    def phase_nsa(self, l):
        nc, P, dr, ps = self.nc, self.P, self.dr, self.ps
        pt = self.pt
        with contextlib.ExitStack() as st:
            T = lambda n, sh, dt: st.enter_context(nc.sbuf_tensor(self.uname(n), sh, dt))
            QT = T("n_QT", [128, 4, S], BF16)
            KsT = T("n_KsT", [128, S], BF16)
            KwT = T("n_KwT", [128, S], BF16)
            kcT = T("n_kcT", [128, 256], BF16)
            V4 = T("n_V4", [128, 32, 4, 65], BF16)
            VC = T("n_VC", [128, 2, 2, 65], BF16)
            ovl = T("n_ovl", [128, 2, 64], BF16)
            mgt = T("n_mgt", [128, 4, 512], BF16)
            mle = T("n_mle", [128, 4, 512], BF16)
            esel = T("n_esel", [128, 32, 128], BF16)
            ident = T("n_ident", [128, 128], BF16)
            mcmp = [T("n_mcmp%d" % i, [128, 2, CH], BF16) for i in range(2)]
            sbias = [T("n_sbias%d" % i, [128, 64], F32) for i in range(4)]
            gts = [T("n_gt%d" % i, [128, 24], F32) for i in range(8)]
            Pt = [T("n_P%d" % i, [128, CH], BF16) for i in range(3)]
            Pc = [T("n_Pc%d" % i, [128, CH], BF16) for i in range(4)]
            onsa = [T("n_o%d" % i, [128, 8, 64], F32) for i in range(8)]
            impg = [T("n_imp%d" % i, [128, 64], F32) for i in range(4)]
            score = T("n_score", [128, 64], F32)
            sc2 = T("n_sc2", [128, 64], F32)
            sc3 = T("n_sc3", [128, 64], F32)
            m8a = T("n_m8a", [128, 8], F32)
            m8b = T("n_m8b", [128, 8], F32)
            seln = T("n_seln", [128, 64], BF16)
            selT = T("n_selT", [128, CH], BF16)
            rc4 = T("n_rc4", [128, 4], F32)
            rg = T("n_rg", [128, 1], F32)
            rs1 = T("n_rs1", [128, 1], F32)
            self._nsa_tmp = ([T("n_r4%d" % i, [128, 1], F32) for i in range(4)], [T("n_g4%d" % i, [128, 1], F32) for i in range(4)],
                             [T("n_s4%d" % i, [128, 64], F32) for i in range(4)])
            for n, t_ in (("m_gt", mgt), ("m_le", mle), ("esel", esel), ("ident", ident), ("ovl", ovl)):
                P.dma("sp", I("dma_start", out=t_[:], in_=dr[n]), writes=[n])
            for j in range(4):
                P.dma("sp" if j % 2 == 0 else "act", I("dma_start", out=QT[:, j, :], in_=dr["FT"][j]), writes=[("Q", j)])
            P.dma("sp", I("dma_start", out=KsT[:], in_=dr["FT"][5]), writes=["KsT"])
            P.dma("act", I("dma_start", out=KwT[:], in_=dr["FT"][6]), writes=["KwT"])
            P.dma("sp", I("dma_start", out=kcT[:], in_=dr["kcT"]), writes=["kcT"])
            P.dma("sp", I("dma_start", out=VC[:], in_=dr["VC"]), writes=["VC"])
            for q8 in range(8):
                P.dma("sp", I("dma_start", out=V4[:, q8 * 4:(q8 + 1) * 4, :, :],
                              in_=dr["VA"][q8 * 512:(q8 + 1) * 512, 0:4, :].rearrange("(sb p) h c -> p sb h c", p=128)), writes=["V4"])
            P.op("pool", I("memset", selT[:], 0.0), writes=["selT"])
            cn = {"st": 0, "p": 0}

            def st_bank():
                b = cn["st"] % 3
                cn["st"] += 1
                return b

            def exp_to(b, dst, dstk, scale=0.125):
                P.op("act", I("activation", out=dst[:], in_=ps[b][:, :], func=AF.Exp, scale=scale), reads=[("ps", b)], writes=[dstk])

            for qc in range(NCH):
                csl = slice(qc * CH, (qc + 1) * CH)
                mb = qc % 2
                P.dma("sp", I("dma_start", out=mcmp[mb][:], in_=dr["m_cmp"][:, :, csl]), writes=[("mcmp", mb)])
                for ti in range(4):
                    rows = slice((qc * 4 + ti) * 128, (qc * 4 + ti + 1) * 128)
                    P.dma("sp", I("dma_start", out=sbias[ti][:], in_=dr["selbias"][rows]), writes=[("sbias", ti)])
                    P.dma("sp", I("dma_start", out=gts[mb * 4 + ti][:], in_=dr["GT"][rows]), writes=[("gts", mb * 4 + ti)])
                nbs = [0] if qc < 4 else [0, 1]
                for g in range(2):
                    gs = slice(64 * g, 64 * g + 64)
                    self.run_deferred()
                    for hh in range(4):
                        head = 4 * g + hh
                        for nb in nbs:
                            b = st_bank()
                            P.op("pe", I("matmul", ps[b][:, :], lhsT=kcT[gs, nb * 128:(nb + 1) * 128], rhs=QT[gs, hh, csl], start=True, stop=False),
                                 reads=["kcT", ("Q", hh)], writes=[("ps", b)])
                            P.op("pe", I("matmul", ps[b][:, :], lhsT=ident[:, :], rhs=mcmp[mb][:, nb, :], start=False, stop=True),
                                 reads=["ident", ("mcmp", mb)], writes=[("ps", b)])
                            pk = (hh % 2) * 2 + nb
                            exp_to(b, Pc[pk], ("Pc", pk))
                        for ti in range(4):
                            for nb in nbs:
                                pk = (hh % 2) * 2 + nb
                                P.op("pe", I("matmul", ps[3 + ti][:, 0:65], lhsT=Pc[pk][:, ti * 128:(ti + 1) * 128], rhs=VC[:, g, nb, :],
                                             start=(nb == nbs[0]), stop=(nb == nbs[-1])), reads=[("Pc", pk), "VC"], writes=[("ps", 3 + ti)])
                            for nb in nbs:
                                pk = (hh % 2) * 2 + nb
                                P.op("pe", I("matmul", ps[3 + ti][:, 128:192], lhsT=Pc[pk][:, ti * 128:(ti + 1) * 128], rhs=ovl[:, nb, :],
                                             start=(nb == nbs[0]), stop=(nb == nbs[-1])), reads=[("Pc", pk), "ovl"], writes=[("ps", 3 + ti)])
                        r4, g4, s4 = self._nsa_tmp
                        for ti in range(4):
                            P.op("dve", I("tensor_scalar", out=r4[ti][:], in0=ps[3 + ti][:, 64:65], scalar1=1e-20, scalar2=None, op0=ALU.max),
                                 reads=[("ps", 3 + ti)], writes=[("r4", ti)])
                        for ti in range(4):
                            P.op("dve", I("reciprocal", out=r4[ti][:], in_=r4[ti][:]), reads=[("r4", ti)], writes=[("r4", ti)])
                        for ti in range(4):
                            o = mb * 4 + ti
                            P.op("dve", I("tensor_tensor", out=g4[ti][:], in0=r4[ti][:], in1=gts[o][:, head * 3:head * 3 + 1], op=ALU.mult),
                                 reads=[("r4", ti), ("gts", o)], writes=[("g4", ti)])
                        for ti in range(4):
                            if hh == 0:
                                P.op("dve", I("tensor_scalar", out=impg[ti][:], in0=ps[3 + ti][:, 128:192], scalar1=r4[ti][:, 0:1], scalar2=None, op0=ALU.mult),
                                     reads=[("ps", 3 + ti), ("r4", ti)], writes=[("impg", ti)])
                            else:
                                P.op("dve", I("tensor_scalar", out=s4[ti][:], in0=ps[3 + ti][:, 128:192], scalar1=r4[ti][:, 0:1], scalar2=None, op0=ALU.mult),
                                     reads=[("ps", 3 + ti), ("r4", ti)], writes=[("s4", ti)])
                        if hh > 0:
                            for ti in range(4):
                                P.op("pool", I("tensor_tensor", out=impg[ti][:], in0=impg[ti][:], in1=s4[ti][:], op=ALU.add),
                                     reads=[("s4", ti), ("impg", ti)], writes=[("impg", ti)])
                        for ti in range(4):
                            o = mb * 4 + ti
                            P.op("dve", I("tensor_scalar", out=onsa[o][:, head, :], in0=ps[3 + ti][:, 0:64], scalar1=g4[ti][:, 0:1], scalar2=None, op0=ALU.mult),
                                 reads=[("ps", 3 + ti), ("g4", ti)], writes=[("onsa", o, head)])
                    if NSA_STOP == 1:
                        continue
                    for ti in range(4):
                        P.op("dve", I("tensor_tensor", out=score[:], in0=impg[ti][:], in1=sbias[ti][:], op=ALU.add),
                             reads=[("impg", ti), ("sbias", ti)], writes=["score"])
                        P.op("dve", I("max", out=m8a[:], in_=score[:]), reads=["score"], writes=["m8a"])
                        P.op("dve", I("match_replace", out=sc3[:], in_to_replace=m8a[:], in_values=score[:], imm_value=-3.0e9),
                             reads=["score", "m8a"], writes=["sc3"])
                        P.op("dve", I("max", out=m8b[:], in_=sc3[:]), reads=["sc3"], writes=["m8b"])
                        P.op("dve", I("tensor_scalar", out=seln[:], in0=score[:], scalar1=m8b[:, 7:8], scalar2=-BIG, op0=ALU.is_lt, op1=ALU.mult),
                             reads=["score", "m8b"], writes=["seln"])
                        P.op("pe", I("transpose", out=pt[0:64, ti * 128:(ti + 1) * 128], in_=seln[:, :], identity=ident[:, :]),
                             reads=["seln", "ident"], writes=[("ps", 7)])
                    P.op("dve", I("tensor_copy", out=selT[0:64, :], in_=pt[0:64, 0:512]), reads=[("ps", 7)], writes=["selT"])
                    if NSA_STOP == 2:
                        continue
                    for branch in ((1, 2) if NSA_STOP != 3 else (1,)):
                        for hh in range(4):
                            head = 4 * g + hh
                            if branch == 1:
                                seq = [(sb, None) for sb in range(4 * qc + 4)]
                            else:
                                seq = [(4 * qc - 4 + r, r) for r in range(8) if 4 * qc - 4 + r >= 0]
                            first = {}
                            last = {}
                            for (sb, r) in seq:
                                for ti in range(4):
                                    if branch == 1:
                                        ok = sb <= 4 * qc + ti
                                    else:
                                        ok = (ti <= r) if r < 4 else (r - 4 <= ti)
                                    if ok:
                                        first.setdefault(ti, sb)
                                        last[ti] = sb
                            for (sb, r) in seq:
                                b = st_bank()
                                KT_ = KsT if branch == 1 else KwT
                                kk = "KsT" if branch == 1 else "KwT"
                                P.op("pe", I("matmul", ps[b][:, :], lhsT=KT_[gs, sb * 128:(sb + 1) * 128], rhs=QT[gs, hh, csl], start=True, stop=False),
                                     reads=[kk, ("Q", hh)], writes=[("ps", b)])
                                if branch == 1:
                                    diag = sb >= 4 * qc
                                    P.op("pe", I("matmul", ps[b][:, :], lhsT=esel[:, sb, :], rhs=selT[:, :], start=False, stop=not diag),
                                         reads=["esel", "selT"], writes=[("ps", b)])
                                    if diag:
                                        P.op("pe", I("matmul", ps[b][:, :], lhsT=ident[:, :], rhs=mgt[:, sb - 4 * qc, :], start=False, stop=True),
                                             reads=["ident", "m_gt"], writes=[("ps", b)])
                                else:
                                    msk = mle[:, r, :] if r < 4 else mgt[:, r - 4, :]
                                    P.op("pe", I("matmul", ps[b][:, :], lhsT=ident[:, :], rhs=msk, start=False, stop=True),
                                         reads=["ident", "m_gt", "m_le"], writes=[("ps", b)])
                                pb = cn["p"] % 3
                                cn["p"] += 1
                                exp_to(b, Pt[pb], ("P", pb))
                                def back(sb, r, pb, branch, g, first, last):
                                    for ti in range(4):
                                        if ti in first and first[ti] <= sb <= last[ti]:
                                            if branch == 2:
                                                ok = (ti <= r) if r < 4 else (r - 4 <= ti)
                                                if not ok:
                                                    continue
                                            vg = g if branch == 1 else 2 + g
                                            P.op("pe", I("matmul", ps[3 + ti][:, 0:65], lhsT=Pt[pb][:, ti * 128:(ti + 1) * 128], rhs=V4[:, sb, vg, :],
                                                         start=(sb == first[ti]), stop=(sb == last[ti])), reads=[("P", pb), "V4"], writes=[("ps", 3 + ti)])
                                self.run_deferred()
                                self.defer(back, sb, r, pb, branch, g, first, last)
                            self.defer(self._nsa_fin, mb, head, branch, gts, rs1, rg, sc2, onsa)
                            for ti in range(0):
                                o = mb * 4 + ti
                                gtile = gts[o]
                                P.op("dve", I("tensor_scalar", out=rs1[:], in0=ps[3 + ti][:, 64:65], scalar1=1e-20, scalar2=None, op0=ALU.max),
                                     reads=[("ps", 3 + ti)], writes=["rs1"])
                                P.op("dve", I("reciprocal", out=rs1[:], in_=rs1[:]), reads=["rs1"], writes=["rs1"])
                                P.op("dve", I("tensor_tensor", out=rg[:], in0=rs1[:], in1=gtile[:, head * 3 + branch:head * 3 + branch + 1], op=ALU.mult),
                                     reads=["rs1", ("gts", o)], writes=["rg"])
                                P.op("dve", I("tensor_scalar", out=sc2[:], in0=ps[3 + ti][:, 0:64], scalar1=rg[:, 0:1], scalar2=None, op0=ALU.mult),
                                     reads=[("ps", 3 + ti), "rg"], writes=["sc2"])
                                P.op("pool", I("tensor_tensor", out=onsa[o][:, head, :], in0=onsa[o][:, head, :], in1=sc2[:], op=ALU.add),
                                     reads=["sc2", ("onsa", o, head)], writes=[("onsa", o, head)])
                def store(qc, mb):
                    for ti in range(4):
                        o = mb * 4 + ti
                        rows = slice((qc * 4 + ti) * 128, (qc * 4 + ti + 1) * 128)
                        P.dma("sp", I("dma_start", out=dr["oS"][rows, 0:512], in_=onsa[o][:].rearrange("p h d -> p (h d)")),
                              reads=[("onsa", o, hd) for hd in range(8)], writes=[("oS", "n", qc, ti)])
                self.defer(store, qc, mb)
            self.run_deferred()
            P.end_phase()

    def _nsa_fin(self, mb, head, branch, gts, rs1, rg, sc2, onsa):
        P, ps = self.P, self.ps
        r4, g4, s4 = self._nsa_tmp
        for ti in range(4):
            P.op("dve", I("tensor_scalar", out=r4[ti][:], in0=ps[3 + ti][:, 64:65], scalar1=1e-20, scalar2=None, op0=ALU.max),
                 reads=[("ps", 3 + ti)], writes=[("r4", ti)])
        for ti in range(4):
            P.op("dve", I("reciprocal", out=r4[ti][:], in_=r4[ti][:]), reads=[("r4", ti)], writes=[("r4", ti)])
        for ti in range(4):
            o = mb * 4 + ti
            P.op("dve", I("tensor_tensor", out=g4[ti][:], in0=r4[ti][:], in1=gts[o][:, head * 3 + branch:head * 3 + branch + 1], op=ALU.mult),
                 reads=[("r4", ti), ("gts", o)], writes=[("g4", ti)])
        for ti in range(4):
            P.op("dve", I("tensor_scalar", out=s4[ti][:], in0=ps[3 + ti][:, 0:64], scalar1=g4[ti][:, 0:1], scalar2=None, op0=ALU.mult),
                 reads=[("ps", 3 + ti), ("g4", ti)], writes=[("s4", ti)])
        for ti in range(4):
            o = mb * 4 + ti
            P.op("pool", I("tensor_tensor", out=onsa[o][:, head, :], in0=onsa[o][:, head, :], in1=s4[ti][:], op=ALU.add),
                 reads=[("s4", ti), ("onsa", o, head)], writes=[("onsa", o, head)])

    def phase_D(self, l, last):
        self.phase_D1(l)
        self.phase_D2(l, last)

    def phase_D1(self, l):
        nc, P, dr, ps = self.nc, self.P, self.dr, self.ps
        hsrc = dr["x"] if l == 0 else dr["hS"]
        with contextlib.ExitStack() as st:
            T = lambda n, sh, dt: st.enter_context(nc.sbuf_tensor(self.uname(n), sh, dt))
            Wout = T("d_Wout", [128, 8, D], BF16)
            Wd = T("d_Wd", [128, NF, D], BF16)
            stage = [T("d_stage%d" % i, [128, DFF], F32) for i in range(2)]
            cvt = [T("d_cvt%d" % i, [128, DFF], BF16) for i in range(2)]
            ghead = T("d_ghead", [128, 8], F32)
            gffn = T("d_gffn", [128, 8], F32)
            ident = T("d_ident", [128, 128], BF16)
            h = [T("d_h%d" % i, [128, D], F32) for i in range(4)]
            ot = [T("d_ot%d" % i, [128, D], F32) for i in range(2)]
            osq = T("d_osq", [128, D], F32)
            ssh = T("d_ssh", [128, 16], F32)
            on = [T("d_on%d" % i, [128, D], BF16) for i in range(2)]
            T4 = [dict(junk=T("d_junk%d" % i, [128, D], BF16), ss=T("d_ss%d" % i, [128, 1], F32),
                       rs=T("d_rs%d" % i, [128, 1], F32)) for i in range(2)]
            xT = T("d_xT", [128, 8, CH], BF16)
            actT = T("d_actT", [128, NF, CH], BF16)
            wgu = [T("d_wgu%d" % i, [128, 2, 8, 128], BF16) for i in range(3)]
            sg = [T("d_sg%d" % i, [128, CH], F32) for i in range(2)]
            P.dma("sp", I("dma_start", out=ghead[:], in_=dr["head_norm"][l]), writes=["ghead"])
            P.dma("sp", I("dma_start", out=gffn[:], in_=dr["norm_ffn"][l]), writes=["gffn"])
            P.dma("sp", I("dma_start", out=ident[:], in_=dr["ident"]), writes=["ident"])
            n = 0
            for k in range(8):
                self.load_weight_bf(Wout[:, k, :], dr["w_out"][l, k * 128:(k + 1) * 128, :], stage[n % 2][:, 0:D], ("stage", n % 2),
                                    ("Wout", k), scale_ap=ghead[:, k:k + 1], scalek="ghead", eng=("dve" if n % 2 == 0 else "pool"),
                                    q=("sp" if n % 2 == 0 else "act"))
                n += 1
            for f in range(NF):
                self.load_weight_bf(Wd[:, f, :], dr["w_ffn_down"][l, f * 128:(f + 1) * 128, :], stage[n % 2][:, 0:D], ("stage", n % 2),
                                    ("Wd", f), eng=("dve" if n % 2 == 0 else "pool"), q=("sp" if n % 2 == 0 else "act"))
                n += 1
            for gi, wn in enumerate(("w_ffn_gate", "w_ffn_up")):
                for k in range(8):
                    b = n % 2
                    self.load_weight_bf(cvt[b][:], dr[wn][l, k * 128:(k + 1) * 128, :], stage[b][:], ("stage", b), ("cvt", b),
                                        scale_ap=gffn[:, k:k + 1], scalek="gffn", eng=("dve" if b == 0 else "pool"),
                                        q=("sp" if b == 0 else "act"))
                    for f0, f1 in ((0, 8), (8, 16), (16, NF)):
                        P.dma("sp", I("dma_start", out=dr["WGU"][f0:f1, :, gi, k, :].rearrange("f p c -> p f c"),
                                      in_=cvt[b][:, f0 * 128:f1 * 128].rearrange("p (f c) -> p f c", c=128)),
                              reads=[("cvt", b)], writes=[("WGU", gi, k, f0)])
                    n += 1
            wgu_ready = [("WGU", gi, k, f0) for gi in range(2) for k in range(8) for f0 in (0, 8, 16)]
            Woutk = [("Wout", k) for k in range(8)]
            Wdk = [("Wd", f) for f in range(NF)]
            wl = 0
            for c in range(NCH):
                for i in range(4):
                    ti = c * 4 + i
                    b = ti % 2
                    rows = slice(ti * 128, (ti + 1) * 128)
                    P.dma("sp", I("dma_start", out=h[i][:], in_=hsrc[rows, :]), writes=[("h", i)])
                    P.dma("act", I("dma_start", out=ot[b][:], in_=dr["oS"][rows, :]), writes=[("ot", b)])
                    P.op("pool", I("tensor_tensor", out=osq[:], in0=ot[b][:], in1=ot[b][:], op=ALU.mult), reads=[("ot", b)], writes=["osq"])
                    P.op("dve", I("tensor_reduce", out=ssh[:], in_=osq[:].rearrange("p (h d) -> p h d", d=64), axis=AX.X, op=ALU.add),
                         reads=["osq"], writes=["ssh"])
                    P.op("dve", I("tensor_scalar", out=ssh[:], in0=ssh[:], scalar1=1.0 / 64, scalar2=1e-6, op0=ALU.mult, op1=ALU.add),
                         reads=["ssh"], writes=["ssh"])
                    P.op("act", I("sqrt", out=ssh[:], in_=ssh[:]), reads=["ssh"], writes=["ssh"])
                    P.op("dve", I("reciprocal", out=ssh[:], in_=ssh[:]), reads=["ssh"], writes=["ssh"])
                    P.op("dve", I("tensor_tensor", out=on[b][:].rearrange("p (h d) -> p h d", d=64),
                                  in0=ot[b][:].rearrange("p (h d) -> p h d", d=64),
                                  in1=ssh[:, :].unsqueeze(2).to_broadcast([128, 16, 64]), op=ALU.mult),
                         reads=[("ot", b), "ssh"], writes=[("on", b)])
                    self.transpose8(on[b], ("on", b), xT[:, :, i * 128:(i + 1) * 128], ("xT", i), ident, eng=("dve" if i % 2 == 0 else "act"))
                    for half in range(2):
                        pa = self.psn()
                        for k in range(8):
                            P.op("pe", I("matmul", ps[pa][:, :], lhsT=xT[:, k, i * 128:(i + 1) * 128], rhs=Wout[:, k, half * 512:(half + 1) * 512],
                                         start=(k == 0), stop=(k == 7)), reads=[("xT", i)] + Woutk, writes=[("ps", pa)])
                        P.op("dve", I("tensor_tensor", out=h[i][:, half * 512:(half + 1) * 512], in0=ps[pa][:, :],
                                      in1=h[i][:, half * 512:(half + 1) * 512], op=ALU.add), reads=[("ps", pa), ("h", i)], writes=[("h", i)])
                if "hmix" in self.dr:
                    for i in range(4):
                        rows = slice((c * 4 + i) * 128, (c * 4 + i + 1) * 128)
                        P.dma("sp", I("dma_start", out=dr["hmix"][rows, :], in_=h[i][:]), reads=[("h", i)], writes=[("hmix", c, i)])
                for i in range(4):
                    b = i % 2
                    self.rms_tile(T4[b], b, h[i], ("h", i), ("junk", b), ("ss", b), ("rs", b), on[b], ("on", b))
                    self.transpose8(on[b], ("on", b), xT[:, :, i * 128:(i + 1) * 128], ("xT", i), ident, eng=("dve" if i % 2 == 0 else "act"))
                xk = [("xT", i) for i in range(4)]
                for f in range(NF):
                    wb = wl % 3
                    wl += 1
                    P.dma("sp" if f % 2 == 0 else "act", I("dma_start", out=wgu[wb][:], in_=dr["WGU"][f]), reads=wgu_ready, writes=[("wgu", wb)])
                    pg, pu = self.psn(), self.psn()
                    for gi, pp in ((0, pg), (1, pu)):
                        for k in range(8):
                            P.op("pe", I("matmul", ps[pp][:, :], lhsT=wgu[wb][:, gi, k, :], rhs=xT[:, k, :], start=(k == 0), stop=(k == 7)),
                                 reads=xk + [("wgu", wb)], writes=[("ps", pp)])
                    sb_ = f % 2
                    P.op("act", I("activation", out=sg[sb_][:], in_=ps[pg][:, :], func=AF.Silu), reads=[("ps", pg)], writes=[("sg", sb_)])
                    P.op("dve", I("tensor_tensor", out=actT[:, f, :], in0=ps[pu][:, :], in1=sg[sb_][:], op=ALU.mult),
                         reads=[("ps", pu), ("sg", sb_)], writes=[("actT", f)])
                ak = [("actT", f) for f in range(NF)]
                for i in range(4):
                    for half in range(2):
                        pa = self.psn()
                        for f in range(NF):
                            P.op("pe", I("matmul", ps[pa][:, :], lhsT=actT[:, f, i * 128:(i + 1) * 128], rhs=Wd[:, f, half * 512:(half + 1) * 512],
                                         start=(f == 0), stop=(f == NF - 1)), reads=ak + Wdk, writes=[("ps", pa)])
                        P.op("dve", I("tensor_tensor", out=h[i][:, half * 512:(half + 1) * 512], in0=ps[pa][:, :],
                                      in1=h[i][:, half * 512:(half + 1) * 512], op=ALU.add), reads=[("ps", pa), ("h", i)], writes=[("h", i)])
                    rows = slice((c * 4 + i) * 128, (c * 4 + i + 1) * 128)
                    P.dma("sp", I("dma_start", out=dr["hS"][rows, :], in_=h[i][:]), reads=[("h", i)], writes=[("hS", c, i)])
            P.end_phase()

    def phase_D2(self, l, last):
        nc, P, dr, ps = self.nc, self.P, self.dr, self.ps
        with contextlib.ExitStack() as st:
            T = lambda n, sh, dt: st.enter_context(nc.sbuf_tensor(self.uname(n), sh, dt))
            Wpg = T("e_Wpg", [128, 8, D], BF16)
            Wpp = T("e_Wpp", [128, 2, D], BF16)
            stage = [T("e_stage%d" % i, [128, D], F32) for i in range(2)]
            gple = T("e_gple", [128, 8], F32)
            gfin = T("e_gfin", [128, D], F32)
            ident = T("e_ident", [128, 128], BF16)
            h = [T("e_h%d" % i, [128, D], F32) for i in range(2)]
            p32 = [T("e_p32%d" % i, [128, 256], F32) for i in range(2)]
            pbf = [T("e_pbf%d" % i, [128, 256], BF16) for i in range(2)]
            hn = [T("e_hn%d" % i, [128, D], BF16) for i in range(2)]
            T4 = [dict(junk=T("e_junk%d" % i, [128, D], BF16), ss=T("e_ss%d" % i, [128, 1], F32),
                       rs=T("e_rs%d" % i, [128, 1], F32)) for i in range(2)]
            xT = [T("e_xT%d" % i, [128, 8, 128], BF16) for i in range(2)]
            pT = [T("e_pT%d" % i, [128, 2, 128], BF16) for i in range(2)]
            sig = [T("e_sig%d" % i, [128, CH], F32) for i in range(2)]
            tmp = [T("e_tmp%d" % i, [128, CH], F32) for i in range(2)]
            outt = [T("e_out%d" % i, [128, D], F32) for i in range(2)]
            P.dma("sp", I("dma_start", out=gple[:], in_=dr["norm_ple"][l]), writes=["gple"])
            P.dma("sp", I("dma_start", out=ident[:], in_=dr["ident"]), writes=["ident"])
            if last:
                P.dma("sp", I("dma_start", out=gfin[:], in_=dr["norm_final"].to_broadcast([128, D])), writes=["gfin"])
            n = 0
            for k in range(8):
                self.load_weight_bf(Wpg[:, k, :], dr["w_ple_gate"][l, k * 128:(k + 1) * 128, :], stage[n % 2][:], ("stage", n % 2),
                                    ("Wpg", k), scale_ap=gple[:, k:k + 1], scalek="gple", eng=("dve" if n % 2 == 0 else "pool"),
                                    q=("sp" if n % 2 == 0 else "act"))
                n += 1
            for k in range(2):
                self.load_weight_bf(Wpp[:, k, :], dr["w_ple_proj"][l, k * 128:(k + 1) * 128, :], stage[n % 2][:], ("stage", n % 2),
                                    ("Wpp", k), eng=("dve" if n % 2 == 0 else "pool"), q=("sp" if n % 2 == 0 else "act"))
                n += 1
            Wpgk = [("Wpg", k) for k in range(8)]
            Wppk = [("Wpp", k) for k in range(2)]
            for ti in range(NTILE):
                b = ti % 2
                rows = slice(ti * 128, (ti + 1) * 128)
                P.dma("sp", I("dma_start", out=h[b][:], in_=dr["hS"][rows, :]), writes=[("h", b)])
                P.dma("act", I("dma_start", out=p32[b][:], in_=dr["p"][l, rows, :]), writes=[("p32", b)])
                P.op("pool", I("tensor_copy", out=pbf[b][:], in_=p32[b][:]), reads=[("p32", b)], writes=[("pbf", b)])
                self.rms_tile(T4[b], b, h[b], ("h", b), ("junk", b), ("ss", b), ("rs", b), hn[b], ("hn", b))
                self.transpose8(hn[b], ("hn", b), xT[b][:, :, :], ("xT", b), ident, eng="dve")
                self.transpose8(pbf[b], ("pbf", b), pT[b][:, :, :], ("pT", b), ident, nblk=2, eng="act")
                for half in range(2):
                    hs = slice(half * 512, (half + 1) * 512)
                    pg, pp = self.psn(), self.psn()
                    for k in range(8):
                        P.op("pe", I("matmul", ps[pg][:, :], lhsT=xT[b][:, k, :], rhs=Wpg[:, k, hs], start=(k == 0), stop=(k == 7)),
                             reads=[("xT", b)] + Wpgk, writes=[("ps", pg)])
                    for k in range(2):
                        P.op("pe", I("matmul", ps[pp][:, :], lhsT=pT[b][:, k, :], rhs=Wpp[:, k, hs], start=(k == 0), stop=(k == 1)),
                             reads=[("pT", b)] + Wppk, writes=[("ps", pp)])
                    P.op("act", I("activation", out=sig[half][:], in_=ps[pg][:, :], func=AF.Sigmoid), reads=[("ps", pg)], writes=[("sig", half)])
                    P.op("dve", I("tensor_tensor", out=tmp[half][:], in0=ps[pp][:, :], in1=sig[half][:], op=ALU.mult),
                         reads=[("ps", pp), ("sig", half)], writes=[("tmp", half)])
                    P.op("pool", I("tensor_tensor", out=h[b][:, hs], in0=h[b][:, hs], in1=tmp[half][:], op=ALU.add),
                         reads=[("tmp", half), ("h", b)], writes=[("h", b)])
                if not last:
                    P.dma("sp", I("dma_start", out=dr["hS"][rows, :], in_=h[b][:]), reads=[("h", b)], writes=[("hS", ti)])
                else:
                    self.rms_tile(T4[b], b, h[b], ("h", b), ("junk", b), ("ss", b), ("rs", b), None, None) if False else None
                    junk, ss, rs = T4[b]["junk"], T4[b]["ss"], T4[b]["rs"]
                    P.op("act", I("activation", out=junk[:], in_=h[b][:], func=AF.Square, accum_out=ss[:]), reads=[("h", b)], writes=[("junk", b), ("ss", b)])
                    P.op("dve", I("tensor_scalar", out=rs[:], in0=ss[:], scalar1=1.0 / D, scalar2=1e-6, op0=ALU.mult, op1=ALU.add),
                         reads=[("ss", b)], writes=[("rs", b)])
                    P.op("act", I("sqrt", out=rs[:], in_=rs[:]), reads=[("rs", b)], writes=[("rs", b)])
                    P.op("dve", I("reciprocal", out=rs[:], in_=rs[:]), reads=[("rs", b)], writes=[("rs", b)])
                    P.op("dve", I("scalar_tensor_tensor", out=outt[b][:], in0=h[b][:], scalar=rs[:, 0:1], in1=gfin[:], op0=ALU.mult, op1=ALU.mult),
                         reads=[("h", b), ("rs", b), "gfin"], writes=[("outt", b)])
                    P.dma("sp", I("dma_start", out=self.out[rows, :], in_=outt[b][:]), reads=[("outt", b)], writes=[("out", ti)])
            P.end_phase()


def make_in_maps(inputs, cores):
    inp = {k: np.asarray(v) for k, v in inputs.items()}
    sh = _prep_shared(inp)
    maps = []
    for b in cores:
        m = dict(sh)
        m["x"] = np.ascontiguousarray(inp["x"][b])
        m["p"] = np.ascontiguousarray(inp["p"][:, b])
        m["pos"] = np.ascontiguousarray(inp["positions"][b].reshape(1, S).astype(np.int32))
        maps.append(m)
    return maps


_NC_CACHE = {}


def kernel(**inputs):
    if "nc" not in _NC_CACHE:
        _NC_CACHE["nc"] = Builder().build()
    nc = _NC_CACHE["nc"]
    maps = make_in_maps(inputs, list(range(8)))
    res = run_bass_kernel_spmd(nc, maps, core_ids=list(range(8)))
    out = np.stack([np.asarray(r["out"]) for r in res.results], axis=0)
    return out.astype(np.float32)
```

```python
import contextlib
import numpy as np
import ml_dtypes
import concourse.bass as bass
import concourse.mybir as mybir
from concourse.bass_utils import run_bass_kernel_spmd

F32 = mybir.dt.float32
BF16 = mybir.dt.bfloat16
I32 = mybir.dt.int32
AF = mybir.ActivationFunctionType
ALU = mybir.AluOpType
AX = mybir.AxisListType

S = 4096
D = 1024
L = 2
NTILE = 32
CH = 512
NCH = 8
DFF = 2816
NF = 22
BIG = 30000.0
WCOLS = 3740
COMPUTE = ("pe", "act", "dve", "pool", "sp")
import os as _os
NSA_STOP = int(_os.environ.get("NSA_STOP", "0"))
DMAQ = ("sp", "pool", "act")


def I(m, *a, **k):
    return lambda e: getattr(e, m)(*a, **k)


class Op:
    __slots__ = ("eng", "fn", "waits", "signal", "cnt", "dma_sem", "dma_val", "is_dma", "idx")

    def __init__(self, eng, fn, is_dma):
        self.eng = eng
        self.fn = fn
        self.waits = []
        self.signal = False
        self.cnt = None
        self.dma_sem = None
        self.dma_val = None
        self.is_dma = is_dma
        self.idx = None


class Prog:
    def __init__(self, nc, st, n_dma_sems=8):
        self.nc = nc
        self.lists = {e: [] for e in COMPUTE}
        self.last_w = {}
        self.readers = {}
        self.n_dma_sems = n_dma_sems
        self.dma_count = {q: 0 for q in DMAQ}
        self.csem = {e: st.enter_context(nc.semaphore("c_" + e)) for e in COMPUTE}
        self.dsem = {(q, j): st.enter_context(nc.semaphore("d_%s%d" % (q, j)))
                     for q in DMAQ for j in range(n_dma_sems)}
        self.cbase = {e: 0 for e in COMPUTE}
        self.gidx = {e: 0 for e in COMPUTE}
        self.barrier = {}
        self.dma_last = {}

    def _deps(self, reads, writes):
        deps = []
        for k in reads:
            w = self.last_w.get(k)
            if w is not None:
                deps.append(w)
        for k in writes:
            w = self.last_w.get(k)
            if w is not None:
                deps.append(w)
            deps.extend(self.readers.get(k, ()))
        return deps

    def _record(self, h, reads, writes):
        for k in reads:
            self.readers.setdefault(k, []).append(h)
        for k in writes:
            self.last_w[k] = h
            self.readers[k] = []

    def _attach(self, h, deps):
        best = {}
        for d in deps:
            if d is h or d.fn is None:
                continue
            if d.is_dma:
                key = ("d",) + d.dma_sem
                cur = best.get(key)
                if cur is None or d.dma_val > cur.dma_val:
                    best[key] = d
            else:
                if d.eng == "pe" and h.eng == "pe" and not h.is_dma:
                    continue
                cur = best.get(d.eng)
                if cur is None or d.idx > cur.idx:
                    best[d.eng] = d
        for d in best.values():
            d.signal = True
            h.waits.append(d)

    def op(self, eng, fn, reads=(), writes=(), extra=()):
        h = Op(eng, fn, False)
        self._attach(h, self._deps(reads, writes) + list(extra))
        h.idx = self.gidx[eng]
        self.gidx[eng] += 1
        self.lists[eng].append(h)
        self._record(h, reads, writes)
        return h

    def dma(self, q, fn, reads=(), writes=(), extra=()):
        h = Op(q, fn, True)
        self._attach(h, self._deps(reads, writes) + list(extra))
        i = self.dma_count[q]
        self.dma_count[q] += 1
        h.dma_sem = (q, i % self.n_dma_sems)
        h.dma_val = 16 * (i // self.n_dma_sems + 1)
        h.idx = self.gidx[q]
        self.gidx[q] += 1
        self.lists[q].append(h)
        self._record(h, reads, writes)
        self.dma_last[h.dma_sem] = h.dma_val
        return h

    def flush(self, final=False):
        nc = self.nc
        for e in COMPUTE:
            c = self.cbase[e]
            for h in self.lists[e]:
                if not h.is_dma and h.signal:
                    c += 1
                    h.cnt = c
        if final:
            pass
        barrier = dict(self.barrier)
        with nc.Block() as block:
            engs = {"pe": block.tensor, "act": block.scalar, "dve": block.vector,
                    "pool": block.gpsimd, "sp": block.sync}

            def make(ename):
                lst = self.lists[ename]

                def body(eng):
                    waited = {}
                    for key, val in barrier.items():
                        if val <= 0:
                            continue
                        sem = self.csem[key[1]] if key[0] == "c" else self.dsem[key[1:]]
                        eng.wait_ge(sem, val)
                        waited[key] = val
                    for h in lst:
                        for d in h.waits:
                            if d.is_dma:
                                key = ("d",) + d.dma_sem
                                sem = self.dsem[d.dma_sem]
                                val = d.dma_val
                            else:
                                key = ("c", d.eng)
                                sem = self.csem[d.eng]
                                val = d.cnt
                            if waited.get(key, 0) >= val:
                                continue
                            waited[key] = val
                            eng.wait_ge(sem, val)
                        if h.is_dma:
                            prev = h.dma_val - 16
                            key = ("d",) + h.dma_sem
                            if prev > 0 and waited.get(key, 0) < prev:
                                eng.wait_ge(self.dsem[h.dma_sem], prev)
                                waited[key] = prev
                            ins = h.fn(eng)
                            ins.then_inc(self.dsem[h.dma_sem], 16)
                        else:
                            ins = h.fn(eng)
                            if h.signal:
                                ins.then_inc(self.csem[ename], 1)
                    if final and ename == "sp":
                        for key, val in self._barrier_now().items():
                            if val > 0 and waited.get(key, 0) < val and key != ("c", "sp"):
                                sem = self.csem[key[1]] if key[0] == "c" else self.dsem[key[1:]]
                                eng.wait_ge(sem, val)
                return body

            for ename in ("sp", "pool", "act", "dve", "pe"):
                if self.lists[ename] or barrier or final:
                    engs[ename](make(ename))
        self.barrier = self._barrier_now()
        for e in COMPUTE:
            for h in self.lists[e]:
                h.fn = None
            self.lists[e] = []
        self.last_w = {}
        self.readers = {}

    def _barrier_now(self):
        b = {}
        for e in COMPUTE:
            c = self.cbase[e]
            for h in self.lists[e]:
                if h.cnt is not None and h.cnt > c:
                    c = h.cnt
            b[("c", e)] = c
        for k, v in self.dma_last.items():
            b[("d",) + k] = v
        return b

    def end_phase(self, final=False):
        for e in COMPUTE:
            for h in reversed(self.lists[e]):
                if not h.is_dma:
                    h.signal = True
                    break
        self.flush(final=final)
        for e in COMPUTE:
            self.cbase[e] = self.barrier[("c", e)]


def _win_cols():
    o = {}
    names = ["nq", "nkc", "nvc", "nks", "nvs", "nkw", "nvw", "ngate", "fq", "fk", "fv", "ff", "sq", "sk", "sv"]
    sizes = [512, 128, 128, 128, 128, 128, 128, 24, 256, 256, 256, 4, 256, 256, 256]
    off = 0
    for n, s in zip(names, sizes):
        o[n] = np.arange(off, off + s)
        off += s
    assert off == 2844

    def rot(c):
        c = c.reshape(-1, 2, 32)
        return c[:, ::-1, :].reshape(-1)

    ft = []
    for j in range(4):
        ft.append(np.concatenate([o["nq"][64 * j:64 * j + 64], o["nq"][64 * (4 + j):64 * (4 + j) + 64]]))
    ft += [o["nkc"], o["nks"], o["nkw"]]
    ft += [rot(c) for c in ft[:7]]
    ft.append(o["nvc"])
    for n in ("fq", "fk", "sq", "sk"):
        ft += [o[n][:128], o[n][128:]]
    cols = np.concatenate(ft + [o["ff"], o["nvs"], o["nvw"], o["fv"], o["sv"], o["ngate"]])
    assert cols.shape[0] == WCOLS
    return cols


def _consts():
    bf = ml_dtypes.bfloat16
    c = {}
    c["ident"] = np.eye(128, dtype=np.float32).astype(bf)
    s = np.arange(128)[:, None, None]
    r = np.arange(4)[None, :, None]
    t = np.arange(512)[None, None, :]
    sa = 128 * r + s
    c["m_gt"] = np.where(sa > t, -BIG, 0.0).astype(bf)
    c["m_ge"] = np.where(sa >= t, -BIG, 0.0).astype(bf)
    c["m_le"] = np.where(sa <= t, -BIG, 0.0).astype(bf)
    n = np.arange(128)[:, None, None] + 128 * np.arange(2)[None, :, None]
    tt = np.arange(S)[None, None, :]
    cm = np.where((16 * n + 31 > tt) | (n >= 255), -BIG, 0.0)
    c["m_cmp"] = cm.astype(bf)
    j = np.arange(64)[:, None, None]
    sb = np.arange(32)[None, :, None]
    ss = np.arange(128)[None, None, :]
    es = np.zeros((128, 32, 128), np.float32)
    es[0:64] = (j == 2 * sb + (ss >= 64))
    c["esel"] = es.astype(bf)
    nn = np.arange(256)
    cs = nn[:, None] * 16
    bs = np.arange(64)[None, :] * 64
    ov = ((cs < bs + 64) & (cs + 32 > bs) & (nn[:, None] < 255)).astype(np.float32)
    c["ovl"] = ov.reshape(2, 128, 64).transpose(1, 0, 2).astype(bf).copy()
    tq = np.arange(S)[:, None]
    jb = np.arange(64)[None, :]
    cur = tq // 64
    forced = (jb == 0) | (jb == cur) | (jb == cur - 1)
    valid = jb <= cur
    c["selbias"] = np.where(forced, 1e9, np.where(valid, 0.0, -1e9)).astype(np.float32)
    half = 32
    invf = (10000.0 ** (-np.arange(half, dtype=np.float32) / half)).astype(np.float32)
    rr = np.arange(128)
    c["invf"] = invf[rr % 32].reshape(128, 1).astype(np.float32)
    c["sgn"] = np.where((rr % 64) < 32, -1.0, 1.0).reshape(128, 1).astype(np.float32)
    jj = np.arange(128)
    c["ntri"] = np.where(jj[:, None] >= jj[None, :], -1.0, 0.0).astype(np.float32).astype(bf)
    c["nones"] = np.full((1, 128), -1.0, np.float32).astype(bf)
    c["onec"] = np.ones((128, 1), np.float32).astype(bf)
    return c


def _col8(v):
    return np.ascontiguousarray(v.reshape(8, 128).T)


def _prep_shared(inp):
    sh = {}
    cols = _win_cols()
    sh["w_in"] = np.ascontiguousarray(inp["w_in"][:, :, cols])
    for n in ("norm_mix", "norm_ffn", "norm_ple", "head_norm"):
        sh[n] = np.stack([_col8(inp[n][l]) for l in range(L)])
    sh["norm_final"] = inp["norm_final"].reshape(1, D)
    sh["b_gate"] = inp["b_nsa_gate"].reshape(L, 1, 24)
    sh["b_forget"] = inp["b_forget"].reshape(L, 4, 1)
    for kv in ("k", "v"):
        w1 = inp["nsa_cmp_w1_" + kv].reshape(L, 32, 64, 128).transpose(0, 2, 1, 3)
        sh["w1_" + kv] = np.ascontiguousarray(np.concatenate([w1, w1], axis=1).reshape(L, 128, 32 * 128))
        pt = inp["nsa_cmp_pos_" + kv].transpose(0, 2, 1)
        sh["pos_" + kv] = np.ascontiguousarray(np.concatenate([pt, pt], axis=1))
        sh["w2_" + kv] = inp["nsa_cmp_w2_" + kv]
    for n in ("w_out", "w_ffn_gate", "w_ffn_up", "w_ffn_down", "w_ple_proj", "w_ple_gate"):
        sh[n] = inp[n]
    sh.update(_consts())
    return sh


class Builder:
    def __init__(self, debug=(), nlayers=L, phases=None):
        self.debug = set(debug)
        self.nlayers = nlayers
        self.phases = phases
        self.nc = bass.Bass("TRN2", target_bir_lowering=False)
        self.dr = {}

    def din(self, name, shape, dt):
        self.dr[name] = self.nc.dram_tensor(name, list(shape), dt, kind="ExternalInput").ap()
        return self.dr[name]

    def dscr(self, name, shape, dt):
        kind = "ExternalOutput" if name in self.debug else "Internal"
        self.dr[name] = self.nc.dram_tensor(name, list(shape), dt, kind=kind).ap()
        return self.dr[name]

    def want(self, ph):
        return self.phases is None or ph in self.phases

    def build(self):
        nc = self.nc
        din, dscr = self.din, self.dscr
        din("x", [S, D], F32)
        din("p", [L, S, 256], F32)
        din("pos", [1, S], I32)
        din("w_in", [L, D, WCOLS], F32)
        for n in ("norm_mix", "norm_ffn", "norm_ple", "head_norm"):
            din(n, [L, 128, 8], F32)
        din("norm_final", [1, D], F32)
        din("b_gate", [L, 1, 24], F32)
        din("b_forget", [L, 4, 1], F32)
        for kv in ("k", "v"):
            din("w1_" + kv, [L, 128, 4096], F32)
            din("pos_" + kv, [L, 128, 32], F32)
            din("w2_" + kv, [L, 128, 64], F32)
        din("w_out", [L, D, D], F32)
        din("w_ffn_gate", [L, D, DFF], F32)
        din("w_ffn_up", [L, D, DFF], F32)
        din("w_ffn_down", [L, DFF, D], F32)
        din("w_ple_proj", [L, 256, D], F32)
        din("w_ple_gate", [L, D, D], F32)
        din("ident", [128, 128], BF16)
        for n in ("m_gt", "m_ge", "m_le"):
            din(n, [128, 4, 512], BF16)
        din("m_cmp", [128, 2, S], BF16)
        din("esel", [128, 32, 128], BF16)
        din("ovl", [128, 2, 64], BF16)
        din("selbias", [S, 64], F32)
        din("invf", [128, 1], F32)
        din("sgn", [128, 1], F32)
        din("ntri", [128, 128], BF16)
        din("nones", [1, 128], BF16)
        din("onec", [128, 1], BF16)
        self.out = nc.dram_tensor("out", [S, D], F32, kind="ExternalOutput").ap()
        dscr("hS", [S, D], F32)
        dscr("cosS", [128, S], F32)
        dscr("sinS", [128, S], F32)
        dscr("FT", [16, 128, S], BF16)
        dscr("cT", [4, S], F32)
        dscr("VA", [S, 8, 65], BF16)
        dscr("SV", [S, 256], BF16)
        dscr("GT", [S, 24], F32)
        dscr("kcT", [128, 256], BF16)
        dscr("VC", [128, 2, 2, 65], BF16)
        dscr("oS", [S, D], F32)
        dscr("WGU", [NF, 128, 2, 8, 128], BF16)
        if "hmix" in self.debug:
            dscr("hmix", [S, D], F32)

        with contextlib.ExitStack() as st:
            self.P = Prog(nc, st)
            self.ps = [st.enter_context(nc.psum_tensor("ps%d" % i, [128, 512], F32)) for i in range(8)]
            self.pt = self.ps[7][:, :].bitcast(BF16)
            self.ps_i = 0
            if self.want("T"):
                self.phase_tables()
            for l in range(self.nlayers):
                if self.want("A"):
                    self.phase_A(l)
                if self.want("B"):
                    self.phase_B(l)
                if self.want("N"):
                    self.phase_nsa(l)
                if self.want("F"):
                    self.phase_fox(l)
                if self.want("SB"):
                    self.phase_sb(l)
                if self.want("D"):
                    self.phase_D(l, last=(l == self.nlayers - 1))
            self.P.op("sp", I("nop"))
            self.P.end_phase(final=True)
        return nc

    def defer(self, fn, *a):
        if not hasattr(self, "_pend"):
            self._pend = []
        self._pend.append((fn, a))

    def run_deferred(self):
        pend = getattr(self, "_pend", [])
        self._pend = []
        for fn, a in pend:
            fn(*a)

    def uname(self, n):
        self._un = getattr(self, "_un", 0) + 1
        return "%s_u%d" % (n, self._un)

    def psn(self):
        i = self.ps_i
        self.ps_i = (i + 1) % 7
        return i

    def phase_tables(self):
        nc, P, dr = self.nc, self.P, self.dr
        with contextlib.ExitStack() as st:
            T = lambda n, sh, dt: st.enter_context(nc.sbuf_tensor(self.uname(n), sh, dt))
            posi = T("t_posi", [128, S], I32)
            ang = T("t_ang", [128, S], F32)
            kk = T("t_kk", [128, S], F32)
            rr = T("t_r", [128, S], F32)
            oo = T("t_o", [128, S], F32)
            invf = T("t_invf", [128, 1], F32)
            sgn = T("t_sgn", [128, 1], F32)
            hpi = T("t_hpi", [128, 1], F32)
            P.dma("sp", I("dma_start", out=posi[:], in_=dr["pos"].to_broadcast([128, S])), writes=["posi"])
            P.dma("sp", I("dma_start", out=invf[:], in_=dr["invf"]), writes=["invf"])
            P.dma("sp", I("dma_start", out=sgn[:], in_=dr["sgn"]), writes=["sgn"])
            P.op("pool", I("memset", hpi[:], float(np.pi / 2)), writes=["hpi"])
            P.op("dve", I("tensor_copy", out=ang[:], in_=posi[:]), reads=["posi"], writes=["ang"])
            P.op("dve", I("tensor_scalar", out=ang[:], in0=ang[:], scalar1=invf[:, 0:1], scalar2=None, op0=ALU.mult),
                 reads=["ang", "invf"], writes=["ang"])
            MAGIC = 12582912.0
            P.op("dve", I("tensor_scalar", out=kk[:], in0=ang[:], scalar1=float(1.0 / (2 * np.pi)), scalar2=MAGIC,
                                                   op0=ALU.mult, op1=ALU.add), reads=["ang"], writes=["kk"])
            P.op("dve", I("tensor_scalar", out=kk[:], in0=kk[:], scalar1=-MAGIC, scalar2=None, op0=ALU.add),
                 reads=["kk"], writes=["kk"])
            C1 = 6.28125
            C2 = float(np.float32(2 * np.pi - C1))
            P.op("dve", I("scalar_tensor_tensor", out=rr[:], in0=kk[:], scalar=-C1, in1=ang[:], op0=ALU.mult, op1=ALU.add),
                 reads=["kk", "ang"], writes=["rr"])
            P.op("dve", I("scalar_tensor_tensor", out=rr[:], in0=kk[:], scalar=-C2, in1=rr[:], op0=ALU.mult, op1=ALU.add),
                 reads=["kk", "rr"], writes=["rr"])
            PL = 3.1415925
            P.op("dve", I("tensor_scalar", out=rr[:], in0=rr[:], scalar1=-PL, scalar2=PL, op0=ALU.max, op1=ALU.min),
                 reads=["rr"], writes=["rr"])
            P.op("act", I("activation", out=oo[:], in_=rr[:], func=AF.Sin), reads=["rr"], writes=["oo"])
            P.op("dve", I("tensor_scalar", out=oo[:], in0=oo[:], scalar1=sgn[:, 0:1], scalar2=None, op0=ALU.mult),
                 reads=["oo", "sgn"], writes=["oo"])
            P.dma("sp", I("dma_start", out=dr["sinS"], in_=oo[:]), reads=["oo"], writes=["sinS"])
            P.op("dve", I("scalar_tensor_tensor", out=kk[:], in0=rr[:], scalar=-1.0, in1=rr[:], op0=ALU.mult, op1=ALU.max),
                 reads=["rr"], writes=["kk"])
            P.op("act", I("activation", out=ang[:], in_=kk[:], func=AF.Sin, bias=hpi[:, 0:1], scale=-1.0),
                 reads=["kk", "hpi", "ang"], writes=["ang"])
            P.dma("sp", I("dma_start", out=dr["cosS"], in_=ang[:]), reads=["ang"], writes=["cosS"])
            P.end_phase()

    def rms_tile(self, T4, i, hx, hxk, jk, ssk, rsk, hn, hnk):
        P = self.P
        junk, ss, rs = T4["junk"], T4["ss"], T4["rs"]
        P.op("act", I("activation", out=junk[:], in_=hx[:], func=AF.Square, accum_out=ss[:]),
             reads=[hxk], writes=[jk, ssk])
        P.op("dve", I("tensor_scalar", out=rs[:], in0=ss[:], scalar1=1.0 / D, scalar2=1e-6, op0=ALU.mult, op1=ALU.add),
             reads=[ssk], writes=[rsk])
        P.op("act", I("sqrt", out=rs[:], in_=rs[:]), reads=[rsk], writes=[rsk])
        P.op("dve", I("reciprocal", out=rs[:], in_=rs[:]), reads=[rsk], writes=[rsk])
        P.op("dve", I("tensor_scalar", out=hn[:], in0=hx[:], scalar1=rs[:, 0:1], scalar2=None, op0=ALU.mult),
             reads=[hxk, rsk], writes=[hnk])

    def transpose8(self, src, srck, dst_ap, dstk, ident, nblk=8, eng="dve"):
        P = self.P
        pt = self.pt
        for c in range(nblk):
            P.op("pe", I("transpose", out=pt[:, c * 128:(c + 1) * 128], in_=src[:, c * 128:(c + 1) * 128],
                                                  identity=ident[:]), reads=[srck, "ident"], writes=[("ps", 7)])
        view = pt[:, 0:nblk * 128].rearrange("p (c t) -> p c t", t=128)
        if eng == "dve":
            P.op("dve", I("tensor_copy", out=dst_ap, in_=view), reads=[("ps", 7)], writes=[dstk])
        else:
            P.op("act", I("copy", out=dst_ap, in_=view), reads=[("ps", 7)], writes=[dstk])

    def load_weight_bf(self, dst_ap, src_ap, stage, stagek, dstk, scale_ap=None, scalek=None, eng="dve", q="sp"):
        P = self.P
        P.dma(q, I("dma_start", out=stage, in_=src_ap), writes=[stagek])
        en = "dve" if eng == "dve" else "pool"
        if scale_ap is not None:
            P.op(en, I("tensor_scalar", out=dst_ap, in0=stage, scalar1=scale_ap, scalar2=None, op0=ALU.mult),
                 reads=[stagek, scalek], writes=[dstk])
        else:
            P.op(en, I("tensor_copy", out=dst_ap, in_=stage), reads=[stagek], writes=[dstk])

    def phase_A(self, l):
        nc, P, dr = self.nc, self.P, self.dr
        hsrc = dr["x"] if l == 0 else dr["hS"]
        with contextlib.ExitStack() as st:
            T = lambda n, sh, dt: st.enter_context(nc.sbuf_tensor(self.uname(n), sh, dt))
            W = T("a_W", [128, 8, WCOLS], BF16)
            stage = [T("a_stage%d" % i, [128, WCOLS], F32) for i in range(2)]
            cosT = T("a_cos", [128, S], F32)
            sinT = T("a_sin", [128, S], F32)
            gcol = T("a_gcol", [128, 8], F32)
            ident = T("a_ident", [128, 128], BF16)
            bgate = T("a_bgate", [128, 24], F32)
            negb = T("a_negb", [4, 1], F32)
            ones4 = T("a_ones4", [4, CH], F32)
            cc = T("a_cc", [4, S], F32)
            hx = [T("a_hx%d" % i, [128, D], F32) for i in range(2)]
            T4 = [dict(junk=T("a_junk%d" % i, [128, D], BF16), ss=T("a_ss%d" % i, [128, 1], F32),
                       rs=T("a_rs%d" % i, [128, 1], F32)) for i in range(2)]
            hn = [T("a_hn%d" % i, [128, D], BF16) for i in range(2)]
            hnT = [T("a_hnT%d" % i, [128, 8, CH], BF16) for i in range(2)]
            t1 = [T("a_t1%d" % i, [128, CH], F32) for i in range(2)]
            t2 = [T("a_t2%d" % i, [128, CH], F32) for i in range(2)]
            ob = [T("a_ob%d" % i, [128, CH], BF16) for i in range(4)]
            va = [T("a_va%d" % i, [128, 8, 65], BF16) for i in range(2)]
            svt = [T("a_sv%d" % i, [128, 256], BF16) for i in range(2)]
            gt = [T("a_gt%d" % i, [128, 24], F32) for i in range(2)]
            e4 = T("a_e4", [4, CH], F32)
            sp4 = T("a_sp4", [4, CH], F32)

            P.dma("sp", I("dma_start", out=gcol[:], in_=dr["norm_mix"][l]), writes=["gcol"])
            P.dma("sp", I("dma_start", out=ident[:], in_=dr["ident"]), writes=["ident"])
            P.dma("sp", I("dma_start", out=cosT[:], in_=dr["cosS"]), writes=["cosT"])
            P.dma("sp", I("dma_start", out=sinT[:], in_=dr["sinS"]), writes=["sinT"])
            P.dma("sp", I("dma_start", out=bgate[:], in_=dr["b_gate"][l].to_broadcast([128, 24])), writes=["bgate"])
            P.dma("sp", I("dma_start", out=negb[:], in_=dr["b_forget"][l]), writes=["negb"])
            P.op("dve", I("tensor_scalar", out=negb[:], in0=negb[:], scalar1=-1.0, scalar2=None, op0=ALU.mult),
                 reads=["negb"], writes=["negb"])
            P.op("pool", I("memset", ones4[:], 1.0), writes=["ones4"])
            for i in range(2):
                P.op("pool", I("memset", va[i][:], 1.0), writes=[("va", i)])
            for k in range(8):
                self.load_weight_bf(W[:, k, :], dr["w_in"][l, k * 128:(k + 1) * 128, :], stage[k % 2][:], ("stage", k % 2),
                                    ("W", k), scale_ap=gcol[:, k:k + 1], scalek="gcol", eng=("dve" if k % 2 == 0 else "pool"),
                                    q=("sp" if k % 2 == 0 else "act"))
            Wk = [("W", k) for k in range(8)]
            obi = 0
            for c in range(NCH):
                hb = c % 2
                csl = slice(c * CH, (c + 1) * CH)
                for i in range(4):
                    ti = c * 4 + i
                    b = ti % 2
                    P.dma("sp", I("dma_start", out=hx[b][:], in_=hsrc[ti * 128:(ti + 1) * 128, :]),
                          writes=[("hx", b)])
                    self.rms_tile(T4[b], b, hx[b], ("hx", b), ("junk", b), ("ss", b), ("rs", b), hn[b], ("hn", b))
                    self.transpose8(hn[b], ("hn", b), hnT[hb][:, :, i * 128:(i + 1) * 128], ("hnT", hb, i), ident,
                                    eng=("dve" if i % 2 == 0 else "act"))
                hk = [("hnT", hb, i) for i in range(4)]

                def fm_matmul(pi, col0, ncols=128):
                    for k in range(8):
                        P.op("pe", I("matmul", self.ps[pi][0:ncols, :], lhsT=W[:, k, col0:col0 + ncols],
                                                          rhs=hnT[hb][:, k, :], start=(k == 0), stop=(k == 7)),
                             reads=hk + Wk, writes=[("ps", pi)])
                for ft in range(7):
                    pa, pb = self.psn(), self.psn()
                    fm_matmul(pa, ft * 128)
                    fm_matmul(pb, (7 + ft) * 128)
                    tb = ft % 2
                    P.op("dve", I("tensor_tensor", out=t1[tb][:], in0=self.ps[pa][:], in1=cosT[:, csl], op=ALU.mult),
                         reads=[("ps", pa), "cosT"], writes=[("t1", tb)])
                    P.op("dve", I("tensor_tensor", out=t2[tb][:], in0=self.ps[pb][:], in1=sinT[:, csl], op=ALU.mult),
                         reads=[("ps", pb), "sinT"], writes=[("t2", tb)])
                    o = obi % 4
                    obi += 1
                    P.op("pool", I("tensor_tensor", out=ob[o][:], in0=t1[tb][:], in1=t2[tb][:], op=ALU.add),
                         reads=[("t1", tb), ("t2", tb)], writes=[("ob", o)])
                    P.dma("sp", I("dma_start", out=dr["FT"][ft, :, csl], in_=ob[o][:]),
                          reads=[("ob", o)], writes=[("FT", ft, c)])
                for ft in range(14, 23):
                    pa = self.psn()
                    fm_matmul(pa, ft * 128)
                    o = obi % 4
                    obi += 1
                    sc = 0.125 if ft in (15, 16, 19, 20) else 1.0
                    P.op("act", I("activation", out=ob[o][:], in_=self.ps[pa][:], func=AF.Copy, scale=sc),
                         reads=[("ps", pa)], writes=[("ob", o)])
                    P.dma("sp", I("dma_start", out=dr["FT"][ft - 7, :, csl], in_=ob[o][:]),
                          reads=[("ob", o)], writes=[("FT", ft, c)])
                pa = self.psn()
                fm_matmul(pa, 23 * 128, ncols=4)
                P.op("act", I("activation", out=e4[:], in_=self.ps[pa][0:4, :], func=AF.Exp, bias=negb[:, 0:1], scale=-1.0),
                     reads=[("ps", pa), "negb"], writes=["e4"])
                P.op("act", I("activation", out=sp4[:], in_=e4[:], func=AF.Ln, bias=1.0, scale=1.0), reads=["e4"], writes=["sp4"])
                if c == 0:
                    P.op("dve", I("tensor_tensor_scan", out=cc[:, csl], data0=ones4[:], data1=sp4[:], initial=0.0,
                                                                op0=ALU.mult, op1=ALU.subtract), reads=["sp4", "ones4"], writes=["cc"])
                else:
                    P.op("dve", I("tensor_tensor_scan", out=cc[:, csl], data0=ones4[:], data1=sp4[:],
                                                                     initial=cc[:, c * CH - 1:c * CH],
                                                                     op0=ALU.mult, op1=ALU.subtract), reads=["sp4", "ones4", "cc"], writes=["cc"])
                c0 = 23 * 128 + 4
                for i in range(4):
                    ti = c * 4 + i
                    b = ti % 2
                    pa, pb = self.psn(), self.psn()
                    for k in range(8):
                        P.op("pe", I("matmul", self.ps[pa][:, 0:512], lhsT=hnT[hb][:, k, i * 128:(i + 1) * 128],
                                                                    rhs=W[:, k, c0:c0 + 512], start=(k == 0), stop=(k == 7)),
                             reads=hk + Wk, writes=[("ps", pa)])
                    for k in range(8):
                        P.op("pe", I("matmul", self.ps[pb][:, 0:280], lhsT=hnT[hb][:, k, i * 128:(i + 1) * 128],
                                                                    rhs=W[:, k, c0 + 512:c0 + 792], start=(k == 0), stop=(k == 7)),
                             reads=hk + Wk, writes=[("ps", pb)])
                    P.op("act", I("copy", out=va[b][:, :, 0:64], in_=self.ps[pa][:, 0:512].rearrange("p (g d) -> p g d", d=64)),
                         reads=[("ps", pa)], writes=[("va", b)])
                    P.op("dve", I("tensor_copy", out=svt[b][:], in_=self.ps[pb][:, 0:256]),
                         reads=[("ps", pb)], writes=[("svt", b)])
                    P.op("dve", I("tensor_tensor", out=gt[b][:], in0=self.ps[pb][:, 256:280], in1=bgate[:], op=ALU.add),
                         reads=[("ps", pb), "bgate"], writes=[("gt", b)])
                    P.op("act", I("activation", out=gt[b][:], in_=gt[b][:], func=AF.Sigmoid), reads=[("gt", b)], writes=[("gt", b)])
                    rows = slice(ti * 128, (ti + 1) * 128)
                    P.dma("sp", I("dma_start", out=dr["VA"][rows], in_=va[b][:]), reads=[("va", b)], writes=[("VA", ti)])
                    P.dma("sp", I("dma_start", out=dr["SV"][rows], in_=svt[b][:]), reads=[("svt", b)], writes=[("SV", ti)])
                    P.dma("sp", I("dma_start", out=dr["GT"][rows], in_=gt[b][:]), reads=[("gt", b)], writes=[("GT", ti)])
            P.dma("sp", I("dma_start", out=dr["cT"], in_=cc[:]), reads=["cc"], writes=["cT"])
            P.end_phase()

    def phase_B(self, l):
        nc, P, dr = self.nc, self.P, self.dr
        with contextlib.ExitStack() as st:
            T = lambda n, sh, dt: st.enter_context(nc.sbuf_tensor(self.uname(n), sh, dt))
            kvT = {"k": T("b_kT", [128, S], BF16), "v": T("b_vT", [128, S], BF16)}
            stage = T("b_stage", [128, 4096], F32)
            W1 = {kv: T("b_w1" + kv, [128, 32, 128], BF16) for kv in "kv"}
            posT = {kv: T("b_pos" + kv, [128, 32], BF16) for kv in "kv"}
            W2 = {kv: T("b_w2" + kv, [128, 64], BF16) for kv in "kv"}
            st32 = T("b_st32", [128, 32], F32)
            st64 = T("b_st64", [128, 64], F32)
            bias = T("b_bias", [128, 1], F32)
            xs = T("b_xs", [128, 255], F32)
            x2 = T("b_x2", [128, 255], F32)
            sg = T("b_sg", [128, 255], F32)
            gl = T("b_gl", [128, 256], BF16)
            kc = T("b_kc", [128, 256], BF16)
            vc = T("b_vc", [128, 2, 2, 65], BF16)
            P.dma("sp", I("dma_start", out=kvT["k"][:], in_=dr["FT"][4]), writes=["kT"])
            P.dma("sp", I("dma_start", out=kvT["v"][:], in_=dr["FT"][7]), writes=["vT"])
            P.op("pool", I("memset", vc[:], 1.0), writes=["vc"])
            P.op("pool", I("memset", gl[:], 0.0), writes=["gl"])
            for kv in "kv":
                self.load_weight_bf(W1[kv][:].rearrange("p l h -> p (l h)"), dr["w1_" + kv][l], stage[:], "stage", "W1" + kv)
                self.load_weight_bf(posT[kv][:], dr["pos_" + kv][l], st32[:], "st32", "pos" + kv)
                self.load_weight_bf(W2[kv][:], dr["w2_" + kv][l], st64[:], "st64", "W2" + kv)
            for kv in "kv":
                pb = self.psn()
                for ll in range(32):
                    P.op("pe", I("matmul", self.ps[pb][:, 0:1], lhsT=W1[kv][0:64, ll, :], rhs=posT[kv][0:64, ll:ll + 1],
                                                                start=(ll == 0), stop=(ll == 31)),
                         reads=["W1" + kv, "pos" + kv], writes=[("ps", pb)])
                P.op("dve", I("tensor_copy", out=bias[:], in_=self.ps[pb][:, 0:1]), reads=[("ps", pb)], writes=["bias"])
                for g in range(2):
                    gs = slice(64 * g, 64 * g + 64)
                    pa = self.psn()
                    for ll in range(32):
                        P.op("pe", I("matmul",
                            self.ps[pa][:, 0:255], lhsT=W1[kv][gs, ll, :], rhs=kvT[kv][gs, ll:ll + 16 * 254 + 1:16],
                            start=(ll == 0), stop=(ll == 31)), reads=["W1" + kv, kv + "T"], writes=[("ps", pa)])
                    P.op("act", I("activation", out=xs[:], in_=self.ps[pa][:, 0:255], func=AF.Identity, bias=bias[:, 0:1], scale=1.0),
                         reads=[("ps", pa), "bias"], writes=["xs"])
                    P.op("dve", I("tensor_tensor", out=x2[:], in0=xs[:], in1=xs[:], op=ALU.mult), reads=["xs"], writes=["x2"])
                    P.op("dve", I("tensor_scalar", out=x2[:], in0=x2[:], scalar1=0.044715, scalar2=1.0, op0=ALU.mult, op1=ALU.add),
                         reads=["x2"], writes=["x2"])
                    P.op("dve", I("tensor_tensor", out=x2[:], in0=x2[:], in1=xs[:], op=ALU.mult), reads=["x2", "xs"], writes=["x2"])
                    P.op("act", I("activation", out=sg[:], in_=x2[:], func=AF.Sigmoid, scale=1.5957691216057308),
                         reads=["x2"], writes=["sg"])
                    P.op("dve", I("tensor_tensor", out=gl[:, 0:255], in0=xs[:], in1=sg[:], op=ALU.mult), reads=["xs", "sg"], writes=["gl"])
                    if kv == "k":
                        po = self.psn()
                        P.op("pe", I("matmul", self.ps[po][gs, 0:256], lhsT=W2["k"][:, :], rhs=gl[:, :], start=True, stop=True),
                             reads=["W2k", "gl"], writes=[("ps", po)])
                        P.op("dve", I("tensor_copy", out=kc[gs, :], in_=self.ps[po][gs, 0:256]),
                             reads=[("ps", po)], writes=[("kc", g)])
                    else:
                        for nb in range(2):
                            po = self.psn()
                            P.op("pe", I("matmul", self.ps[po][:, 0:64], lhsT=gl[:, nb * 128:(nb + 1) * 128], rhs=W2["v"][:, :],
                                                                     start=True, stop=True), reads=["W2v", "gl"], writes=[("ps", po)])
                            P.op("dve", I("tensor_copy", out=vc[:, g, nb, 0:64], in_=self.ps[po][:, 0:64]),
                                 reads=[("ps", po)], writes=["vc"])
            P.dma("sp", I("dma_start", out=dr["kcT"], in_=kc[:]), reads=[("kc", 0), ("kc", 1)], writes=["kcT"])
            P.dma("sp", I("dma_start", out=dr["VC"], in_=vc[:]), reads=["vc"], writes=["VC"])
            P.end_phase()

    def phase_fox(self, l):
        nc, P, dr, ps = self.nc, self.P, self.dr, self.ps
        if "cH" not in dr:
            self.dscr("cH", [4, 3, S], BF16)
        with contextlib.ExitStack() as st:
            T = lambda n, sh, dt: st.enter_context(nc.sbuf_tensor(self.uname(n), sh, dt))
            QaT = T("f_QaT", [70, 4, S], BF16)
            KaT = T("f_KaT", [70, 4, S], BF16)
            V = T("f_V", [128, 32, 4, 65], BF16)
            c4 = T("f_c4", [4, S], F32)
            rr = T("f_rr", [4, S], F32)
            H = T("f_H", [4, 3, S], BF16)
            mgt = T("f_mgt", [128, 4, 512], BF16)
            ident = T("f_ident", [128, 128], BF16)
            Pt = [T("f_P%d" % i, [128, CH], BF16) for i in range(3)]
            rs = [T("f_rs%d" % i, [128, 1], F32) for i in range(4)]
            of = [T("f_of%d" % i, [128, 4, 64], F32) for i in range(8)]
            P.dma("sp", I("dma_start", out=mgt[:], in_=dr["m_gt"]), writes=["mgt"])
            P.dma("sp", I("dma_start", out=ident[:], in_=dr["ident"]), writes=["ident"])
            P.dma("sp", I("dma_start", out=c4[:], in_=dr["cT"]), writes=["c4"])
            for q8 in range(8):
                P.dma("sp", I("dma_start", out=V[:, q8 * 4:(q8 + 1) * 4, :, :],
                              in_=dr["VA"][q8 * 512:(q8 + 1) * 512, 4:8, :].rearrange("(sb p) h c -> p sb h c", p=128)), writes=["V"])
            P.op("pool", I("memset", QaT[64:70, :, :], -1.0), writes=["Qaug"])
            P.op("pool", I("memset", KaT[64:70, :, :], 1.0), writes=["Kaug"])
            for hh in range(4):
                src_q = dr["FT"][8 + hh // 2, (hh % 2) * 64:(hh % 2) * 64 + 64, :]
                src_k = dr["FT"][10 + hh // 2, (hh % 2) * 64:(hh % 2) * 64 + 64, :]
                P.dma("sp", I("dma_start", out=QaT[0:64, hh, :], in_=src_q), writes=[("Q", hh)])
                P.dma("act", I("dma_start", out=KaT[0:64, hh, :], in_=src_k), writes=[("K", hh)])
            P.op("dve", I("tensor_copy", out=H[:, 0, :], in_=c4[:]), reads=["c4"], writes=["H0"])
            P.op("dve", I("tensor_tensor", out=rr[:], in0=c4[:], in1=H[:, 0, :], op=ALU.subtract), reads=["c4", "H0"], writes=["rr"])
            P.op("dve", I("tensor_copy", out=H[:, 1, :], in_=rr[:]), reads=["rr"], writes=["H1"])
            P.op("dve", I("tensor_tensor", out=rr[:], in0=rr[:], in1=H[:, 1, :], op=ALU.subtract), reads=["rr", "H1"], writes=["rr"])
            P.op("dve", I("tensor_copy", out=H[:, 2, :], in_=rr[:]), reads=["rr"], writes=["H2"])
            P.dma("sp", I("dma_start", out=dr["cH"], in_=H[:]), reads=["H0", "H1", "H2"], writes=["cH"])
            for hh in range(4):
                P.dma("sp", I("dma_start", out=QaT[64:67, hh, :], in_=dr["cH"][hh]), reads=["cH", "Qaug"], writes=[("Qa", hh)])
                P.dma("sp", I("dma_start", out=KaT[67:70, hh, :], in_=dr["cH"][hh]), reads=["cH", "Kaug"], writes=[("Ka", hh)])
            sti = 0
            pi = 0
            for qc in range(NCH):
                csl = slice(qc * CH, (qc + 1) * CH)
                for hh in range(4):
                    qk = [("Q", hh), ("Qa", hh), ("K", hh), ("Ka", hh), "Qaug", "Kaug"]
                    for sb in range(4 * qc + 4):
                        diag = sb >= 4 * qc
                        b = sti % 3
                        sti += 1
                        P.op("pe", I("matmul", ps[b][:, :], lhsT=KaT[0:70, hh, sb * 128:(sb + 1) * 128], rhs=QaT[0:70, hh, csl],
                                     start=True, stop=not diag), reads=qk, writes=[("ps", b)])
                        if diag:
                            P.op("pe", I("matmul", ps[b][:, :], lhsT=ident[:, :], rhs=mgt[:, sb - 4 * qc, :], start=False, stop=True),
                                 reads=["ident", "mgt"], writes=[("ps", b)])
                        pb = pi % 3
                        pi += 1
                        P.op("act", I("activation", out=Pt[pb][:], in_=ps[b][:, :], func=AF.Exp), reads=[("ps", b)], writes=[("P", pb)])
                        def back(sb, pb, hh, qc):
                            for ti in range(4):
                                if sb <= 4 * qc + ti:
                                    P.op("pe", I("matmul", ps[3 + ti][:, 0:65], lhsT=Pt[pb][:, ti * 128:(ti + 1) * 128], rhs=V[:, sb, hh, :],
                                                 start=(sb == 0), stop=(sb == 4 * qc + ti)), reads=[("P", pb), "V"], writes=[("ps", 3 + ti)])
                        self.run_deferred()
                        self.defer(back, sb, pb, hh, qc)

                    def fin(hh, qc):
                        for ti in range(4):
                            P.op("dve", I("tensor_scalar", out=rs[ti][:], in0=ps[3 + ti][:, 64:65], scalar1=1e-20, scalar2=None, op0=ALU.max),
                                 reads=[("ps", 3 + ti)], writes=[("rs", ti)])
                        for ti in range(4):
                            P.op("dve", I("reciprocal", out=rs[ti][:], in_=rs[ti][:]), reads=[("rs", ti)], writes=[("rs", ti)])
                        for ti in range(4):
                            o = (qc % 2) * 4 + ti
                            P.op("dve", I("tensor_scalar", out=of[o][:, hh, :], in0=ps[3 + ti][:, 0:64], scalar1=rs[ti][:, 0:1], scalar2=None, op0=ALU.mult),
                                 reads=[("ps", 3 + ti), ("rs", ti)], writes=[("of", o, hh)])
                    self.defer(fin, hh, qc)

                def store(qc):
                    for ti in range(4):
                        o = (qc % 2) * 4 + ti
                        rows = slice((qc * 4 + ti) * 128, (qc * 4 + ti + 1) * 128)
                        P.dma("sp", I("dma_start", out=dr["oS"][rows, 512:768], in_=of[o][:].rearrange("p h d -> p (h d)")),
                              reads=[("of", o, hh) for hh in range(4)], writes=[("oS", "f", qc, ti)])
                self.defer(store, qc)
            self.run_deferred()
            P.end_phase()

    def phase_sb(self, l):
        nc, P, dr, ps = self.nc, self.P, self.dr, self.ps
        with contextlib.ExitStack() as st:
            T = lambda n, sh, dt: st.enter_context(nc.sbuf_tensor(self.uname(n), sh, dt))
            QT = T("s_QT", [128, 2, S], BF16)
            KT = T("s_KT", [128, 2, S], BF16)
            V = T("s_V", [128, 32, 256], BF16)
            mge = T("s_mge", [128, 4, 512], BF16)
            ident = T("s_ident", [128, 128], BF16)
            ntri = T("s_ntri", [128, 128], BF16)
            nones = T("s_nones", [1, 128], BF16)
            onec = T("s_onec", [128, 1], BF16)
            et = [T("s_e%d" % i, [128, CH], F32) for i in range(2)]
            SP = [T("s_SP%d" % i, [128, CH], BF16) for i in range(3)]
            at = [T("s_a%d" % i, [128, CH], BF16) for i in range(2)]
            carry = T("s_carry", [1, CH], F32)
            ctmp = T("s_ctmp", [1, CH], F32)
            chi = [T("s_chi%d" % i, [1, CH], BF16) for i in range(3)]
            clo = [T("s_clo%d" % i, [1, CH], BF16) for i in range(3)]
            osb = [T("s_o%d" % i, [128, 4, 64], F32) for i in range(8)]
            for n, t_ in (("m_ge", mge), ("ident", ident), ("ntri", ntri), ("nones", nones), ("onec", onec)):
                P.dma("sp", I("dma_start", out=t_[:], in_=dr[n]), writes=[n])
            for j in range(2):
                P.dma("sp", I("dma_start", out=QT[:, j, :], in_=dr["FT"][12 + j]), writes=[("Q", j)])
                P.dma("act", I("dma_start", out=KT[:, j, :], in_=dr["FT"][14 + j]), writes=[("K", j)])
            for q8 in range(8):
                P.dma("sp", I("dma_start", out=V[:, q8 * 4:(q8 + 1) * 4, :],
                              in_=dr["SV"][q8 * 512:(q8 + 1) * 512, :].rearrange("(sb p) c -> p sb c", p=128)), writes=["V"])
            cnt = {"a": 0, "e": 0, "sp": 0, "at": 0, "c": 0}
            for qc in range(NCH):
                csl = slice(qc * CH, (qc + 1) * CH)
                for h in range(4):
                    hb = slice(64 * (h % 2), 64 * (h % 2) + 64)
                    j = h // 2
                    qk = [("Q", j), ("K", j)]
                    blocks = list(range(4 * qc + 3, -1, -1))
                    nblk = len(blocks)
                    info = {}

                    def S1a(idx):
                        sb = blocks[idx]
                        diag = sb >= 4 * qc
                        P.op("pe", I("matmul", ps[0][:, :], lhsT=KT[hb, j, sb * 128:(sb + 1) * 128], rhs=QT[hb, j, csl], start=True, stop=not diag),
                             reads=qk, writes=[("ps", 0)])
                        if diag:
                            P.op("pe", I("matmul", ps[0][:, :], lhsT=ident[:, :], rhs=mge[:, sb - 4 * qc, :], start=False, stop=True),
                                 reads=["ident", "m_ge"], writes=[("ps", 0)])
                        eb = cnt["e"] % 2
                        cnt["e"] += 1
                        P.op("act", I("activation", out=et[eb][:], in_=ps[0][:, :], func=AF.Exp), reads=[("ps", 0)], writes=[("e", eb)])
                        sb_i = cnt["sp"] % 3
                        cnt["sp"] += 1
                        P.op("act", I("activation", out=SP[sb_i][:], in_=et[eb][:], func=AF.Ln, bias=1.0, scale=1.0),
                             reads=[("e", eb)], writes=[("SP", sb_i)])
                        info[idx] = sb_i

                    def S1b(idx):
                        sb_i = info[idx]
                        P.op("pe", I("matmul", ps[3][0:1, :], lhsT=onec[:, 0:1], rhs=SP[sb_i][:, :], start=(idx == 0), stop=(idx + 2 == nblk)),
                             reads=["onec", ("SP", sb_i)], writes=[("ps", 3)])
                        cb = (idx + 1) % 3
                        P.op("dve", I("tensor_copy", out=chi[cb][:], in_=ps[3][0:1, :]), reads=[("ps", 3)], writes=[("chi", cb)])
                        P.op("dve", I("tensor_tensor", out=clo[cb][:], in0=ps[3][0:1, :], in1=chi[cb][:], op=ALU.subtract),
                             reads=[("ps", 3), ("chi", cb)], writes=[("clo", cb)])

                    def S2a(idx):
                        sb = blocks[idx]
                        diag = sb >= 4 * qc
                        sb_i = info[idx]
                        bb = 1 + (cnt["a"] % 2)
                        cnt["a"] += 1
                        P.op("pe", I("matmul", ps[bb][:, :], lhsT=KT[hb, j, sb * 128:(sb + 1) * 128], rhs=QT[hb, j, csl], start=True, stop=False),
                             reads=qk, writes=[("ps", bb)])
                        if diag:
                            P.op("pe", I("matmul", ps[bb][:, :], lhsT=ident[:, :], rhs=mge[:, sb - 4 * qc, :], start=False, stop=False),
                                 reads=["ident", "m_ge"], writes=[("ps", bb)])
                        last = (idx == 0)
                        P.op("pe", I("matmul", ps[bb][:, :], lhsT=ntri[:, :], rhs=SP[sb_i][:, :], start=False, stop=last),
                             reads=["ntri", ("SP", sb_i)], writes=[("ps", bb)])
                        if idx > 0:
                            cb = idx % 3
                            P.op("pe", I("matmul", ps[bb][:, :], lhsT=nones[0:1, :], rhs=chi[cb][0:1, :], start=False, stop=False),
                                 reads=["nones", ("chi", cb)], writes=[("ps", bb)])
                            P.op("pe", I("matmul", ps[bb][:, :], lhsT=nones[0:1, :], rhs=clo[cb][0:1, :], start=False, stop=True),
                                 reads=["nones", ("clo", cb)], writes=[("ps", bb)])
                        ab = cnt["at"] % 2
                        cnt["at"] += 1
                        P.op("act", I("activation", out=at[ab][:], in_=ps[bb][:, :], func=AF.Exp), reads=[("ps", bb)], writes=[("at", ab)])
                        info[("ab", idx)] = ab

                    def S2b(idx):
                        sb = blocks[idx]
                        ab = info[("ab", idx)]
                        for ti in range(4):
                            if sb <= 4 * qc + ti:
                                P.op("pe", I("matmul", ps[4 + ti][:, 0:64], lhsT=at[ab][:, ti * 128:(ti + 1) * 128], rhs=V[:, sb, h * 64:(h + 1) * 64],
                                             start=(sb == 4 * qc + ti), stop=(sb == 0)), reads=[("at", ab), "V"], writes=[("ps", 4 + ti)])

                    S1a(0)
                    if nblk > 1:
                        S1a(1)
                        S1b(0)
                    for idx in range(nblk):
                        if idx + 2 < nblk:
                            S1a(idx + 2)
                            S1b(idx + 1)
                        S2a(idx)
                        if idx >= 1:
                            S2b(idx - 1)
                    S2b(nblk - 1)
                    for ti in range(4):
                        o = (qc % 2) * 4 + ti
                        P.op("dve", I("tensor_copy", out=osb[o][:, h, :], in_=ps[4 + ti][:, 0:64]), reads=[("ps", 4 + ti)], writes=[("osb", o, h)])
                for ti in range(4):
                    o = (qc % 2) * 4 + ti
                    rows = slice((qc * 4 + ti) * 128, (qc * 4 + ti + 1) * 128)
                    P.dma("sp", I("dma_start", out=dr["oS"][rows, 768:1024], in_=osb[o][:].rearrange("p h d -> p (h d)")),
                          reads=[("osb", o, h) for h in range(4)], writes=[("oS", "s", qc, ti)])
            P.end_phase()

    def phase_nsa(self, l):
        nc, P, dr, ps = self.nc, self.P, self.dr, self.ps
        pt = self.pt
        with contextlib.ExitStack() as st:
            T = lambda n, sh, dt: st.enter_context(nc.sbuf_tensor(self.uname(n), sh, dt))
            QT = T("n_QT", [128, 4, S], BF16)
            KsT = T("n_KsT", [128, S], BF16)
            KwT = T("n_KwT", [128, S], BF16)
            kcT = T("n_kcT", [128, 256], BF16)
            V4 = T("n_V4", [128, 32, 4, 65], BF16)
            VC = T("n_VC", [128, 2, 2, 65], BF16)
            ovl = T("n_ovl", [128, 2, 64], BF16)
            mgt = T("n_mgt", [128, 4, 512], BF16)
            mle = T("n_mle", [128, 4, 512], BF16)
            esel = T("n_esel", [128, 32, 128], BF16)
            ident = T("n_ident", [128, 128], BF16)
            mcmp = [T("n_mcmp%d" % i, [128, 2, CH], BF16) for i in range(2)]
            sbias = [T("n_sbias%d" % i, [128, 64], F32) for i in range(4)]
            gts = [T("n_gt%d" % i, [128, 24], F32) for i in range(8)]
            Pt = [T("n_P%d" % i, [128, CH], BF16) for i in range(3)]
            Pc = [T("n_Pc%d" % i, [128, CH], BF16) for i in range(4)]
            onsa = [T("n_o%d" % i, [128, 8, 64], F32) for i in range(8)]
            impg = [T("n_imp%d" % i, [128, 64], F32) for i in range(4)]
            score = T("n_score", [128, 64], F32)
            sc2 = T("n_sc2", [128, 64], F32)
            sc3 = T("n_sc3", [128, 64], F32)
            m8a = T("n_m8a", [128, 8], F32)
            m8b = T("n_m8b", [128, 8], F32)
            seln = T("n_seln", [128, 64], BF16)
            selT = T("n_selT", [128, CH], BF16)
            rc4 = T("n_rc4", [128, 4], F32)
            rg = T("n_rg", [128, 1], F32)
            rs1 = T("n_rs1", [128, 1], F32)
            self._nsa_tmp = ([T("n_r4%d" % i, [128, 1], F32) for i in range(4)], [T("n_g4%d" % i, [128, 1], F32) for i in range(4)],
                             [T("n_s4%d" % i, [128, 64], F32) for i in range(4)])
            for n, t_ in (("m_gt", mgt), ("m_le", mle), ("esel", esel), ("ident", ident), ("ovl", ovl)):
                P.dma("sp", I("dma_start", out=t_[:], in_=dr[n]), writes=[n])
            for j in range(4):
                P.dma("sp" if j % 2 == 0 else "act", I("dma_start", out=QT[:, j, :], in_=dr["FT"][j]), writes=[("Q", j)])
            P.dma("sp", I("dma_start", out=KsT[:], in_=dr["FT"][5]), writes=["KsT"])
            P.dma("act", I("dma_start", out=KwT[:], in_=dr["FT"][6]), writes=["KwT"])
            P.dma("sp", I("dma_start", out=kcT[:], in_=dr["kcT"]), writes=["kcT"])
            P.dma("sp", I("dma_start", out=VC[:], in_=dr["VC"]), writes=["VC"])
            for q8 in range(8):
                P.dma("sp", I("dma_start", out=V4[:, q8 * 4:(q8 + 1) * 4, :, :],
                              in_=dr["VA"][q8 * 512:(q8 + 1) * 512, 0:4, :].rearrange("(sb p) h c -> p sb h c", p=128)), writes=["V4"])
            P.op("pool", I("memset", selT[:], 0.0), writes=["selT"])
            cn = {"st": 0, "p": 0}

            def st_bank():
                b = cn["st"] % 3
                cn["st"] += 1
                return b

            def exp_to(b, dst, dstk, scale=0.125):
                P.op("act", I("activation", out=dst[:], in_=ps[b][:, :], func=AF.Exp, scale=scale), reads=[("ps", b)], writes=[dstk])

            for qc in range(NCH):
                csl = slice(qc * CH, (qc + 1) * CH)
                mb = qc % 2
                P.dma("sp", I("dma_start", out=mcmp[mb][:], in_=dr["m_cmp"][:, :, csl]), writes=[("mcmp", mb)])
                for ti in range(4):
                    rows = slice((qc * 4 + ti) * 128, (qc * 4 + ti + 1) * 128)
                    P.dma("sp", I("dma_start", out=sbias[ti][:], in_=dr["selbias"][rows]), writes=[("sbias", ti)])
                    P.dma("sp", I("dma_start", out=gts[mb * 4 + ti][:], in_=dr["GT"][rows]), writes=[("gts", mb * 4 + ti)])
                nbs = [0] if qc < 4 else [0, 1]
                for g in range(2):
                    gs = slice(64 * g, 64 * g + 64)
                    self.run_deferred()
                    for hh in range(4):
                        head = 4 * g + hh
                        for nb in nbs:
                            b = st_bank()
                            P.op("pe", I("matmul", ps[b][:, :], lhsT=kcT[gs, nb * 128:(nb + 1) * 128], rhs=QT[gs, hh, csl], start=True, stop=False),
                                 reads=["kcT", ("Q", hh)], writes=[("ps", b)])
                            P.op("pe", I("matmul", ps[b][:, :], lhsT=ident[:, :], rhs=mcmp[mb][:, nb, :], start=False, stop=True),
                                 reads=["ident", ("mcmp", mb)], writes=[("ps", b)])
                            pk = (hh % 2) * 2 + nb
                            exp_to(b, Pc[pk], ("Pc", pk))
                        for ti in range(4):
                            for nb in nbs:
                                pk = (hh % 2) * 2 + nb
                                P.op("pe", I("matmul", ps[3 + ti][:, 0:65], lhsT=Pc[pk][:, ti * 128:(ti + 1) * 128], rhs=VC[:, g, nb, :],
                                             start=(nb == nbs[0]), stop=(nb == nbs[-1])), reads=[("Pc", pk), "VC"], writes=[("ps", 3 + ti)])
                            for nb in nbs:
                                pk = (hh % 2) * 2 + nb
                                P.op("pe", I("matmul", ps[3 + ti][:, 128:192], lhsT=Pc[pk][:, ti * 128:(ti + 1) * 128], rhs=ovl[:, nb, :],
                                             start=(nb == nbs[0]), stop=(nb == nbs[-1])), reads=[("Pc", pk), "ovl"], writes=[("ps", 3 + ti)])
                        r4, g4, s4 = self._nsa_tmp
                        for ti in range(4):
                            P.op("dve", I("tensor_scalar", out=r4[ti][:], in0=ps[3 + ti][:, 64:65], scalar1=1e-20, scalar2=None, op0=ALU.max),
                                 reads=[("ps", 3 + ti)], writes=[("r4", ti)])
                        for ti in range(4):
                            P.op("dve", I("reciprocal", out=r4[ti][:], in_=r4[ti][:]), reads=[("r4", ti)], writes=[("r4", ti)])
                        for ti in range(4):
                            o = mb * 4 + ti
                            P.op("dve", I("tensor_tensor", out=g4[ti][:], in0=r4[ti][:], in1=gts[o][:, head * 3:head * 3 + 1], op=ALU.mult),
                                 reads=[("r4", ti), ("gts", o)], writes=[("g4", ti)])
                        for ti in range(4):
                            if hh == 0:
                                P.op("dve", I("tensor_scalar", out=impg[ti][:], in0=ps[3 + ti][:, 128:192], scalar1=r4[ti][:, 0:1], scalar2=None, op0=ALU.mult),
                                     reads=[("ps", 3 + ti), ("r4", ti)], writes=[("impg", ti)])
                            else:
                                P.op("dve", I("tensor_scalar", out=s4[ti][:], in0=ps[3 + ti][:, 128:192], scalar1=r4[ti][:, 0:1], scalar2=None, op0=ALU.mult),
                                     reads=[("ps", 3 + ti), ("r4", ti)], writes=[("s4", ti)])
                        if hh > 0:
                            for ti in range(4):
                                P.op("pool", I("tensor_tensor", out=impg[ti][:], in0=impg[ti][:], in1=s4[ti][:], op=ALU.add),
                                     reads=[("s4", ti), ("impg", ti)], writes=[("impg", ti)])
                        for ti in range(4):
                            o = mb * 4 + ti
                            P.op("dve", I("tensor_scalar", out=onsa[o][:, head, :], in0=ps[3 + ti][:, 0:64], scalar1=g4[ti][:, 0:1], scalar2=None, op0=ALU.mult),
                                 reads=[("ps", 3 + ti), ("g4", ti)], writes=[("onsa", o, head)])
                    if NSA_STOP == 1:
                        continue
                    for ti in range(4):
                        P.op("dve", I("tensor_tensor", out=score[:], in0=impg[ti][:], in1=sbias[ti][:], op=ALU.add),
                             reads=[("impg", ti), ("sbias", ti)], writes=["score"])
                        P.op("dve", I("max", out=m8a[:], in_=score[:]), reads=["score"], writes=["m8a"])
                        P.op("dve", I("match_replace", out=sc3[:], in_to_replace=m8a[:], in_values=score[:], imm_value=-3.0e9),
                             reads=["score", "m8a"], writes=["sc3"])
                        P.op("dve", I("max", out=m8b[:], in_=sc3[:]), reads=["sc3"], writes=["m8b"])
                        P.op("dve", I("tensor_scalar", out=seln[:], in0=score[:], scalar1=m8b[:, 7:8], scalar2=-BIG, op0=ALU.is_lt, op1=ALU.mult),
                             reads=["score", "m8b"], writes=["seln"])
                        P.op("pe", I("transpose", out=pt[0:64, ti * 128:(ti + 1) * 128], in_=seln[:, :], identity=ident[:, :]),
                             reads=["seln", "ident"], writes=[("ps", 7)])
                    P.op("dve", I("tensor_copy", out=selT[0:64, :], in_=pt[0:64, 0:512]), reads=[("ps", 7)], writes=["selT"])
                    if NSA_STOP == 2:
                        continue
                    for branch in ((1, 2) if NSA_STOP != 3 else (1,)):
                        for hh in range(4):
                            head = 4 * g + hh
                            if branch == 1:
                                seq = [(sb, None) for sb in range(4 * qc + 4)]
                            else:
                                seq = [(4 * qc - 4 + r, r) for r in range(8) if 4 * qc - 4 + r >= 0]
                            first = {}
                            last = {}
                            for (sb, r) in seq:
                                for ti in range(4):
                                    if branch == 1:
                                        ok = sb <= 4 * qc + ti
                                    else:
                                        ok = (ti <= r) if r < 4 else (r - 4 <= ti)
                                    if ok:
                                        first.setdefault(ti, sb)
                                        last[ti] = sb
                            for (sb, r) in seq:
                                b = st_bank()
                                KT_ = KsT if branch == 1 else KwT
                                kk = "KsT" if branch == 1 else "KwT"
                                P.op("pe", I("matmul", ps[b][:, :], lhsT=KT_[gs, sb * 128:(sb + 1) * 128], rhs=QT[gs, hh, csl], start=True, stop=False),
                                     reads=[kk, ("Q", hh)], writes=[("ps", b)])
                                if branch == 1:
                                    diag = sb >= 4 * qc
                                    P.op("pe", I("matmul", ps[b][:, :], lhsT=esel[:, sb, :], rhs=selT[:, :], start=False, stop=not diag),
                                         reads=["esel", "selT"], writes=[("ps", b)])
                                    if diag:
                                        P.op("pe", I("matmul", ps[b][:, :], lhsT=ident[:, :], rhs=mgt[:, sb - 4 * qc, :], start=False, stop=True),
                                             reads=["ident", "m_gt"], writes=[("ps", b)])
                                else:
                                    msk = mle[:, r, :] if r < 4 else mgt[:, r - 4, :]
                                    P.op("pe", I("matmul", ps[b][:, :], lhsT=ident[:, :], rhs=msk, start=False, stop=True),
                                         reads=["ident", "m_gt", "m_le"], writes=[("ps", b)])
                                pb = cn["p"] % 3
                                cn["p"] += 1
                                exp_to(b, Pt[pb], ("P", pb))
                                def back(sb, r, pb, branch, g, first, last):
                                    for ti in range(4):
                                        if ti in first and first[ti] <= sb <= last[ti]:
                                            if branch == 2:
                                                ok = (ti <= r) if r < 4 else (r - 4 <= ti)
                                                if not ok:
                                                    continue
                                            vg = g if branch == 1 else 2 + g
                                            P.op("pe", I("matmul", ps[3 + ti][:, 0:65], lhsT=Pt[pb][:, ti * 128:(ti + 1) * 128], rhs=V4[:, sb, vg, :],
                                                         start=(sb == first[ti]), stop=(sb == last[ti])), reads=[("P", pb), "V4"], writes=[("ps", 3 + ti)])
                                self.run_deferred()
                                self.defer(back, sb, r, pb, branch, g, first, last)
                            self.defer(self._nsa_fin, mb, head, branch, gts, rs1, rg, sc2, onsa)
                            for ti in range(0):
                                o = mb * 4 + ti
                                gtile = gts[o]
                                P.op("dve", I("tensor_scalar", out=rs1[:], in0=ps[3 + ti][:, 64:65], scalar1=1e-20, scalar2=None, op0=ALU.max),
                                     reads=[("ps", 3 + ti)], writes=["rs1"])
                                P.op("dve", I("reciprocal", out=rs1[:], in_=rs1[:]), reads=["rs1"], writes=["rs1"])
                                P.op("dve", I("tensor_tensor", out=rg[:], in0=rs1[:], in1=gtile[:, head * 3 + branch:head * 3 + branch + 1], op=ALU.mult),
                                     reads=["rs1", ("gts", o)], writes=["rg"])
                                P.op("dve", I("tensor_scalar", out=sc2[:], in0=ps[3 + ti][:, 0:64], scalar1=rg[:, 0:1], scalar2=None, op0=ALU.mult),
                                     reads=[("ps", 3 + ti), "rg"], writes=["sc2"])
                                P.op("pool", I("tensor_tensor", out=onsa[o][:, head, :], in0=onsa[o][:, head, :], in1=sc2[:], op=ALU.add),
                                     reads=["sc2", ("onsa", o, head)], writes=[("onsa", o, head)])
                def store(qc, mb):
                    for ti in range(4):
                        o = mb * 4 + ti
                        rows = slice((qc * 4 + ti) * 128, (qc * 4 + ti + 1) * 128)
                        P.dma("sp", I("dma_start", out=dr["oS"][rows, 0:512], in_=onsa[o][:].rearrange("p h d -> p (h d)")),
                              reads=[("onsa", o, hd) for hd in range(8)], writes=[("oS", "n", qc, ti)])
                self.defer(store, qc, mb)
            self.run_deferred()
            P.end_phase()

    def _nsa_fin(self, mb, head, branch, gts, rs1, rg, sc2, onsa):
        P, ps = self.P, self.ps
        r4, g4, s4 = self._nsa_tmp
        for ti in range(4):
            P.op("dve", I("tensor_scalar", out=r4[ti][:], in0=ps[3 + ti][:, 64:65], scalar1=1e-20, scalar2=None, op0=ALU.max),
                 reads=[("ps", 3 + ti)], writes=[("r4", ti)])
        for ti in range(4):
            P.op("dve", I("reciprocal", out=r4[ti][:], in_=r4[ti][:]), reads=[("r4", ti)], writes=[("r4", ti)])
        for ti in range(4):
            o = mb * 4 + ti
            P.op("dve", I("tensor_tensor", out=g4[ti][:], in0=r4[ti][:], in1=gts[o][:, head * 3 + branch:head * 3 + branch + 1], op=ALU.mult),
                 reads=[("r4", ti), ("gts", o)], writes=[("g4", ti)])
        for ti in range(4):
            P.op("dve", I("tensor_scalar", out=s4[ti][:], in0=ps[3 + ti][:, 0:64], scalar1=g4[ti][:, 0:1], scalar2=None, op0=ALU.mult),
                 reads=[("ps", 3 + ti), ("g4", ti)], writes=[("s4", ti)])
        for ti in range(4):
            o = mb * 4 + ti
            P.op("pool", I("tensor_tensor", out=onsa[o][:, head, :], in0=onsa[o][:, head, :], in1=s4[ti][:], op=ALU.add),
                 reads=[("s4", ti), ("onsa", o, head)], writes=[("onsa", o, head)])

    def phase_D(self, l, last):
        self.phase_D1(l)
        self.phase_D2(l, last)

    def phase_D1(self, l):
        nc, P, dr, ps = self.nc, self.P, self.dr, self.ps
        hsrc = dr["x"] if l == 0 else dr["hS"]
        with contextlib.ExitStack() as st:
            T = lambda n, sh, dt: st.enter_context(nc.sbuf_tensor(self.uname(n), sh, dt))
            Wout = T("d_Wout", [128, 8, D], BF16)
            Wd = T("d_Wd", [128, NF, D], BF16)
            stage = [T("d_stage%d" % i, [128, DFF], F32) for i in range(2)]
            cvt = [T("d_cvt%d" % i, [128, DFF], BF16) for i in range(2)]
            ghead = T("d_ghead", [128, 8], F32)
            gffn = T("d_gffn", [128, 8], F32)
            ident = T("d_ident", [128, 128], BF16)
            h = [T("d_h%d" % i, [128, D], F32) for i in range(4)]
            ot = [T("d_ot%d" % i, [128, D], F32) for i in range(2)]
            osq = T("d_osq", [128, D], F32)
            ssh = T("d_ssh", [128, 16], F32)
            on = [T("d_on%d" % i, [128, D], BF16) for i in range(2)]
            T4 = [dict(junk=T("d_junk%d" % i, [128, D], BF16), ss=T("d_ss%d" % i, [128, 1], F32),
                       rs=T("d_rs%d" % i, [128, 1], F32)) for i in range(2)]
            xT = T("d_xT", [128, 8, CH], BF16)
            actT = T("d_actT", [128, NF, CH], BF16)
            wgu = [T("d_wgu%d" % i, [128, 2, 8, 128], BF16) for i in range(3)]
            sg = [T("d_sg%d" % i, [128, CH], F32) for i in range(2)]
            P.dma("sp", I("dma_start", out=ghead[:], in_=dr["head_norm"][l]), writes=["ghead"])
            P.dma("sp", I("dma_start", out=gffn[:], in_=dr["norm_ffn"][l]), writes=["gffn"])
            P.dma("sp", I("dma_start", out=ident[:], in_=dr["ident"]), writes=["ident"])
            n = 0
            for k in range(8):
                self.load_weight_bf(Wout[:, k, :], dr["w_out"][l, k * 128:(k + 1) * 128, :], stage[n % 2][:, 0:D], ("stage", n % 2),
                                    ("Wout", k), scale_ap=ghead[:, k:k + 1], scalek="ghead", eng=("dve" if n % 2 == 0 else "pool"),
                                    q=("sp" if n % 2 == 0 else "act"))
                n += 1
            for f in range(NF):
                self.load_weight_bf(Wd[:, f, :], dr["w_ffn_down"][l, f * 128:(f + 1) * 128, :], stage[n % 2][:, 0:D], ("stage", n % 2),
                                    ("Wd", f), eng=("dve" if n % 2 == 0 else "pool"), q=("sp" if n % 2 == 0 else "act"))
                n += 1
            for gi, wn in enumerate(("w_ffn_gate", "w_ffn_up")):
                for k in range(8):
                    b = n % 2
                    self.load_weight_bf(cvt[b][:], dr[wn][l, k * 128:(k + 1) * 128, :], stage[b][:], ("stage", b), ("cvt", b),
                                        scale_ap=gffn[:, k:k + 1], scalek="gffn", eng=("dve" if b == 0 else "pool"),
                                        q=("sp" if b == 0 else "act"))
                    for f0, f1 in ((0, 8), (8, 16), (16, NF)):
                        P.dma("sp", I("dma_start", out=dr["WGU"][f0:f1, :, gi, k, :].rearrange("f p c -> p f c"),
                                      in_=cvt[b][:, f0 * 128:f1 * 128].rearrange("p (f c) -> p f c", c=128)),
                              reads=[("cvt", b)], writes=[("WGU", gi, k, f0)])
                    n += 1
            wgu_ready = [("WGU", gi, k, f0) for gi in range(2) for k in range(8) for f0 in (0, 8, 16)]
            Woutk = [("Wout", k) for k in range(8)]
            Wdk = [("Wd", f) for f in range(NF)]
            wl = 0
            for c in range(NCH):
                for i in range(4):
                    ti = c * 4 + i
                    b = ti % 2
                    rows = slice(ti * 128, (ti + 1) * 128)
                    P.dma("sp", I("dma_start", out=h[i][:], in_=hsrc[rows, :]), writes=[("h", i)])
                    P.dma("act", I("dma_start", out=ot[b][:], in_=dr["oS"][rows, :]), writes=[("ot", b)])
                    P.op("pool", I("tensor_tensor", out=osq[:], in0=ot[b][:], in1=ot[b][:], op=ALU.mult), reads=[("ot", b)], writes=["osq"])
                    P.op("dve", I("tensor_reduce", out=ssh[:], in_=osq[:].rearrange("p (h d) -> p h d", d=64), axis=AX.X, op=ALU.add),
                         reads=["osq"], writes=["ssh"])
                    P.op("dve", I("tensor_scalar", out=ssh[:], in0=ssh[:], scalar1=1.0 / 64, scalar2=1e-6, op0=ALU.mult, op1=ALU.add),
                         reads=["ssh"], writes=["ssh"])
                    P.op("act", I("sqrt", out=ssh[:], in_=ssh[:]), reads=["ssh"], writes=["ssh"])
                    P.op("dve", I("reciprocal", out=ssh[:], in_=ssh[:]), reads=["ssh"], writes=["ssh"])
                    P.op("dve", I("tensor_tensor", out=on[b][:].rearrange("p (h d) -> p h d", d=64),
                                  in0=ot[b][:].rearrange("p (h d) -> p h d", d=64),
                                  in1=ssh[:, :].unsqueeze(2).to_broadcast([128, 16, 64]), op=ALU.mult),
                         reads=[("ot", b), "ssh"], writes=[("on", b)])
                    self.transpose8(on[b], ("on", b), xT[:, :, i * 128:(i + 1) * 128], ("xT", i), ident, eng=("dve" if i % 2 == 0 else "act"))
                    for half in range(2):
                        pa = self.psn()
                        for k in range(8):
                            P.op("pe", I("matmul", ps[pa][:, :], lhsT=xT[:, k, i * 128:(i + 1) * 128], rhs=Wout[:, k, half * 512:(half + 1) * 512],
                                         start=(k == 0), stop=(k == 7)), reads=[("xT", i)] + Woutk, writes=[("ps", pa)])
                        P.op("dve", I("tensor_tensor", out=h[i][:, half * 512:(half + 1) * 512], in0=ps[pa][:, :],
                                      in1=h[i][:, half * 512:(half + 1) * 512], op=ALU.add), reads=[("ps", pa), ("h", i)], writes=[("h", i)])
                if "hmix" in self.dr:
                    for i in range(4):
                        rows = slice((c * 4 + i) * 128, (c * 4 + i + 1) * 128)
                        P.dma("sp", I("dma_start", out=dr["hmix"][rows, :], in_=h[i][:]), reads=[("h", i)], writes=[("hmix", c, i)])
                for i in range(4):
                    b = i % 2
                    self.rms_tile(T4[b], b, h[i], ("h", i), ("junk", b), ("ss", b), ("rs", b), on[b], ("on", b))
                    self.transpose8(on[b], ("on", b), xT[:, :, i * 128:(i + 1) * 128], ("xT", i), ident, eng=("dve" if i % 2 == 0 else "act"))
                xk = [("xT", i) for i in range(4)]
                for f in range(NF):
                    wb = wl % 3
                    wl += 1
                    P.dma("sp" if f % 2 == 0 else "act", I("dma_start", out=wgu[wb][:], in_=dr["WGU"][f]), reads=wgu_ready, writes=[("wgu", wb)])
                    pg, pu = self.psn(), self.psn()
                    for gi, pp in ((0, pg), (1, pu)):
                        for k in range(8):
                            P.op("pe", I("matmul", ps[pp][:, :], lhsT=wgu[wb][:, gi, k, :], rhs=xT[:, k, :], start=(k == 0), stop=(k == 7)),
                                 reads=xk + [("wgu", wb)], writes=[("ps", pp)])
                    sb_ = f % 2
                    P.op("act", I("activation", out=sg[sb_][:], in_=ps[pg][:, :], func=AF.Silu), reads=[("ps", pg)], writes=[("sg", sb_)])
                    P.op("dve", I("tensor_tensor", out=actT[:, f, :], in0=ps[pu][:, :], in1=sg[sb_][:], op=ALU.mult),
                         reads=[("ps", pu), ("sg", sb_)], writes=[("actT", f)])
                ak = [("actT", f) for f in range(NF)]
                for i in range(4):
                    for half in range(2):
                        pa = self.psn()
                        for f in range(NF):
                            P.op("pe", I("matmul", ps[pa][:, :], lhsT=actT[:, f, i * 128:(i + 1) * 128], rhs=Wd[:, f, half * 512:(half + 1) * 512],
                                         start=(f == 0), stop=(f == NF - 1)), reads=ak + Wdk, writes=[("ps", pa)])
                        P.op("dve", I("tensor_tensor", out=h[i][:, half * 512:(half + 1) * 512], in0=ps[pa][:, :],
                                      in1=h[i][:, half * 512:(half + 1) * 512], op=ALU.add), reads=[("ps", pa), ("h", i)], writes=[("h", i)])
                    rows = slice((c * 4 + i) * 128, (c * 4 + i + 1) * 128)
                    P.dma("sp", I("dma_start", out=dr["hS"][rows, :], in_=h[i][:]), reads=[("h", i)], writes=[("hS", c, i)])
            P.end_phase()

    def phase_D2(self, l, last):
        nc, P, dr, ps = self.nc, self.P, self.dr, self.ps
        with contextlib.ExitStack() as st:
            T = lambda n, sh, dt: st.enter_context(nc.sbuf_tensor(self.uname(n), sh, dt))
            Wpg = T("e_Wpg", [128, 8, D], BF16)
            Wpp = T("e_Wpp", [128, 2, D], BF16)
            stage = [T("e_stage%d" % i, [128, D], F32) for i in range(2)]
            gple = T("e_gple", [128, 8], F32)
            gfin = T("e_gfin", [128, D], F32)
            ident = T("e_ident", [128, 128], BF16)
            h = [T("e_h%d" % i, [128, D], F32) for i in range(2)]
            p32 = [T("e_p32%d" % i, [128, 256], F32) for i in range(2)]
            pbf = [T("e_pbf%d" % i, [128, 256], BF16) for i in range(2)]
            hn = [T("e_hn%d" % i, [128, D], BF16) for i in range(2)]
            T4 = [dict(junk=T("e_junk%d" % i, [128, D], BF16), ss=T("e_ss%d" % i, [128, 1], F32),
                       rs=T("e_rs%d" % i, [128, 1], F32)) for i in range(2)]
            xT = [T("e_xT%d" % i, [128, 8, 128], BF16) for i in range(2)]
            pT = [T("e_pT%d" % i, [128, 2, 128], BF16) for i in range(2)]
            sig = [T("e_sig%d" % i, [128, CH], F32) for i in range(2)]
            tmp = [T("e_tmp%d" % i, [128, CH], F32) for i in range(2)]
            outt = [T("e_out%d" % i, [128, D], F32) for i in range(2)]
            P.dma("sp", I("dma_start", out=gple[:], in_=dr["norm_ple"][l]), writes=["gple"])
            P.dma("sp", I("dma_start", out=ident[:], in_=dr["ident"]), writes=["ident"])
            if last:
                P.dma("sp", I("dma_start", out=gfin[:], in_=dr["norm_final"].to_broadcast([128, D])), writes=["gfin"])
            n = 0
            for k in range(8):
                self.load_weight_bf(Wpg[:, k, :], dr["w_ple_gate"][l, k * 128:(k + 1) * 128, :], stage[n % 2][:], ("stage", n % 2),
                                    ("Wpg", k), scale_ap=gple[:, k:k + 1], scalek="gple", eng=("dve" if n % 2 == 0 else "pool"),
                                    q=("sp" if n % 2 == 0 else "act"))
                n += 1
            for k in range(2):
                self.load_weight_bf(Wpp[:, k, :], dr["w_ple_proj"][l, k * 128:(k + 1) * 128, :], stage[n % 2][:], ("stage", n % 2),
                                    ("Wpp", k), eng=("dve" if n % 2 == 0 else "pool"), q=("sp" if n % 2 == 0 else "act"))
                n += 1
            Wpgk = [("Wpg", k) for k in range(8)]
            Wppk = [("Wpp", k) for k in range(2)]
            for ti in range(NTILE):
                b = ti % 2
                rows = slice(ti * 128, (ti + 1) * 128)
                P.dma("sp", I("dma_start", out=h[b][:], in_=dr["hS"][rows, :]), writes=[("h", b)])
                P.dma("act", I("dma_start", out=p32[b][:], in_=dr["p"][l, rows, :]), writes=[("p32", b)])
                P.op("pool", I("tensor_copy", out=pbf[b][:], in_=p32[b][:]), reads=[("p32", b)], writes=[("pbf", b)])
                self.rms_tile(T4[b], b, h[b], ("h", b), ("junk", b), ("ss", b), ("rs", b), hn[b], ("hn", b))
                self.transpose8(hn[b], ("hn", b), xT[b][:, :, :], ("xT", b), ident, eng="dve")
                self.transpose8(pbf[b], ("pbf", b), pT[b][:, :, :], ("pT", b), ident, nblk=2, eng="act")
                for half in range(2):
                    hs = slice(half * 512, (half + 1) * 512)
                    pg, pp = self.psn(), self.psn()
                    for k in range(8):
                        P.op("pe", I("matmul", ps[pg][:, :], lhsT=xT[b][:, k, :], rhs=Wpg[:, k, hs], start=(k == 0), stop=(k == 7)),
                             reads=[("xT", b)] + Wpgk, writes=[("ps", pg)])
                    for k in range(2):
                        P.op("pe", I("matmul", ps[pp][:, :], lhsT=pT[b][:, k, :], rhs=Wpp[:, k, hs], start=(k == 0), stop=(k == 1)),
                             reads=[("pT", b)] + Wppk, writes=[("ps", pp)])
                    P.op("act", I("activation", out=sig[half][:], in_=ps[pg][:, :], func=AF.Sigmoid), reads=[("ps", pg)], writes=[("sig", half)])
                    P.op("dve", I("tensor_tensor", out=tmp[half][:], in0=ps[pp][:, :], in1=sig[half][:], op=ALU.mult),
                         reads=[("ps", pp), ("sig", half)], writes=[("tmp", half)])
                    P.op("pool", I("tensor_tensor", out=h[b][:, hs], in0=h[b][:, hs], in1=tmp[half][:], op=ALU.add),
                         reads=[("tmp", half), ("h", b)], writes=[("h", b)])
                if not last:
                    P.dma("sp", I("dma_start", out=dr["hS"][rows, :], in_=h[b][:]), reads=[("h", b)], writes=[("hS", ti)])
                else:
                    self.rms_tile(T4[b], b, h[b], ("h", b), ("junk", b), ("ss", b), ("rs", b), None, None) if False else None
                    junk, ss, rs = T4[b]["junk"], T4[b]["ss"], T4[b]["rs"]
                    P.op("act", I("activation", out=junk[:], in_=h[b][:], func=AF.Square, accum_out=ss[:]), reads=[("h", b)], writes=[("junk", b), ("ss", b)])
                    P.op("dve", I("tensor_scalar", out=rs[:], in0=ss[:], scalar1=1.0 / D, scalar2=1e-6, op0=ALU.mult, op1=ALU.add),
                         reads=[("ss", b)], writes=[("rs", b)])
                    P.op("act", I("sqrt", out=rs[:], in_=rs[:]), reads=[("rs", b)], writes=[("rs", b)])
                    P.op("dve", I("reciprocal", out=rs[:], in_=rs[:]), reads=[("rs", b)], writes=[("rs", b)])
                    P.op("dve", I("scalar_tensor_tensor", out=outt[b][:], in0=h[b][:], scalar=rs[:, 0:1], in1=gfin[:], op0=ALU.mult, op1=ALU.mult),
                         reads=[("h", b), ("rs", b), "gfin"], writes=[("outt", b)])
                    P.dma("sp", I("dma_start", out=self.out[rows, :], in_=outt[b][:]), reads=[("outt", b)], writes=[("out", ti)])
            P.end_phase()


def make_in_maps(inputs, cores):
    inp = {k: np.asarray(v) for k, v in inputs.items()}
    sh = _prep_shared(inp)
    maps = []
    for b in cores:
        m = dict(sh)
        m["x"] = np.ascontiguousarray(inp["x"][b])
        m["p"] = np.ascontiguousarray(inp["p"][:, b])
        m["pos"] = np.ascontiguousarray(inp["positions"][b].reshape(1, S).astype(np.int32))
        maps.append(m)
    return maps


_NC_CACHE = {}


def kernel(**inputs):
    if "nc" not in _NC_CACHE:
        _NC_CACHE["nc"] = Builder().build()
    nc = _NC_CACHE["nc"]
    maps = make_in_maps(inputs, list(range(8)))
    res = run_bass_kernel_spmd(nc, maps, core_ids=list(range(8)))
    out = np.stack([np.asarray(r["out"]) for r in res.results], axis=0)
    return out.astype(np.float32)
```

```python
import contextlib
import numpy as np
import ml_dtypes
import concourse.bass as bass
import concourse.mybir as mybir
from concourse.bass_utils import run_bass_kernel_spmd

F32 = mybir.dt.float32
BF16 = mybir.dt.bfloat16
I32 = mybir.dt.int32
AF = mybir.ActivationFunctionType
ALU = mybir.AluOpType
AX = mybir.AxisListType

S = 4096
D = 1024
L = 2
NTILE = 32
CH = 512
NCH = 8
DFF = 2816
NF = 22
BIG = 30000.0
WCOLS = 3740
COMPUTE = ("pe", "act", "dve", "pool", "sp")
import os as _os
NSA_STOP = int(_os.environ.get("NSA_STOP", "0"))
DMAQ = ("sp", "pool", "act")


def I(m, *a, **k):
    return lambda e: getattr(e, m)(*a, **k)


class Op:
    __slots__ = ("eng", "fn", "waits", "signal", "cnt", "dma_sem", "dma_val", "is_dma", "idx")

    def __init__(self, eng, fn, is_dma):
        self.eng = eng
        self.fn = fn
        self.waits = []
        self.signal = False
        self.cnt = None
        self.dma_sem = None
        self.dma_val = None
        self.is_dma = is_dma
        self.idx = None


class Prog:
    def __init__(self, nc, st, n_dma_sems=8):
        self.nc = nc
        self.lists = {e: [] for e in COMPUTE}
        self.last_w = {}
        self.readers = {}
        self.n_dma_sems = n_dma_sems
        self.dma_count = {q: 0 for q in DMAQ}
        self.csem = {e: st.enter_context(nc.semaphore("c_" + e)) for e in COMPUTE}
        self.dsem = {(q, j): st.enter_context(nc.semaphore("d_%s%d" % (q, j)))
                     for q in DMAQ for j in range(n_dma_sems)}
        self.cbase = {e: 0 for e in COMPUTE}
        self.gidx = {e: 0 for e in COMPUTE}
        self.barrier = {}
        self.dma_last = {}

    def _deps(self, reads, writes):
        deps = []
        for k in reads:
            w = self.last_w.get(k)
            if w is not None:
                deps.append(w)
        for k in writes:
            w = self.last_w.get(k)
            if w is not None:
                deps.append(w)
            deps.extend(self.readers.get(k, ()))
        return deps

    def _record(self, h, reads, writes):
        for k in reads:
            self.readers.setdefault(k, []).append(h)
        for k in writes:
            self.last_w[k] = h
            self.readers[k] = []

    def _attach(self, h, deps):
        best = {}
        for d in deps:
            if d is h or d.fn is None:
                continue
            if d.is_dma:
                key = ("d",) + d.dma_sem
                cur = best.get(key)
                if cur is None or d.dma_val > cur.dma_val:
                    best[key] = d
            else:
                if d.eng == "pe" and h.eng == "pe" and not h.is_dma:
                    continue
                cur = best.get(d.eng)
                if cur is None or d.idx > cur.idx:
                    best[d.eng] = d
        for d in best.values():
            d.signal = True
            h.waits.append(d)

    def op(self, eng, fn, reads=(), writes=(), extra=()):
        h = Op(eng, fn, False)
        self._attach(h, self._deps(reads, writes) + list(extra))
        h.idx = self.gidx[eng]
        self.gidx[eng] += 1
        self.lists[eng].append(h)
        self._record(h, reads, writes)
        return h

    def dma(self, q, fn, reads=(), writes=(), extra=()):
        h = Op(q, fn, True)
        self._attach(h, self._deps(reads, writes) + list(extra))
        i = self.dma_count[q]
        self.dma_count[q] += 1
        h.dma_sem = (q, i % self.n_dma_sems)
        h.dma_val = 16 * (i // self.n_dma_sems + 1)
        h.idx = self.gidx[q]
        self.gidx[q] += 1
        self.lists[q].append(h)
        self._record(h, reads, writes)
        self.dma_last[h.dma_sem] = h.dma_val
        return h

    def flush(self, final=False):
        nc = self.nc
        for e in COMPUTE:
            c = self.cbase[e]
            for h in self.lists[e]:
                if not h.is_dma and h.signal:
                    c += 1
                    h.cnt = c
        if final:
            pass
        barrier = dict(self.barrier)
        with nc.Block() as block:
            engs = {"pe": block.tensor, "act": block.scalar, "dve": block.vector,
                    "pool": block.gpsimd, "sp": block.sync}

            def make(ename):
                lst = self.lists[ename]

                def body(eng):
                    waited = {}
                    for key, val in barrier.items():
                        if val <= 0:
                            continue
                        sem = self.csem[key[1]] if key[0] == "c" else self.dsem[key[1:]]
                        eng.wait_ge(sem, val)
                        waited[key] = val
                    for h in lst:
                        for d in h.waits:
                            if d.is_dma:
                                key = ("d",) + d.dma_sem
                                sem = self.dsem[d.dma_sem]
                                val = d.dma_val
                            else:
                                key = ("c", d.eng)
                                sem = self.csem[d.eng]
                                val = d.cnt
                            if waited.get(key, 0) >= val:
                                continue
                            waited[key] = val
                            eng.wait_ge(sem, val)
                        if h.is_dma:
                            prev = h.dma_val - 16
                            key = ("d",) + h.dma_sem
                            if prev > 0 and waited.get(key, 0) < prev:
                                eng.wait_ge(self.dsem[h.dma_sem], prev)
                                waited[key] = prev
                            ins = h.fn(eng)
                            ins.then_inc(self.dsem[h.dma_sem], 16)
                        else:
                            ins = h.fn(eng)
                            if h.signal:
                                ins.then_inc(self.csem[ename], 1)
                    if final and ename == "sp":
                        for key, val in self._barrier_now().items():
                            if val > 0 and waited.get(key, 0) < val and key != ("c", "sp"):
                                sem = self.csem[key[1]] if key[0] == "c" else self.dsem[key[1:]]
                                eng.wait_ge(sem, val)
                return body

            for ename in ("sp", "pool", "act", "dve", "pe"):
                if self.lists[ename] or barrier or final:
                    engs[ename](make(ename))
        self.barrier = self._barrier_now()
        for e in COMPUTE:
            for h in self.lists[e]:
                h.fn = None
            self.lists[e] = []
        self.last_w = {}
        self.readers = {}

    def _barrier_now(self):
        b = {}
        for e in COMPUTE:
            c = self.cbase[e]
            for h in self.lists[e]:
                if h.cnt is not None and h.cnt > c:
                    c = h.cnt
            b[("c", e)] = c
        for k, v in self.dma_last.items():
            b[("d",) + k] = v
        return b

    def end_phase(self, final=False):
        for e in COMPUTE:
            for h in reversed(self.lists[e]):
                if not h.is_dma:
                    h.signal = True
                    break
        self.flush(final=final)
        for e in COMPUTE:
            self.cbase[e] = self.barrier[("c", e)]


def _win_cols():
    o = {}
    names = ["nq", "nkc", "nvc", "nks", "nvs", "nkw", "nvw", "ngate", "fq", "fk", "fv", "ff", "sq", "sk", "sv"]
    sizes = [512, 128, 128, 128, 128, 128, 128, 24, 256, 256, 256, 4, 256, 256, 256]
    off = 0
    for n, s in zip(names, sizes):
        o[n] = np.arange(off, off + s)
        off += s
    assert off == 2844

    def rot(c):
        c = c.reshape(-1, 2, 32)
        return c[:, ::-1, :].reshape(-1)

    ft = []
    for j in range(4):
        ft.append(np.concatenate([o["nq"][64 * j:64 * j + 64], o["nq"][64 * (4 + j):64 * (4 + j) + 64]]))
    ft += [o["nkc"], o["nks"], o["nkw"]]
    ft += [rot(c) for c in ft[:7]]
    ft.append(o["nvc"])
    for n in ("fq", "fk", "sq", "sk"):
        ft += [o[n][:128], o[n][128:]]
    cols = np.concatenate(ft + [o["ff"], o["nvs"], o["nvw"], o["fv"], o["sv"], o["ngate"]])
    assert cols.shape[0] == WCOLS
    return cols


def _consts():
    bf = ml_dtypes.bfloat16
    c = {}
    c["ident"] = np.eye(128, dtype=np.float32).astype(bf)
    s = np.arange(128)[:, None, None]
    r = np.arange(4)[None, :, None]
    t = np.arange(512)[None, None, :]
    sa = 128 * r + s
    c["m_gt"] = np.where(sa > t, -BIG, 0.0).astype(bf)
    c["m_ge"] = np.where(sa >= t, -BIG, 0.0).astype(bf)
    c["m_le"] = np.where(sa <= t, -BIG, 0.0).astype(bf)
    n = np.arange(128)[:, None, None] + 128 * np.arange(2)[None, :, None]
    tt = np.arange(S)[None, None, :]
    cm = np.where((16 * n + 31 > tt) | (n >= 255), -BIG, 0.0)
    c["m_cmp"] = cm.astype(bf)
    j = np.arange(64)[:, None, None]
    sb = np.arange(32)[None, :, None]
    ss = np.arange(128)[None, None, :]
    c["eblk"] = (np.arange(64)[:, None] == (np.arange(S)[None, :] // 64)).astype(np.float32).astype(bf)
    nn = np.arange(256)
    cs = nn[:, None] * 16
    bs = np.arange(64)[None, :] * 64
    ov = ((cs < bs + 64) & (cs + 32 > bs) & (nn[:, None] < 255)).astype(np.float32)
    c["ovl"] = ov.reshape(2, 128, 64).transpose(1, 0, 2).astype(bf).copy()
    tq = np.arange(S)[:, None]
    jb = np.arange(64)[None, :]
    cur = tq // 64
    forced = (jb == 0) | (jb == cur) | (jb == cur - 1)
    valid = jb <= cur
    c["selbias"] = np.where(forced, 1e9, np.where(valid, 0.0, -1e9)).astype(np.float32)
    half = 32
    invf = (10000.0 ** (-np.arange(half, dtype=np.float32) / half)).astype(np.float32)
    rr = np.arange(128)
    c["invf"] = invf[rr % 32].reshape(128, 1).astype(np.float32)
    c["sgn"] = np.where((rr % 64) < 32, -1.0, 1.0).reshape(128, 1).astype(np.float32)
    jj = np.arange(128)
    c["ntri"] = np.where(jj[:, None] >= jj[None, :], -1.0, 0.0).astype(np.float32).astype(bf)
    c["nones"] = np.full((1, 128), -1.0, np.float32).astype(bf)
    c["onec"] = np.ones((128, 1), np.float32).astype(bf)
    return c


def _col8(v):
    return np.ascontiguousarray(v.reshape(8, 128).T)


def _prep_shared(inp):
    sh = {}
    cols = _win_cols()
    sh["w_in"] = np.ascontiguousarray(inp["w_in"][:, :, cols])
    for n in ("norm_mix", "norm_ffn", "norm_ple", "head_norm"):
        sh[n] = np.stack([_col8(inp[n][l]) for l in range(L)])
    sh["norm_final"] = inp["norm_final"].reshape(1, D)
    sh["b_gate"] = inp["b_nsa_gate"].reshape(L, 1, 24)
    sh["b_forget"] = inp["b_forget"].reshape(L, 4, 1)
    for kv in ("k", "v"):
        w1 = inp["nsa_cmp_w1_" + kv].reshape(L, 32, 64, 128).transpose(0, 2, 1, 3)
        sh["w1_" + kv] = np.ascontiguousarray(np.concatenate([w1, w1], axis=1).reshape(L, 128, 32 * 128))
        pt = inp["nsa_cmp_pos_" + kv].transpose(0, 2, 1)
        sh["pos_" + kv] = np.ascontiguousarray(np.concatenate([pt, pt], axis=1))
        sh["w2_" + kv] = inp["nsa_cmp_w2_" + kv]
    for n in ("w_out", "w_ffn_gate", "w_ffn_up", "w_ffn_down", "w_ple_proj", "w_ple_gate"):
        sh[n] = inp[n]
    sh.update(_consts())
    return sh


class Builder:
    def __init__(self, debug=(), nlayers=L, phases=None):
        self.debug = set(debug)
        self.nlayers = nlayers
        self.phases = phases
        self.nc = bass.Bass("TRN2", target_bir_lowering=False)
        self.dr = {}

    def din(self, name, shape, dt):
        self.dr[name] = self.nc.dram_tensor(name, list(shape), dt, kind="ExternalInput").ap()
        return self.dr[name]

    def dscr(self, name, shape, dt):
        kind = "ExternalOutput" if name in self.debug else "Internal"
        self.dr[name] = self.nc.dram_tensor(name, list(shape), dt, kind=kind).ap()
        return self.dr[name]

    def want(self, ph):
        return self.phases is None or ph in self.phases

    def build(self):
        nc = self.nc
        din, dscr = self.din, self.dscr
        din("x", [S, D], F32)
        din("p", [L, S, 256], F32)
        din("pos", [1, S], I32)
        din("w_in", [L, D, WCOLS], F32)
        for n in ("norm_mix", "norm_ffn", "norm_ple", "head_norm"):
            din(n, [L, 128, 8], F32)
        din("norm_final", [1, D], F32)
        din("b_gate", [L, 1, 24], F32)
        din("b_forget", [L, 4, 1], F32)
        for kv in ("k", "v"):
            din("w1_" + kv, [L, 128, 4096], F32)
            din("pos_" + kv, [L, 128, 32], F32)
            din("w2_" + kv, [L, 128, 64], F32)
        din("w_out", [L, D, D], F32)
        din("w_ffn_gate", [L, D, DFF], F32)
        din("w_ffn_up", [L, D, DFF], F32)
        din("w_ffn_down", [L, DFF, D], F32)
        din("w_ple_proj", [L, 256, D], F32)
        din("w_ple_gate", [L, D, D], F32)
        din("ident", [128, 128], BF16)
        for n in ("m_gt", "m_ge", "m_le"):
            din(n, [128, 4, 512], BF16)
        din("m_cmp", [128, 2, S], BF16)
        din("eblk", [64, S], BF16)
        din("ovl", [128, 2, 64], BF16)
        din("selbias", [S, 64], F32)
        din("invf", [128, 1], F32)
        din("sgn", [128, 1], F32)
        din("ntri", [128, 128], BF16)
        din("nones", [1, 128], BF16)
        din("onec", [128, 1], BF16)
        self.out = nc.dram_tensor("out", [S, D], F32, kind="ExternalOutput").ap()
        dscr("hS", [S, D], F32)
        dscr("cosS", [128, S], F32)
        dscr("sinS", [128, S], F32)
        dscr("FT", [16, 128, S], BF16)
        dscr("cT", [4, S], F32)
        dscr("VA", [S, 8, 65], BF16)
        dscr("SV", [S, 256], BF16)
        dscr("GT", [S, 24], F32)
        dscr("kcT", [128, 256], BF16)
        dscr("VC", [128, 2, 2, 65], BF16)
        dscr("oS", [S, D], F32)
        dscr("WGU", [NF, 128, 2, 8, 128], BF16)
        if "hmix" in self.debug:
            dscr("hmix", [S, D], F32)

        with contextlib.ExitStack() as st:
            self.P = Prog(nc, st)
            self.ps = [st.enter_context(nc.psum_tensor("ps%d" % i, [128, 512], F32)) for i in range(8)]
            self.pt = self.ps[7][:, :].bitcast(BF16)
            self.ps_i = 0
            if self.want("T"):
                self.phase_tables()
            for l in range(self.nlayers):
                if self.want("A"):
                    self.phase_A(l)
                if self.want("B"):
                    self.phase_B(l)
                if self.want("N"):
                    self.phase_nsa(l)
                if self.want("F"):
                    self.phase_fox(l)
                if self.want("SB"):
                    self.phase_sb(l)
                if self.want("D"):
                    self.phase_D(l, last=(l == self.nlayers - 1))
            self.P.op("sp", I("nop"))
            self.P.end_phase(final=True)
        return nc

    def defer(self, fn, *a):
        if not hasattr(self, "_pend"):
            self._pend = []
        self._pend.append((fn, a))

    def run_deferred(self):
        pend = getattr(self, "_pend", [])
        self._pend = []
        for fn, a in pend:
            fn(*a)

    def uname(self, n):
        self._un = getattr(self, "_un", 0) + 1
        return "%s_u%d" % (n, self._un)

    def psn(self):
        i = self.ps_i
        self.ps_i = (i + 1) % 7
        return i

    def phase_tables(self):
        nc, P, dr = self.nc, self.P, self.dr
        with contextlib.ExitStack() as st:
            T = lambda n, sh, dt: st.enter_context(nc.sbuf_tensor(self.uname(n), sh, dt))
            posi = T("t_posi", [128, S], I32)
            ang = T("t_ang", [128, S], F32)
            kk = T("t_kk", [128, S], F32)
            rr = T("t_r", [128, S], F32)
            oo = T("t_o", [128, S], F32)
            invf = T("t_invf", [128, 1], F32)
            sgn = T("t_sgn", [128, 1], F32)
            hpi = T("t_hpi", [128, 1], F32)
            P.dma("sp", I("dma_start", out=posi[:], in_=dr["pos"].to_broadcast([128, S])), writes=["posi"])
            P.dma("sp", I("dma_start", out=invf[:], in_=dr["invf"]), writes=["invf"])
            P.dma("sp", I("dma_start", out=sgn[:], in_=dr["sgn"]), writes=["sgn"])
            P.op("pool", I("memset", hpi[:], float(np.pi / 2)), writes=["hpi"])
            P.op("dve", I("tensor_copy", out=ang[:], in_=posi[:]), reads=["posi"], writes=["ang"])
            P.op("dve", I("tensor_scalar", out=ang[:], in0=ang[:], scalar1=invf[:, 0:1], scalar2=None, op0=ALU.mult),
                 reads=["ang", "invf"], writes=["ang"])
            MAGIC = 12582912.0
            P.op("dve", I("tensor_scalar", out=kk[:], in0=ang[:], scalar1=float(1.0 / (2 * np.pi)), scalar2=MAGIC,
                                                   op0=ALU.mult, op1=ALU.add), reads=["ang"], writes=["kk"])
            P.op("dve", I("tensor_scalar", out=kk[:], in0=kk[:], scalar1=-MAGIC, scalar2=None, op0=ALU.add),
                 reads=["kk"], writes=["kk"])
            C1 = 6.28125
            C2 = float(np.float32(2 * np.pi - C1))
            P.op("dve", I("scalar_tensor_tensor", out=rr[:], in0=kk[:], scalar=-C1, in1=ang[:], op0=ALU.mult, op1=ALU.add),
                 reads=["kk", "ang"], writes=["rr"])
            P.op("dve", I("scalar_tensor_tensor", out=rr[:], in0=kk[:], scalar=-C2, in1=rr[:], op0=ALU.mult, op1=ALU.add),
                 reads=["kk", "rr"], writes=["rr"])
            PL = 3.1415925
            P.op("dve", I("tensor_scalar", out=rr[:], in0=rr[:], scalar1=-PL, scalar2=PL, op0=ALU.max, op1=ALU.min),
                 reads=["rr"], writes=["rr"])
            P.op("act", I("activation", out=oo[:], in_=rr[:], func=AF.Sin), reads=["rr"], writes=["oo"])
            P.op("dve", I("tensor_scalar", out=oo[:], in0=oo[:], scalar1=sgn[:, 0:1], scalar2=None, op0=ALU.mult),
                 reads=["oo", "sgn"], writes=["oo"])
            P.dma("sp", I("dma_start", out=dr["sinS"], in_=oo[:]), reads=["oo"], writes=["sinS"])
            P.op("dve", I("scalar_tensor_tensor", out=kk[:], in0=rr[:], scalar=-1.0, in1=rr[:], op0=ALU.mult, op1=ALU.max),
                 reads=["rr"], writes=["kk"])
            P.op("act", I("activation", out=ang[:], in_=kk[:], func=AF.Sin, bias=hpi[:, 0:1], scale=-1.0),
                 reads=["kk", "hpi", "ang"], writes=["ang"])
            P.dma("sp", I("dma_start", out=dr["cosS"], in_=ang[:]), reads=["ang"], writes=["cosS"])
            P.end_phase()

    def rms_tile(self, T4, i, hx, hxk, jk, ssk, rsk, hn, hnk):
        P = self.P
        junk, ss, rs = T4["junk"], T4["ss"], T4["rs"]
        P.op("act", I("activation", out=junk[:], in_=hx[:], func=AF.Square, accum_out=ss[:]),
             reads=[hxk], writes=[jk, ssk])
        P.op("dve", I("tensor_scalar", out=rs[:], in0=ss[:], scalar1=1.0 / D, scalar2=1e-6, op0=ALU.mult, op1=ALU.add),
             reads=[ssk], writes=[rsk])
        P.op("act", I("sqrt", out=rs[:], in_=rs[:]), reads=[rsk], writes=[rsk])
        P.op("dve", I("reciprocal", out=rs[:], in_=rs[:]), reads=[rsk], writes=[rsk])
        P.op("dve", I("tensor_scalar", out=hn[:], in0=hx[:], scalar1=rs[:, 0:1], scalar2=None, op0=ALU.mult),
             reads=[hxk, rsk], writes=[hnk])

    def transpose8(self, src, srck, dst_ap, dstk, ident, nblk=8, eng="dve"):
        P = self.P
        pt = self.pt
        for c in range(nblk):
            P.op("pe", I("transpose", out=pt[:, c * 128:(c + 1) * 128], in_=src[:, c * 128:(c + 1) * 128],
                                                  identity=ident[:]), reads=[srck, "ident"], writes=[("ps", 7)])
        view = pt[:, 0:nblk * 128].rearrange("p (c t) -> p c t", t=128)
        if eng == "dve":
            P.op("dve", I("tensor_copy", out=dst_ap, in_=view), reads=[("ps", 7)], writes=[dstk])
        else:
            P.op("act", I("copy", out=dst_ap, in_=view), reads=[("ps", 7)], writes=[dstk])

    def load_weight_bf(self, dst_ap, src_ap, stage, stagek, dstk, scale_ap=None, scalek=None, eng="dve", q="sp"):
        P = self.P
        P.dma(q, I("dma_start", out=stage, in_=src_ap), writes=[stagek])
        en = "dve" if eng == "dve" else "pool"
        if scale_ap is not None:
            P.op(en, I("tensor_scalar", out=dst_ap, in0=stage, scalar1=scale_ap, scalar2=None, op0=ALU.mult),
                 reads=[stagek, scalek], writes=[dstk])
        else:
            P.op(en, I("tensor_copy", out=dst_ap, in_=stage), reads=[stagek], writes=[dstk])

    def phase_A(self, l):
        nc, P, dr = self.nc, self.P, self.dr
        hsrc = dr["x"] if l == 0 else dr["hS"]
        with contextlib.ExitStack() as st:
            T = lambda n, sh, dt: st.enter_context(nc.sbuf_tensor(self.uname(n), sh, dt))
            W = T("a_W", [128, 8, WCOLS], BF16)
            stage = [T("a_stage%d" % i, [128, WCOLS], F32) for i in range(2)]
            cosT = T("a_cos", [128, S], F32)
            sinT = T("a_sin", [128, S], F32)
            gcol = T("a_gcol", [128, 8], F32)
            ident = T("a_ident", [128, 128], BF16)
            bgate = T("a_bgate", [128, 24], F32)
            negb = T("a_negb", [4, 1], F32)
            ones4 = T("a_ones4", [4, CH], F32)
            cc = T("a_cc", [4, S], F32)
            hx = [T("a_hx%d" % i, [128, D], F32) for i in range(2)]
            T4 = [dict(junk=T("a_junk%d" % i, [128, D], BF16), ss=T("a_ss%d" % i, [128, 1], F32),
                       rs=T("a_rs%d" % i, [128, 1], F32)) for i in range(2)]
            hn = [T("a_hn%d" % i, [128, D], BF16) for i in range(2)]
            hnT = [T("a_hnT%d" % i, [128, 8, CH], BF16) for i in range(2)]
            t1 = [T("a_t1%d" % i, [128, CH], F32) for i in range(2)]
            t2 = [T("a_t2%d" % i, [128, CH], F32) for i in range(2)]
            ob = [T("a_ob%d" % i, [128, CH], BF16) for i in range(4)]
            va = [T("a_va%d" % i, [128, 8, 65], BF16) for i in range(2)]
            svt = [T("a_sv%d" % i, [128, 256], BF16) for i in range(2)]
            gt = [T("a_gt%d" % i, [128, 24], F32) for i in range(2)]
            e4 = T("a_e4", [4, CH], F32)
            sp4 = T("a_sp4", [4, CH], F32)

            P.dma("sp", I("dma_start", out=gcol[:], in_=dr["norm_mix"][l]), writes=["gcol"])
            P.dma("sp", I("dma_start", out=ident[:], in_=dr["ident"]), writes=["ident"])
            P.dma("sp", I("dma_start", out=cosT[:], in_=dr["cosS"]), writes=["cosT"])
            P.dma("sp", I("dma_start", out=sinT[:], in_=dr["sinS"]), writes=["sinT"])
            P.dma("sp", I("dma_start", out=bgate[:], in_=dr["b_gate"][l].to_broadcast([128, 24])), writes=["bgate"])
            P.dma("sp", I("dma_start", out=negb[:], in_=dr["b_forget"][l]), writes=["negb"])
            P.op("dve", I("tensor_scalar", out=negb[:], in0=negb[:], scalar1=-1.0, scalar2=None, op0=ALU.mult),
                 reads=["negb"], writes=["negb"])
            P.op("pool", I("memset", ones4[:], 1.0), writes=["ones4"])
            for i in range(2):
                P.op("pool", I("memset", va[i][:], 1.0), writes=[("va", i)])
            for k in range(8):
                self.load_weight_bf(W[:, k, :], dr["w_in"][l, k * 128:(k + 1) * 128, :], stage[k % 2][:], ("stage", k % 2),
                                    ("W", k), scale_ap=gcol[:, k:k + 1], scalek="gcol", eng=("dve" if k % 2 == 0 else "pool"),
                                    q=("sp" if k % 2 == 0 else "act"))
            Wk = [("W", k) for k in range(8)]
            obi = 0
            for c in range(NCH):
                hb = c % 2
                csl = slice(c * CH, (c + 1) * CH)
                for i in range(4):
                    ti = c * 4 + i
                    b = ti % 2
                    P.dma("sp", I("dma_start", out=hx[b][:], in_=hsrc[ti * 128:(ti + 1) * 128, :]),
                          writes=[("hx", b)])
                    self.rms_tile(T4[b], b, hx[b], ("hx", b), ("junk", b), ("ss", b), ("rs", b), hn[b], ("hn", b))
                    self.transpose8(hn[b], ("hn", b), hnT[hb][:, :, i * 128:(i + 1) * 128], ("hnT", hb, i), ident,
                                    eng=("dve" if i % 2 == 0 else "act"))
                hk = [("hnT", hb, i) for i in range(4)]

                def fm_matmul(pi, col0, ncols=128):
                    for k in range(8):
                        P.op("pe", I("matmul", self.ps[pi][0:ncols, :], lhsT=W[:, k, col0:col0 + ncols],
                                                          rhs=hnT[hb][:, k, :], start=(k == 0), stop=(k == 7)),
                             reads=hk + Wk, writes=[("ps", pi)])
                for ft in range(7):
                    pa, pb = self.psn(), self.psn()
                    fm_matmul(pa, ft * 128)
                    fm_matmul(pb, (7 + ft) * 128)
                    tb = ft % 2
                    P.op("dve", I("tensor_tensor", out=t1[tb][:], in0=self.ps[pa][:], in1=cosT[:, csl], op=ALU.mult),
                         reads=[("ps", pa), "cosT"], writes=[("t1", tb)])
                    P.op("dve", I("tensor_tensor", out=t2[tb][:], in0=self.ps[pb][:], in1=sinT[:, csl], op=ALU.mult),
                         reads=[("ps", pb), "sinT"], writes=[("t2", tb)])
                    o = obi % 4
                    obi += 1
                    P.op("pool", I("tensor_tensor", out=ob[o][:], in0=t1[tb][:], in1=t2[tb][:], op=ALU.add),
                         reads=[("t1", tb), ("t2", tb)], writes=[("ob", o)])
                    P.dma("sp", I("dma_start", out=dr["FT"][ft, :, csl], in_=ob[o][:]),
                          reads=[("ob", o)], writes=[("FT", ft, c)])
                for ft in range(14, 23):
                    pa = self.psn()
                    fm_matmul(pa, ft * 128)
                    o = obi % 4
                    obi += 1
                    sc = 0.125 if ft in (15, 16, 19, 20) else 1.0
                    P.op("act", I("activation", out=ob[o][:], in_=self.ps[pa][:], func=AF.Copy, scale=sc),
                         reads=[("ps", pa)], writes=[("ob", o)])
                    P.dma("sp", I("dma_start", out=dr["FT"][ft - 7, :, csl], in_=ob[o][:]),
                          reads=[("ob", o)], writes=[("FT", ft, c)])
                pa = self.psn()
                fm_matmul(pa, 23 * 128, ncols=4)
                P.op("act", I("activation", out=e4[:], in_=self.ps[pa][0:4, :], func=AF.Exp, bias=negb[:, 0:1], scale=-1.0),
                     reads=[("ps", pa), "negb"], writes=["e4"])
                P.op("act", I("activation", out=sp4[:], in_=e4[:], func=AF.Ln, bias=1.0, scale=1.0), reads=["e4"], writes=["sp4"])
                if c == 0:
                    P.op("dve", I("tensor_tensor_scan", out=cc[:, csl], data0=ones4[:], data1=sp4[:], initial=0.0,
                                                                op0=ALU.mult, op1=ALU.subtract), reads=["sp4", "ones4"], writes=["cc"])
                else:
                    P.op("dve", I("tensor_tensor_scan", out=cc[:, csl], data0=ones4[:], data1=sp4[:],
                                                                     initial=cc[:, c * CH - 1:c * CH],
                                                                     op0=ALU.mult, op1=ALU.subtract), reads=["sp4", "ones4", "cc"], writes=["cc"])
                c0 = 23 * 128 + 4
                for i in range(4):
                    ti = c * 4 + i
                    b = ti % 2
                    pa, pb = self.psn(), self.psn()
                    for k in range(8):
                        P.op("pe", I("matmul", self.ps[pa][:, 0:512], lhsT=hnT[hb][:, k, i * 128:(i + 1) * 128],
                                                                    rhs=W[:, k, c0:c0 + 512], start=(k == 0), stop=(k == 7)),
                             reads=hk + Wk, writes=[("ps", pa)])
                    for k in range(8):
                        P.op("pe", I("matmul", self.ps[pb][:, 0:280], lhsT=hnT[hb][:, k, i * 128:(i + 1) * 128],
                                                                    rhs=W[:, k, c0 + 512:c0 + 792], start=(k == 0), stop=(k == 7)),
                             reads=hk + Wk, writes=[("ps", pb)])
                    P.op("act", I("copy", out=va[b][:, :, 0:64], in_=self.ps[pa][:, 0:512].rearrange("p (g d) -> p g d", d=64)),
                         reads=[("ps", pa)], writes=[("va", b)])
                    P.op("dve", I("tensor_copy", out=svt[b][:], in_=self.ps[pb][:, 0:256]),
                         reads=[("ps", pb)], writes=[("svt", b)])
                    P.op("dve", I("tensor_tensor", out=gt[b][:], in0=self.ps[pb][:, 256:280], in1=bgate[:], op=ALU.add),
                         reads=[("ps", pb), "bgate"], writes=[("gt", b)])
                    P.op("act", I("activation", out=gt[b][:], in_=gt[b][:], func=AF.Sigmoid), reads=[("gt", b)], writes=[("gt", b)])
                    rows = slice(ti * 128, (ti + 1) * 128)
                    P.dma("sp", I("dma_start", out=dr["VA"][rows], in_=va[b][:]), reads=[("va", b)], writes=[("VA", ti)])
                    P.dma("sp", I("dma_start", out=dr["SV"][rows], in_=svt[b][:]), reads=[("svt", b)], writes=[("SV", ti)])
                    P.dma("sp", I("dma_start", out=dr["GT"][rows], in_=gt[b][:]), reads=[("gt", b)], writes=[("GT", ti)])
            P.dma("sp", I("dma_start", out=dr["cT"], in_=cc[:]), reads=["cc"], writes=["cT"])
            P.end_phase()

    def phase_B(self, l):
        nc, P, dr = self.nc, self.P, self.dr
        with contextlib.ExitStack() as st:
            T = lambda n, sh, dt: st.enter_context(nc.sbuf_tensor(self.uname(n), sh, dt))
            kvT = {"k": T("b_kT", [128, S], BF16), "v": T("b_vT", [128, S], BF16)}
            stage = T("b_stage", [128, 4096], F32)
            W1 = {kv: T("b_w1" + kv, [128, 32, 128], BF16) for kv in "kv"}
            posT = {kv: T("b_pos" + kv, [128, 32], BF16) for kv in "kv"}
            W2 = {kv: T("b_w2" + kv, [128, 64], BF16) for kv in "kv"}
            st32 = T("b_st32", [128, 32], F32)
            st64 = T("b_st64", [128, 64], F32)
            bias = T("b_bias", [128, 1], F32)
            xs = T("b_xs", [128, 255], F32)
            x2 = T("b_x2", [128, 255], F32)
            sg = T("b_sg", [128, 255], F32)
            gl = T("b_gl", [128, 256], BF16)
            kc = T("b_kc", [128, 256], BF16)
            vc = T("b_vc", [128, 2, 2, 65], BF16)
            P.dma("sp", I("dma_start", out=kvT["k"][:], in_=dr["FT"][4]), writes=["kT"])
            P.dma("sp", I("dma_start", out=kvT["v"][:], in_=dr["FT"][7]), writes=["vT"])
            P.op("pool", I("memset", vc[:], 1.0), writes=["vc"])
            P.op("pool", I("memset", gl[:], 0.0), writes=["gl"])
            for kv in "kv":
                self.load_weight_bf(W1[kv][:].rearrange("p l h -> p (l h)"), dr["w1_" + kv][l], stage[:], "stage", "W1" + kv)
                self.load_weight_bf(posT[kv][:], dr["pos_" + kv][l], st32[:], "st32", "pos" + kv)
                self.load_weight_bf(W2[kv][:], dr["w2_" + kv][l], st64[:], "st64", "W2" + kv)
            for kv in "kv":
                pb = self.psn()
                for ll in range(32):
                    P.op("pe", I("matmul", self.ps[pb][:, 0:1], lhsT=W1[kv][0:64, ll, :], rhs=posT[kv][0:64, ll:ll + 1],
                                                                start=(ll == 0), stop=(ll == 31)),
                         reads=["W1" + kv, "pos" + kv], writes=[("ps", pb)])
                P.op("dve", I("tensor_copy", out=bias[:], in_=self.ps[pb][:, 0:1]), reads=[("ps", pb)], writes=["bias"])
                for g in range(2):
                    gs = slice(64 * g, 64 * g + 64)
                    pa = self.psn()
                    for ll in range(32):
                        P.op("pe", I("matmul",
                            self.ps[pa][:, 0:255], lhsT=W1[kv][gs, ll, :], rhs=kvT[kv][gs, ll:ll + 16 * 254 + 1:16],
                            start=(ll == 0), stop=(ll == 31)), reads=["W1" + kv, kv + "T"], writes=[("ps", pa)])
                    P.op("act", I("activation", out=xs[:], in_=self.ps[pa][:, 0:255], func=AF.Identity, bias=bias[:, 0:1], scale=1.0),
                         reads=[("ps", pa), "bias"], writes=["xs"])
                    P.op("dve", I("tensor_tensor", out=x2[:], in0=xs[:], in1=xs[:], op=ALU.mult), reads=["xs"], writes=["x2"])
                    P.op("dve", I("tensor_scalar", out=x2[:], in0=x2[:], scalar1=0.044715, scalar2=1.0, op0=ALU.mult, op1=ALU.add),
                         reads=["x2"], writes=["x2"])
                    P.op("dve", I("tensor_tensor", out=x2[:], in0=x2[:], in1=xs[:], op=ALU.mult), reads=["x2", "xs"], writes=["x2"])
                    P.op("act", I("activation", out=sg[:], in_=x2[:], func=AF.Sigmoid, scale=1.5957691216057308),
                         reads=["x2"], writes=["sg"])
                    P.op("dve", I("tensor_tensor", out=gl[:, 0:255], in0=xs[:], in1=sg[:], op=ALU.mult), reads=["xs", "sg"], writes=["gl"])
                    if kv == "k":
                        po = self.psn()
                        P.op("pe", I("matmul", self.ps[po][gs, 0:256], lhsT=W2["k"][:, :], rhs=gl[:, :], start=True, stop=True),
                             reads=["W2k", "gl"], writes=[("ps", po)])
                        P.op("dve", I("tensor_copy", out=kc[gs, :], in_=self.ps[po][gs, 0:256]),
                             reads=[("ps", po)], writes=[("kc", g)])
                    else:
                        for nb in range(2):
                            po = self.psn()
                            P.op("pe", I("matmul", self.ps[po][:, 0:64], lhsT=gl[:, nb * 128:(nb + 1) * 128], rhs=W2["v"][:, :],
                                                                     start=True, stop=True), reads=["W2v", "gl"], writes=[("ps", po)])
                            P.op("dve", I("tensor_copy", out=vc[:, g, nb, 0:64], in_=self.ps[po][:, 0:64]),
                                 reads=[("ps", po)], writes=["vc"])
            P.dma("sp", I("dma_start", out=dr["kcT"], in_=kc[:]), reads=[("kc", 0), ("kc", 1)], writes=["kcT"])
            P.dma("sp", I("dma_start", out=dr["VC"], in_=vc[:]), reads=["vc"], writes=["VC"])
            P.end_phase()

    def phase_fox(self, l):
        nc, P, dr, ps = self.nc, self.P, self.dr, self.ps
        if "cH" not in dr:
            self.dscr("cH", [4, 3, S], BF16)
        with contextlib.ExitStack() as st:
            T = lambda n, sh, dt: st.enter_context(nc.sbuf_tensor(self.uname(n), sh, dt))
            QaT = T("f_QaT", [70, 4, S], BF16)
            KaT = T("f_KaT", [70, 4, S], BF16)
            V = T("f_V", [128, 32, 4, 65], BF16)
            c4 = T("f_c4", [4, S], F32)
            rr = T("f_rr", [4, S], F32)
            H = T("f_H", [4, 3, S], BF16)
            mgt = T("f_mgt", [128, 4, 512], BF16)
            ident = T("f_ident", [128, 128], BF16)
            Pt = [T("f_P%d" % i, [128, CH], BF16) for i in range(3)]
            rs = [T("f_rs%d" % i, [128, 1], F32) for i in range(4)]
            of = [T("f_of%d" % i, [128, 4, 64], F32) for i in range(8)]
            P.dma("sp", I("dma_start", out=mgt[:], in_=dr["m_gt"]), writes=["mgt"])
            P.dma("sp", I("dma_start", out=ident[:], in_=dr["ident"]), writes=["ident"])
            P.dma("sp", I("dma_start", out=c4[:], in_=dr["cT"]), writes=["c4"])
            for q8 in range(8):
                P.dma("sp", I("dma_start", out=V[:, q8 * 4:(q8 + 1) * 4, :, :],
                              in_=dr["VA"][q8 * 512:(q8 + 1) * 512, 4:8, :].rearrange("(sb p) h c -> p sb h c", p=128)), writes=["V"])
            P.op("pool", I("memset", QaT[64:70, :, :], -1.0), writes=["Qaug"])
            P.op("pool", I("memset", KaT[64:70, :, :], 1.0), writes=["Kaug"])
            for hh in range(4):
                src_q = dr["FT"][8 + hh // 2, (hh % 2) * 64:(hh % 2) * 64 + 64, :]
                src_k = dr["FT"][10 + hh // 2, (hh % 2) * 64:(hh % 2) * 64 + 64, :]
                P.dma("sp", I("dma_start", out=QaT[0:64, hh, :], in_=src_q), writes=[("Q", hh)])
                P.dma("act", I("dma_start", out=KaT[0:64, hh, :], in_=src_k), writes=[("K", hh)])
            P.op("dve", I("tensor_copy", out=H[:, 0, :], in_=c4[:]), reads=["c4"], writes=["H0"])
            P.op("dve", I("tensor_tensor", out=rr[:], in0=c4[:], in1=H[:, 0, :], op=ALU.subtract), reads=["c4", "H0"], writes=["rr"])
            P.op("dve", I("tensor_copy", out=H[:, 1, :], in_=rr[:]), reads=["rr"], writes=["H1"])
            P.op("dve", I("tensor_tensor", out=rr[:], in0=rr[:], in1=H[:, 1, :], op=ALU.subtract), reads=["rr", "H1"], writes=["rr"])
            P.op("dve", I("tensor_copy", out=H[:, 2, :], in_=rr[:]), reads=["rr"], writes=["H2"])
            P.dma("sp", I("dma_start", out=dr["cH"], in_=H[:]), reads=["H0", "H1", "H2"], writes=["cH"])
            for hh in range(4):
                P.dma("sp", I("dma_start", out=QaT[64:67, hh, :], in_=dr["cH"][hh]), reads=["cH", "Qaug"], writes=[("Qa", hh)])
                P.dma("sp", I("dma_start", out=KaT[67:70, hh, :], in_=dr["cH"][hh]), reads=["cH", "Kaug"], writes=[("Ka", hh)])
            sti = 0
            pi = 0
            for qc in range(NCH):
                csl = slice(qc * CH, (qc + 1) * CH)
                for hh in range(4):
                    qk = [("Q", hh), ("Qa", hh), ("K", hh), ("Ka", hh), "Qaug", "Kaug"]
                    for sb in range(4 * qc + 4):
                        diag = sb >= 4 * qc
                        b = sti % 3
                        sti += 1
                        P.op("pe", I("matmul", ps[b][:, :], lhsT=KaT[0:70, hh, sb * 128:(sb + 1) * 128], rhs=QaT[0:70, hh, csl],
                                     start=True, stop=not diag), reads=qk, writes=[("ps", b)])
                        if diag:
                            rq = sb - 4 * qc
                            P.op("pe", I("matmul", ps[b][:, rq * 128:(rq + 1) * 128], lhsT=ident[:, :], rhs=mgt[:, rq, rq * 128:(rq + 1) * 128],
                                         start=False, stop=True), reads=["ident", "mgt"], writes=[("ps", b)])
                        pb = pi % 3
                        pi += 1
                        P.op("act", I("activation", out=Pt[pb][:], in_=ps[b][:, :], func=AF.Exp), reads=[("ps", b)], writes=[("P", pb)])
                        def back(sb, pb, hh, qc):
                            for ti in range(4):
                                if sb <= 4 * qc + ti:
                                    P.op("pe", I("matmul", ps[3 + ti][:, 0:65], lhsT=Pt[pb][:, ti * 128:(ti + 1) * 128], rhs=V[:, sb, hh, :],
                                                 start=(sb == 0), stop=(sb == 4 * qc + ti)), reads=[("P", pb), "V"], writes=[("ps", 3 + ti)])
                        self.run_deferred()
                        self.defer(back, sb, pb, hh, qc)

                    def fin(hh, qc):
                        for ti in range(4):
                            P.op("dve", I("tensor_scalar", out=rs[ti][:], in0=ps[3 + ti][:, 64:65], scalar1=1e-20, scalar2=None, op0=ALU.max),
                                 reads=[("ps", 3 + ti)], writes=[("rs", ti)])
                        for ti in range(4):
                            P.op("dve", I("reciprocal", out=rs[ti][:], in_=rs[ti][:]), reads=[("rs", ti)], writes=[("rs", ti)])
                        for ti in range(4):
                            o = (qc % 2) * 4 + ti
                            P.op("dve", I("tensor_scalar", out=of[o][:, hh, :], in0=ps[3 + ti][:, 0:64], scalar1=rs[ti][:, 0:1], scalar2=None, op0=ALU.mult),
                                 reads=[("ps", 3 + ti), ("rs", ti)], writes=[("of", o, hh)])
                    self.defer(fin, hh, qc)

                def store(qc):
                    for ti in range(4):
                        o = (qc % 2) * 4 + ti
                        rows = slice((qc * 4 + ti) * 128, (qc * 4 + ti + 1) * 128)
                        P.dma("sp", I("dma_start", out=dr["oS"][rows, 512:768], in_=of[o][:].rearrange("p h d -> p (h d)")),
                              reads=[("of", o, hh) for hh in range(4)], writes=[("oS", "f", qc, ti)])
                self.defer(store, qc)
            self.run_deferred()
            P.end_phase()

    def phase_sb(self, l):
        nc, P, dr, ps = self.nc, self.P, self.dr, self.ps
        with contextlib.ExitStack() as st:
            T = lambda n, sh, dt: st.enter_context(nc.sbuf_tensor(self.uname(n), sh, dt))
            QT = T("s_QT", [128, 2, S], BF16)
            KT = T("s_KT", [128, 2, S], BF16)
            V = T("s_V", [128, 32, 256], BF16)
            mge = T("s_mge", [128, 4, 512], BF16)
            ident = T("s_ident", [128, 128], BF16)
            ntri = T("s_ntri", [128, 128], BF16)
            nones = T("s_nones", [1, 128], BF16)
            onec = T("s_onec", [128, 1], BF16)
            et = [T("s_e%d" % i, [128, CH], F32) for i in range(2)]
            SP = [T("s_SP%d" % i, [128, CH], BF16) for i in range(3)]
            at = [T("s_a%d" % i, [128, CH], BF16) for i in range(2)]
            carry = T("s_carry", [1, CH], F32)
            ctmp = T("s_ctmp", [1, CH], F32)
            chi = [T("s_chi%d" % i, [1, CH], BF16) for i in range(3)]
            clo = [T("s_clo%d" % i, [1, CH], BF16) for i in range(3)]
            osb = [T("s_o%d" % i, [128, 4, 64], F32) for i in range(8)]
            for n, t_ in (("m_ge", mge), ("ident", ident), ("ntri", ntri), ("nones", nones), ("onec", onec)):
                P.dma("sp", I("dma_start", out=t_[:], in_=dr[n]), writes=[n])
            for j in range(2):
                P.dma("sp", I("dma_start", out=QT[:, j, :], in_=dr["FT"][12 + j]), writes=[("Q", j)])
                P.dma("act", I("dma_start", out=KT[:, j, :], in_=dr["FT"][14 + j]), writes=[("K", j)])
            for q8 in range(8):
                P.dma("sp", I("dma_start", out=V[:, q8 * 4:(q8 + 1) * 4, :],
                              in_=dr["SV"][q8 * 512:(q8 + 1) * 512, :].rearrange("(sb p) c -> p sb c", p=128)), writes=["V"])
            cnt = {"a": 0, "e": 0, "sp": 0, "at": 0, "c": 0}
            for qc in range(NCH):
                csl = slice(qc * CH, (qc + 1) * CH)
                for h in range(4):
                    hb = slice(64 * (h % 2), 64 * (h % 2) + 64)
                    j = h // 2
                    qk = [("Q", j), ("K", j)]
                    blocks = list(range(4 * qc + 3, -1, -1))
                    nblk = len(blocks)
                    info = {}

                    def S1a(idx):
                        sb = blocks[idx]
                        diag = sb >= 4 * qc
                        P.op("pe", I("matmul", ps[0][:, :], lhsT=KT[hb, j, sb * 128:(sb + 1) * 128], rhs=QT[hb, j, csl], start=True, stop=not diag),
                             reads=qk, writes=[("ps", 0)])
                        if diag:
                            P.op("pe", I("matmul", ps[0][:, :], lhsT=ident[:, :], rhs=mge[:, sb - 4 * qc, :], start=False, stop=True),
                                 reads=["ident", "m_ge"], writes=[("ps", 0)])
                        eb = cnt["e"] % 2
                        cnt["e"] += 1
                        P.op("act", I("activation", out=et[eb][:], in_=ps[0][:, :], func=AF.Exp), reads=[("ps", 0)], writes=[("e", eb)])
                        sb_i = cnt["sp"] % 3
                        cnt["sp"] += 1
                        P.op("act", I("activation", out=SP[sb_i][:], in_=et[eb][:], func=AF.Ln, bias=1.0, scale=1.0),
                             reads=[("e", eb)], writes=[("SP", sb_i)])
                        info[idx] = sb_i

                    def S1b(idx):
                        sb_i = info[idx]
                        P.op("pe", I("matmul", ps[3][0:1, :], lhsT=onec[:, 0:1], rhs=SP[sb_i][:, :], start=(idx == 0), stop=(idx + 2 == nblk)),
                             reads=["onec", ("SP", sb_i)], writes=[("ps", 3)])
                        cb = (idx + 1) % 3
                        P.op("dve", I("tensor_copy", out=chi[cb][:], in_=ps[3][0:1, :]), reads=[("ps", 3)], writes=[("chi", cb)])
                        P.op("dve", I("tensor_tensor", out=clo[cb][:], in0=ps[3][0:1, :], in1=chi[cb][:], op=ALU.subtract),
                             reads=[("ps", 3), ("chi", cb)], writes=[("clo", cb)])

                    def S2a(idx):
                        sb = blocks[idx]
                        diag = sb >= 4 * qc
                        sb_i = info[idx]
                        bb = 1 + (cnt["a"] % 2)
                        cnt["a"] += 1
                        P.op("pe", I("matmul", ps[bb][:, :], lhsT=KT[hb, j, sb * 128:(sb + 1) * 128], rhs=QT[hb, j, csl], start=True, stop=False),
                             reads=qk, writes=[("ps", bb)])
                        if diag:
                            P.op("pe", I("matmul", ps[bb][:, :], lhsT=ident[:, :], rhs=mge[:, sb - 4 * qc, :], start=False, stop=False),
                                 reads=["ident", "m_ge"], writes=[("ps", bb)])
                        last = (idx == 0)
                        P.op("pe", I("matmul", ps[bb][:, :], lhsT=ntri[:, :], rhs=SP[sb_i][:, :], start=False, stop=last),
                             reads=["ntri", ("SP", sb_i)], writes=[("ps", bb)])
                        if idx > 0:
                            cb = idx % 3
                            P.op("pe", I("matmul", ps[bb][:, :], lhsT=nones[0:1, :], rhs=chi[cb][0:1, :], start=False, stop=False),
                                 reads=["nones", ("chi", cb)], writes=[("ps", bb)])
                            P.op("pe", I("matmul", ps[bb][:, :], lhsT=nones[0:1, :], rhs=clo[cb][0:1, :], start=False, stop=True),
                                 reads=["nones", ("clo", cb)], writes=[("ps", bb)])
                        ab = cnt["at"] % 2
                        cnt["at"] += 1
                        P.op("act", I("activation", out=at[ab][:], in_=ps[bb][:, :], func=AF.Exp), reads=[("ps", bb)], writes=[("at", ab)])
                        info[("ab", idx)] = ab

                    def S2b(idx):
                        sb = blocks[idx]
                        ab = info[("ab", idx)]
                        for ti in range(4):
                            if sb <= 4 * qc + ti:
                                P.op("pe", I("matmul", ps[4 + ti][:, 0:64], lhsT=at[ab][:, ti * 128:(ti + 1) * 128], rhs=V[:, sb, h * 64:(h + 1) * 64],
                                             start=(sb == 4 * qc + ti), stop=(sb == 0)), reads=[("at", ab), "V"], writes=[("ps", 4 + ti)])

                    S1a(0)
                    if nblk > 1:
                        S1a(1)
                        S1b(0)
                    for idx in range(nblk):
                        if idx + 2 < nblk:
                            S1a(idx + 2)
                            S1b(idx + 1)
                        S2a(idx)
                        if idx >= 1:
                            S2b(idx - 1)
                    S2b(nblk - 1)
                    for ti in range(4):
                        o = (qc % 2) * 4 + ti
                        P.op("dve", I("tensor_copy", out=osb[o][:, h, :], in_=ps[4 + ti][:, 0:64]), reads=[("ps", 4 + ti)], writes=[("osb", o, h)])
                for ti in range(4):
                    o = (qc % 2) * 4 + ti
                    rows = slice((qc * 4 + ti) * 128, (qc * 4 + ti + 1) * 128)
                    P.dma("sp", I("dma_start", out=dr["oS"][rows, 768:1024], in_=osb[o][:].rearrange("p h d -> p (h d)")),
                          reads=[("osb", o, h) for h in range(4)], writes=[("oS", "s", qc, ti)])
            P.end_phase()

    def phase_nsa(self, l):
        nc, P, dr, ps = self.nc, self.P, self.dr, self.ps
        pt = self.pt
        with contextlib.ExitStack() as st:
            T = lambda n, sh, dt: st.enter_context(nc.sbuf_tensor(self.uname(n), sh, dt))
            QT = T("n_QT", [128, 4, S], BF16)
            KsE = T("n_KsE", [128, 2, S], BF16)
            Qs = T("n_Qs", [128, 4, CH], BF16)
            KwT = T("n_KwT", [128, S], BF16)
            kcT = T("n_kcT", [128, 256], BF16)
            V4 = T("n_V4", [128, 32, 4, 65], BF16)
            VC = T("n_VC", [128, 2, 2, 65], BF16)
            ovl = T("n_ovl", [128, 2, 64], BF16)
            mgt = T("n_mgt", [128, 4, 512], BF16)
            mle = T("n_mle", [128, 4, 512], BF16)
            ident = T("n_ident", [128, 128], BF16)
            mcmp = [T("n_mcmp%d" % i, [128, 2, CH], BF16) for i in range(2)]
            sbias = [T("n_sbias%d" % i, [128, 64], F32) for i in range(4)]
            gts = [T("n_gt%d" % i, [128, 24], F32) for i in range(8)]
            Pt = [T("n_P%d" % i, [128, CH], BF16) for i in range(3)]
            Pc = [T("n_Pc%d" % i, [128, CH], BF16) for i in range(4)]
            onsa = [T("n_o%d" % i, [128, 8, 64], F32) for i in range(8)]
            impg = [T("n_imp%d" % i, [128, 64], F32) for i in range(4)]
            score = T("n_score", [128, 64], F32)
            sc2 = T("n_sc2", [128, 64], F32)
            sc3 = T("n_sc3", [128, 64], F32)
            m8a = T("n_m8a", [128, 8], F32)
            m8b = T("n_m8b", [128, 8], F32)
            seln = T("n_seln", [128, 64], BF16)
            rc4 = T("n_rc4", [128, 4], F32)
            rg = T("n_rg", [128, 1], F32)
            rs1 = T("n_rs1", [128, 1], F32)
            self._nsa_tmp = ([T("n_r4%d" % i, [128, 1], F32) for i in range(4)], [T("n_g4%d" % i, [128, 1], F32) for i in range(4)],
                             [T("n_s4%d" % i, [128, 64], F32) for i in range(4)])
            for n, t_ in (("m_gt", mgt), ("m_le", mle), ("ident", ident), ("ovl", ovl)):
                P.dma("sp", I("dma_start", out=t_[:], in_=dr[n]), writes=[n])
            for j in range(4):
                P.dma("sp" if j % 2 == 0 else "act", I("dma_start", out=QT[:, j, :], in_=dr["FT"][j]), writes=[("Q", j)])
            for g_ in range(2):
                P.dma("sp", I("dma_start", out=KsE[0:64, g_, :], in_=dr["FT"][5, 64 * g_:64 * g_ + 64, :]), writes=[("KsE", g_, 0)])
                P.dma("act", I("dma_start", out=KsE[64:128, g_, :], in_=dr["eblk"]), writes=[("KsE", g_, 1)])
            P.dma("act", I("dma_start", out=KwT[:], in_=dr["FT"][6]), writes=["KwT"])
            P.dma("sp", I("dma_start", out=kcT[:], in_=dr["kcT"]), writes=["kcT"])
            P.dma("sp", I("dma_start", out=VC[:], in_=dr["VC"]), writes=["VC"])
            for q8 in range(8):
                P.dma("sp", I("dma_start", out=V4[:, q8 * 4:(q8 + 1) * 4, :, :],
                              in_=dr["VA"][q8 * 512:(q8 + 1) * 512, 0:4, :].rearrange("(sb p) h c -> p sb h c", p=128)), writes=["V4"])
            cn = {"st": 0, "p": 0}

            def st_bank():
                b = cn["st"] % 3
                cn["st"] += 1
                return b

            def exp_to(b, dst, dstk, scale=0.125):
                P.op("act", I("activation", out=dst[:], in_=ps[b][:, :], func=AF.Exp, scale=scale), reads=[("ps", b)], writes=[dstk])

            for qc in range(NCH):
                csl = slice(qc * CH, (qc + 1) * CH)
                mb = qc % 2
                P.dma("sp", I("dma_start", out=mcmp[mb][:], in_=dr["m_cmp"][:, :, csl]), writes=[("mcmp", mb)])
                for ti in range(4):
                    rows = slice((qc * 4 + ti) * 128, (qc * 4 + ti + 1) * 128)
                    P.dma("sp", I("dma_start", out=sbias[ti][:], in_=dr["selbias"][rows]), writes=[("sbias", ti)])
                    P.dma("sp", I("dma_start", out=gts[mb * 4 + ti][:], in_=dr["GT"][rows]), writes=[("gts", mb * 4 + ti)])
                nbs = [0] if qc < 4 else [0, 1]
                for g in range(2):
                    gs = slice(64 * g, 64 * g + 64)
                    self.run_deferred()
                    for hh in range(4):
                        head = 4 * g + hh
                        for nb in nbs:
                            b = st_bank()
                            P.op("pe", I("matmul", ps[b][:, :], lhsT=kcT[gs, nb * 128:(nb + 1) * 128], rhs=QT[gs, hh, csl], start=True, stop=False),
                                 reads=["kcT", ("Q", hh)], writes=[("ps", b)])
                            P.op("pe", I("matmul", ps[b][:, :], lhsT=ident[:, :], rhs=mcmp[mb][:, nb, :], start=False, stop=True),
                                 reads=["ident", ("mcmp", mb)], writes=[("ps", b)])
                            pk = (hh % 2) * 2 + nb
                            exp_to(b, Pc[pk], ("Pc", pk))
                        for ti in range(4):
                            for nb in nbs:
                                pk = (hh % 2) * 2 + nb
                                P.op("pe", I("matmul", ps[3 + ti][:, 0:65], lhsT=Pc[pk][:, ti * 128:(ti + 1) * 128], rhs=VC[:, g, nb, :],
                                             start=(nb == nbs[0]), stop=(nb == nbs[-1])), reads=[("Pc", pk), "VC"], writes=[("ps", 3 + ti)])
                            for nb in nbs:
                                pk = (hh % 2) * 2 + nb
                                P.op("pe", I("matmul", ps[3 + ti][:, 128:192], lhsT=Pc[pk][:, ti * 128:(ti + 1) * 128], rhs=ovl[:, nb, :],
                                             start=(nb == nbs[0]), stop=(nb == nbs[-1])), reads=[("Pc", pk), "ovl"], writes=[("ps", 3 + ti)])
                        r4, g4, s4 = self._nsa_tmp
                        for ti in range(4):
                            P.op("dve", I("tensor_scalar", out=r4[ti][:], in0=ps[3 + ti][:, 64:65], scalar1=1e-20, scalar2=None, op0=ALU.max),
                                 reads=[("ps", 3 + ti)], writes=[("r4", ti)])
                        for ti in range(4):
                            P.op("dve", I("reciprocal", out=r4[ti][:], in_=r4[ti][:]), reads=[("r4", ti)], writes=[("r4", ti)])
                        for ti in range(4):
                            o = mb * 4 + ti
                            P.op("dve", I("tensor_tensor", out=g4[ti][:], in0=r4[ti][:], in1=gts[o][:, head * 3:head * 3 + 1], op=ALU.mult),
                                 reads=[("r4", ti), ("gts", o)], writes=[("g4", ti)])
                        for ti in range(4):
                            if hh == 0:
                                P.op("dve", I("tensor_scalar", out=impg[ti][:], in0=ps[3 + ti][:, 128:192], scalar1=r4[ti][:, 0:1], scalar2=None, op0=ALU.mult),
                                     reads=[("ps", 3 + ti), ("r4", ti)], writes=[("impg", ti)])
                            else:
                                P.op("dve", I("tensor_scalar", out=s4[ti][:], in0=ps[3 + ti][:, 128:192], scalar1=r4[ti][:, 0:1], scalar2=None, op0=ALU.mult),
                                     reads=[("ps", 3 + ti), ("r4", ti)], writes=[("s4", ti)])
                        if hh > 0:
                            for ti in range(4):
                                P.op("pool", I("tensor_tensor", out=impg[ti][:], in0=impg[ti][:], in1=s4[ti][:], op=ALU.add),
                                     reads=[("s4", ti), ("impg", ti)], writes=[("impg", ti)])
                        for ti in range(4):
                            o = mb * 4 + ti
                            P.op("dve", I("tensor_scalar", out=onsa[o][:, head, :], in0=ps[3 + ti][:, 0:64], scalar1=g4[ti][:, 0:1], scalar2=None, op0=ALU.mult),
                                 reads=[("ps", 3 + ti), ("g4", ti)], writes=[("onsa", o, head)])
                    if NSA_STOP == 1:
                        continue
                    for ti in range(4):
                        P.op("dve", I("tensor_tensor", out=score[:], in0=impg[ti][:], in1=sbias[ti][:], op=ALU.add),
                             reads=[("impg", ti), ("sbias", ti)], writes=["score"])
                        P.op("dve", I("max", out=m8a[:], in_=score[:]), reads=["score"], writes=["m8a"])
                        P.op("dve", I("match_replace", out=sc3[:], in_to_replace=m8a[:], in_values=score[:], imm_value=-3.0e9),
                             reads=["score", "m8a"], writes=["sc3"])
                        P.op("dve", I("max", out=m8b[:], in_=sc3[:]), reads=["sc3"], writes=["m8b"])
                        P.op("dve", I("tensor_scalar", out=seln[:], in0=score[:], scalar1=m8b[:, 7:8], scalar2=-BIG, op0=ALU.is_lt, op1=ALU.mult),
                             reads=["score", "m8b"], writes=["seln"])
                        P.op("pe", I("transpose", out=pt[0:64, ti * 128:(ti + 1) * 128], in_=seln[:, :], identity=ident[:, :]),
                             reads=["seln", "ident"], writes=[("ps", 7)])
                    for hh in range(4):
                        P.op("pool", I("tensor_copy", out=Qs[0:64, hh, :], in_=QT[gs, hh, csl]), reads=[("Q", hh)], writes=[("Qs", hh, 0)])
                        P.op("dve", I("tensor_copy", out=Qs[64:128, hh, :], in_=pt[0:64, 0:512]), reads=[("ps", 7)], writes=[("Qs", hh, 1)])
                    if NSA_STOP == 2:
                        continue
                    for branch in ((1, 2) if NSA_STOP != 3 else (1,)):
                        for hh in range(4):
                            head = 4 * g + hh
                            if branch == 1:
                                seq = [(sb, None) for sb in range(4 * qc + 4)]
                            else:
                                seq = [(4 * qc - 4 + r, r) for r in range(8) if 4 * qc - 4 + r >= 0]
                            first = {}
                            last = {}
                            for (sb, r) in seq:
                                for ti in range(4):
                                    if branch == 1:
                                        ok = sb <= 4 * qc + ti
                                    else:
                                        ok = (ti <= r) if r < 4 else (r - 4 <= ti)
                                    if ok:
                                        first.setdefault(ti, sb)
                                        last[ti] = sb
                            for (sb, r) in seq:
                                b = st_bank()
                                if branch == 1:
                                    diag = sb >= 4 * qc
                                    P.op("pe", I("matmul", ps[b][:, :], lhsT=KsE[:, g, sb * 128:(sb + 1) * 128], rhs=Qs[:, hh, :], start=True, stop=not diag),
                                         reads=[("KsE", g, 0), ("KsE", g, 1), ("Qs", hh, 0), ("Qs", hh, 1)], writes=[("ps", b)])
                                    if diag:
                                        rq = sb - 4 * qc
                                        P.op("pe", I("matmul", ps[b][:, rq * 128:(rq + 1) * 128], lhsT=ident[:, :],
                                                     rhs=mgt[:, rq, rq * 128:(rq + 1) * 128], start=False, stop=True),
                                             reads=["ident", "m_gt"], writes=[("ps", b)])
                                else:
                                    P.op("pe", I("matmul", ps[b][:, :], lhsT=KwT[gs, sb * 128:(sb + 1) * 128], rhs=QT[gs, hh, csl], start=True, stop=False),
                                         reads=["KwT", ("Q", hh)], writes=[("ps", b)])
                                    rq = r if r < 4 else r - 4
                                    msk = mle[:, rq, rq * 128:(rq + 1) * 128] if r < 4 else mgt[:, rq, rq * 128:(rq + 1) * 128]
                                    P.op("pe", I("matmul", ps[b][:, rq * 128:(rq + 1) * 128], lhsT=ident[:, :], rhs=msk, start=False, stop=True),
                                         reads=["ident", "m_gt", "m_le"], writes=[("ps", b)])
                                pb = cn["p"] % 3
                                cn["p"] += 1
                                exp_to(b, Pt[pb], ("P", pb))
                                def back(sb, r, pb, branch, g, first, last):
                                    for ti in range(4):
                                        if ti in first and first[ti] <= sb <= last[ti]:
                                            if branch == 2:
                                                ok = (ti <= r) if r < 4 else (r - 4 <= ti)
                                                if not ok:
                                                    continue
                                            vg = g if branch == 1 else 2 + g
                                            P.op("pe", I("matmul", ps[3 + ti][:, 0:65], lhsT=Pt[pb][:, ti * 128:(ti + 1) * 128], rhs=V4[:, sb, vg, :],
                                                         start=(sb == first[ti]), stop=(sb == last[ti])), reads=[("P", pb), "V4"], writes=[("ps", 3 + ti)])
                                self.run_deferred()
                                self.defer(back, sb, r, pb, branch, g, first, last)
                            self.defer(self._nsa_fin, mb, head, branch, gts, rs1, rg, sc2, onsa)
                            for ti in range(0):
                                o = mb * 4 + ti
                                gtile = gts[o]
                                P.op("dve", I("tensor_scalar", out=rs1[:], in0=ps[3 + ti][:, 64:65], scalar1=1e-20, scalar2=None, op0=ALU.max),
                                     reads=[("ps", 3 + ti)], writes=["rs1"])
                                P.op("dve", I("reciprocal", out=rs1[:], in_=rs1[:]), reads=["rs1"], writes=["rs1"])
                                P.op("dve", I("tensor_tensor", out=rg[:], in0=rs1[:], in1=gtile[:, head * 3 + branch:head * 3 + branch + 1], op=ALU.mult),
                                     reads=["rs1", ("gts", o)], writes=["rg"])
                                P.op("dve", I("tensor_scalar", out=sc2[:], in0=ps[3 + ti][:, 0:64], scalar1=rg[:, 0:1], scalar2=None, op0=ALU.mult),
                                     reads=[("ps", 3 + ti), "rg"], writes=["sc2"])
                                P.op("pool", I("tensor_tensor", out=onsa[o][:, head, :], in0=onsa[o][:, head, :], in1=sc2[:], op=ALU.add),
                                     reads=["sc2", ("onsa", o, head)], writes=[("onsa", o, head)])
                def store(qc, mb):
                    for ti in range(4):
                        o = mb * 4 + ti
                        rows = slice((qc * 4 + ti) * 128, (qc * 4 + ti + 1) * 128)
                        P.dma("sp", I("dma_start", out=dr["oS"][rows, 0:512], in_=onsa[o][:].rearrange("p h d -> p (h d)")),
                              reads=[("onsa", o, hd) for hd in range(8)], writes=[("oS", "n", qc, ti)])
                self.defer(store, qc, mb)
            self.run_deferred()
            P.end_phase()

    def _nsa_fin(self, mb, head, branch, gts, rs1, rg, sc2, onsa):
        P, ps = self.P, self.ps
        r4, g4, s4 = self._nsa_tmp
        for ti in range(4):
            P.op("dve", I("tensor_scalar", out=r4[ti][:], in0=ps[3 + ti][:, 64:65], scalar1=1e-20, scalar2=None, op0=ALU.max),
                 reads=[("ps", 3 + ti)], writes=[("r4", ti)])
        for ti in range(4):
            P.op("dve", I("reciprocal", out=r4[ti][:], in_=r4[ti][:]), reads=[("r4", ti)], writes=[("r4", ti)])
        for ti in range(4):
            o = mb * 4 + ti
            P.op("dve", I("tensor_tensor", out=g4[ti][:], in0=r4[ti][:], in1=gts[o][:, head * 3 + branch:head * 3 + branch + 1], op=ALU.mult),
                 reads=[("r4", ti), ("gts", o)], writes=[("g4", ti)])
        for ti in range(4):
            P.op("dve", I("tensor_scalar", out=s4[ti][:], in0=ps[3 + ti][:, 0:64], scalar1=g4[ti][:, 0:1], scalar2=None, op0=ALU.mult),
                 reads=[("ps", 3 + ti), ("g4", ti)], writes=[("s4", ti)])
        for ti in range(4):
            o = mb * 4 + ti
            P.op("pool", I("tensor_tensor", out=onsa[o][:, head, :], in0=onsa[o][:, head, :], in1=s4[ti][:], op=ALU.add),
                 reads=[("s4", ti), ("onsa", o, head)], writes=[("onsa", o, head)])

    def phase_D(self, l, last):
        self.phase_D1(l)
        self.phase_D2(l, last)

    def phase_D1(self, l):
        nc, P, dr, ps = self.nc, self.P, self.dr, self.ps
        hsrc = dr["x"] if l == 0 else dr["hS"]
        with contextlib.ExitStack() as st:
            T = lambda n, sh, dt: st.enter_context(nc.sbuf_tensor(self.uname(n), sh, dt))
            Wout = T("d_Wout", [128, 8, D], BF16)
            Wd = T("d_Wd", [128, NF, D], BF16)
            stage = [T("d_stage%d" % i, [128, DFF], F32) for i in range(2)]
            cvt = [T("d_cvt%d" % i, [128, DFF], BF16) for i in range(2)]
            ghead = T("d_ghead", [128, 8], F32)
            gffn = T("d_gffn", [128, 8], F32)
            ident = T("d_ident", [128, 128], BF16)
            h = [T("d_h%d" % i, [128, D], F32) for i in range(4)]
            ot = [T("d_ot%d" % i, [128, D], F32) for i in range(2)]
            osq = T("d_osq", [128, D], F32)
            ssh = T("d_ssh", [128, 16], F32)
            on = [T("d_on%d" % i, [128, D], BF16) for i in range(2)]
            T4 = [dict(junk=T("d_junk%d" % i, [128, D], BF16), ss=T("d_ss%d" % i, [128, 1], F32),
                       rs=T("d_rs%d" % i, [128, 1], F32)) for i in range(2)]
            xT = T("d_xT", [128, 8, CH], BF16)
            actT = T("d_actT", [128, NF, CH], BF16)
            wgu = [T("d_wgu%d" % i, [128, 2, 8, 128], BF16) for i in range(3)]
            sg = [T("d_sg%d" % i, [128, CH], F32) for i in range(2)]
            P.dma("sp", I("dma_start", out=ghead[:], in_=dr["head_norm"][l]), writes=["ghead"])
            P.dma("sp", I("dma_start", out=gffn[:], in_=dr["norm_ffn"][l]), writes=["gffn"])
            P.dma("sp", I("dma_start", out=ident[:], in_=dr["ident"]), writes=["ident"])
            n = 0
            for k in range(8):
                self.load_weight_bf(Wout[:, k, :], dr["w_out"][l, k * 128:(k + 1) * 128, :], stage[n % 2][:, 0:D], ("stage", n % 2),
                                    ("Wout", k), scale_ap=ghead[:, k:k + 1], scalek="ghead", eng=("dve" if n % 2 == 0 else "pool"),
                                    q=("sp" if n % 2 == 0 else "act"))
                n += 1
            for f in range(NF):
                self.load_weight_bf(Wd[:, f, :], dr["w_ffn_down"][l, f * 128:(f + 1) * 128, :], stage[n % 2][:, 0:D], ("stage", n % 2),
                                    ("Wd", f), eng=("dve" if n % 2 == 0 else "pool"), q=("sp" if n % 2 == 0 else "act"))
                n += 1
            for gi, wn in enumerate(("w_ffn_gate", "w_ffn_up")):
                for k in range(8):
                    b = n % 2
                    self.load_weight_bf(cvt[b][:], dr[wn][l, k * 128:(k + 1) * 128, :], stage[b][:], ("stage", b), ("cvt", b),
                                        scale_ap=gffn[:, k:k + 1], scalek="gffn", eng=("dve" if b == 0 else "pool"),
                                        q=("sp" if b == 0 else "act"))
                    for f0, f1 in ((0, 8), (8, 16), (16, NF)):
                        P.dma("sp", I("dma_start", out=dr["WGU"][f0:f1, :, gi, k, :].rearrange("f p c -> p f c"),
                                      in_=cvt[b][:, f0 * 128:f1 * 128].rearrange("p (f c) -> p f c", c=128)),
                              reads=[("cvt", b)], writes=[("WGU", gi, k, f0)])
                    n += 1
            wgu_ready = [("WGU", gi, k, f0) for gi in range(2) for k in range(8) for f0 in (0, 8, 16)]
            Woutk = [("Wout", k) for k in range(8)]
            Wdk = [("Wd", f) for f in range(NF)]
            wl = 0
            for c in range(NCH):
                for i in range(4):
                    ti = c * 4 + i
                    b = ti % 2
                    rows = slice(ti * 128, (ti + 1) * 128)
                    P.dma("sp", I("dma_start", out=h[i][:], in_=hsrc[rows, :]), writes=[("h", i)])
                    P.dma("act", I("dma_start", out=ot[b][:], in_=dr["oS"][rows, :]), writes=[("ot", b)])
                    P.op("pool", I("tensor_tensor", out=osq[:], in0=ot[b][:], in1=ot[b][:], op=ALU.mult), reads=[("ot", b)], writes=["osq"])
                    P.op("dve", I("tensor_reduce", out=ssh[:], in_=osq[:].rearrange("p (h d) -> p h d", d=64), axis=AX.X, op=ALU.add),
                         reads=["osq"], writes=["ssh"])
                    P.op("dve", I("tensor_scalar", out=ssh[:], in0=ssh[:], scalar1=1.0 / 64, scalar2=1e-6, op0=ALU.mult, op1=ALU.add),
                         reads=["ssh"], writes=["ssh"])
                    P.op("act", I("sqrt", out=ssh[:], in_=ssh[:]), reads=["ssh"], writes=["ssh"])
                    P.op("dve", I("reciprocal", out=ssh[:], in_=ssh[:]), reads=["ssh"], writes=["ssh"])
                    P.op("dve", I("tensor_tensor", out=on[b][:].rearrange("p (h d) -> p h d", d=64),
                                  in0=ot[b][:].rearrange("p (h d) -> p h d", d=64),
                                  in1=ssh[:, :].unsqueeze(2).to_broadcast([128, 16, 64]), op=ALU.mult),
                         reads=[("ot", b), "ssh"], writes=[("on", b)])
                    self.transpose8(on[b], ("on", b), xT[:, :, i * 128:(i + 1) * 128], ("xT", i), ident, eng=("dve" if i % 2 == 0 else "act"))
                    for half in range(2):
                        pa = self.psn()
                        for k in range(8):
                            P.op("pe", I("matmul", ps[pa][:, :], lhsT=xT[:, k, i * 128:(i + 1) * 128], rhs=Wout[:, k, half * 512:(half + 1) * 512],
                                         start=(k == 0), stop=(k == 7)), reads=[("xT", i)] + Woutk, writes=[("ps", pa)])
                        P.op("dve", I("tensor_tensor", out=h[i][:, half * 512:(half + 1) * 512], in0=ps[pa][:, :],
                                      in1=h[i][:, half * 512:(half + 1) * 512], op=ALU.add), reads=[("ps", pa), ("h", i)], writes=[("h", i)])
                if "hmix" in self.dr:
                    for i in range(4):
                        rows = slice((c * 4 + i) * 128, (c * 4 + i + 1) * 128)
                        P.dma("sp", I("dma_start", out=dr["hmix"][rows, :], in_=h[i][:]), reads=[("h", i)], writes=[("hmix", c, i)])
                for i in range(4):
                    b = i % 2
                    self.rms_tile(T4[b], b, h[i], ("h", i), ("junk", b), ("ss", b), ("rs", b), on[b], ("on", b))
                    self.transpose8(on[b], ("on", b), xT[:, :, i * 128:(i + 1) * 128], ("xT", i), ident, eng=("dve" if i % 2 == 0 else "act"))
                xk = [("xT", i) for i in range(4)]
                for f in range(NF):
                    wb = wl % 3
                    wl += 1
                    P.dma("sp" if f % 2 == 0 else "act", I("dma_start", out=wgu[wb][:], in_=dr["WGU"][f]), reads=wgu_ready, writes=[("wgu", wb)])
                    pg, pu = self.psn(), self.psn()
                    for gi, pp in ((0, pg), (1, pu)):
                        for k in range(8):
                            P.op("pe", I("matmul", ps[pp][:, :], lhsT=wgu[wb][:, gi, k, :], rhs=xT[:, k, :], start=(k == 0), stop=(k == 7)),
                                 reads=xk + [("wgu", wb)], writes=[("ps", pp)])
                    sb_ = f % 2
                    P.op("act", I("activation", out=sg[sb_][:], in_=ps[pg][:, :], func=AF.Silu), reads=[("ps", pg)], writes=[("sg", sb_)])
                    P.op("dve", I("tensor_tensor", out=actT[:, f, :], in0=ps[pu][:, :], in1=sg[sb_][:], op=ALU.mult),
                         reads=[("ps", pu), ("sg", sb_)], writes=[("actT", f)])
                ak = [("actT", f) for f in range(NF)]
                for i in range(4):
                    for half in range(2):
                        pa = self.psn()
                        for f in range(NF):
                            P.op("pe", I("matmul", ps[pa][:, :], lhsT=actT[:, f, i * 128:(i + 1) * 128], rhs=Wd[:, f, half * 512:(half + 1) * 512],
                                         start=(f == 0), stop=(f == NF - 1)), reads=ak + Wdk, writes=[("ps", pa)])
                        P.op("dve", I("tensor_tensor", out=h[i][:, half * 512:(half + 1) * 512], in0=ps[pa][:, :],
                                      in1=h[i][:, half * 512:(half + 1) * 512], op=ALU.add), reads=[("ps", pa), ("h", i)], writes=[("h", i)])
                    rows = slice((c * 4 + i) * 128, (c * 4 + i + 1) * 128)
                    P.dma("sp", I("dma_start", out=dr["hS"][rows, :], in_=h[i][:]), reads=[("h", i)], writes=[("hS", c, i)])
            P.end_phase()

    def phase_D2(self, l, last):
        nc, P, dr, ps = self.nc, self.P, self.dr, self.ps
        with contextlib.ExitStack() as st:
            T = lambda n, sh, dt: st.enter_context(nc.sbuf_tensor(self.uname(n), sh, dt))
            Wpg = T("e_Wpg", [128, 8, D], BF16)
            Wpp = T("e_Wpp", [128, 2, D], BF16)
            stage = [T("e_stage%d" % i, [128, D], F32) for i in range(2)]
            gple = T("e_gple", [128, 8], F32)
            gfin = T("e_gfin", [128, D], F32)
            ident = T("e_ident", [128, 128], BF16)
            h = [T("e_h%d" % i, [128, D], F32) for i in range(2)]
            p32 = [T("e_p32%d" % i, [128, 256], F32) for i in range(2)]
            pbf = [T("e_pbf%d" % i, [128, 256], BF16) for i in range(2)]
            hn = [T("e_hn%d" % i, [128, D], BF16) for i in range(2)]
            T4 = [dict(junk=T("e_junk%d" % i, [128, D], BF16), ss=T("e_ss%d" % i, [128, 1], F32),
                       rs=T("e_rs%d" % i, [128, 1], F32)) for i in range(2)]
            xT = [T("e_xT%d" % i, [128, 8, 128], BF16) for i in range(2)]
            pT = [T("e_pT%d" % i, [128, 2, 128], BF16) for i in range(2)]
            sig = [T("e_sig%d" % i, [128, CH], F32) for i in range(2)]
            tmp = [T("e_tmp%d" % i, [128, CH], F32) for i in range(2)]
            outt = [T("e_out%d" % i, [128, D], F32) for i in range(2)]
            P.dma("sp", I("dma_start", out=gple[:], in_=dr["norm_ple"][l]), writes=["gple"])
            P.dma("sp", I("dma_start", out=ident[:], in_=dr["ident"]), writes=["ident"])
            if last:
                P.dma("sp", I("dma_start", out=gfin[:], in_=dr["norm_final"].to_broadcast([128, D])), writes=["gfin"])
            n = 0
            for k in range(8):
                self.load_weight_bf(Wpg[:, k, :], dr["w_ple_gate"][l, k * 128:(k + 1) * 128, :], stage[n % 2][:], ("stage", n % 2),
                                    ("Wpg", k), scale_ap=gple[:, k:k + 1], scalek="gple", eng=("dve" if n % 2 == 0 else "pool"),
                                    q=("sp" if n % 2 == 0 else "act"))
                n += 1
            for k in range(2):
                self.load_weight_bf(Wpp[:, k, :], dr["w_ple_proj"][l, k * 128:(k + 1) * 128, :], stage[n % 2][:], ("stage", n % 2),
                                    ("Wpp", k), eng=("dve" if n % 2 == 0 else "pool"), q=("sp" if n % 2 == 0 else "act"))
                n += 1
            Wpgk = [("Wpg", k) for k in range(8)]
            Wppk = [("Wpp", k) for k in range(2)]
            for ti in range(NTILE):
                b = ti % 2
                rows = slice(ti * 128, (ti + 1) * 128)
                P.dma("sp", I("dma_start", out=h[b][:], in_=dr["hS"][rows, :]), writes=[("h", b)])
                P.dma("act", I("dma_start", out=p32[b][:], in_=dr["p"][l, rows, :]), writes=[("p32", b)])
                P.op("pool", I("tensor_copy", out=pbf[b][:], in_=p32[b][:]), reads=[("p32", b)], writes=[("pbf", b)])
                self.rms_tile(T4[b], b, h[b], ("h", b), ("junk", b), ("ss", b), ("rs", b), hn[b], ("hn", b))
                self.transpose8(hn[b], ("hn", b), xT[b][:, :, :], ("xT", b), ident, eng="dve")
                self.transpose8(pbf[b], ("pbf", b), pT[b][:, :, :], ("pT", b), ident, nblk=2, eng="act")
                for half in range(2):
                    hs = slice(half * 512, (half + 1) * 512)
                    pg, pp = self.psn(), self.psn()
                    for k in range(8):
                        P.op("pe", I("matmul", ps[pg][:, :], lhsT=xT[b][:, k, :], rhs=Wpg[:, k, hs], start=(k == 0), stop=(k == 7)),
                             reads=[("xT", b)] + Wpgk, writes=[("ps", pg)])
                    for k in range(2):
                        P.op("pe", I("matmul", ps[pp][:, :], lhsT=pT[b][:, k, :], rhs=Wpp[:, k, hs], start=(k == 0), stop=(k == 1)),
                             reads=[("pT", b)] + Wppk, writes=[("ps", pp)])
                    P.op("act", I("activation", out=sig[half][:], in_=ps[pg][:, :], func=AF.Sigmoid), reads=[("ps", pg)], writes=[("sig", half)])
                    P.op("dve", I("tensor_tensor", out=tmp[half][:], in0=ps[pp][:, :], in1=sig[half][:], op=ALU.mult),
                         reads=[("ps", pp), ("sig", half)], writes=[("tmp", half)])
                    P.op("pool", I("tensor_tensor", out=h[b][:, hs], in0=h[b][:, hs], in1=tmp[half][:], op=ALU.add),
                         reads=[("tmp", half), ("h", b)], writes=[("h", b)])
                if not last:
                    P.dma("sp", I("dma_start", out=dr["hS"][rows, :], in_=h[b][:]), reads=[("h", b)], writes=[("hS", ti)])
                else:
                    self.rms_tile(T4[b], b, h[b], ("h", b), ("junk", b), ("ss", b), ("rs", b), None, None) if False else None
                    junk, ss, rs = T4[b]["junk"], T4[b]["ss"], T4[b]["rs"]
                    P.op("act", I("activation", out=junk[:], in_=h[b][:], func=AF.Square, accum_out=ss[:]), reads=[("h", b)], writes=[("junk", b), ("ss", b)])
                    P.op("dve", I("tensor_scalar", out=rs[:], in0=ss[:], scalar1=1.0 / D, scalar2=1e-6, op0=ALU.mult, op1=ALU.add),
                         reads=[("ss", b)], writes=[("rs", b)])
                    P.op("act", I("sqrt", out=rs[:], in_=rs[:]), reads=[("rs", b)], writes=[("rs", b)])
                    P.op("dve", I("reciprocal", out=rs[:], in_=rs[:]), reads=[("rs", b)], writes=[("rs", b)])
                    P.op("dve", I("scalar_tensor_tensor", out=outt[b][:], in0=h[b][:], scalar=rs[:, 0:1], in1=gfin[:], op0=ALU.mult, op1=ALU.mult),
                         reads=[("h", b), ("rs", b), "gfin"], writes=[("outt", b)])
                    P.dma("sp", I("dma_start", out=self.out[rows, :], in_=outt[b][:]), reads=[("outt", b)], writes=[("out", ti)])
            P.end_phase()


def make_in_maps(inputs, cores):
    inp = {k: np.asarray(v) for k, v in inputs.items()}
    sh = _prep_shared(inp)
    maps = []
    for b in cores:
        m = dict(sh)
        m["x"] = np.ascontiguousarray(inp["x"][b])
        m["p"] = np.ascontiguousarray(inp["p"][:, b])
        m["pos"] = np.ascontiguousarray(inp["positions"][b].reshape(1, S).astype(np.int32))
        maps.append(m)
    return maps


_NC_CACHE = {}


def kernel(**inputs):
    if "nc" not in _NC_CACHE:
        _NC_CACHE["nc"] = Builder().build()
    nc = _NC_CACHE["nc"]
    maps = make_in_maps(inputs, list(range(8)))
    res = run_bass_kernel_spmd(nc, maps, core_ids=list(range(8)))
    out = np.stack([np.asarray(r["out"]) for r in res.results], axis=0)
    return out.astype(np.float32)
```

```python
import contextlib
import numpy as np
import ml_dtypes
import concourse.bass as bass
import concourse.mybir as mybir
from concourse.bass_utils import run_bass_kernel_spmd

F32 = mybir.dt.float32
BF16 = mybir.dt.bfloat16
I32 = mybir.dt.int32
AF = mybir.ActivationFunctionType
ALU = mybir.AluOpType
AX = mybir.AxisListType

S = 4096
D = 1024
L = 2
NTILE = 32
CH = 512
NCH = 8
DFF = 2816
NF = 22
BIG = 30000.0
WCOLS = 3740
COMPUTE = ("pe", "act", "dve", "pool", "sp")
import os as _os
NSA_STOP = int(_os.environ.get("NSA_STOP", "0"))
DMAQ = ("sp", "pool", "act")


def I(m, *a, **k):
    return lambda e: getattr(e, m)(*a, **k)


class Op:
    __slots__ = ("eng", "fn", "waits", "signal", "cnt", "dma_sem", "dma_val", "is_dma", "idx")

    def __init__(self, eng, fn, is_dma):
        self.eng = eng
        self.fn = fn
        self.waits = []
        self.signal = False
        self.cnt = None
        self.dma_sem = None
        self.dma_val = None
        self.is_dma = is_dma
        self.idx = None


class Prog:
    def __init__(self, nc, st, n_dma_sems=8):
        self.nc = nc
        self.lists = {e: [] for e in COMPUTE}
        self.last_w = {}
        self.readers = {}
        self.n_dma_sems = n_dma_sems
        self.dma_count = {q: 0 for q in DMAQ}
        self.csem = {e: st.enter_context(nc.semaphore("c_" + e)) for e in COMPUTE}
        self.dsem = {(q, j): st.enter_context(nc.semaphore("d_%s%d" % (q, j)))
                     for q in DMAQ for j in range(n_dma_sems)}
        self.cbase = {e: 0 for e in COMPUTE}
        self.gidx = {e: 0 for e in COMPUTE}
        self.barrier = {}
        self.dma_last = {}

    def _deps(self, reads, writes):
        deps = []
        for k in reads:
            w = self.last_w.get(k)
            if w is not None:
                deps.append(w)
        for k in writes:
            w = self.last_w.get(k)
            if w is not None:
                deps.append(w)
            deps.extend(self.readers.get(k, ()))
        return deps

    def _record(self, h, reads, writes):
        for k in reads:
            self.readers.setdefault(k, []).append(h)
        for k in writes:
            self.last_w[k] = h
            self.readers[k] = []

    def _attach(self, h, deps):
        best = {}
        for d in deps:
            if d is h or d.fn is None:
                continue
            if d.is_dma:
                key = ("d",) + d.dma_sem
                cur = best.get(key)
                if cur is None or d.dma_val > cur.dma_val:
                    best[key] = d
            else:
                if d.eng == "pe" and h.eng == "pe" and not h.is_dma:
                    continue
                cur = best.get(d.eng)
                if cur is None or d.idx > cur.idx:
                    best[d.eng] = d
        for d in best.values():
            d.signal = True
            h.waits.append(d)

    def op(self, eng, fn, reads=(), writes=(), extra=()):
        h = Op(eng, fn, False)
        self._attach(h, self._deps(reads, writes) + list(extra))
        h.idx = self.gidx[eng]
        self.gidx[eng] += 1
        self.lists[eng].append(h)
        self._record(h, reads, writes)
        return h

    def dma(self, q, fn, reads=(), writes=(), extra=()):
        h = Op(q, fn, True)
        self._attach(h, self._deps(reads, writes) + list(extra))
        i = self.dma_count[q]
        self.dma_count[q] += 1
        h.dma_sem = (q, i % self.n_dma_sems)
        h.dma_val = 16 * (i // self.n_dma_sems + 1)
        h.idx = self.gidx[q]
        self.gidx[q] += 1
        self.lists[q].append(h)
        self._record(h, reads, writes)
        self.dma_last[h.dma_sem] = h.dma_val
        return h

    def flush(self, final=False):
        nc = self.nc
        for e in COMPUTE:
            c = self.cbase[e]
            for h in self.lists[e]:
                if not h.is_dma and h.signal:
                    c += 1
                    h.cnt = c
        if final:
            pass
        barrier = dict(self.barrier)
        with nc.Block() as block:
            engs = {"pe": block.tensor, "act": block.scalar, "dve": block.vector,
                    "pool": block.gpsimd, "sp": block.sync}

            def make(ename):
                lst = self.lists[ename]

                def body(eng):
                    waited = {}
                    for key, val in barrier.items():
                        if val <= 0:
                            continue
                        sem = self.csem[key[1]] if key[0] == "c" else self.dsem[key[1:]]
                        eng.wait_ge(sem, val)
                        waited[key] = val
                    for h in lst:
                        for d in h.waits:
                            if d.is_dma:
                                key = ("d",) + d.dma_sem
                                sem = self.dsem[d.dma_sem]
                                val = d.dma_val
                            else:
                                key = ("c", d.eng)
                                sem = self.csem[d.eng]
                                val = d.cnt
                            if waited.get(key, 0) >= val:
                                continue
                            waited[key] = val
                            eng.wait_ge(sem, val)
                        if h.is_dma:
                            prev = h.dma_val - 16
                            key = ("d",) + h.dma_sem
                            if prev > 0 and waited.get(key, 0) < prev:
                                eng.wait_ge(self.dsem[h.dma_sem], prev)
                                waited[key] = prev
                            ins = h.fn(eng)
                            ins.then_inc(self.dsem[h.dma_sem], 16)
                        else:
                            ins = h.fn(eng)
                            if h.signal:
                                ins.then_inc(self.csem[ename], 1)
                    if final and ename == "sp":
                        for key, val in self._barrier_now().items():
                            if val > 0 and waited.get(key, 0) < val and key != ("c", "sp"):
                                sem = self.csem[key[1]] if key[0] == "c" else self.dsem[key[1:]]
                                eng.wait_ge(sem, val)
                return body

            for ename in ("sp", "pool", "act", "dve", "pe"):
                if self.lists[ename] or barrier or final:
                    engs[ename](make(ename))
        self.barrier = self._barrier_now()
        for e in COMPUTE:
            for h in self.lists[e]:
                h.fn = None
            self.lists[e] = []
        self.last_w = {}
        self.readers = {}

    def _barrier_now(self):
        b = {}
        for e in COMPUTE:
            c = self.cbase[e]
            for h in self.lists[e]:
                if h.cnt is not None and h.cnt > c:
                    c = h.cnt
            b[("c", e)] = c
        for k, v in self.dma_last.items():
            b[("d",) + k] = v
        return b

    def end_phase(self, final=False):
        for e in COMPUTE:
            for h in reversed(self.lists[e]):
                if not h.is_dma:
                    h.signal = True
                    break
        self.flush(final=final)
        for e in COMPUTE:
            self.cbase[e] = self.barrier[("c", e)]


def _win_cols():
    o = {}
    names = ["nq", "nkc", "nvc", "nks", "nvs", "nkw", "nvw", "ngate", "fq", "fk", "fv", "ff", "sq", "sk", "sv"]
    sizes = [512, 128, 128, 128, 128, 128, 128, 24, 256, 256, 256, 4, 256, 256, 256]
    off = 0
    for n, s in zip(names, sizes):
        o[n] = np.arange(off, off + s)
        off += s
    assert off == 2844

    def rot(c):
        c = c.reshape(-1, 2, 32)
        return c[:, ::-1, :].reshape(-1)

    ft = []
    for j in range(4):
        ft.append(np.concatenate([o["nq"][64 * j:64 * j + 64], o["nq"][64 * (4 + j):64 * (4 + j) + 64]]))
    ft += [o["nkc"], o["nks"], o["nkw"]]
    ft += [rot(c) for c in ft[:7]]
    ft.append(o["nvc"])
    for n in ("fq", "fk", "sq", "sk"):
        ft += [o[n][:128], o[n][128:]]
    cols = np.concatenate(ft + [o["ff"], o["nvs"], o["nvw"], o["fv"], o["sv"], o["ngate"]])
    assert cols.shape[0] == WCOLS
    return cols


def _consts():
    bf = ml_dtypes.bfloat16
    c = {}
    c["ident"] = np.eye(128, dtype=np.float32).astype(bf)
    s = np.arange(128)[:, None, None]
    r = np.arange(4)[None, :, None]
    t = np.arange(512)[None, None, :]
    sa = 128 * r + s
    c["m_gt"] = np.where(sa > t, -BIG, 0.0).astype(bf)
    c["m_ge"] = np.where(sa >= t, -BIG, 0.0).astype(bf)
    c["m_le"] = np.where(sa <= t, -BIG, 0.0).astype(bf)
    n = np.arange(128)[:, None, None] + 128 * np.arange(2)[None, :, None]
    tt = np.arange(S)[None, None, :]
    cm = np.where((16 * n + 31 > tt) | (n >= 255), -BIG, 0.0)
    c["m_cmp"] = cm.astype(bf)
    j = np.arange(64)[:, None, None]
    sb = np.arange(32)[None, :, None]
    ss = np.arange(128)[None, None, :]
    c["eblk"] = (np.arange(64)[:, None] == (np.arange(S)[None, :] // 64)).astype(np.float32).astype(bf)
    nn = np.arange(256)
    cs = nn[:, None] * 16
    bs = np.arange(64)[None, :] * 64
    ov = ((cs < bs + 64) & (cs + 32 > bs) & (nn[:, None] < 255)).astype(np.float32)
    c["ovl"] = ov.reshape(2, 128, 64).transpose(1, 0, 2).astype(bf).copy()
    tq = np.arange(S)[:, None]
    jb = np.arange(64)[None, :]
    cur = tq // 64
    forced = (jb == 0) | (jb == cur) | (jb == cur - 1)
    valid = jb <= cur
    c["selbias"] = np.where(forced, 1e9, np.where(valid, 0.0, -1e9)).astype(np.float32)
    half = 32
    invf = (10000.0 ** (-np.arange(half, dtype=np.float32) / half)).astype(np.float32)
    rr = np.arange(128)
    c["invf"] = invf[rr % 32].reshape(128, 1).astype(np.float32)
    c["sgn"] = np.where((rr % 64) < 32, -1.0, 1.0).reshape(128, 1).astype(np.float32)
    jj = np.arange(128)
    c["ntri"] = np.where(jj[:, None] >= jj[None, :], -1.0, 0.0).astype(np.float32).astype(bf)
    c["nones"] = np.full((1, 128), -1.0, np.float32).astype(bf)
    c["onec"] = np.ones((128, 1), np.float32).astype(bf)
    return c


def _col8(v):
    return np.ascontiguousarray(v.reshape(8, 128).T)


def _prep_shared(inp):
    sh = {}
    cols = _win_cols()
    sh["w_in"] = np.ascontiguousarray(inp["w_in"][:, :, cols])
    for n in ("norm_mix", "norm_ffn", "norm_ple", "head_norm"):
        sh[n] = np.stack([_col8(inp[n][l]) for l in range(L)])
    sh["norm_final"] = inp["norm_final"].reshape(1, D)
    sh["b_gate"] = inp["b_nsa_gate"].reshape(L, 1, 24)
    sh["b_forget"] = inp["b_forget"].reshape(L, 4, 1)
    for kv in ("k", "v"):
        w1 = inp["nsa_cmp_w1_" + kv].reshape(L, 32, 64, 128).transpose(0, 2, 1, 3)
        sh["w1_" + kv] = np.ascontiguousarray(np.concatenate([w1, w1], axis=1).reshape(L, 128, 32 * 128))
        pt = inp["nsa_cmp_pos_" + kv].transpose(0, 2, 1)
        sh["pos_" + kv] = np.ascontiguousarray(np.concatenate([pt, pt], axis=1))
        sh["w2_" + kv] = inp["nsa_cmp_w2_" + kv]
    for n in ("w_out", "w_ffn_gate", "w_ffn_up", "w_ffn_down", "w_ple_proj", "w_ple_gate"):
        sh[n] = inp[n]
    sh.update(_consts())
    return sh


class Builder:
    def __init__(self, debug=(), nlayers=L, phases=None):
        self.debug = set(debug)
        self.nlayers = nlayers
        self.phases = phases
        self.nc = bass.Bass("TRN2", target_bir_lowering=False)
        self.dr = {}

    def din(self, name, shape, dt):
        self.dr[name] = self.nc.dram_tensor(name, list(shape), dt, kind="ExternalInput").ap()
        return self.dr[name]

    def dscr(self, name, shape, dt):
        kind = "ExternalOutput" if name in self.debug else "Internal"
        self.dr[name] = self.nc.dram_tensor(name, list(shape), dt, kind=kind).ap()
        return self.dr[name]

    def want(self, ph):
        return self.phases is None or ph in self.phases

    def build(self):
        nc = self.nc
        din, dscr = self.din, self.dscr
        din("x", [S, D], F32)
        din("p", [L, S, 256], F32)
        din("pos", [1, S], I32)
        din("w_in", [L, D, WCOLS], F32)
        for n in ("norm_mix", "norm_ffn", "norm_ple", "head_norm"):
            din(n, [L, 128, 8], F32)
        din("norm_final", [1, D], F32)
        din("b_gate", [L, 1, 24], F32)
        din("b_forget", [L, 4, 1], F32)
        for kv in ("k", "v"):
            din("w1_" + kv, [L, 128, 4096], F32)
            din("pos_" + kv, [L, 128, 32], F32)
            din("w2_" + kv, [L, 128, 64], F32)
        din("w_out", [L, D, D], F32)
        din("w_ffn_gate", [L, D, DFF], F32)
        din("w_ffn_up", [L, D, DFF], F32)
        din("w_ffn_down", [L, DFF, D], F32)
        din("w_ple_proj", [L, 256, D], F32)
        din("w_ple_gate", [L, D, D], F32)
        din("ident", [128, 128], BF16)
        for n in ("m_gt", "m_ge", "m_le"):
            din(n, [128, 4, 512], BF16)
        din("m_cmp", [128, 2, S], BF16)
        din("eblk", [64, S], BF16)
        din("ovl", [128, 2, 64], BF16)
        din("selbias", [S, 64], F32)
        din("invf", [128, 1], F32)
        din("sgn", [128, 1], F32)
        din("ntri", [128, 128], BF16)
        din("nones", [1, 128], BF16)
        din("onec", [128, 1], BF16)
        self.out = nc.dram_tensor("out", [S, D], F32, kind="ExternalOutput").ap()
        dscr("hS", [S, D], F32)
        dscr("cosS", [128, S], F32)
        dscr("sinS", [128, S], F32)
        dscr("FT", [16, 128, S], BF16)
        dscr("cT", [4, S], F32)
        dscr("VA", [S, 8, 65], BF16)
        dscr("SV", [S, 256], BF16)
        dscr("GT", [S, 24], F32)
        dscr("kcT", [128, 256], BF16)
        dscr("VC", [128, 2, 2, 65], BF16)
        dscr("oS", [S, D], F32)
        dscr("WGU", [NF, 128, 2, 8, 128], BF16)
        if "hmix" in self.debug:
            dscr("hmix", [S, D], F32)

        with contextlib.ExitStack() as st:
            self.P = Prog(nc, st)
            self.ps = [st.enter_context(nc.psum_tensor("ps%d" % i, [128, 512], F32)) for i in range(8)]
            self.pt = self.ps[7][:, :].bitcast(BF16)
            self.ps_i = 0
            if self.want("T"):
                self.phase_tables()
            for l in range(self.nlayers):
                if self.want("A"):
                    self.phase_A(l)
                if self.want("B"):
                    self.phase_B(l)
                if self.want("N"):
                    self.phase_nsa(l)
                if self.want("F"):
                    self.phase_fox(l)
                if self.want("SB"):
                    self.phase_sb(l)
                if self.want("D"):
                    self.phase_D(l, last=(l == self.nlayers - 1))
            self.P.op("sp", I("nop"))
            self.P.end_phase(final=True)
        return nc

    def defer(self, fn, *a):
        if not hasattr(self, "_pend"):
            self._pend = []
        self._pend.append((fn, a))

    def run_deferred(self):
        pend = getattr(self, "_pend", [])
        self._pend = []
        for fn, a in pend:
            fn(*a)

    def uname(self, n):
        self._un = getattr(self, "_un", 0) + 1
        return "%s_u%d" % (n, self._un)

    def psn(self):
        i = self.ps_i
        self.ps_i = (i + 1) % 7
        return i

    def phase_tables(self):
        nc, P, dr = self.nc, self.P, self.dr
        with contextlib.ExitStack() as st:
            T = lambda n, sh, dt: st.enter_context(nc.sbuf_tensor(self.uname(n), sh, dt))
            posi = T("t_posi", [128, S], I32)
            ang = T("t_ang", [128, S], F32)
            kk = T("t_kk", [128, S], F32)
            rr = T("t_r", [128, S], F32)
            oo = T("t_o", [128, S], F32)
            invf = T("t_invf", [128, 1], F32)
            sgn = T("t_sgn", [128, 1], F32)
            hpi = T("t_hpi", [128, 1], F32)
            P.dma("sp", I("dma_start", out=posi[:], in_=dr["pos"].to_broadcast([128, S])), writes=["posi"])
            P.dma("sp", I("dma_start", out=invf[:], in_=dr["invf"]), writes=["invf"])
            P.dma("sp", I("dma_start", out=sgn[:], in_=dr["sgn"]), writes=["sgn"])
            P.op("pool", I("memset", hpi[:], float(np.pi / 2)), writes=["hpi"])
            P.op("dve", I("tensor_copy", out=ang[:], in_=posi[:]), reads=["posi"], writes=["ang"])
            P.op("dve", I("tensor_scalar", out=ang[:], in0=ang[:], scalar1=invf[:, 0:1], scalar2=None, op0=ALU.mult),
                 reads=["ang", "invf"], writes=["ang"])
            MAGIC = 12582912.0
            P.op("dve", I("tensor_scalar", out=kk[:], in0=ang[:], scalar1=float(1.0 / (2 * np.pi)), scalar2=MAGIC,
                                                   op0=ALU.mult, op1=ALU.add), reads=["ang"], writes=["kk"])
            P.op("dve", I("tensor_scalar", out=kk[:], in0=kk[:], scalar1=-MAGIC, scalar2=None, op0=ALU.add),
                 reads=["kk"], writes=["kk"])
            C1 = 6.28125
            C2 = float(np.float32(2 * np.pi - C1))
            P.op("dve", I("scalar_tensor_tensor", out=rr[:], in0=kk[:], scalar=-C1, in1=ang[:], op0=ALU.mult, op1=ALU.add),
                 reads=["kk", "ang"], writes=["rr"])
            P.op("dve", I("scalar_tensor_tensor", out=rr[:], in0=kk[:], scalar=-C2, in1=rr[:], op0=ALU.mult, op1=ALU.add),
                 reads=["kk", "rr"], writes=["rr"])
            PL = 3.1415925
            P.op("dve", I("tensor_scalar", out=rr[:], in0=rr[:], scalar1=-PL, scalar2=PL, op0=ALU.max, op1=ALU.min),
                 reads=["rr"], writes=["rr"])
            P.op("act", I("activation", out=oo[:], in_=rr[:], func=AF.Sin), reads=["rr"], writes=["oo"])
            P.op("dve", I("tensor_scalar", out=oo[:], in0=oo[:], scalar1=sgn[:, 0:1], scalar2=None, op0=ALU.mult),
                 reads=["oo", "sgn"], writes=["oo"])
            P.dma("sp", I("dma_start", out=dr["sinS"], in_=oo[:]), reads=["oo"], writes=["sinS"])
            P.op("dve", I("scalar_tensor_tensor", out=kk[:], in0=rr[:], scalar=-1.0, in1=rr[:], op0=ALU.mult, op1=ALU.max),
                 reads=["rr"], writes=["kk"])
            P.op("act", I("activation", out=ang[:], in_=kk[:], func=AF.Sin, bias=hpi[:, 0:1], scale=-1.0),
                 reads=["kk", "hpi", "ang"], writes=["ang"])
            P.dma("sp", I("dma_start", out=dr["cosS"], in_=ang[:]), reads=["ang"], writes=["cosS"])
            P.end_phase()

    def rms_tile(self, T4, i, hx, hxk, jk, ssk, rsk, hn, hnk):
        P = self.P
        junk, ss, rs = T4["junk"], T4["ss"], T4["rs"]
        P.op("act", I("activation", out=junk[:], in_=hx[:], func=AF.Square, accum_out=ss[:]),
             reads=[hxk], writes=[jk, ssk])
        P.op("dve", I("tensor_scalar", out=rs[:], in0=ss[:], scalar1=1.0 / D, scalar2=1e-6, op0=ALU.mult, op1=ALU.add),
             reads=[ssk], writes=[rsk])
        P.op("act", I("sqrt", out=rs[:], in_=rs[:]), reads=[rsk], writes=[rsk])
        P.op("dve", I("reciprocal", out=rs[:], in_=rs[:]), reads=[rsk], writes=[rsk])
        P.op("dve", I("tensor_scalar", out=hn[:], in0=hx[:], scalar1=rs[:, 0:1], scalar2=None, op0=ALU.mult),
             reads=[hxk, rsk], writes=[hnk])

    def transpose8(self, src, srck, dst_ap, dstk, ident, nblk=8, eng="dve"):
        P = self.P
        pt = self.pt
        for c in range(nblk):
            P.op("pe", I("transpose", out=pt[:, c * 128:(c + 1) * 128], in_=src[:, c * 128:(c + 1) * 128],
                                                  identity=ident[:]), reads=[srck, "ident"], writes=[("ps", 7)])
        view = pt[:, 0:nblk * 128].rearrange("p (c t) -> p c t", t=128)
        if eng == "dve":
            P.op("dve", I("tensor_copy", out=dst_ap, in_=view), reads=[("ps", 7)], writes=[dstk])
        else:
            P.op("act", I("copy", out=dst_ap, in_=view), reads=[("ps", 7)], writes=[dstk])

    def load_weight_bf(self, dst_ap, src_ap, stage, stagek, dstk, scale_ap=None, scalek=None, eng="dve", q="sp"):
        P = self.P
        P.dma(q, I("dma_start", out=stage, in_=src_ap), writes=[stagek])
        en = "dve" if eng == "dve" else "pool"
        if scale_ap is not None:
            P.op(en, I("tensor_scalar", out=dst_ap, in0=stage, scalar1=scale_ap, scalar2=None, op0=ALU.mult),
                 reads=[stagek, scalek], writes=[dstk])
        else:
            P.op(en, I("tensor_copy", out=dst_ap, in_=stage), reads=[stagek], writes=[dstk])

    def phase_A(self, l):
        nc, P, dr = self.nc, self.P, self.dr
        hsrc = dr["x"] if l == 0 else dr["hS"]
        with contextlib.ExitStack() as st:
            T = lambda n, sh, dt: st.enter_context(nc.sbuf_tensor(self.uname(n), sh, dt))
            W = T("a_W", [128, 8, WCOLS], BF16)
            stage = [T("a_stage%d" % i, [128, WCOLS], F32) for i in range(2)]
            cosT = T("a_cos", [128, S], F32)
            sinT = T("a_sin", [128, S], F32)
            gcol = T("a_gcol", [128, 8], F32)
            ident = T("a_ident", [128, 128], BF16)
            bgate = T("a_bgate", [128, 24], F32)
            negb = T("a_negb", [4, 1], F32)
            ones4 = T("a_ones4", [4, CH], F32)
            cc = T("a_cc", [4, S], F32)
            hx = [T("a_hx%d" % i, [128, D], F32) for i in range(2)]
            T4 = [dict(junk=T("a_junk%d" % i, [128, D], BF16), ss=T("a_ss%d" % i, [128, 1], F32),
                       rs=T("a_rs%d" % i, [128, 1], F32)) for i in range(2)]
            hn = [T("a_hn%d" % i, [128, D], BF16) for i in range(2)]
            hnT = [T("a_hnT%d" % i, [128, 8, CH], BF16) for i in range(2)]
            t1 = [T("a_t1%d" % i, [128, CH], F32) for i in range(2)]
            t2 = [T("a_t2%d" % i, [128, CH], F32) for i in range(2)]
            ob = [T("a_ob%d" % i, [128, CH], BF16) for i in range(4)]
            va = [T("a_va%d" % i, [128, 8, 65], BF16) for i in range(2)]
            svt = [T("a_sv%d" % i, [128, 256], BF16) for i in range(2)]
            gt = [T("a_gt%d" % i, [128, 24], F32) for i in range(2)]
            e4 = T("a_e4", [4, CH], F32)
            sp4 = T("a_sp4", [4, CH], F32)

            P.dma("sp", I("dma_start", out=gcol[:], in_=dr["norm_mix"][l]), writes=["gcol"])
            P.dma("sp", I("dma_start", out=ident[:], in_=dr["ident"]), writes=["ident"])
            P.dma("sp", I("dma_start", out=cosT[:], in_=dr["cosS"]), writes=["cosT"])
            P.dma("sp", I("dma_start", out=sinT[:], in_=dr["sinS"]), writes=["sinT"])
            P.dma("sp", I("dma_start", out=bgate[:], in_=dr["b_gate"][l].to_broadcast([128, 24])), writes=["bgate"])
            P.dma("sp", I("dma_start", out=negb[:], in_=dr["b_forget"][l]), writes=["negb"])
            P.op("dve", I("tensor_scalar", out=negb[:], in0=negb[:], scalar1=-1.0, scalar2=None, op0=ALU.mult),
                 reads=["negb"], writes=["negb"])
            P.op("pool", I("memset", ones4[:], 1.0), writes=["ones4"])
            for i in range(2):
                P.op("pool", I("memset", va[i][:], 1.0), writes=[("va", i)])
            for k in range(8):
                self.load_weight_bf(W[:, k, :], dr["w_in"][l, k * 128:(k + 1) * 128, :], stage[k % 2][:], ("stage", k % 2),
                                    ("W", k), scale_ap=gcol[:, k:k + 1], scalek="gcol", eng=("dve" if k % 2 == 0 else "pool"),
                                    q=("sp" if k % 2 == 0 else "act"))
            Wk = [("W", k) for k in range(8)]
            cn_a = {"obi": 0}
            for c in range(NCH):
                hb = c % 2
                csl = slice(c * CH, (c + 1) * CH)
                for i in range(4):
                    ti = c * 4 + i
                    b = ti % 2
                    P.dma("sp", I("dma_start", out=hx[b][:], in_=hsrc[ti * 128:(ti + 1) * 128, :]),
                          writes=[("hx", b)])
                    self.rms_tile(T4[b], b, hx[b], ("hx", b), ("junk", b), ("ss", b), ("rs", b), hn[b], ("hn", b))
                    self.transpose8(hn[b], ("hn", b), hnT[hb][:, :, i * 128:(i + 1) * 128], ("hnT", hb, i), ident,
                                    eng=("dve" if i % 2 == 0 else "act"))
                hk = [("hnT", hb, i) for i in range(4)]

                def back(c, hb, csl, hk):

                    def fm_matmul(pi, col0, ncols=128):
                        for k in range(8):
                            P.op("pe", I("matmul", self.ps[pi][0:ncols, :], lhsT=W[:, k, col0:col0 + ncols],
                                                              rhs=hnT[hb][:, k, :], start=(k == 0), stop=(k == 7)),
                                 reads=hk + Wk, writes=[("ps", pi)])
                    for ft in range(7):
                        pa, pb = self.psn(), self.psn()
                        fm_matmul(pa, ft * 128)
                        fm_matmul(pb, (7 + ft) * 128)
                        tb = ft % 2
                        P.op("dve", I("tensor_tensor", out=t1[tb][:], in0=self.ps[pa][:], in1=cosT[:, csl], op=ALU.mult),
                             reads=[("ps", pa), "cosT"], writes=[("t1", tb)])
                        P.op("dve", I("tensor_tensor", out=t2[tb][:], in0=self.ps[pb][:], in1=sinT[:, csl], op=ALU.mult),
                             reads=[("ps", pb), "sinT"], writes=[("t2", tb)])
                        o = cn_a["obi"] % 4
                        cn_a["obi"] += 1
                        P.op("pool", I("tensor_tensor", out=ob[o][:], in0=t1[tb][:], in1=t2[tb][:], op=ALU.add),
                             reads=[("t1", tb), ("t2", tb)], writes=[("ob", o)])
                        P.dma("sp", I("dma_start", out=dr["FT"][ft, :, csl], in_=ob[o][:]),
                              reads=[("ob", o)], writes=[("FT", ft, c)])
                    for ft in range(14, 23):
                        pa = self.psn()
                        fm_matmul(pa, ft * 128)
                        o = cn_a["obi"] % 4
                        cn_a["obi"] += 1
                        sc = 0.125 if ft in (15, 16, 19, 20) else 1.0
                        P.op("act", I("activation", out=ob[o][:], in_=self.ps[pa][:], func=AF.Copy, scale=sc),
                             reads=[("ps", pa)], writes=[("ob", o)])
                        P.dma("sp", I("dma_start", out=dr["FT"][ft - 7, :, csl], in_=ob[o][:]),
                              reads=[("ob", o)], writes=[("FT", ft, c)])
                    pa = self.psn()
                    fm_matmul(pa, 23 * 128, ncols=4)
                    P.op("act", I("activation", out=e4[:], in_=self.ps[pa][0:4, :], func=AF.Exp, bias=negb[:, 0:1], scale=-1.0),
                         reads=[("ps", pa), "negb"], writes=["e4"])
                    P.op("act", I("activation", out=sp4[:], in_=e4[:], func=AF.Ln, bias=1.0, scale=1.0), reads=["e4"], writes=["sp4"])
                    if c == 0:
                        P.op("dve", I("tensor_tensor_scan", out=cc[:, csl], data0=ones4[:], data1=sp4[:], initial=0.0,
                                                                    op0=ALU.mult, op1=ALU.subtract), reads=["sp4", "ones4"], writes=["cc"])
                    else:
                        P.op("dve", I("tensor_tensor_scan", out=cc[:, csl], data0=ones4[:], data1=sp4[:],
                                                                         initial=cc[:, c * CH - 1:c * CH],
                                                                         op0=ALU.mult, op1=ALU.subtract), reads=["sp4", "ones4", "cc"], writes=["cc"])
                    c0 = 23 * 128 + 4
                    for i in range(4):
                        ti = c * 4 + i
                        b = ti % 2
                        pa, pb = self.psn(), self.psn()
                        for k in range(8):
                            P.op("pe", I("matmul", self.ps[pa][:, 0:512], lhsT=hnT[hb][:, k, i * 128:(i + 1) * 128],
                                                                        rhs=W[:, k, c0:c0 + 512], start=(k == 0), stop=(k == 7)),
                                 reads=hk + Wk, writes=[("ps", pa)])
                        for k in range(8):
                            P.op("pe", I("matmul", self.ps[pb][:, 0:280], lhsT=hnT[hb][:, k, i * 128:(i + 1) * 128],
                                                                        rhs=W[:, k, c0 + 512:c0 + 792], start=(k == 0), stop=(k == 7)),
                                 reads=hk + Wk, writes=[("ps", pb)])
                        P.op("act", I("copy", out=va[b][:, :, 0:64], in_=self.ps[pa][:, 0:512].rearrange("p (g d) -> p g d", d=64)),
                             reads=[("ps", pa)], writes=[("va", b)])
                        P.op("dve", I("tensor_copy", out=svt[b][:], in_=self.ps[pb][:, 0:256]),
                             reads=[("ps", pb)], writes=[("svt", b)])
                        P.op("dve", I("tensor_tensor", out=gt[b][:], in0=self.ps[pb][:, 256:280], in1=bgate[:], op=ALU.add),
                             reads=[("ps", pb), "bgate"], writes=[("gt", b)])
                        P.op("act", I("activation", out=gt[b][:], in_=gt[b][:], func=AF.Sigmoid), reads=[("gt", b)], writes=[("gt", b)])
                        rows = slice(ti * 128, (ti + 1) * 128)
                        P.dma("sp", I("dma_start", out=dr["VA"][rows], in_=va[b][:]), reads=[("va", b)], writes=[("VA", ti)])
                        P.dma("sp", I("dma_start", out=dr["SV"][rows], in_=svt[b][:]), reads=[("svt", b)], writes=[("SV", ti)])
                        P.dma("sp", I("dma_start", out=dr["GT"][rows], in_=gt[b][:]), reads=[("gt", b)], writes=[("GT", ti)])

                self.run_deferred()
                self.defer(back, c, hb, csl, hk)
            self.run_deferred()
            P.dma("sp", I("dma_start", out=dr["cT"], in_=cc[:]), reads=["cc"], writes=["cT"])
            P.end_phase()

    def phase_B(self, l):
        nc, P, dr = self.nc, self.P, self.dr
        with contextlib.ExitStack() as st:
            T = lambda n, sh, dt: st.enter_context(nc.sbuf_tensor(self.uname(n), sh, dt))
            kvT = {"k": T("b_kT", [128, S], BF16), "v": T("b_vT", [128, S], BF16)}
            stage = T("b_stage", [128, 4096], F32)
            W1 = {kv: T("b_w1" + kv, [128, 32, 128], BF16) for kv in "kv"}
            posT = {kv: T("b_pos" + kv, [128, 32], BF16) for kv in "kv"}
            W2 = {kv: T("b_w2" + kv, [128, 64], BF16) for kv in "kv"}
            st32 = T("b_st32", [128, 32], F32)
            st64 = T("b_st64", [128, 64], F32)
            bias = T("b_bias", [128, 1], F32)
            xs = T("b_xs", [128, 255], F32)
            x2 = T("b_x2", [128, 255], F32)
            sg = T("b_sg", [128, 255], F32)
            gl = T("b_gl", [128, 256], BF16)
            kc = T("b_kc", [128, 256], BF16)
            vc = T("b_vc", [128, 2, 2, 65], BF16)
            P.dma("sp", I("dma_start", out=kvT["k"][:], in_=dr["FT"][4]), writes=["kT"])
            P.dma("sp", I("dma_start", out=kvT["v"][:], in_=dr["FT"][7]), writes=["vT"])
            P.op("pool", I("memset", vc[:], 1.0), writes=["vc"])
            P.op("pool", I("memset", gl[:], 0.0), writes=["gl"])
            for kv in "kv":
                self.load_weight_bf(W1[kv][:].rearrange("p l h -> p (l h)"), dr["w1_" + kv][l], stage[:], "stage", "W1" + kv)
                self.load_weight_bf(posT[kv][:], dr["pos_" + kv][l], st32[:], "st32", "pos" + kv)
                self.load_weight_bf(W2[kv][:], dr["w2_" + kv][l], st64[:], "st64", "W2" + kv)
            for kv in "kv":
                pb = self.psn()
                for ll in range(32):
                    P.op("pe", I("matmul", self.ps[pb][:, 0:1], lhsT=W1[kv][0:64, ll, :], rhs=posT[kv][0:64, ll:ll + 1],
                                                                start=(ll == 0), stop=(ll == 31)),
                         reads=["W1" + kv, "pos" + kv], writes=[("ps", pb)])
                P.op("dve", I("tensor_copy", out=bias[:], in_=self.ps[pb][:, 0:1]), reads=[("ps", pb)], writes=["bias"])
                for g in range(2):
                    gs = slice(64 * g, 64 * g + 64)
                    pa = self.psn()
                    for ll in range(32):
                        P.op("pe", I("matmul",
                            self.ps[pa][:, 0:255], lhsT=W1[kv][gs, ll, :], rhs=kvT[kv][gs, ll:ll + 16 * 254 + 1:16],
                            start=(ll == 0), stop=(ll == 31)), reads=["W1" + kv, kv + "T"], writes=[("ps", pa)])
                    P.op("act", I("activation", out=xs[:], in_=self.ps[pa][:, 0:255], func=AF.Identity, bias=bias[:, 0:1], scale=1.0),
                         reads=[("ps", pa), "bias"], writes=["xs"])
                    P.op("dve", I("tensor_tensor", out=x2[:], in0=xs[:], in1=xs[:], op=ALU.mult), reads=["xs"], writes=["x2"])
                    P.op("dve", I("tensor_scalar", out=x2[:], in0=x2[:], scalar1=0.044715, scalar2=1.0, op0=ALU.mult, op1=ALU.add),
                         reads=["x2"], writes=["x2"])
                    P.op("dve", I("tensor_tensor", out=x2[:], in0=x2[:], in1=xs[:], op=ALU.mult), reads=["x2", "xs"], writes=["x2"])
                    P.op("act", I("activation", out=sg[:], in_=x2[:], func=AF.Sigmoid, scale=1.5957691216057308),
                         reads=["x2"], writes=["sg"])
                    P.op("dve", I("tensor_tensor", out=gl[:, 0:255], in0=xs[:], in1=sg[:], op=ALU.mult), reads=["xs", "sg"], writes=["gl"])
                    if kv == "k":
                        po = self.psn()
                        P.op("pe", I("matmul", self.ps[po][gs, 0:256], lhsT=W2["k"][:, :], rhs=gl[:, :], start=True, stop=True),
                             reads=["W2k", "gl"], writes=[("ps", po)])
                        P.op("dve", I("tensor_copy", out=kc[gs, :], in_=self.ps[po][gs, 0:256]),
                             reads=[("ps", po)], writes=[("kc", g)])
                    else:
                        for nb in range(2):
                            po = self.psn()
                            P.op("pe", I("matmul", self.ps[po][:, 0:64], lhsT=gl[:, nb * 128:(nb + 1) * 128], rhs=W2["v"][:, :],
                                                                     start=True, stop=True), reads=["W2v", "gl"], writes=[("ps", po)])
                            P.op("dve", I("tensor_copy", out=vc[:, g, nb, 0:64], in_=self.ps[po][:, 0:64]),
                                 reads=[("ps", po)], writes=["vc"])
            P.dma("sp", I("dma_start", out=dr["kcT"], in_=kc[:]), reads=[("kc", 0), ("kc", 1)], writes=["kcT"])
            P.dma("sp", I("dma_start", out=dr["VC"], in_=vc[:]), reads=["vc"], writes=["VC"])
            P.end_phase()

    def phase_fox(self, l):
        nc, P, dr, ps = self.nc, self.P, self.dr, self.ps
        if "cH" not in dr:
            self.dscr("cH", [4, 3, S], BF16)
        with contextlib.ExitStack() as st:
            T = lambda n, sh, dt: st.enter_context(nc.sbuf_tensor(self.uname(n), sh, dt))
            QaT = T("f_QaT", [70, 4, S], BF16)
            KaT = T("f_KaT", [70, 4, S], BF16)
            V = T("f_V", [128, 32, 4, 65], BF16)
            c4 = T("f_c4", [4, S], F32)
            rr = T("f_rr", [4, S], F32)
            H = T("f_H", [4, 3, S], BF16)
            mgt = T("f_mgt", [128, 4, 512], BF16)
            ident = T("f_ident", [128, 128], BF16)
            Pt = [T("f_P%d" % i, [128, CH], BF16) for i in range(3)]
            rs = [T("f_rs%d" % i, [128, 1], F32) for i in range(4)]
            of = [T("f_of%d" % i, [128, 4, 64], F32) for i in range(8)]
            P.dma("sp", I("dma_start", out=mgt[:], in_=dr["m_gt"]), writes=["mgt"])
            P.dma("sp", I("dma_start", out=ident[:], in_=dr["ident"]), writes=["ident"])
            P.dma("sp", I("dma_start", out=c4[:], in_=dr["cT"]), writes=["c4"])
            for q8 in range(8):
                P.dma("sp", I("dma_start", out=V[:, q8 * 4:(q8 + 1) * 4, :, :],
                              in_=dr["VA"][q8 * 512:(q8 + 1) * 512, 4:8, :].rearrange("(sb p) h c -> p sb h c", p=128)), writes=["V"])
            P.op("pool", I("memset", QaT[64:70, :, :], -1.0), writes=["Qaug"])
            P.op("pool", I("memset", KaT[64:70, :, :], 1.0), writes=["Kaug"])
            for hh in range(4):
                src_q = dr["FT"][8 + hh // 2, (hh % 2) * 64:(hh % 2) * 64 + 64, :]
                src_k = dr["FT"][10 + hh // 2, (hh % 2) * 64:(hh % 2) * 64 + 64, :]
                P.dma("sp", I("dma_start", out=QaT[0:64, hh, :], in_=src_q), writes=[("Q", hh)])
                P.dma("act", I("dma_start", out=KaT[0:64, hh, :], in_=src_k), writes=[("K", hh)])
            P.op("dve", I("tensor_copy", out=H[:, 0, :], in_=c4[:]), reads=["c4"], writes=["H0"])
            P.op("dve", I("tensor_tensor", out=rr[:], in0=c4[:], in1=H[:, 0, :], op=ALU.subtract), reads=["c4", "H0"], writes=["rr"])
            P.op("dve", I("tensor_copy", out=H[:, 1, :], in_=rr[:]), reads=["rr"], writes=["H1"])
            P.op("dve", I("tensor_tensor", out=rr[:], in0=rr[:], in1=H[:, 1, :], op=ALU.subtract), reads=["rr", "H1"], writes=["rr"])
            P.op("dve", I("tensor_copy", out=H[:, 2, :], in_=rr[:]), reads=["rr"], writes=["H2"])
            P.dma("sp", I("dma_start", out=dr["cH"], in_=H[:]), reads=["H0", "H1", "H2"], writes=["cH"])
            for hh in range(4):
                P.dma("sp", I("dma_start", out=QaT[64:67, hh, :], in_=dr["cH"][hh]), reads=["cH", "Qaug"], writes=[("Qa", hh)])
                P.dma("sp", I("dma_start", out=KaT[67:70, hh, :], in_=dr["cH"][hh]), reads=["cH", "Kaug"], writes=[("Ka", hh)])
            sti = 0
            pi = 0
            for qc in range(NCH):
                csl = slice(qc * CH, (qc + 1) * CH)
                for hh in range(4):
                    qk = [("Q", hh), ("Qa", hh), ("K", hh), ("Ka", hh), "Qaug", "Kaug"]
                    for sb in range(4 * qc + 4):
                        diag = sb >= 4 * qc
                        b = sti % 3
                        sti += 1
                        P.op("pe", I("matmul", ps[b][:, :], lhsT=KaT[0:70, hh, sb * 128:(sb + 1) * 128], rhs=QaT[0:70, hh, csl],
                                     start=True, stop=not diag), reads=qk, writes=[("ps", b)])
                        if diag:
                            rq = sb - 4 * qc
                            P.op("pe", I("matmul", ps[b][:, rq * 128:(rq + 1) * 128], lhsT=ident[:, :], rhs=mgt[:, rq, rq * 128:(rq + 1) * 128],
                                         start=False, stop=True), reads=["ident", "mgt"], writes=[("ps", b)])
                        pb = pi % 3
                        pi += 1
                        P.op("act", I("activation", out=Pt[pb][:], in_=ps[b][:, :], func=AF.Exp), reads=[("ps", b)], writes=[("P", pb)])
                        def back(sb, pb, hh, qc):
                            for ti in range(4):
                                if sb <= 4 * qc + ti:
                                    P.op("pe", I("matmul", ps[3 + ti][:, 0:65], lhsT=Pt[pb][:, ti * 128:(ti + 1) * 128], rhs=V[:, sb, hh, :],
                                                 start=(sb == 0), stop=(sb == 4 * qc + ti)), reads=[("P", pb), "V"], writes=[("ps", 3 + ti)])
                        self.run_deferred()
                        self.defer(back, sb, pb, hh, qc)

                    def fin(hh, qc):
                        for ti in range(4):
                            P.op("dve", I("tensor_scalar", out=rs[ti][:], in0=ps[3 + ti][:, 64:65], scalar1=1e-20, scalar2=None, op0=ALU.max),
                                 reads=[("ps", 3 + ti)], writes=[("rs", ti)])
                        for ti in range(4):
                            P.op("dve", I("reciprocal", out=rs[ti][:], in_=rs[ti][:]), reads=[("rs", ti)], writes=[("rs", ti)])
                        for ti in range(4):
                            o = (qc % 2) * 4 + ti
                            P.op("dve", I("tensor_scalar", out=of[o][:, hh, :], in0=ps[3 + ti][:, 0:64], scalar1=rs[ti][:, 0:1], scalar2=None, op0=ALU.mult),
                                 reads=[("ps", 3 + ti), ("rs", ti)], writes=[("of", o, hh)])
                    self.defer(fin, hh, qc)

                def store(qc):
                    for ti in range(4):
                        o = (qc % 2) * 4 + ti
                        rows = slice((qc * 4 + ti) * 128, (qc * 4 + ti + 1) * 128)
                        P.dma("sp", I("dma_start", out=dr["oS"][rows, 512:768], in_=of[o][:].rearrange("p h d -> p (h d)")),
                              reads=[("of", o, hh) for hh in range(4)], writes=[("oS", "f", qc, ti)])
                self.defer(store, qc)
            self.run_deferred()
            P.end_phase()

    def phase_sb(self, l):
        nc, P, dr, ps = self.nc, self.P, self.dr, self.ps
        with contextlib.ExitStack() as st:
            T = lambda n, sh, dt: st.enter_context(nc.sbuf_tensor(self.uname(n), sh, dt))
            QT = T("s_QT", [128, 2, S], BF16)
            KT = T("s_KT", [128, 2, S], BF16)
            V = T("s_V", [128, 32, 256], BF16)
            mge = T("s_mge", [128, 4, 512], BF16)
            ident = T("s_ident", [128, 128], BF16)
            ntri = T("s_ntri", [128, 128], BF16)
            nones = T("s_nones", [1, 128], BF16)
            onec = T("s_onec", [128, 1], BF16)
            et = [T("s_e%d" % i, [128, CH], F32) for i in range(2)]
            SP = [T("s_SP%d" % i, [128, CH], BF16) for i in range(3)]
            at = [T("s_a%d" % i, [128, CH], BF16) for i in range(2)]
            carry = T("s_carry", [1, CH], F32)
            ctmp = T("s_ctmp", [1, CH], F32)
            chi = [T("s_chi%d" % i, [1, CH], BF16) for i in range(3)]
            clo = [T("s_clo%d" % i, [1, CH], BF16) for i in range(3)]
            osb = [T("s_o%d" % i, [128, 4, 64], F32) for i in range(8)]
            for n, t_ in (("m_ge", mge), ("ident", ident), ("ntri", ntri), ("nones", nones), ("onec", onec)):
                P.dma("sp", I("dma_start", out=t_[:], in_=dr[n]), writes=[n])
            for j in range(2):
                P.dma("sp", I("dma_start", out=QT[:, j, :], in_=dr["FT"][12 + j]), writes=[("Q", j)])
                P.dma("act", I("dma_start", out=KT[:, j, :], in_=dr["FT"][14 + j]), writes=[("K", j)])
            for q8 in range(8):
                P.dma("sp", I("dma_start", out=V[:, q8 * 4:(q8 + 1) * 4, :],
                              in_=dr["SV"][q8 * 512:(q8 + 1) * 512, :].rearrange("(sb p) c -> p sb c", p=128)), writes=["V"])
            cnt = {"a": 0, "e": 0, "sp": 0, "at": 0, "c": 0}
            for qc in range(NCH):
                csl = slice(qc * CH, (qc + 1) * CH)
                for h in range(4):
                    hb = slice(64 * (h % 2), 64 * (h % 2) + 64)
                    j = h // 2
                    qk = [("Q", j), ("K", j)]
                    blocks = list(range(4 * qc + 3, -1, -1))
                    nblk = len(blocks)
                    info = {}

                    def S1a(idx):
                        sb = blocks[idx]
                        diag = sb >= 4 * qc
                        P.op("pe", I("matmul", ps[0][:, :], lhsT=KT[hb, j, sb * 128:(sb + 1) * 128], rhs=QT[hb, j, csl], start=True, stop=not diag),
                             reads=qk, writes=[("ps", 0)])
                        if diag:
                            P.op("pe", I("matmul", ps[0][:, :], lhsT=ident[:, :], rhs=mge[:, sb - 4 * qc, :], start=False, stop=True),
                                 reads=["ident", "m_ge"], writes=[("ps", 0)])
                        eb = cnt["e"] % 2
                        cnt["e"] += 1
                        P.op("act", I("activation", out=et[eb][:], in_=ps[0][:, :], func=AF.Exp), reads=[("ps", 0)], writes=[("e", eb)])
                        sb_i = cnt["sp"] % 3
                        cnt["sp"] += 1
                        P.op("act", I("activation", out=SP[sb_i][:], in_=et[eb][:], func=AF.Ln, bias=1.0, scale=1.0),
                             reads=[("e", eb)], writes=[("SP", sb_i)])
                        info[idx] = sb_i

                    def S1b(idx):
                        sb_i = info[idx]
                        P.op("pe", I("matmul", ps[3][0:1, :], lhsT=onec[:, 0:1], rhs=SP[sb_i][:, :], start=(idx == 0), stop=(idx + 2 == nblk)),
                             reads=["onec", ("SP", sb_i)], writes=[("ps", 3)])
                        cb = (idx + 1) % 3
                        P.op("dve", I("tensor_copy", out=chi[cb][:], in_=ps[3][0:1, :]), reads=[("ps", 3)], writes=[("chi", cb)])
                        P.op("dve", I("tensor_tensor", out=clo[cb][:], in0=ps[3][0:1, :], in1=chi[cb][:], op=ALU.subtract),
                             reads=[("ps", 3), ("chi", cb)], writes=[("clo", cb)])

                    def S2a(idx):
                        sb = blocks[idx]
                        diag = sb >= 4 * qc
                        sb_i = info[idx]
                        bb = 1 + (cnt["a"] % 2)
                        cnt["a"] += 1
                        P.op("pe", I("matmul", ps[bb][:, :], lhsT=KT[hb, j, sb * 128:(sb + 1) * 128], rhs=QT[hb, j, csl], start=True, stop=False),
                             reads=qk, writes=[("ps", bb)])
                        if diag:
                            P.op("pe", I("matmul", ps[bb][:, :], lhsT=ident[:, :], rhs=mge[:, sb - 4 * qc, :], start=False, stop=False),
                                 reads=["ident", "m_ge"], writes=[("ps", bb)])
                        last = (idx == 0)
                        P.op("pe", I("matmul", ps[bb][:, :], lhsT=ntri[:, :], rhs=SP[sb_i][:, :], start=False, stop=last),
                             reads=["ntri", ("SP", sb_i)], writes=[("ps", bb)])
                        if idx > 0:
                            cb = idx % 3
                            P.op("pe", I("matmul", ps[bb][:, :], lhsT=nones[0:1, :], rhs=chi[cb][0:1, :], start=False, stop=False),
                                 reads=["nones", ("chi", cb)], writes=[("ps", bb)])
                            P.op("pe", I("matmul", ps[bb][:, :], lhsT=nones[0:1, :], rhs=clo[cb][0:1, :], start=False, stop=True),
                                 reads=["nones", ("clo", cb)], writes=[("ps", bb)])
                        ab = cnt["at"] % 2
                        cnt["at"] += 1
                        P.op("act", I("activation", out=at[ab][:], in_=ps[bb][:, :], func=AF.Exp), reads=[("ps", bb)], writes=[("at", ab)])
                        info[("ab", idx)] = ab

                    def S2b(idx):
                        sb = blocks[idx]
                        ab = info[("ab", idx)]
                        for ti in range(4):
                            if sb <= 4 * qc + ti:
                                P.op("pe", I("matmul", ps[4 + ti][:, 0:64], lhsT=at[ab][:, ti * 128:(ti + 1) * 128], rhs=V[:, sb, h * 64:(h + 1) * 64],
                                             start=(sb == 4 * qc + ti), stop=(sb == 0)), reads=[("at", ab), "V"], writes=[("ps", 4 + ti)])

                    S1a(0)
                    if nblk > 1:
                        S1a(1)
                        S1b(0)
                    for idx in range(nblk):
                        if idx + 2 < nblk:
                            S1a(idx + 2)
                            S1b(idx + 1)
                        S2a(idx)
                        if idx >= 1:
                            S2b(idx - 1)
                    S2b(nblk - 1)
                    for ti in range(4):
                        o = (qc % 2) * 4 + ti
                        P.op("dve", I("tensor_copy", out=osb[o][:, h, :], in_=ps[4 + ti][:, 0:64]), reads=[("ps", 4 + ti)], writes=[("osb", o, h)])
                for ti in range(4):
                    o = (qc % 2) * 4 + ti
                    rows = slice((qc * 4 + ti) * 128, (qc * 4 + ti + 1) * 128)
                    P.dma("sp", I("dma_start", out=dr["oS"][rows, 768:1024], in_=osb[o][:].rearrange("p h d -> p (h d)")),
                          reads=[("osb", o, h) for h in range(4)], writes=[("oS", "s", qc, ti)])
            P.end_phase()

    def phase_nsa(self, l):
        nc, P, dr, ps = self.nc, self.P, self.dr, self.ps
        pt = self.pt
        with contextlib.ExitStack() as st:
            T = lambda n, sh, dt: st.enter_context(nc.sbuf_tensor(self.uname(n), sh, dt))
            QT = T("n_QT", [128, 4, S], BF16)
            KsE = T("n_KsE", [128, 2, S], BF16)
            Qs = T("n_Qs", [128, 4, CH], BF16)
            KwT = T("n_KwT", [128, S], BF16)
            kcT = T("n_kcT", [128, 256], BF16)
            V4 = T("n_V4", [128, 32, 4, 65], BF16)
            VC = T("n_VC", [128, 2, 2, 65], BF16)
            ovl = T("n_ovl", [128, 2, 64], BF16)
            mgt = T("n_mgt", [128, 4, 512], BF16)
            mle = T("n_mle", [128, 4, 512], BF16)
            ident = T("n_ident", [128, 128], BF16)
            mcmp = [T("n_mcmp%d" % i, [128, 2, CH], BF16) for i in range(2)]
            sbias = [T("n_sbias%d" % i, [128, 64], F32) for i in range(4)]
            gts = [T("n_gt%d" % i, [128, 24], F32) for i in range(8)]
            Pt = [T("n_P%d" % i, [128, CH], BF16) for i in range(3)]
            Pc = [T("n_Pc%d" % i, [128, CH], BF16) for i in range(4)]
            onsa = [T("n_o%d" % i, [128, 8, 64], F32) for i in range(8)]
            impg = [T("n_imp%d" % i, [128, 64], F32) for i in range(4)]
            score = T("n_score", [128, 64], F32)
            sc2 = T("n_sc2", [128, 64], F32)
            sc3 = T("n_sc3", [128, 64], F32)
            m8a = T("n_m8a", [128, 8], F32)
            m8b = T("n_m8b", [128, 8], F32)
            seln = T("n_seln", [128, 64], BF16)
            rc4 = T("n_rc4", [128, 4], F32)
            rg = T("n_rg", [128, 1], F32)
            rs1 = T("n_rs1", [128, 1], F32)
            self._nsa_tmp = ([T("n_r4%d" % i, [128, 1], F32) for i in range(4)], [T("n_g4%d" % i, [128, 1], F32) for i in range(4)],
                             [T("n_s4%d" % i, [128, 64], F32) for i in range(4)])
            for n, t_ in (("m_gt", mgt), ("m_le", mle), ("ident", ident), ("ovl", ovl)):
                P.dma("sp", I("dma_start", out=t_[:], in_=dr[n]), writes=[n])
            for j in range(4):
                P.dma("sp" if j % 2 == 0 else "act", I("dma_start", out=QT[:, j, :], in_=dr["FT"][j]), writes=[("Q", j)])
            for g_ in range(2):
                P.dma("sp", I("dma_start", out=KsE[0:64, g_, :], in_=dr["FT"][5, 64 * g_:64 * g_ + 64, :]), writes=[("KsE", g_, 0)])
                P.dma("act", I("dma_start", out=KsE[64:128, g_, :], in_=dr["eblk"]), writes=[("KsE", g_, 1)])
            P.dma("act", I("dma_start", out=KwT[:], in_=dr["FT"][6]), writes=["KwT"])
            P.dma("sp", I("dma_start", out=kcT[:], in_=dr["kcT"]), writes=["kcT"])
            P.dma("sp", I("dma_start", out=VC[:], in_=dr["VC"]), writes=["VC"])
            for q8 in range(8):
                P.dma("sp", I("dma_start", out=V4[:, q8 * 4:(q8 + 1) * 4, :, :],
                              in_=dr["VA"][q8 * 512:(q8 + 1) * 512, 0:4, :].rearrange("(sb p) h c -> p sb h c", p=128)), writes=["V4"])
            cn = {"st": 0, "p": 0}

            def st_bank():
                b = cn["st"] % 3
                cn["st"] += 1
                return b

            def exp_to(b, dst, dstk, scale=0.125):
                P.op("act", I("activation", out=dst[:], in_=ps[b][:, :], func=AF.Exp, scale=scale), reads=[("ps", b)], writes=[dstk])

            for qc in range(NCH):
                csl = slice(qc * CH, (qc + 1) * CH)
                mb = qc % 2
                P.dma("sp", I("dma_start", out=mcmp[mb][:], in_=dr["m_cmp"][:, :, csl]), writes=[("mcmp", mb)])
                for ti in range(4):
                    rows = slice((qc * 4 + ti) * 128, (qc * 4 + ti + 1) * 128)
                    P.dma("sp", I("dma_start", out=sbias[ti][:], in_=dr["selbias"][rows]), writes=[("sbias", ti)])
                    P.dma("sp", I("dma_start", out=gts[mb * 4 + ti][:], in_=dr["GT"][rows]), writes=[("gts", mb * 4 + ti)])
                nbs = [0] if qc < 4 else [0, 1]
                for g in range(2):
                    gs = slice(64 * g, 64 * g + 64)
                    self.run_deferred()
                    for hh in range(4):
                        head = 4 * g + hh
                        for nb in nbs:
                            b = st_bank()
                            P.op("pe", I("matmul", ps[b][:, :], lhsT=kcT[gs, nb * 128:(nb + 1) * 128], rhs=QT[gs, hh, csl], start=True, stop=False),
                                 reads=["kcT", ("Q", hh)], writes=[("ps", b)])
                            P.op("pe", I("matmul", ps[b][:, :], lhsT=ident[:, :], rhs=mcmp[mb][:, nb, :], start=False, stop=True),
                                 reads=["ident", ("mcmp", mb)], writes=[("ps", b)])
                            pk = (hh % 2) * 2 + nb
                            exp_to(b, Pc[pk], ("Pc", pk))
                        for ti in range(4):
                            for nb in nbs:
                                pk = (hh % 2) * 2 + nb
                                P.op("pe", I("matmul", ps[3 + ti][:, 0:65], lhsT=Pc[pk][:, ti * 128:(ti + 1) * 128], rhs=VC[:, g, nb, :],
                                             start=(nb == nbs[0]), stop=(nb == nbs[-1])), reads=[("Pc", pk), "VC"], writes=[("ps", 3 + ti)])
                            for nb in nbs:
                                pk = (hh % 2) * 2 + nb
                                P.op("pe", I("matmul", ps[3 + ti][:, 128:192], lhsT=Pc[pk][:, ti * 128:(ti + 1) * 128], rhs=ovl[:, nb, :],
                                             start=(nb == nbs[0]), stop=(nb == nbs[-1])), reads=[("Pc", pk), "ovl"], writes=[("ps", 3 + ti)])
                        r4, g4, s4 = self._nsa_tmp
                        for ti in range(4):
                            P.op("dve", I("tensor_scalar", out=r4[ti][:], in0=ps[3 + ti][:, 64:65], scalar1=1e-20, scalar2=None, op0=ALU.max),
                                 reads=[("ps", 3 + ti)], writes=[("r4", ti)])
                        for ti in range(4):
                            P.op("dve", I("reciprocal", out=r4[ti][:], in_=r4[ti][:]), reads=[("r4", ti)], writes=[("r4", ti)])
                        for ti in range(4):
                            o = mb * 4 + ti
                            P.op("dve", I("tensor_tensor", out=g4[ti][:], in0=r4[ti][:], in1=gts[o][:, head * 3:head * 3 + 1], op=ALU.mult),
                                 reads=[("r4", ti), ("gts", o)], writes=[("g4", ti)])
                        for ti in range(4):
                            if hh == 0:
                                P.op("dve", I("tensor_scalar", out=impg[ti][:], in0=ps[3 + ti][:, 128:192], scalar1=r4[ti][:, 0:1], scalar2=None, op0=ALU.mult),
                                     reads=[("ps", 3 + ti), ("r4", ti)], writes=[("impg", ti)])
                            else:
                                P.op("dve", I("tensor_scalar", out=s4[ti][:], in0=ps[3 + ti][:, 128:192], scalar1=r4[ti][:, 0:1], scalar2=None, op0=ALU.mult),
                                     reads=[("ps", 3 + ti), ("r4", ti)], writes=[("s4", ti)])
                        if hh > 0:
                            for ti in range(4):
                                P.op("pool", I("tensor_tensor", out=impg[ti][:], in0=impg[ti][:], in1=s4[ti][:], op=ALU.add),
                                     reads=[("s4", ti), ("impg", ti)], writes=[("impg", ti)])
                        for ti in range(4):
                            o = mb * 4 + ti
                            P.op("dve", I("tensor_scalar", out=onsa[o][:, head, :], in0=ps[3 + ti][:, 0:64], scalar1=g4[ti][:, 0:1], scalar2=None, op0=ALU.mult),
                                 reads=[("ps", 3 + ti), ("g4", ti)], writes=[("onsa", o, head)])
                    if NSA_STOP == 1:
                        continue
                    for ti in range(4):
                        P.op("dve", I("tensor_tensor", out=score[:], in0=impg[ti][:], in1=sbias[ti][:], op=ALU.add),
                             reads=[("impg", ti), ("sbias", ti)], writes=["score"])
                        P.op("dve", I("max", out=m8a[:], in_=score[:]), reads=["score"], writes=["m8a"])
                        P.op("dve", I("match_replace", out=sc3[:], in_to_replace=m8a[:], in_values=score[:], imm_value=-3.0e9),
                             reads=["score", "m8a"], writes=["sc3"])
                        P.op("dve", I("max", out=m8b[:], in_=sc3[:]), reads=["sc3"], writes=["m8b"])
                        P.op("dve", I("tensor_scalar", out=seln[:], in0=score[:], scalar1=m8b[:, 7:8], scalar2=-BIG, op0=ALU.is_lt, op1=ALU.mult),
                             reads=["score", "m8b"], writes=["seln"])
                        P.op("pe", I("transpose", out=pt[0:64, ti * 128:(ti + 1) * 128], in_=seln[:, :], identity=ident[:, :]),
                             reads=["seln", "ident"], writes=[("ps", 7)])
                    for hh in range(4):
                        P.op("pool", I("tensor_copy", out=Qs[0:64, hh, :], in_=QT[gs, hh, csl]), reads=[("Q", hh)], writes=[("Qs", hh, 0)])
                        P.op("dve", I("tensor_copy", out=Qs[64:128, hh, :], in_=pt[0:64, 0:512]), reads=[("ps", 7)], writes=[("Qs", hh, 1)])
                    if NSA_STOP == 2:
                        continue
                    for branch in ((1, 2) if NSA_STOP != 3 else (1,)):
                        for hh in range(4):
                            head = 4 * g + hh
                            if branch == 1:
                                seq = [(sb, None) for sb in range(4 * qc + 4)]
                            else:
                                seq = [(4 * qc - 4 + r, r) for r in range(8) if 4 * qc - 4 + r >= 0]
                            first = {}
                            last = {}
                            for (sb, r) in seq:
                                for ti in range(4):
                                    if branch == 1:
                                        ok = sb <= 4 * qc + ti
                                    else:
                                        ok = (ti <= r) if r < 4 else (r - 4 <= ti)
                                    if ok:
                                        first.setdefault(ti, sb)
                                        last[ti] = sb
                            for (sb, r) in seq:
                                b = st_bank()
                                if branch == 1:
                                    diag = sb >= 4 * qc
                                    P.op("pe", I("matmul", ps[b][:, :], lhsT=KsE[:, g, sb * 128:(sb + 1) * 128], rhs=Qs[:, hh, :], start=True, stop=not diag),
                                         reads=[("KsE", g, 0), ("KsE", g, 1), ("Qs", hh, 0), ("Qs", hh, 1)], writes=[("ps", b)])
                                    if diag:
                                        rq = sb - 4 * qc
                                        P.op("pe", I("matmul", ps[b][:, rq * 128:(rq + 1) * 128], lhsT=ident[:, :],
                                                     rhs=mgt[:, rq, rq * 128:(rq + 1) * 128], start=False, stop=True),
                                             reads=["ident", "m_gt"], writes=[("ps", b)])
                                else:
                                    P.op("pe", I("matmul", ps[b][:, :], lhsT=KwT[gs, sb * 128:(sb + 1) * 128], rhs=QT[gs, hh, csl], start=True, stop=False),
                                         reads=["KwT", ("Q", hh)], writes=[("ps", b)])
                                    rq = r if r < 4 else r - 4
                                    msk = mle[:, rq, rq * 128:(rq + 1) * 128] if r < 4 else mgt[:, rq, rq * 128:(rq + 1) * 128]
                                    P.op("pe", I("matmul", ps[b][:, rq * 128:(rq + 1) * 128], lhsT=ident[:, :], rhs=msk, start=False, stop=True),
                                         reads=["ident", "m_gt", "m_le"], writes=[("ps", b)])
                                pb = cn["p"] % 3
                                cn["p"] += 1
                                exp_to(b, Pt[pb], ("P", pb))
                                def back(sb, r, pb, branch, g, first, last):
                                    for ti in range(4):
                                        if ti in first and first[ti] <= sb <= last[ti]:
                                            if branch == 2:
                                                ok = (ti <= r) if r < 4 else (r - 4 <= ti)
                                                if not ok:
                                                    continue
                                            vg = g if branch == 1 else 2 + g
                                            P.op("pe", I("matmul", ps[3 + ti][:, 0:65], lhsT=Pt[pb][:, ti * 128:(ti + 1) * 128], rhs=V4[:, sb, vg, :],
                                                         start=(sb == first[ti]), stop=(sb == last[ti])), reads=[("P", pb), "V4"], writes=[("ps", 3 + ti)])
                                self.run_deferred()
                                self.defer(back, sb, r, pb, branch, g, first, last)
                            self.defer(self._nsa_fin, mb, head, branch, gts, rs1, rg, sc2, onsa)
                            for ti in range(0):
                                o = mb * 4 + ti
                                gtile = gts[o]
                                P.op("dve", I("tensor_scalar", out=rs1[:], in0=ps[3 + ti][:, 64:65], scalar1=1e-20, scalar2=None, op0=ALU.max),
                                     reads=[("ps", 3 + ti)], writes=["rs1"])
                                P.op("dve", I("reciprocal", out=rs1[:], in_=rs1[:]), reads=["rs1"], writes=["rs1"])
                                P.op("dve", I("tensor_tensor", out=rg[:], in0=rs1[:], in1=gtile[:, head * 3 + branch:head * 3 + branch + 1], op=ALU.mult),
                                     reads=["rs1", ("gts", o)], writes=["rg"])
                                P.op("dve", I("tensor_scalar", out=sc2[:], in0=ps[3 + ti][:, 0:64], scalar1=rg[:, 0:1], scalar2=None, op0=ALU.mult),
                                     reads=[("ps", 3 + ti), "rg"], writes=["sc2"])
                                P.op("pool", I("tensor_tensor", out=onsa[o][:, head, :], in0=onsa[o][:, head, :], in1=sc2[:], op=ALU.add),
                                     reads=["sc2", ("onsa", o, head)], writes=[("onsa", o, head)])
                def store(qc, mb):
                    for ti in range(4):
                        o = mb * 4 + ti
                        rows = slice((qc * 4 + ti) * 128, (qc * 4 + ti + 1) * 128)
                        P.dma("sp", I("dma_start", out=dr["oS"][rows, 0:512], in_=onsa[o][:].rearrange("p h d -> p (h d)")),
                              reads=[("onsa", o, hd) for hd in range(8)], writes=[("oS", "n", qc, ti)])
                self.defer(store, qc, mb)
            self.run_deferred()
            P.end_phase()

    def _nsa_fin(self, mb, head, branch, gts, rs1, rg, sc2, onsa):
        P, ps = self.P, self.ps
        r4, g4, s4 = self._nsa_tmp
        for ti in range(4):
            P.op("dve", I("tensor_scalar", out=r4[ti][:], in0=ps[3 + ti][:, 64:65], scalar1=1e-20, scalar2=None, op0=ALU.max),
                 reads=[("ps", 3 + ti)], writes=[("r4", ti)])
        for ti in range(4):
            P.op("dve", I("reciprocal", out=r4[ti][:], in_=r4[ti][:]), reads=[("r4", ti)], writes=[("r4", ti)])
        for ti in range(4):
            o = mb * 4 + ti
            P.op("dve", I("tensor_tensor", out=g4[ti][:], in0=r4[ti][:], in1=gts[o][:, head * 3 + branch:head * 3 + branch + 1], op=ALU.mult),
                 reads=[("r4", ti), ("gts", o)], writes=[("g4", ti)])
        for ti in range(4):
            P.op("dve", I("tensor_scalar", out=s4[ti][:], in0=ps[3 + ti][:, 0:64], scalar1=g4[ti][:, 0:1], scalar2=None, op0=ALU.mult),
                 reads=[("ps", 3 + ti), ("g4", ti)], writes=[("s4", ti)])
        for ti in range(4):
            o = mb * 4 + ti
            P.op("pool", I("tensor_tensor", out=onsa[o][:, head, :], in0=onsa[o][:, head, :], in1=s4[ti][:], op=ALU.add),
                 reads=[("s4", ti), ("onsa", o, head)], writes=[("onsa", o, head)])

    def phase_D(self, l, last):
        self.phase_D1(l)
        self.phase_D2(l, last)

    def phase_D1(self, l):
        nc, P, dr, ps = self.nc, self.P, self.dr, self.ps
        hsrc = dr["x"] if l == 0 else dr["hS"]
        with contextlib.ExitStack() as st:
            T = lambda n, sh, dt: st.enter_context(nc.sbuf_tensor(self.uname(n), sh, dt))
            Wout = T("d_Wout", [128, 8, D], BF16)
            Wd = T("d_Wd", [128, NF, D], BF16)
            stage = [T("d_stage%d" % i, [128, DFF], F32) for i in range(2)]
            cvt = [T("d_cvt%d" % i, [128, DFF], BF16) for i in range(2)]
            ghead = T("d_ghead", [128, 8], F32)
            gffn = T("d_gffn", [128, 8], F32)
            ident = T("d_ident", [128, 128], BF16)
            h = [T("d_h%d" % i, [128, D], F32) for i in range(4)]
            ot = [T("d_ot%d" % i, [128, D], F32) for i in range(2)]
            osq = T("d_osq", [128, D], F32)
            ssh = T("d_ssh", [128, 16], F32)
            on = [T("d_on%d" % i, [128, D], BF16) for i in range(2)]
            T4 = [dict(junk=T("d_junk%d" % i, [128, D], BF16), ss=T("d_ss%d" % i, [128, 1], F32),
                       rs=T("d_rs%d" % i, [128, 1], F32)) for i in range(2)]
            xT = T("d_xT", [128, 8, CH], BF16)
            actT = T("d_actT", [128, NF, CH], BF16)
            wgu = [T("d_wgu%d" % i, [128, 2, 8, 128], BF16) for i in range(3)]
            sg = [T("d_sg%d" % i, [128, CH], F32) for i in range(2)]
            P.dma("sp", I("dma_start", out=ghead[:], in_=dr["head_norm"][l]), writes=["ghead"])
            P.dma("sp", I("dma_start", out=gffn[:], in_=dr["norm_ffn"][l]), writes=["gffn"])
            P.dma("sp", I("dma_start", out=ident[:], in_=dr["ident"]), writes=["ident"])
            n = 0
            for k in range(8):
                self.load_weight_bf(Wout[:, k, :], dr["w_out"][l, k * 128:(k + 1) * 128, :], stage[n % 2][:, 0:D], ("stage", n % 2),
                                    ("Wout", k), scale_ap=ghead[:, k:k + 1], scalek="ghead", eng=("dve" if n % 2 == 0 else "pool"),
                                    q=("sp" if n % 2 == 0 else "act"))
                n += 1
            for f in range(NF):
                self.load_weight_bf(Wd[:, f, :], dr["w_ffn_down"][l, f * 128:(f + 1) * 128, :], stage[n % 2][:, 0:D], ("stage", n % 2),
                                    ("Wd", f), eng=("dve" if n % 2 == 0 else "pool"), q=("sp" if n % 2 == 0 else "act"))
                n += 1
            for gi, wn in enumerate(("w_ffn_gate", "w_ffn_up")):
                for k in range(8):
                    b = n % 2
                    self.load_weight_bf(cvt[b][:], dr[wn][l, k * 128:(k + 1) * 128, :], stage[b][:], ("stage", b), ("cvt", b),
                                        scale_ap=gffn[:, k:k + 1], scalek="gffn", eng=("dve" if b == 0 else "pool"),
                                        q=("sp" if b == 0 else "act"))
                    for f0, f1 in ((0, 8), (8, 16), (16, NF)):
                        P.dma("sp", I("dma_start", out=dr["WGU"][f0:f1, :, gi, k, :].rearrange("f p c -> p f c"),
                                      in_=cvt[b][:, f0 * 128:f1 * 128].rearrange("p (f c) -> p f c", c=128)),
                              reads=[("cvt", b)], writes=[("WGU", gi, k, f0)])
                    n += 1
            wgu_ready = [("WGU", gi, k, f0) for gi in range(2) for k in range(8) for f0 in (0, 8, 16)]
            Woutk = [("Wout", k) for k in range(8)]
            Wdk = [("Wd", f) for f in range(NF)]
            wl = 0
            for c in range(NCH):
                for i in range(4):
                    ti = c * 4 + i
                    b = ti % 2
                    rows = slice(ti * 128, (ti + 1) * 128)
                    P.dma("sp", I("dma_start", out=h[i][:], in_=hsrc[rows, :]), writes=[("h", i)])
                    P.dma("act", I("dma_start", out=ot[b][:], in_=dr["oS"][rows, :]), writes=[("ot", b)])
                    P.op("pool", I("tensor_tensor", out=osq[:], in0=ot[b][:], in1=ot[b][:], op=ALU.mult), reads=[("ot", b)], writes=["osq"])
                    P.op("dve", I("tensor_reduce", out=ssh[:], in_=osq[:].rearrange("p (h d) -> p h d", d=64), axis=AX.X, op=ALU.add),
                         reads=["osq"], writes=["ssh"])
                    P.op("dve", I("tensor_scalar", out=ssh[:], in0=ssh[:], scalar1=1.0 / 64, scalar2=1e-6, op0=ALU.mult, op1=ALU.add),
                         reads=["ssh"], writes=["ssh"])
                    P.op("act", I("sqrt", out=ssh[:], in_=ssh[:]), reads=["ssh"], writes=["ssh"])
                    P.op("dve", I("reciprocal", out=ssh[:], in_=ssh[:]), reads=["ssh"], writes=["ssh"])
                    P.op("dve", I("tensor_tensor", out=on[b][:].rearrange("p (h d) -> p h d", d=64),
                                  in0=ot[b][:].rearrange("p (h d) -> p h d", d=64),
                                  in1=ssh[:, :].unsqueeze(2).to_broadcast([128, 16, 64]), op=ALU.mult),
                         reads=[("ot", b), "ssh"], writes=[("on", b)])
                    self.transpose8(on[b], ("on", b), xT[:, :, i * 128:(i + 1) * 128], ("xT", i), ident, eng=("dve" if i % 2 == 0 else "act"))
                    for half in range(2):
                        pa = self.psn()
                        for k in range(8):
                            P.op("pe", I("matmul", ps[pa][:, :], lhsT=xT[:, k, i * 128:(i + 1) * 128], rhs=Wout[:, k, half * 512:(half + 1) * 512],
                                         start=(k == 0), stop=(k == 7)), reads=[("xT", i)] + Woutk, writes=[("ps", pa)])
                        P.op("dve", I("tensor_tensor", out=h[i][:, half * 512:(half + 1) * 512], in0=ps[pa][:, :],
                                      in1=h[i][:, half * 512:(half + 1) * 512], op=ALU.add), reads=[("ps", pa), ("h", i)], writes=[("h", i)])
                if "hmix" in self.dr:
                    for i in range(4):
                        rows = slice((c * 4 + i) * 128, (c * 4 + i + 1) * 128)
                        P.dma("sp", I("dma_start", out=dr["hmix"][rows, :], in_=h[i][:]), reads=[("h", i)], writes=[("hmix", c, i)])
                for i in range(4):
                    b = i % 2
                    self.rms_tile(T4[b], b, h[i], ("h", i), ("junk", b), ("ss", b), ("rs", b), on[b], ("on", b))
                    self.transpose8(on[b], ("on", b), xT[:, :, i * 128:(i + 1) * 128], ("xT", i), ident, eng=("dve" if i % 2 == 0 else "act"))
                xk = [("xT", i) for i in range(4)]
                for f in range(NF):
                    wb = wl % 3
                    wl += 1
                    P.dma("sp" if f % 2 == 0 else "act", I("dma_start", out=wgu[wb][:], in_=dr["WGU"][f]), reads=wgu_ready, writes=[("wgu", wb)])
                    pg, pu = self.psn(), self.psn()
                    for gi, pp in ((0, pg), (1, pu)):
                        for k in range(8):
                            P.op("pe", I("matmul", ps[pp][:, :], lhsT=wgu[wb][:, gi, k, :], rhs=xT[:, k, :], start=(k == 0), stop=(k == 7)),
                                 reads=xk + [("wgu", wb)], writes=[("ps", pp)])
                    sb_ = f % 2
                    P.op("act", I("activation", out=sg[sb_][:], in_=ps[pg][:, :], func=AF.Silu), reads=[("ps", pg)], writes=[("sg", sb_)])
                    P.op("dve", I("tensor_tensor", out=actT[:, f, :], in0=ps[pu][:, :], in1=sg[sb_][:], op=ALU.mult),
                         reads=[("ps", pu), ("sg", sb_)], writes=[("actT", f)])
                ak = [("actT", f) for f in range(NF)]
                for i in range(4):
                    for half in range(2):
                        pa = self.psn()
                        for f in range(NF):
                            P.op("pe", I("matmul", ps[pa][:, :], lhsT=actT[:, f, i * 128:(i + 1) * 128], rhs=Wd[:, f, half * 512:(half + 1) * 512],
                                         start=(f == 0), stop=(f == NF - 1)), reads=ak + Wdk, writes=[("ps", pa)])
                        P.op("dve", I("tensor_tensor", out=h[i][:, half * 512:(half + 1) * 512], in0=ps[pa][:, :],
                                      in1=h[i][:, half * 512:(half + 1) * 512], op=ALU.add), reads=[("ps", pa), ("h", i)], writes=[("h", i)])
                    rows = slice((c * 4 + i) * 128, (c * 4 + i + 1) * 128)
                    P.dma("sp", I("dma_start", out=dr["hS"][rows, :], in_=h[i][:]), reads=[("h", i)], writes=[("hS", c, i)])
            P.end_phase()

    def phase_D2(self, l, last):
        nc, P, dr, ps = self.nc, self.P, self.dr, self.ps
        with contextlib.ExitStack() as st:
            T = lambda n, sh, dt: st.enter_context(nc.sbuf_tensor(self.uname(n), sh, dt))
            Wpg = T("e_Wpg", [128, 8, D], BF16)
            Wpp = T("e_Wpp", [128, 2, D], BF16)
            stage = [T("e_stage%d" % i, [128, D], F32) for i in range(2)]
            gple = T("e_gple", [128, 8], F32)
            gfin = T("e_gfin", [128, D], F32)
            ident = T("e_ident", [128, 128], BF16)
            h = [T("e_h%d" % i, [128, D], F32) for i in range(2)]
            p32 = [T("e_p32%d" % i, [128, 256], F32) for i in range(2)]
            pbf = [T("e_pbf%d" % i, [128, 256], BF16) for i in range(2)]
            hn = [T("e_hn%d" % i, [128, D], BF16) for i in range(2)]
            T4 = [dict(junk=T("e_junk%d" % i, [128, D], BF16), ss=T("e_ss%d" % i, [128, 1], F32),
                       rs=T("e_rs%d" % i, [128, 1], F32)) for i in range(2)]
            xT = [T("e_xT%d" % i, [128, 8, 128], BF16) for i in range(2)]
            pT = [T("e_pT%d" % i, [128, 2, 128], BF16) for i in range(2)]
            sig = [T("e_sig%d" % i, [128, CH], F32) for i in range(2)]
            tmp = [T("e_tmp%d" % i, [128, CH], F32) for i in range(2)]
            outt = [T("e_out%d" % i, [128, D], F32) for i in range(2)]
            P.dma("sp", I("dma_start", out=gple[:], in_=dr["norm_ple"][l]), writes=["gple"])
            P.dma("sp", I("dma_start", out=ident[:], in_=dr["ident"]), writes=["ident"])
            if last:
                P.dma("sp", I("dma_start", out=gfin[:], in_=dr["norm_final"].to_broadcast([128, D])), writes=["gfin"])
            n = 0
            for k in range(8):
                self.load_weight_bf(Wpg[:, k, :], dr["w_ple_gate"][l, k * 128:(k + 1) * 128, :], stage[n % 2][:], ("stage", n % 2),
                                    ("Wpg", k), scale_ap=gple[:, k:k + 1], scalek="gple", eng=("dve" if n % 2 == 0 else "pool"),
                                    q=("sp" if n % 2 == 0 else "act"))
                n += 1
            for k in range(2):
                self.load_weight_bf(Wpp[:, k, :], dr["w_ple_proj"][l, k * 128:(k + 1) * 128, :], stage[n % 2][:], ("stage", n % 2),
                                    ("Wpp", k), eng=("dve" if n % 2 == 0 else "pool"), q=("sp" if n % 2 == 0 else "act"))
                n += 1
            Wpgk = [("Wpg", k) for k in range(8)]
            Wppk = [("Wpp", k) for k in range(2)]
            for ti in range(NTILE):
                b = ti % 2
                rows = slice(ti * 128, (ti + 1) * 128)
                P.dma("sp", I("dma_start", out=h[b][:], in_=dr["hS"][rows, :]), writes=[("h", b)])
                P.dma("act", I("dma_start", out=p32[b][:], in_=dr["p"][l, rows, :]), writes=[("p32", b)])
                P.op("pool", I("tensor_copy", out=pbf[b][:], in_=p32[b][:]), reads=[("p32", b)], writes=[("pbf", b)])
                self.rms_tile(T4[b], b, h[b], ("h", b), ("junk", b), ("ss", b), ("rs", b), hn[b], ("hn", b))
                self.transpose8(hn[b], ("hn", b), xT[b][:, :, :], ("xT", b), ident, eng="dve")
                self.transpose8(pbf[b], ("pbf", b), pT[b][:, :, :], ("pT", b), ident, nblk=2, eng="act")
                self.run_deferred()
                self.defer(self._d2_back, l, last, ti, b, rows, (h, hn, xT, pT, Wpg, Wpp, Wpgk, Wppk, sig, tmp, T4, outt, gfin))
            self.run_deferred()
            P.end_phase()

    def _d2_back(self, l, last, ti, b, rows, tl):
        P, dr, ps = self.P, self.dr, self.ps
        h, hn, xT, pT, Wpg, Wpp, Wpgk, Wppk, sig, tmp, T4, outt, gfin = tl
        if True:
            if True:
                for half in range(2):
                    hs = slice(half * 512, (half + 1) * 512)
                    pg, pp = self.psn(), self.psn()
                    for k in range(8):
                        P.op("pe", I("matmul", ps[pg][:, :], lhsT=xT[b][:, k, :], rhs=Wpg[:, k, hs], start=(k == 0), stop=(k == 7)),
                             reads=[("xT", b)] + Wpgk, writes=[("ps", pg)])
                    for k in range(2):
                        P.op("pe", I("matmul", ps[pp][:, :], lhsT=pT[b][:, k, :], rhs=Wpp[:, k, hs], start=(k == 0), stop=(k == 1)),
                             reads=[("pT", b)] + Wppk, writes=[("ps", pp)])
                    P.op("act", I("activation", out=sig[half][:], in_=ps[pg][:, :], func=AF.Sigmoid), reads=[("ps", pg)], writes=[("sig", half)])
                    P.op("dve", I("tensor_tensor", out=tmp[half][:], in0=ps[pp][:, :], in1=sig[half][:], op=ALU.mult),
                         reads=[("ps", pp), ("sig", half)], writes=[("tmp", half)])
                    P.op("pool", I("tensor_tensor", out=h[b][:, hs], in0=h[b][:, hs], in1=tmp[half][:], op=ALU.add),
                         reads=[("tmp", half), ("h", b)], writes=[("h", b)])
                if not last:
                    P.dma("sp", I("dma_start", out=dr["hS"][rows, :], in_=h[b][:]), reads=[("h", b)], writes=[("hS", ti)])
                else:
                    self.rms_tile(T4[b], b, h[b], ("h", b), ("junk", b), ("ss", b), ("rs", b), None, None) if False else None
                    junk, ss, rs = T4[b]["junk"], T4[b]["ss"], T4[b]["rs"]
                    P.op("act", I("activation", out=junk[:], in_=h[b][:], func=AF.Square, accum_out=ss[:]), reads=[("h", b)], writes=[("junk", b), ("ss", b)])
                    P.op("dve", I("tensor_scalar", out=rs[:], in0=ss[:], scalar1=1.0 / D, scalar2=1e-6, op0=ALU.mult, op1=ALU.add),
                         reads=[("ss", b)], writes=[("rs", b)])
                    P.op("act", I("sqrt", out=rs[:], in_=rs[:]), reads=[("rs", b)], writes=[("rs", b)])
                    P.op("dve", I("reciprocal", out=rs[:], in_=rs[:]), reads=[("rs", b)], writes=[("rs", b)])
                    P.op("dve", I("scalar_tensor_tensor", out=outt[b][:], in0=h[b][:], scalar=rs[:, 0:1], in1=gfin[:], op0=ALU.mult, op1=ALU.mult),
                         reads=[("h", b), ("rs", b), "gfin"], writes=[("outt", b)])
                    P.dma("sp", I("dma_start", out=self.out[rows, :], in_=outt[b][:]), reads=[("outt", b)], writes=[("out", ti)])


def make_in_maps(inputs, cores):
    inp = {k: np.asarray(v) for k, v in inputs.items()}
    sh = _prep_shared(inp)
    maps = []
    for b in cores:
        m = dict(sh)
        m["x"] = np.ascontiguousarray(inp["x"][b])
        m["p"] = np.ascontiguousarray(inp["p"][:, b])
        m["pos"] = np.ascontiguousarray(inp["positions"][b].reshape(1, S).astype(np.int32))
        maps.append(m)
    return maps


_NC_CACHE = {}


def kernel(**inputs):
    if "nc" not in _NC_CACHE:
        _NC_CACHE["nc"] = Builder().build()
    nc = _NC_CACHE["nc"]
    maps = make_in_maps(inputs, list(range(8)))
    res = run_bass_kernel_spmd(nc, maps, core_ids=list(range(8)))
    out = np.stack([np.asarray(r["out"]) for r in res.results], axis=0)
    return out.astype(np.float32)
```

```python
import contextlib
import numpy as np
import ml_dtypes
import concourse.bass as bass
import concourse.mybir as mybir
from concourse.bass_utils import run_bass_kernel_spmd

F32 = mybir.dt.float32
BF16 = mybir.dt.bfloat16
I32 = mybir.dt.int32
AF = mybir.ActivationFunctionType
ALU = mybir.AluOpType
AX = mybir.AxisListType

S = 4096
D = 1024
L = 2
NTILE = 32
CH = 512
NCH = 8
DFF = 2816
NF = 22
BIG = 30000.0
WCOLS = 3740
COMPUTE = ("pe", "act", "dve", "pool", "sp")
import os as _os
NSA_STOP = int(_os.environ.get("NSA_STOP", "0"))
DMAQ = ("sp", "pool", "act")


def I(m, *a, **k):
    return lambda e: getattr(e, m)(*a, **k)


class Op:
    __slots__ = ("eng", "fn", "waits", "signal", "cnt", "dma_sem", "dma_val", "is_dma", "idx")

    def __init__(self, eng, fn, is_dma):
        self.eng = eng
        self.fn = fn
        self.waits = []
        self.signal = False
        self.cnt = None
        self.dma_sem = None
        self.dma_val = None
        self.is_dma = is_dma
        self.idx = None


class Prog:
    def __init__(self, nc, st, n_dma_sems=8):
        self.nc = nc
        self.lists = {e: [] for e in COMPUTE}
        self.last_w = {}
        self.readers = {}
        self.n_dma_sems = n_dma_sems
        self.dma_count = {q: 0 for q in DMAQ}
        self.csem = {e: st.enter_context(nc.semaphore("c_" + e)) for e in COMPUTE}
        self.dsem = {(q, j): st.enter_context(nc.semaphore("d_%s%d" % (q, j)))
                     for q in DMAQ for j in range(n_dma_sems)}
        self.cbase = {e: 0 for e in COMPUTE}
        self.gidx = {e: 0 for e in COMPUTE}
        self.barrier = {}
        self.dma_last = {}

    def _deps(self, reads, writes):
        deps = []
        for k in reads:
            w = self.last_w.get(k)
            if w is not None:
                deps.append(w)
        for k in writes:
            w = self.last_w.get(k)
            if w is not None:
                deps.append(w)
            deps.extend(self.readers.get(k, ()))
        return deps

    def _record(self, h, reads, writes):
        for k in reads:
            self.readers.setdefault(k, []).append(h)
        for k in writes:
            self.last_w[k] = h
            self.readers[k] = []

    def _attach(self, h, deps):
        best = {}
        for d in deps:
            if d is h or d.fn is None:
                continue
            if d.is_dma:
                key = ("d",) + d.dma_sem
                cur = best.get(key)
                if cur is None or d.dma_val > cur.dma_val:
                    best[key] = d
            else:
                if d.eng == "pe" and h.eng == "pe" and not h.is_dma:
                    continue
                cur = best.get(d.eng)
                if cur is None or d.idx > cur.idx:
                    best[d.eng] = d
        for d in best.values():
            d.signal = True
            h.waits.append(d)

    def op(self, eng, fn, reads=(), writes=(), extra=()):
        h = Op(eng, fn, False)
        self._attach(h, self._deps(reads, writes) + list(extra))
        h.idx = self.gidx[eng]
        self.gidx[eng] += 1
        self.lists[eng].append(h)
        self._record(h, reads, writes)
        return h

    def dma(self, q, fn, reads=(), writes=(), extra=()):
        h = Op(q, fn, True)
        self._attach(h, self._deps(reads, writes) + list(extra))
        i = self.dma_count[q]
        self.dma_count[q] += 1
        h.dma_sem = (q, i % self.n_dma_sems)
        h.dma_val = 16 * (i // self.n_dma_sems + 1)
        h.idx = self.gidx[q]
        self.gidx[q] += 1
        self.lists[q].append(h)
        self._record(h, reads, writes)
        self.dma_last[h.dma_sem] = h.dma_val
        return h

    def flush(self, final=False):
        nc = self.nc
        for e in COMPUTE:
            c = self.cbase[e]
            for h in self.lists[e]:
                if not h.is_dma and h.signal:
                    c += 1
                    h.cnt = c
        if final:
            pass
        barrier = dict(self.barrier)
        with nc.Block() as block:
            engs = {"pe": block.tensor, "act": block.scalar, "dve": block.vector,
                    "pool": block.gpsimd, "sp": block.sync}

            def make(ename):
                lst = self.lists[ename]

                def body(eng):
                    waited = {}
                    for key, val in barrier.items():
                        if val <= 0:
                            continue
                        sem = self.csem[key[1]] if key[0] == "c" else self.dsem[key[1:]]
                        eng.wait_ge(sem, val)
                        waited[key] = val
                    for h in lst:
                        for d in h.waits:
                            if d.is_dma:
                                key = ("d",) + d.dma_sem
                                sem = self.dsem[d.dma_sem]
                                val = d.dma_val
                            else:
                                key = ("c", d.eng)
                                sem = self.csem[d.eng]
                                val = d.cnt
                            if waited.get(key, 0) >= val:
                                continue
                            waited[key] = val
                            eng.wait_ge(sem, val)
                        if h.is_dma:
                            prev = h.dma_val - 16
                            key = ("d",) + h.dma_sem
                            if prev > 0 and waited.get(key, 0) < prev:
                                eng.wait_ge(self.dsem[h.dma_sem], prev)
                                waited[key] = prev
                            ins = h.fn(eng)
                            ins.then_inc(self.dsem[h.dma_sem], 16)
                        else:
                            ins = h.fn(eng)
                            if h.signal:
                                ins.then_inc(self.csem[ename], 1)
                    if final and ename == "sp":
                        for key, val in self._barrier_now().items():
                            if val > 0 and waited.get(key, 0) < val and key != ("c", "sp"):
                                sem = self.csem[key[1]] if key[0] == "c" else self.dsem[key[1:]]
                                eng.wait_ge(sem, val)
                return body

            for ename in ("sp", "pool", "act", "dve", "pe"):
                if self.lists[ename] or barrier or final:
                    engs[ename](make(ename))
        self.barrier = self._barrier_now()
        for e in COMPUTE:
            for h in self.lists[e]:
                h.fn = None
            self.lists[e] = []
        self.last_w = {}
        self.readers = {}

    def _barrier_now(self):
        b = {}
        for e in COMPUTE:
            c = self.cbase[e]
            for h in self.lists[e]:
                if h.cnt is not None and h.cnt > c:
                    c = h.cnt
            b[("c", e)] = c
        for k, v in self.dma_last.items():
            b[("d",) + k] = v
        return b

    def end_phase(self, final=False):
        for e in COMPUTE:
            for h in reversed(self.lists[e]):
                if not h.is_dma:
                    h.signal = True
                    break
        self.flush(final=final)
        for e in COMPUTE:
            self.cbase[e] = self.barrier[("c", e)]


def _win_cols():
    o = {}
    names = ["nq", "nkc", "nvc", "nks", "nvs", "nkw", "nvw", "ngate", "fq", "fk", "fv", "ff", "sq", "sk", "sv"]
    sizes = [512, 128, 128, 128, 128, 128, 128, 24, 256, 256, 256, 4, 256, 256, 256]
    off = 0
    for n, s in zip(names, sizes):
        o[n] = np.arange(off, off + s)
        off += s
    assert off == 2844

    def rot(c):
        c = c.reshape(-1, 2, 32)
        return c[:, ::-1, :].reshape(-1)

    ft = []
    for j in range(4):
        ft.append(np.concatenate([o["nq"][64 * j:64 * j + 64], o["nq"][64 * (4 + j):64 * (4 + j) + 64]]))
    ft += [o["nkc"], o["nks"], o["nkw"]]
    ft += [rot(c) for c in ft[:7]]
    ft.append(o["nvc"])
    for n in ("fq", "fk", "sq", "sk"):
        ft += [o[n][:128], o[n][128:]]
    cols = np.concatenate(ft + [o["ff"], o["nvs"], o["nvw"], o["fv"], o["sv"], o["ngate"]])
    assert cols.shape[0] == WCOLS
    return cols


def _consts():
    bf = ml_dtypes.bfloat16
    c = {}
    c["ident"] = np.eye(128, dtype=np.float32).astype(bf)
    s = np.arange(128)[:, None, None]
    r = np.arange(4)[None, :, None]
    t = np.arange(512)[None, None, :]
    sa = 128 * r + s
    c["m_gt"] = np.where(sa > t, -BIG, 0.0).astype(bf)
    c["m_ge"] = np.where(sa >= t, -BIG, 0.0).astype(bf)
    c["m_le"] = np.where(sa <= t, -BIG, 0.0).astype(bf)
    n = np.arange(128)[:, None, None] + 128 * np.arange(2)[None, :, None]
    tt = np.arange(S)[None, None, :]
    cm = np.where((16 * n + 31 > tt) | (n >= 255), -BIG, 0.0)
    c["m_cmp"] = cm.astype(bf)
    j = np.arange(64)[:, None, None]
    sb = np.arange(32)[None, :, None]
    ss = np.arange(128)[None, None, :]
    c["eblk"] = (np.arange(64)[:, None] == (np.arange(S)[None, :] // 64)).astype(np.float32).astype(bf)
    nn = np.arange(256)
    cs = nn[:, None] * 16
    bs = np.arange(64)[None, :] * 64
    ov = ((cs < bs + 64) & (cs + 32 > bs) & (nn[:, None] < 255)).astype(np.float32)
    c["ovl"] = ov.reshape(2, 128, 64).transpose(1, 0, 2).astype(bf).copy()
    tq = np.arange(S)[:, None]
    jb = np.arange(64)[None, :]
    cur = tq // 64
    forced = (jb == 0) | (jb == cur) | (jb == cur - 1)
    valid = jb <= cur
    c["selbias"] = np.where(forced, 1e9, np.where(valid, 0.0, -1e9)).astype(np.float32)
    half = 32
    invf = (10000.0 ** (-np.arange(half, dtype=np.float32) / half)).astype(np.float32)
    rr = np.arange(128)
    c["invf"] = invf[rr % 32].reshape(128, 1).astype(np.float32)
    c["sgn"] = np.where((rr % 64) < 32, -1.0, 1.0).reshape(128, 1).astype(np.float32)
    jj = np.arange(128)
    c["ntri"] = np.where(jj[:, None] >= jj[None, :], -1.0, 0.0).astype(np.float32).astype(bf)
    c["nones"] = np.full((1, 128), -1.0, np.float32).astype(bf)
    c["onec"] = np.ones((128, 1), np.float32).astype(bf)
    return c


def _col8(v):
    return np.ascontiguousarray(v.reshape(8, 128).T)


def _prep_shared(inp):
    sh = {}
    cols = _win_cols()
    sh["w_in"] = np.ascontiguousarray(inp["w_in"][:, :, cols])
    for n in ("norm_mix", "norm_ffn", "norm_ple", "head_norm"):
        sh[n] = np.stack([_col8(inp[n][l]) for l in range(L)])
    sh["norm_final"] = inp["norm_final"].reshape(1, D)
    sh["b_gate"] = inp["b_nsa_gate"].reshape(L, 1, 24)
    sh["b_forget"] = inp["b_forget"].reshape(L, 4, 1)
    for kv in ("k", "v"):
        w1 = inp["nsa_cmp_w1_" + kv].reshape(L, 32, 64, 128).transpose(0, 2, 1, 3)
        sh["w1_" + kv] = np.ascontiguousarray(np.concatenate([w1, w1], axis=1).reshape(L, 128, 32 * 128))
        pt = inp["nsa_cmp_pos_" + kv].transpose(0, 2, 1)
        sh["pos_" + kv] = np.ascontiguousarray(np.concatenate([pt, pt], axis=1))
        sh["w2_" + kv] = inp["nsa_cmp_w2_" + kv]
    for n in ("w_out", "w_ffn_gate", "w_ffn_up", "w_ffn_down", "w_ple_proj", "w_ple_gate"):
        sh[n] = inp[n]
    sh.update(_consts())
    return sh


class Builder:
    def __init__(self, debug=(), nlayers=L, phases=None):
        self.debug = set(debug)
        self.nlayers = nlayers
        self.phases = phases
        self.nc = bass.Bass("TRN2", target_bir_lowering=False)
        self.dr = {}

    def din(self, name, shape, dt):
        self.dr[name] = self.nc.dram_tensor(name, list(shape), dt, kind="ExternalInput").ap()
        return self.dr[name]

    def dscr(self, name, shape, dt):
        kind = "ExternalOutput" if name in self.debug else "Internal"
        self.dr[name] = self.nc.dram_tensor(name, list(shape), dt, kind=kind).ap()
        return self.dr[name]

    def want(self, ph):
        return self.phases is None or ph in self.phases

    def build(self):
        nc = self.nc
        din, dscr = self.din, self.dscr
        din("x", [S, D], F32)
        din("p", [L, S, 256], F32)
        din("pos", [1, S], I32)
        din("w_in", [L, D, WCOLS], F32)
        for n in ("norm_mix", "norm_ffn", "norm_ple", "head_norm"):
            din(n, [L, 128, 8], F32)
        din("norm_final", [1, D], F32)
        din("b_gate", [L, 1, 24], F32)
        din("b_forget", [L, 4, 1], F32)
        for kv in ("k", "v"):
            din("w1_" + kv, [L, 128, 4096], F32)
            din("pos_" + kv, [L, 128, 32], F32)
            din("w2_" + kv, [L, 128, 64], F32)
        din("w_out", [L, D, D], F32)
        din("w_ffn_gate", [L, D, DFF], F32)
        din("w_ffn_up", [L, D, DFF], F32)
        din("w_ffn_down", [L, DFF, D], F32)
        din("w_ple_proj", [L, 256, D], F32)
        din("w_ple_gate", [L, D, D], F32)
        din("ident", [128, 128], BF16)
        for n in ("m_gt", "m_ge", "m_le"):
            din(n, [128, 4, 512], BF16)
        din("m_cmp", [128, 2, S], BF16)
        din("eblk", [64, S], BF16)
        din("ovl", [128, 2, 64], BF16)
        din("selbias", [S, 64], F32)
        din("invf", [128, 1], F32)
        din("sgn", [128, 1], F32)
        din("ntri", [128, 128], BF16)
        din("nones", [1, 128], BF16)
        din("onec", [128, 1], BF16)
        self.out = nc.dram_tensor("out", [S, D], F32, kind="ExternalOutput").ap()
        dscr("hS", [S, D], F32)
        dscr("cosS", [128, S], F32)
        dscr("sinS", [128, S], F32)
        dscr("FT", [16, 128, S], BF16)
        dscr("cT", [4, S], F32)
        dscr("VA", [S, 8, 65], BF16)
        dscr("SV", [S, 256], BF16)
        dscr("GT", [S, 24], F32)
        dscr("kcT", [128, 256], BF16)
        dscr("VC", [128, 2, 2, 65], BF16)
        dscr("oS", [S, D], F32)
        dscr("WGU", [NF, 128, 2, 8, 128], BF16)
        if "hmix" in self.debug:
            dscr("hmix", [S, D], F32)

        with contextlib.ExitStack() as st:
            self.P = Prog(nc, st)
            self.ps = [st.enter_context(nc.psum_tensor("ps%d" % i, [128, 512], F32)) for i in range(8)]
            self.pt = self.ps[7][:, :].bitcast(BF16)
            self.ps_i = 0
            if self.want("T"):
                self.phase_tables()
            for l in range(self.nlayers):
                if self.want("A"):
                    self.phase_A(l)
                if self.want("B"):
                    self.phase_B(l)
                if self.want("N"):
                    self.phase_nsa(l)
                if self.want("F"):
                    self.phase_fox(l)
                if self.want("SB"):
                    self.phase_sb(l)
                if self.want("D"):
                    self.phase_D(l, last=(l == self.nlayers - 1))
            self.P.op("sp", I("nop"))
            self.P.end_phase(final=True)
        return nc

    def defer(self, fn, *a):
        if not hasattr(self, "_pend"):
            self._pend = []
        self._pend.append((fn, a))

    def run_deferred(self):
        pend = getattr(self, "_pend", [])
        self._pend = []
        for fn, a in pend:
            fn(*a)

    def uname(self, n):
        self._un = getattr(self, "_un", 0) + 1
        return "%s_u%d" % (n, self._un)

    def psn(self):
        i = self.ps_i
        self.ps_i = (i + 1) % 7
        return i

    def phase_tables(self):
        nc, P, dr = self.nc, self.P, self.dr
        with contextlib.ExitStack() as st:
            T = lambda n, sh, dt: st.enter_context(nc.sbuf_tensor(self.uname(n), sh, dt))
            posi = T("t_posi", [128, S], I32)
            ang = T("t_ang", [128, S], F32)
            kk = T("t_kk", [128, S], F32)
            rr = T("t_r", [128, S], F32)
            oo = T("t_o", [128, S], F32)
            invf = T("t_invf", [128, 1], F32)
            sgn = T("t_sgn", [128, 1], F32)
            hpi = T("t_hpi", [128, 1], F32)
            P.dma("sp", I("dma_start", out=posi[:], in_=dr["pos"].to_broadcast([128, S])), writes=["posi"])
            P.dma("sp", I("dma_start", out=invf[:], in_=dr["invf"]), writes=["invf"])
            P.dma("sp", I("dma_start", out=sgn[:], in_=dr["sgn"]), writes=["sgn"])
            P.op("pool", I("memset", hpi[:], float(np.pi / 2)), writes=["hpi"])
            P.op("dve", I("tensor_copy", out=ang[:], in_=posi[:]), reads=["posi"], writes=["ang"])
            P.op("dve", I("tensor_scalar", out=ang[:], in0=ang[:], scalar1=invf[:, 0:1], scalar2=None, op0=ALU.mult),
                 reads=["ang", "invf"], writes=["ang"])
            MAGIC = 12582912.0
            P.op("dve", I("tensor_scalar", out=kk[:], in0=ang[:], scalar1=float(1.0 / (2 * np.pi)), scalar2=MAGIC,
                                                   op0=ALU.mult, op1=ALU.add), reads=["ang"], writes=["kk"])
            P.op("dve", I("tensor_scalar", out=kk[:], in0=kk[:], scalar1=-MAGIC, scalar2=None, op0=ALU.add),
                 reads=["kk"], writes=["kk"])
            C1 = 6.28125
            C2 = float(np.float32(2 * np.pi - C1))
            P.op("dve", I("scalar_tensor_tensor", out=rr[:], in0=kk[:], scalar=-C1, in1=ang[:], op0=ALU.mult, op1=ALU.add),
                 reads=["kk", "ang"], writes=["rr"])
            P.op("dve", I("scalar_tensor_tensor", out=rr[:], in0=kk[:], scalar=-C2, in1=rr[:], op0=ALU.mult, op1=ALU.add),
                 reads=["kk", "rr"], writes=["rr"])
            PL = 3.1415925
            P.op("dve", I("tensor_scalar", out=rr[:], in0=rr[:], scalar1=-PL, scalar2=PL, op0=ALU.max, op1=ALU.min),
                 reads=["rr"], writes=["rr"])
            P.op("act", I("activation", out=oo[:], in_=rr[:], func=AF.Sin), reads=["rr"], writes=["oo"])
            P.op("dve", I("tensor_scalar", out=oo[:], in0=oo[:], scalar1=sgn[:, 0:1], scalar2=None, op0=ALU.mult),
                 reads=["oo", "sgn"], writes=["oo"])
            P.dma("sp", I("dma_start", out=dr["sinS"], in_=oo[:]), reads=["oo"], writes=["sinS"])
            P.op("dve", I("scalar_tensor_tensor", out=kk[:], in0=rr[:], scalar=-1.0, in1=rr[:], op0=ALU.mult, op1=ALU.max),
                 reads=["rr"], writes=["kk"])
            P.op("act", I("activation", out=ang[:], in_=kk[:], func=AF.Sin, bias=hpi[:, 0:1], scale=-1.0),
                 reads=["kk", "hpi", "ang"], writes=["ang"])
            P.dma("sp", I("dma_start", out=dr["cosS"], in_=ang[:]), reads=["ang"], writes=["cosS"])
            P.end_phase()

    def rms_tile(self, T4, i, hx, hxk, jk, ssk, rsk, hn, hnk):
        P = self.P
        junk, ss, rs = T4["junk"], T4["ss"], T4["rs"]
        P.op("act", I("activation", out=junk[:], in_=hx[:], func=AF.Square, accum_out=ss[:]),
             reads=[hxk], writes=[jk, ssk])
        P.op("dve", I("tensor_scalar", out=rs[:], in0=ss[:], scalar1=1.0 / D, scalar2=1e-6, op0=ALU.mult, op1=ALU.add),
             reads=[ssk], writes=[rsk])
        P.op("act", I("sqrt", out=rs[:], in_=rs[:]), reads=[rsk], writes=[rsk])
        P.op("dve", I("reciprocal", out=rs[:], in_=rs[:]), reads=[rsk], writes=[rsk])
        P.op("dve", I("tensor_scalar", out=hn[:], in0=hx[:], scalar1=rs[:, 0:1], scalar2=None, op0=ALU.mult),
             reads=[hxk, rsk], writes=[hnk])

    def transpose8(self, src, srck, dst_ap, dstk, ident, nblk=8, eng="dve"):
        P = self.P
        pt = self.pt
        for c in range(nblk):
            P.op("pe", I("transpose", out=pt[:, c * 128:(c + 1) * 128], in_=src[:, c * 128:(c + 1) * 128],
                                                  identity=ident[:]), reads=[srck, "ident"], writes=[("ps", 7)])
        view = pt[:, 0:nblk * 128].rearrange("p (c t) -> p c t", t=128)
        if eng == "dve":
            P.op("dve", I("tensor_copy", out=dst_ap, in_=view), reads=[("ps", 7)], writes=[dstk])
        else:
            P.op("act", I("copy", out=dst_ap, in_=view), reads=[("ps", 7)], writes=[dstk])

    def load_weight_bf(self, dst_ap, src_ap, stage, stagek, dstk, scale_ap=None, scalek=None, eng="dve", q="sp"):
        P = self.P
        P.dma(q, I("dma_start", out=stage, in_=src_ap), writes=[stagek])
        en = "dve" if eng == "dve" else "pool"
        if scale_ap is not None:
            P.op(en, I("tensor_scalar", out=dst_ap, in0=stage, scalar1=scale_ap, scalar2=None, op0=ALU.mult),
                 reads=[stagek, scalek], writes=[dstk])
        else:
            P.op(en, I("tensor_copy", out=dst_ap, in_=stage), reads=[stagek], writes=[dstk])

    def phase_A(self, l):
        nc, P, dr = self.nc, self.P, self.dr
        hsrc = dr["x"] if l == 0 else dr["hS"]
        with contextlib.ExitStack() as st:
            T = lambda n, sh, dt: st.enter_context(nc.sbuf_tensor(self.uname(n), sh, dt))
            W = T("a_W", [128, 8, WCOLS], BF16)
            stage = [T("a_stage%d" % i, [128, WCOLS], F32) for i in range(2)]
            cosT = T("a_cos", [128, S], F32)
            sinT = T("a_sin", [128, S], F32)
            gcol = T("a_gcol", [128, 8], F32)
            ident = T("a_ident", [128, 128], BF16)
            bgate = T("a_bgate", [128, 24], F32)
            negb = T("a_negb", [4, 1], F32)
            ones4 = T("a_ones4", [4, CH], F32)
            cc = T("a_cc", [4, S], F32)
            hx = [T("a_hx%d" % i, [128, D], F32) for i in range(2)]
            T4 = [dict(junk=T("a_junk%d" % i, [128, D], BF16), ss=T("a_ss%d" % i, [128, 1], F32),
                       rs=T("a_rs%d" % i, [128, 1], F32)) for i in range(2)]
            hn = [T("a_hn%d" % i, [128, D], BF16) for i in range(2)]
            hnT = [T("a_hnT%d" % i, [128, 8, CH], BF16) for i in range(2)]
            t1 = [T("a_t1%d" % i, [128, CH], F32) for i in range(2)]
            t2 = [T("a_t2%d" % i, [128, CH], F32) for i in range(2)]
            ob = [T("a_ob%d" % i, [128, CH], BF16) for i in range(4)]
            va = [T("a_va%d" % i, [128, 8, 65], BF16) for i in range(2)]
            svt = [T("a_sv%d" % i, [128, 256], BF16) for i in range(2)]
            gt = [T("a_gt%d" % i, [128, 24], F32) for i in range(2)]
            e4 = T("a_e4", [4, CH], F32)
            sp4 = T("a_sp4", [4, CH], F32)

            P.dma("sp", I("dma_start", out=gcol[:], in_=dr["norm_mix"][l]), writes=["gcol"])
            P.dma("sp", I("dma_start", out=ident[:], in_=dr["ident"]), writes=["ident"])
            P.dma("sp", I("dma_start", out=cosT[:], in_=dr["cosS"]), writes=["cosT"])
            P.dma("sp", I("dma_start", out=sinT[:], in_=dr["sinS"]), writes=["sinT"])
            P.dma("sp", I("dma_start", out=bgate[:], in_=dr["b_gate"][l].to_broadcast([128, 24])), writes=["bgate"])
            P.dma("sp", I("dma_start", out=negb[:], in_=dr["b_forget"][l]), writes=["negb"])
            P.op("dve", I("tensor_scalar", out=negb[:], in0=negb[:], scalar1=-1.0, scalar2=None, op0=ALU.mult),
                 reads=["negb"], writes=["negb"])
            P.op("pool", I("memset", ones4[:], 1.0), writes=["ones4"])
            for i in range(2):
                P.op("pool", I("memset", va[i][:], 1.0), writes=[("va", i)])
            for k in range(8):
                self.load_weight_bf(W[:, k, :], dr["w_in"][l, k * 128:(k + 1) * 128, :], stage[k % 2][:], ("stage", k % 2),
                                    ("W", k), scale_ap=gcol[:, k:k + 1], scalek="gcol", eng=("dve" if k % 2 == 0 else "pool"),
                                    q=("sp" if k % 2 == 0 else "act"))
            Wk = [("W", k) for k in range(8)]
            cn_a = {"obi": 0}
            for c in range(NCH):
                hb = c % 2
                csl = slice(c * CH, (c + 1) * CH)
                for i in range(4):
                    ti = c * 4 + i
                    b = ti % 2
                    P.dma("sp", I("dma_start", out=hx[b][:], in_=hsrc[ti * 128:(ti + 1) * 128, :]),
                          writes=[("hx", b)])
                    self.rms_tile(T4[b], b, hx[b], ("hx", b), ("junk", b), ("ss", b), ("rs", b), hn[b], ("hn", b))
                    self.transpose8(hn[b], ("hn", b), hnT[hb][:, :, i * 128:(i + 1) * 128], ("hnT", hb, i), ident,
                                    eng=("dve" if i % 2 == 0 else "act"))
                hk = [("hnT", hb, i) for i in range(4)]

                def back(c, hb, csl, hk):

                    def fm_matmul(pi, col0, ncols=128):
                        for k in range(8):
                            P.op("pe", I("matmul", self.ps[pi][0:ncols, :], lhsT=W[:, k, col0:col0 + ncols],
                                                              rhs=hnT[hb][:, k, :], start=(k == 0), stop=(k == 7)),
                                 reads=hk + Wk, writes=[("ps", pi)])
                    for ft in range(7):
                        pa, pb = self.psn(), self.psn()
                        fm_matmul(pa, ft * 128)
                        fm_matmul(pb, (7 + ft) * 128)
                        tb = ft % 2
                        P.op("dve", I("tensor_tensor", out=t1[tb][:], in0=self.ps[pa][:], in1=cosT[:, csl], op=ALU.mult),
                             reads=[("ps", pa), "cosT"], writes=[("t1", tb)])
                        P.op("dve", I("tensor_tensor", out=t2[tb][:], in0=self.ps[pb][:], in1=sinT[:, csl], op=ALU.mult),
                             reads=[("ps", pb), "sinT"], writes=[("t2", tb)])
                        o = cn_a["obi"] % 4
                        cn_a["obi"] += 1
                        P.op("pool", I("tensor_tensor", out=ob[o][:], in0=t1[tb][:], in1=t2[tb][:], op=ALU.add),
                             reads=[("t1", tb), ("t2", tb)], writes=[("ob", o)])
                        P.dma("sp", I("dma_start", out=dr["FT"][ft, :, csl], in_=ob[o][:]),
                              reads=[("ob", o)], writes=[("FT", ft, c)])
                    for ft in range(14, 23):
                        pa = self.psn()
                        fm_matmul(pa, ft * 128)
                        o = cn_a["obi"] % 4
                        cn_a["obi"] += 1
                        sc = 0.125 if ft in (15, 16, 19, 20) else 1.0
                        P.op("act", I("activation", out=ob[o][:], in_=self.ps[pa][:], func=AF.Copy, scale=sc),
                             reads=[("ps", pa)], writes=[("ob", o)])
                        P.dma("sp", I("dma_start", out=dr["FT"][ft - 7, :, csl], in_=ob[o][:]),
                              reads=[("ob", o)], writes=[("FT", ft, c)])
                    pa = self.psn()
                    fm_matmul(pa, 23 * 128, ncols=4)
                    P.op("act", I("activation", out=e4[:], in_=self.ps[pa][0:4, :], func=AF.Exp, bias=negb[:, 0:1], scale=-1.0),
                         reads=[("ps", pa), "negb"], writes=["e4"])
                    P.op("act", I("activation", out=sp4[:], in_=e4[:], func=AF.Ln, bias=1.0, scale=1.0), reads=["e4"], writes=["sp4"])
                    if c == 0:
                        P.op("dve", I("tensor_tensor_scan", out=cc[:, csl], data0=ones4[:], data1=sp4[:], initial=0.0,
                                                                    op0=ALU.mult, op1=ALU.subtract), reads=["sp4", "ones4"], writes=["cc"])
                    else:
                        P.op("dve", I("tensor_tensor_scan", out=cc[:, csl], data0=ones4[:], data1=sp4[:],
                                                                         initial=cc[:, c * CH - 1:c * CH],
                                                                         op0=ALU.mult, op1=ALU.subtract), reads=["sp4", "ones4", "cc"], writes=["cc"])
                    c0 = 23 * 128 + 4
                    for i in range(4):
                        ti = c * 4 + i
                        b = ti % 2
                        pa, pb = self.psn(), self.psn()
                        for k in range(8):
                            P.op("pe", I("matmul", self.ps[pa][:, 0:512], lhsT=hnT[hb][:, k, i * 128:(i + 1) * 128],
                                                                        rhs=W[:, k, c0:c0 + 512], start=(k == 0), stop=(k == 7)),
                                 reads=hk + Wk, writes=[("ps", pa)])
                        for k in range(8):
                            P.op("pe", I("matmul", self.ps[pb][:, 0:280], lhsT=hnT[hb][:, k, i * 128:(i + 1) * 128],
                                                                        rhs=W[:, k, c0 + 512:c0 + 792], start=(k == 0), stop=(k == 7)),
                                 reads=hk + Wk, writes=[("ps", pb)])
                        P.op("act", I("copy", out=va[b][:, :, 0:64], in_=self.ps[pa][:, 0:512].rearrange("p (g d) -> p g d", d=64)),
                             reads=[("ps", pa)], writes=[("va", b)])
                        P.op("dve", I("tensor_copy", out=svt[b][:], in_=self.ps[pb][:, 0:256]),
                             reads=[("ps", pb)], writes=[("svt", b)])
                        P.op("dve", I("tensor_tensor", out=gt[b][:], in0=self.ps[pb][:, 256:280], in1=bgate[:], op=ALU.add),
                             reads=[("ps", pb), "bgate"], writes=[("gt", b)])
                        P.op("act", I("activation", out=gt[b][:], in_=gt[b][:], func=AF.Sigmoid), reads=[("gt", b)], writes=[("gt", b)])
                        rows = slice(ti * 128, (ti + 1) * 128)
                        P.dma("sp", I("dma_start", out=dr["VA"][rows], in_=va[b][:]), reads=[("va", b)], writes=[("VA", ti)])
                        P.dma("sp", I("dma_start", out=dr["SV"][rows], in_=svt[b][:]), reads=[("svt", b)], writes=[("SV", ti)])
                        P.dma("sp", I("dma_start", out=dr["GT"][rows], in_=gt[b][:]), reads=[("gt", b)], writes=[("GT", ti)])

                self.run_deferred()
                self.defer(back, c, hb, csl, hk)
            self.run_deferred()
            P.dma("sp", I("dma_start", out=dr["cT"], in_=cc[:]), reads=["cc"], writes=["cT"])
            P.end_phase()

    def phase_B(self, l):
        nc, P, dr = self.nc, self.P, self.dr
        with contextlib.ExitStack() as st:
            T = lambda n, sh, dt: st.enter_context(nc.sbuf_tensor(self.uname(n), sh, dt))
            kvT = {"k": T("b_kT", [128, S], BF16), "v": T("b_vT", [128, S], BF16)}
            stage = T("b_stage", [128, 4096], F32)
            W1 = {kv: T("b_w1" + kv, [128, 32, 128], BF16) for kv in "kv"}
            posT = {kv: T("b_pos" + kv, [128, 32], BF16) for kv in "kv"}
            W2 = {kv: T("b_w2" + kv, [128, 64], BF16) for kv in "kv"}
            st32 = T("b_st32", [128, 32], F32)
            st64 = T("b_st64", [128, 64], F32)
            bias = T("b_bias", [128, 1], F32)
            xs = T("b_xs", [128, 255], F32)
            x2 = T("b_x2", [128, 255], F32)
            sg = T("b_sg", [128, 255], F32)
            gl = T("b_gl", [128, 256], BF16)
            kc = T("b_kc", [128, 256], BF16)
            vc = T("b_vc", [128, 2, 2, 65], BF16)
            P.dma("sp", I("dma_start", out=kvT["k"][:], in_=dr["FT"][4]), writes=["kT"])
            P.dma("sp", I("dma_start", out=kvT["v"][:], in_=dr["FT"][7]), writes=["vT"])
            P.op("pool", I("memset", vc[:], 1.0), writes=["vc"])
            P.op("pool", I("memset", gl[:], 0.0), writes=["gl"])
            for kv in "kv":
                self.load_weight_bf(W1[kv][:].rearrange("p l h -> p (l h)"), dr["w1_" + kv][l], stage[:], "stage", "W1" + kv)
                self.load_weight_bf(posT[kv][:], dr["pos_" + kv][l], st32[:], "st32", "pos" + kv)
                self.load_weight_bf(W2[kv][:], dr["w2_" + kv][l], st64[:], "st64", "W2" + kv)
            for kv in "kv":
                pb = self.psn()
                for ll in range(32):
                    P.op("pe", I("matmul", self.ps[pb][:, 0:1], lhsT=W1[kv][0:64, ll, :], rhs=posT[kv][0:64, ll:ll + 1],
                                                                start=(ll == 0), stop=(ll == 31)),
                         reads=["W1" + kv, "pos" + kv], writes=[("ps", pb)])
                P.op("dve", I("tensor_copy", out=bias[:], in_=self.ps[pb][:, 0:1]), reads=[("ps", pb)], writes=["bias"])
                for g in range(2):
                    gs = slice(64 * g, 64 * g + 64)
                    pa = self.psn()
                    for ll in range(32):
                        P.op("pe", I("matmul",
                            self.ps[pa][:, 0:255], lhsT=W1[kv][gs, ll, :], rhs=kvT[kv][gs, ll:ll + 16 * 254 + 1:16],
                            start=(ll == 0), stop=(ll == 31)), reads=["W1" + kv, kv + "T"], writes=[("ps", pa)])
                    P.op("act", I("activation", out=xs[:], in_=self.ps[pa][:, 0:255], func=AF.Identity, bias=bias[:, 0:1], scale=1.0),
                         reads=[("ps", pa), "bias"], writes=["xs"])
                    P.op("dve", I("tensor_tensor", out=x2[:], in0=xs[:], in1=xs[:], op=ALU.mult), reads=["xs"], writes=["x2"])
                    P.op("dve", I("tensor_scalar", out=x2[:], in0=x2[:], scalar1=0.044715, scalar2=1.0, op0=ALU.mult, op1=ALU.add),
                         reads=["x2"], writes=["x2"])
                    P.op("dve", I("tensor_tensor", out=x2[:], in0=x2[:], in1=xs[:], op=ALU.mult), reads=["x2", "xs"], writes=["x2"])
                    P.op("act", I("activation", out=sg[:], in_=x2[:], func=AF.Sigmoid, scale=1.5957691216057308),
                         reads=["x2"], writes=["sg"])
                    P.op("dve", I("tensor_tensor", out=gl[:, 0:255], in0=xs[:], in1=sg[:], op=ALU.mult), reads=["xs", "sg"], writes=["gl"])
                    if kv == "k":
                        po = self.psn()
                        P.op("pe", I("matmul", self.ps[po][gs, 0:256], lhsT=W2["k"][:, :], rhs=gl[:, :], start=True, stop=True),
                             reads=["W2k", "gl"], writes=[("ps", po)])
                        P.op("dve", I("tensor_copy", out=kc[gs, :], in_=self.ps[po][gs, 0:256]),
                             reads=[("ps", po)], writes=[("kc", g)])
                    else:
                        for nb in range(2):
                            po = self.psn()
                            P.op("pe", I("matmul", self.ps[po][:, 0:64], lhsT=gl[:, nb * 128:(nb + 1) * 128], rhs=W2["v"][:, :],
                                                                     start=True, stop=True), reads=["W2v", "gl"], writes=[("ps", po)])
                            P.op("dve", I("tensor_copy", out=vc[:, g, nb, 0:64], in_=self.ps[po][:, 0:64]),
                                 reads=[("ps", po)], writes=["vc"])
            P.dma("sp", I("dma_start", out=dr["kcT"], in_=kc[:]), reads=[("kc", 0), ("kc", 1)], writes=["kcT"])
            P.dma("sp", I("dma_start", out=dr["VC"], in_=vc[:]), reads=["vc"], writes=["VC"])
            P.end_phase()

    def phase_fox(self, l):
        nc, P, dr, ps = self.nc, self.P, self.dr, self.ps
        if "cH" not in dr:
            self.dscr("cH", [4, 3, S], BF16)
        with contextlib.ExitStack() as st:
            T = lambda n, sh, dt: st.enter_context(nc.sbuf_tensor(self.uname(n), sh, dt))
            QaT = T("f_QaT", [70, 4, S], BF16)
            KaT = T("f_KaT", [70, 4, S], BF16)
            V = T("f_V", [128, 32, 4, 65], BF16)
            c4 = T("f_c4", [4, S], F32)
            rr = T("f_rr", [4, S], F32)
            H = T("f_H", [4, 3, S], BF16)
            mgt = T("f_mgt", [128, 4, 512], BF16)
            ident = T("f_ident", [128, 128], BF16)
            Pt = [T("f_P%d" % i, [128, CH], BF16) for i in range(3)]
            rs = [T("f_rs%d" % i, [128, 1], F32) for i in range(4)]
            of = [T("f_of%d" % i, [128, 4, 64], F32) for i in range(8)]
            P.dma("sp", I("dma_start", out=mgt[:], in_=dr["m_gt"]), writes=["mgt"])
            P.dma("sp", I("dma_start", out=ident[:], in_=dr["ident"]), writes=["ident"])
            P.dma("sp", I("dma_start", out=c4[:], in_=dr["cT"]), writes=["c4"])
            for q8 in range(8):
                P.dma("sp", I("dma_start", out=V[:, q8 * 4:(q8 + 1) * 4, :, :],
                              in_=dr["VA"][q8 * 512:(q8 + 1) * 512, 4:8, :].rearrange("(sb p) h c -> p sb h c", p=128)), writes=["V"])
            P.op("pool", I("memset", QaT[64:70, :, :], -1.0), writes=["Qaug"])
            P.op("pool", I("memset", KaT[64:70, :, :], 1.0), writes=["Kaug"])
            for hh in range(4):
                src_q = dr["FT"][8 + hh // 2, (hh % 2) * 64:(hh % 2) * 64 + 64, :]
                src_k = dr["FT"][10 + hh // 2, (hh % 2) * 64:(hh % 2) * 64 + 64, :]
                P.dma("sp", I("dma_start", out=QaT[0:64, hh, :], in_=src_q), writes=[("Q", hh)])
                P.dma("act", I("dma_start", out=KaT[0:64, hh, :], in_=src_k), writes=[("K", hh)])
            P.op("dve", I("tensor_copy", out=H[:, 0, :], in_=c4[:]), reads=["c4"], writes=["H0"])
            P.op("dve", I("tensor_tensor", out=rr[:], in0=c4[:], in1=H[:, 0, :], op=ALU.subtract), reads=["c4", "H0"], writes=["rr"])
            P.op("dve", I("tensor_copy", out=H[:, 1, :], in_=rr[:]), reads=["rr"], writes=["H1"])
            P.op("dve", I("tensor_tensor", out=rr[:], in0=rr[:], in1=H[:, 1, :], op=ALU.subtract), reads=["rr", "H1"], writes=["rr"])
            P.op("dve", I("tensor_copy", out=H[:, 2, :], in_=rr[:]), reads=["rr"], writes=["H2"])
            P.dma("sp", I("dma_start", out=dr["cH"], in_=H[:]), reads=["H0", "H1", "H2"], writes=["cH"])
            for hh in range(4):
                P.dma("sp", I("dma_start", out=QaT[64:67, hh, :], in_=dr["cH"][hh]), reads=["cH", "Qaug"], writes=[("Qa", hh)])
                P.dma("sp", I("dma_start", out=KaT[67:70, hh, :], in_=dr["cH"][hh]), reads=["cH", "Kaug"], writes=[("Ka", hh)])
            sti = 0
            pi = 0
            for qc in range(NCH):
                csl = slice(qc * CH, (qc + 1) * CH)
                for hh in range(4):
                    qk = [("Q", hh), ("Qa", hh), ("K", hh), ("Ka", hh), "Qaug", "Kaug"]
                    for sb in range(4 * qc + 4):
                        diag = sb >= 4 * qc
                        b = sti % 3
                        sti += 1
                        P.op("pe", I("matmul", ps[b][:, :], lhsT=KaT[0:70, hh, sb * 128:(sb + 1) * 128], rhs=QaT[0:70, hh, csl],
                                     start=True, stop=not diag), reads=qk, writes=[("ps", b)])
                        if diag:
                            rq = sb - 4 * qc
                            P.op("pe", I("matmul", ps[b][:, rq * 128:(rq + 1) * 128], lhsT=ident[:, :], rhs=mgt[:, rq, rq * 128:(rq + 1) * 128],
                                         start=False, stop=True), reads=["ident", "mgt"], writes=[("ps", b)])
                        pb = pi % 3
                        pi += 1
                        P.op("act", I("activation", out=Pt[pb][:], in_=ps[b][:, :], func=AF.Exp), reads=[("ps", b)], writes=[("P", pb)])
                        def back(sb, pb, hh, qc):
                            for ti in range(4):
                                if sb <= 4 * qc + ti:
                                    P.op("pe", I("matmul", ps[3 + ti][:, 0:65], lhsT=Pt[pb][:, ti * 128:(ti + 1) * 128], rhs=V[:, sb, hh, :],
                                                 start=(sb == 0), stop=(sb == 4 * qc + ti)), reads=[("P", pb), "V"], writes=[("ps", 3 + ti)])
                        self.run_deferred()
                        self.defer(back, sb, pb, hh, qc)

                    def fin(hh, qc):
                        for ti in range(4):
                            P.op("dve", I("tensor_scalar", out=rs[ti][:], in0=ps[3 + ti][:, 64:65], scalar1=1e-20, scalar2=None, op0=ALU.max),
                                 reads=[("ps", 3 + ti)], writes=[("rs", ti)])
                        for ti in range(4):
                            P.op("dve", I("reciprocal", out=rs[ti][:], in_=rs[ti][:]), reads=[("rs", ti)], writes=[("rs", ti)])
                        for ti in range(4):
                            o = (qc % 2) * 4 + ti
                            P.op("dve", I("tensor_scalar", out=of[o][:, hh, :], in0=ps[3 + ti][:, 0:64], scalar1=rs[ti][:, 0:1], scalar2=None, op0=ALU.mult),
                                 reads=[("ps", 3 + ti), ("rs", ti)], writes=[("of", o, hh)])
                    self.defer(fin, hh, qc)

                def store(qc):
                    for ti in range(4):
                        o = (qc % 2) * 4 + ti
                        rows = slice((qc * 4 + ti) * 128, (qc * 4 + ti + 1) * 128)
                        P.dma("sp", I("dma_start", out=dr["oS"][rows, 512:768], in_=of[o][:].rearrange("p h d -> p (h d)")),
                              reads=[("of", o, hh) for hh in range(4)], writes=[("oS", "f", qc, ti)])
                self.defer(store, qc)
            self.run_deferred()
            P.end_phase()

    def phase_sb(self, l):
        nc, P, dr, ps = self.nc, self.P, self.dr, self.ps
        with contextlib.ExitStack() as st:
            T = lambda n, sh, dt: st.enter_context(nc.sbuf_tensor(self.uname(n), sh, dt))
            QT = T("s_QT", [128, 2, S], BF16)
            KT = T("s_KT", [128, 2, S], BF16)
            V = T("s_V", [128, 32, 256], BF16)
            mge = T("s_mge", [128, 4, 512], BF16)
            ident = T("s_ident", [128, 128], BF16)
            ntri = T("s_ntri", [128, 128], BF16)
            nones = T("s_nones", [1, 128], BF16)
            onec = T("s_onec", [128, 1], BF16)
            et = [T("s_e%d" % i, [128, CH], F32) for i in range(2)]
            SP = [T("s_SP%d" % i, [128, CH], BF16) for i in range(3)]
            at = [T("s_a%d" % i, [128, CH], BF16) for i in range(2)]
            carry = T("s_carry", [1, CH], F32)
            ctmp = T("s_ctmp", [1, CH], F32)
            chi = [T("s_chi%d" % i, [1, CH], BF16) for i in range(3)]
            clo = [T("s_clo%d" % i, [1, CH], BF16) for i in range(3)]
            osb = [T("s_o%d" % i, [128, 4, 64], F32) for i in range(8)]
            for n, t_ in (("m_ge", mge), ("ident", ident), ("ntri", ntri), ("nones", nones), ("onec", onec)):
                P.dma("sp", I("dma_start", out=t_[:], in_=dr[n]), writes=[n])
            for j in range(2):
                P.dma("sp", I("dma_start", out=QT[:, j, :], in_=dr["FT"][12 + j]), writes=[("Q", j)])
                P.dma("act", I("dma_start", out=KT[:, j, :], in_=dr["FT"][14 + j]), writes=[("K", j)])
            for q8 in range(8):
                P.dma("sp", I("dma_start", out=V[:, q8 * 4:(q8 + 1) * 4, :],
                              in_=dr["SV"][q8 * 512:(q8 + 1) * 512, :].rearrange("(sb p) c -> p sb c", p=128)), writes=["V"])
            cnt = {"a": 0, "e": 0, "sp": 0, "at": 0, "c": 0}
            for qc in range(NCH):
                csl = slice(qc * CH, (qc + 1) * CH)
                for h in range(4):
                    hb = slice(64 * (h % 2), 64 * (h % 2) + 64)
                    j = h // 2
                    qk = [("Q", j), ("K", j)]
                    blocks = list(range(4 * qc + 3, -1, -1))
                    nblk = len(blocks)
                    info = {}

                    def S1a(idx):
                        sb = blocks[idx]
                        diag = sb >= 4 * qc
                        P.op("pe", I("matmul", ps[0][:, :], lhsT=KT[hb, j, sb * 128:(sb + 1) * 128], rhs=QT[hb, j, csl], start=True, stop=not diag),
                             reads=qk, writes=[("ps", 0)])
                        if diag:
                            P.op("pe", I("matmul", ps[0][:, :], lhsT=ident[:, :], rhs=mge[:, sb - 4 * qc, :], start=False, stop=True),
                                 reads=["ident", "m_ge"], writes=[("ps", 0)])
                        eb = cnt["e"] % 2
                        cnt["e"] += 1
                        P.op("act", I("activation", out=et[eb][:], in_=ps[0][:, :], func=AF.Exp), reads=[("ps", 0)], writes=[("e", eb)])
                        sb_i = cnt["sp"] % 3
                        cnt["sp"] += 1
                        P.op("act", I("activation", out=SP[sb_i][:], in_=et[eb][:], func=AF.Ln, bias=1.0, scale=1.0),
                             reads=[("e", eb)], writes=[("SP", sb_i)])
                        info[idx] = sb_i

                    def S1b(idx):
                        sb_i = info[idx]
                        P.op("pe", I("matmul", ps[3][0:1, :], lhsT=onec[:, 0:1], rhs=SP[sb_i][:, :], start=(idx == 0), stop=(idx + 2 == nblk)),
                             reads=["onec", ("SP", sb_i)], writes=[("ps", 3)])
                        cb = (idx + 1) % 3
                        P.op("dve", I("tensor_copy", out=chi[cb][:], in_=ps[3][0:1, :]), reads=[("ps", 3)], writes=[("chi", cb)])
                        P.op("dve", I("tensor_tensor", out=clo[cb][:], in0=ps[3][0:1, :], in1=chi[cb][:], op=ALU.subtract),
                             reads=[("ps", 3), ("chi", cb)], writes=[("clo", cb)])

                    def S2a(idx):
                        sb = blocks[idx]
                        diag = sb >= 4 * qc
                        sb_i = info[idx]
                        bb = 1 + (cnt["a"] % 2)
                        cnt["a"] += 1
                        c0 = 128 * (sb - 4 * qc) if diag else 0
                        qsl = slice(qc * CH + c0, (qc + 1) * CH)
                        P.op("pe", I("matmul", ps[bb][:, c0:CH], lhsT=KT[hb, j, sb * 128:(sb + 1) * 128], rhs=QT[hb, j, qsl], start=True, stop=False),
                             reads=qk, writes=[("ps", bb)])
                        if diag:
                            rq = sb - 4 * qc
                            P.op("pe", I("matmul", ps[bb][:, c0:c0 + 128], lhsT=ident[:, :], rhs=mge[:, rq, c0:c0 + 128], start=False, stop=False),
                                 reads=["ident", "m_ge"], writes=[("ps", bb)])
                        last = (idx == 0)
                        P.op("pe", I("matmul", ps[bb][:, c0:CH], lhsT=ntri[:, :], rhs=SP[sb_i][:, c0:CH], start=False, stop=last),
                             reads=["ntri", ("SP", sb_i)], writes=[("ps", bb)])
                        if idx > 0:
                            cb = idx % 3
                            P.op("pe", I("matmul", ps[bb][:, c0:CH], lhsT=nones[0:1, :], rhs=chi[cb][0:1, c0:CH], start=False, stop=False),
                                 reads=["nones", ("chi", cb)], writes=[("ps", bb)])
                            P.op("pe", I("matmul", ps[bb][:, c0:CH], lhsT=nones[0:1, :], rhs=clo[cb][0:1, c0:CH], start=False, stop=True),
                                 reads=["nones", ("clo", cb)], writes=[("ps", bb)])
                        ab = cnt["at"] % 2
                        cnt["at"] += 1
                        P.op("act", I("activation", out=at[ab][:, c0:CH], in_=ps[bb][:, c0:CH], func=AF.Exp), reads=[("ps", bb)], writes=[("at", ab)])
                        info[("ab", idx)] = ab

                    def S2b(idx):
                        sb = blocks[idx]
                        ab = info[("ab", idx)]
                        for ti in range(4):
                            if sb <= 4 * qc + ti:
                                P.op("pe", I("matmul", ps[4 + ti][:, 0:64], lhsT=at[ab][:, ti * 128:(ti + 1) * 128], rhs=V[:, sb, h * 64:(h + 1) * 64],
                                             start=(sb == 4 * qc + ti), stop=(sb == 0)), reads=[("at", ab), "V"], writes=[("ps", 4 + ti)])

                    S1a(0)
                    if nblk > 1:
                        S1a(1)
                        S1b(0)
                    for idx in range(nblk):
                        if idx + 2 < nblk:
                            S1a(idx + 2)
                            S1b(idx + 1)
                        S2a(idx)
                        if idx >= 1:
                            S2b(idx - 1)
                    S2b(nblk - 1)
                    for ti in range(4):
                        o = (qc % 2) * 4 + ti
                        P.op("dve", I("tensor_copy", out=osb[o][:, h, :], in_=ps[4 + ti][:, 0:64]), reads=[("ps", 4 + ti)], writes=[("osb", o, h)])
                for ti in range(4):
                    o = (qc % 2) * 4 + ti
                    rows = slice((qc * 4 + ti) * 128, (qc * 4 + ti + 1) * 128)
                    P.dma("sp", I("dma_start", out=dr["oS"][rows, 768:1024], in_=osb[o][:].rearrange("p h d -> p (h d)")),
                          reads=[("osb", o, h) for h in range(4)], writes=[("oS", "s", qc, ti)])
            P.end_phase()

    def phase_nsa(self, l):
        nc, P, dr, ps = self.nc, self.P, self.dr, self.ps
        pt = self.pt
        with contextlib.ExitStack() as st:
            T = lambda n, sh, dt: st.enter_context(nc.sbuf_tensor(self.uname(n), sh, dt))
            QT = T("n_QT", [128, 4, S], BF16)
            KsE = T("n_KsE", [128, 2, S], BF16)
            Qs = T("n_Qs", [128, 4, CH], BF16)
            KwT = T("n_KwT", [128, S], BF16)
            kcT = T("n_kcT", [128, 256], BF16)
            V4 = T("n_V4", [128, 32, 4, 65], BF16)
            VC = T("n_VC", [128, 2, 2, 65], BF16)
            ovl = T("n_ovl", [128, 2, 64], BF16)
            mgt = T("n_mgt", [128, 4, 512], BF16)
            mle = T("n_mle", [128, 4, 512], BF16)
            ident = T("n_ident", [128, 128], BF16)
            mcmp = [T("n_mcmp%d" % i, [128, 2, CH], BF16) for i in range(2)]
            sbias = [T("n_sbias%d" % i, [128, 64], F32) for i in range(4)]
            gts = [T("n_gt%d" % i, [128, 24], F32) for i in range(8)]
            Pt = [T("n_P%d" % i, [128, CH], BF16) for i in range(3)]
            Pc = [T("n_Pc%d" % i, [128, CH], BF16) for i in range(4)]
            onsa = [T("n_o%d" % i, [128, 8, 64], F32) for i in range(8)]
            impg = [T("n_imp%d" % i, [128, 64], F32) for i in range(4)]
            score = T("n_score", [128, 64], F32)
            sc2 = T("n_sc2", [128, 64], F32)
            sc3 = T("n_sc3", [128, 64], F32)
            m8a = T("n_m8a", [128, 8], F32)
            m8b = T("n_m8b", [128, 8], F32)
            seln = T("n_seln", [128, 64], BF16)
            rc4 = T("n_rc4", [128, 4], F32)
            rg = T("n_rg", [128, 1], F32)
            rs1 = T("n_rs1", [128, 1], F32)
            self._nsa_tmp = ([T("n_r4%d" % i, [128, 1], F32) for i in range(4)], [T("n_g4%d" % i, [128, 1], F32) for i in range(4)],
                             [T("n_s4%d" % i, [128, 64], F32) for i in range(4)])
            for n, t_ in (("m_gt", mgt), ("m_le", mle), ("ident", ident), ("ovl", ovl)):
                P.dma("sp", I("dma_start", out=t_[:], in_=dr[n]), writes=[n])
            for j in range(4):
                P.dma("sp" if j % 2 == 0 else "act", I("dma_start", out=QT[:, j, :], in_=dr["FT"][j]), writes=[("Q", j)])
            for g_ in range(2):
                P.dma("sp", I("dma_start", out=KsE[0:64, g_, :], in_=dr["FT"][5, 64 * g_:64 * g_ + 64, :]), writes=[("KsE", g_, 0)])
                P.dma("act", I("dma_start", out=KsE[64:128, g_, :], in_=dr["eblk"]), writes=[("KsE", g_, 1)])
            P.dma("act", I("dma_start", out=KwT[:], in_=dr["FT"][6]), writes=["KwT"])
            P.dma("sp", I("dma_start", out=kcT[:], in_=dr["kcT"]), writes=["kcT"])
            P.dma("sp", I("dma_start", out=VC[:], in_=dr["VC"]), writes=["VC"])
            for q8 in range(8):
                P.dma("sp", I("dma_start", out=V4[:, q8 * 4:(q8 + 1) * 4, :, :],
                              in_=dr["VA"][q8 * 512:(q8 + 1) * 512, 0:4, :].rearrange("(sb p) h c -> p sb h c", p=128)), writes=["V4"])
            cn = {"st": 0, "p": 0}

            def st_bank():
                b = cn["st"] % 3
                cn["st"] += 1
                return b

            def exp_to(b, dst, dstk, scale=0.125):
                P.op("act", I("activation", out=dst[:], in_=ps[b][:, :], func=AF.Exp, scale=scale), reads=[("ps", b)], writes=[dstk])

            for qc in range(NCH):
                csl = slice(qc * CH, (qc + 1) * CH)
                mb = qc % 2
                P.dma("sp", I("dma_start", out=mcmp[mb][:], in_=dr["m_cmp"][:, :, csl]), writes=[("mcmp", mb)])
                for ti in range(4):
                    rows = slice((qc * 4 + ti) * 128, (qc * 4 + ti + 1) * 128)
                    P.dma("sp", I("dma_start", out=sbias[ti][:], in_=dr["selbias"][rows]), writes=[("sbias", ti)])
                    P.dma("sp", I("dma_start", out=gts[mb * 4 + ti][:], in_=dr["GT"][rows]), writes=[("gts", mb * 4 + ti)])
                nbs = [0] if qc < 4 else [0, 1]
                for g in range(2):
                    gs = slice(64 * g, 64 * g + 64)
                    self.run_deferred()
                    for hh in range(4):
                        head = 4 * g + hh
                        for nb in nbs:
                            b = st_bank()
                            P.op("pe", I("matmul", ps[b][:, :], lhsT=kcT[gs, nb * 128:(nb + 1) * 128], rhs=QT[gs, hh, csl], start=True, stop=False),
                                 reads=["kcT", ("Q", hh)], writes=[("ps", b)])
                            P.op("pe", I("matmul", ps[b][:, :], lhsT=ident[:, :], rhs=mcmp[mb][:, nb, :], start=False, stop=True),
                                 reads=["ident", ("mcmp", mb)], writes=[("ps", b)])
                            pk = (hh % 2) * 2 + nb
                            exp_to(b, Pc[pk], ("Pc", pk))
                        for ti in range(4):
                            for nb in nbs:
                                pk = (hh % 2) * 2 + nb
                                P.op("pe", I("matmul", ps[3 + ti][:, 0:65], lhsT=Pc[pk][:, ti * 128:(ti + 1) * 128], rhs=VC[:, g, nb, :],
                                             start=(nb == nbs[0]), stop=(nb == nbs[-1])), reads=[("Pc", pk), "VC"], writes=[("ps", 3 + ti)])
                            for nb in nbs:
                                pk = (hh % 2) * 2 + nb
                                P.op("pe", I("matmul", ps[3 + ti][:, 128:192], lhsT=Pc[pk][:, ti * 128:(ti + 1) * 128], rhs=ovl[:, nb, :],
                                             start=(nb == nbs[0]), stop=(nb == nbs[-1])), reads=[("Pc", pk), "ovl"], writes=[("ps", 3 + ti)])
                        r4, g4, s4 = self._nsa_tmp
                        for ti in range(4):
                            P.op("dve", I("tensor_scalar", out=r4[ti][:], in0=ps[3 + ti][:, 64:65], scalar1=1e-20, scalar2=None, op0=ALU.max),
                                 reads=[("ps", 3 + ti)], writes=[("r4", ti)])
                        for ti in range(4):
                            P.op("dve", I("reciprocal", out=r4[ti][:], in_=r4[ti][:]), reads=[("r4", ti)], writes=[("r4", ti)])
                        for ti in range(4):
                            o = mb * 4 + ti
                            P.op("dve", I("tensor_tensor", out=g4[ti][:], in0=r4[ti][:], in1=gts[o][:, head * 3:head * 3 + 1], op=ALU.mult),
                                 reads=[("r4", ti), ("gts", o)], writes=[("g4", ti)])
                        for ti in range(4):
                            if hh == 0:
                                P.op("dve", I("tensor_scalar", out=impg[ti][:], in0=ps[3 + ti][:, 128:192], scalar1=r4[ti][:, 0:1], scalar2=None, op0=ALU.mult),
                                     reads=[("ps", 3 + ti), ("r4", ti)], writes=[("impg", ti)])
                            else:
                                P.op("dve", I("tensor_scalar", out=s4[ti][:], in0=ps[3 + ti][:, 128:192], scalar1=r4[ti][:, 0:1], scalar2=None, op0=ALU.mult),
                                     reads=[("ps", 3 + ti), ("r4", ti)], writes=[("s4", ti)])
                        if hh > 0:
                            for ti in range(4):
                                P.op("pool", I("tensor_tensor", out=impg[ti][:], in0=impg[ti][:], in1=s4[ti][:], op=ALU.add),
                                     reads=[("s4", ti), ("impg", ti)], writes=[("impg", ti)])
                        for ti in range(4):
                            o = mb * 4 + ti
                            P.op("dve", I("tensor_scalar", out=onsa[o][:, head, :], in0=ps[3 + ti][:, 0:64], scalar1=g4[ti][:, 0:1], scalar2=None, op0=ALU.mult),
                                 reads=[("ps", 3 + ti), ("g4", ti)], writes=[("onsa", o, head)])
                    if NSA_STOP == 1:
                        continue
                    for ti in range(4):
                        P.op("dve", I("tensor_tensor", out=score[:], in0=impg[ti][:], in1=sbias[ti][:], op=ALU.add),
                             reads=[("impg", ti), ("sbias", ti)], writes=["score"])
                        P.op("dve", I("max", out=m8a[:], in_=score[:]), reads=["score"], writes=["m8a"])
                        P.op("dve", I("match_replace", out=sc3[:], in_to_replace=m8a[:], in_values=score[:], imm_value=-3.0e9),
                             reads=["score", "m8a"], writes=["sc3"])
                        P.op("dve", I("max", out=m8b[:], in_=sc3[:]), reads=["sc3"], writes=["m8b"])
                        P.op("dve", I("tensor_scalar", out=seln[:], in0=score[:], scalar1=m8b[:, 7:8], scalar2=-BIG, op0=ALU.is_lt, op1=ALU.mult),
                             reads=["score", "m8b"], writes=["seln"])
                        P.op("pe", I("transpose", out=pt[0:64, ti * 128:(ti + 1) * 128], in_=seln[:, :], identity=ident[:, :]),
                             reads=["seln", "ident"], writes=[("ps", 7)])
                    for hh in range(4):
                        P.op("pool", I("tensor_copy", out=Qs[0:64, hh, :], in_=QT[gs, hh, csl]), reads=[("Q", hh)], writes=[("Qs", hh, 0)])
                        P.op("dve", I("tensor_copy", out=Qs[64:128, hh, :], in_=pt[0:64, 0:512]), reads=[("ps", 7)], writes=[("Qs", hh, 1)])
                    if NSA_STOP == 2:
                        continue
                    for branch in ((1, 2) if NSA_STOP != 3 else (1,)):
                        for hh in range(4):
                            head = 4 * g + hh
                            if branch == 1:
                                seq = [(sb, None) for sb in range(4 * qc + 4)]
                            else:
                                seq = [(4 * qc - 4 + r, r) for r in range(8) if 4 * qc - 4 + r >= 0]
                            first = {}
                            last = {}
                            for (sb, r) in seq:
                                for ti in range(4):
                                    if branch == 1:
                                        ok = sb <= 4 * qc + ti
                                    else:
                                        ok = (ti <= r) if r < 4 else (r - 4 <= ti)
                                    if ok:
                                        first.setdefault(ti, sb)
                                        last[ti] = sb
                            for (sb, r) in seq:
                                b = st_bank()
                                if branch == 1:
                                    diag = sb >= 4 * qc
                                    P.op("pe", I("matmul", ps[b][:, :], lhsT=KsE[:, g, sb * 128:(sb + 1) * 128], rhs=Qs[:, hh, :], start=True, stop=not diag),
                                         reads=[("KsE", g, 0), ("KsE", g, 1), ("Qs", hh, 0), ("Qs", hh, 1)], writes=[("ps", b)])
                                    if diag:
                                        rq = sb - 4 * qc
                                        P.op("pe", I("matmul", ps[b][:, rq * 128:(rq + 1) * 128], lhsT=ident[:, :],
                                                     rhs=mgt[:, rq, rq * 128:(rq + 1) * 128], start=False, stop=True),
                                             reads=["ident", "m_gt"], writes=[("ps", b)])
                                else:
                                    P.op("pe", I("matmul", ps[b][:, :], lhsT=KwT[gs, sb * 128:(sb + 1) * 128], rhs=QT[gs, hh, csl], start=True, stop=False),
                                         reads=["KwT", ("Q", hh)], writes=[("ps", b)])
                                    rq = r if r < 4 else r - 4
                                    msk = mle[:, rq, rq * 128:(rq + 1) * 128] if r < 4 else mgt[:, rq, rq * 128:(rq + 1) * 128]
                                    P.op("pe", I("matmul", ps[b][:, rq * 128:(rq + 1) * 128], lhsT=ident[:, :], rhs=msk, start=False, stop=True),
                                         reads=["ident", "m_gt", "m_le"], writes=[("ps", b)])
                                pb = cn["p"] % 3
                                cn["p"] += 1
                                exp_to(b, Pt[pb], ("P", pb))
                                def back(sb, r, pb, branch, g, first, last):
                                    for ti in range(4):
                                        if ti in first and first[ti] <= sb <= last[ti]:
                                            if branch == 2:
                                                ok = (ti <= r) if r < 4 else (r - 4 <= ti)
                                                if not ok:
                                                    continue
                                            vg = g if branch == 1 else 2 + g
                                            P.op("pe", I("matmul", ps[3 + ti][:, 0:65], lhsT=Pt[pb][:, ti * 128:(ti + 1) * 128], rhs=V4[:, sb, vg, :],
                                                         start=(sb == first[ti]), stop=(sb == last[ti])), reads=[("P", pb), "V4"], writes=[("ps", 3 + ti)])
                                self.run_deferred()
                                self.defer(back, sb, r, pb, branch, g, first, last)
                            self.defer(self._nsa_fin, mb, head, branch, gts, rs1, rg, sc2, onsa)
                            for ti in range(0):
                                o = mb * 4 + ti
                                gtile = gts[o]
                                P.op("dve", I("tensor_scalar", out=rs1[:], in0=ps[3 + ti][:, 64:65], scalar1=1e-20, scalar2=None, op0=ALU.max),
                                     reads=[("ps", 3 + ti)], writes=["rs1"])
                                P.op("dve", I("reciprocal", out=rs1[:], in_=rs1[:]), reads=["rs1"], writes=["rs1"])
                                P.op("dve", I("tensor_tensor", out=rg[:], in0=rs1[:], in1=gtile[:, head * 3 + branch:head * 3 + branch + 1], op=ALU.mult),
                                     reads=["rs1", ("gts", o)], writes=["rg"])
                                P.op("dve", I("tensor_scalar", out=sc2[:], in0=ps[3 + ti][:, 0:64], scalar1=rg[:, 0:1], scalar2=None, op0=ALU.mult),
                                     reads=[("ps", 3 + ti), "rg"], writes=["sc2"])
                                P.op("pool", I("tensor_tensor", out=onsa[o][:, head, :], in0=onsa[o][:, head, :], in1=sc2[:], op=ALU.add),
                                     reads=["sc2", ("onsa", o, head)], writes=[("onsa", o, head)])
                def store(qc, mb):
                    for ti in range(4):
                        o = mb * 4 + ti
                        rows = slice((qc * 4 + ti) * 128, (qc * 4 + ti + 1) * 128)
                        P.dma("sp", I("dma_start", out=dr["oS"][rows, 0:512], in_=onsa[o][:].rearrange("p h d -> p (h d)")),
                              reads=[("onsa", o, hd) for hd in range(8)], writes=[("oS", "n", qc, ti)])
                self.defer(store, qc, mb)
            self.run_deferred()
            P.end_phase()

    def _nsa_fin(self, mb, head, branch, gts, rs1, rg, sc2, onsa):
        P, ps = self.P, self.ps
        r4, g4, s4 = self._nsa_tmp
        for ti in range(4):
            P.op("dve", I("tensor_scalar", out=r4[ti][:], in0=ps[3 + ti][:, 64:65], scalar1=1e-20, scalar2=None, op0=ALU.max),
                 reads=[("ps", 3 + ti)], writes=[("r4", ti)])
        for ti in range(4):
            P.op("dve", I("reciprocal", out=r4[ti][:], in_=r4[ti][:]), reads=[("r4", ti)], writes=[("r4", ti)])
        for ti in range(4):
            o = mb * 4 + ti
            P.op("dve", I("tensor_tensor", out=g4[ti][:], in0=r4[ti][:], in1=gts[o][:, head * 3 + branch:head * 3 + branch + 1], op=ALU.mult),
                 reads=[("r4", ti), ("gts", o)], writes=[("g4", ti)])
        for ti in range(4):
            P.op("dve", I("tensor_scalar", out=s4[ti][:], in0=ps[3 + ti][:, 0:64], scalar1=g4[ti][:, 0:1], scalar2=None, op0=ALU.mult),
                 reads=[("ps", 3 + ti), ("g4", ti)], writes=[("s4", ti)])
        for ti in range(4):
            o = mb * 4 + ti
            P.op("pool", I("tensor_tensor", out=onsa[o][:, head, :], in0=onsa[o][:, head, :], in1=s4[ti][:], op=ALU.add),
                 reads=[("s4", ti), ("onsa", o, head)], writes=[("onsa", o, head)])

    def phase_D(self, l, last):
        self.phase_D1(l)
        self.phase_D2(l, last)

    def phase_D1(self, l):
        nc, P, dr, ps = self.nc, self.P, self.dr, self.ps
        hsrc = dr["x"] if l == 0 else dr["hS"]
        with contextlib.ExitStack() as st:
            T = lambda n, sh, dt: st.enter_context(nc.sbuf_tensor(self.uname(n), sh, dt))
            Wout = T("d_Wout", [128, 8, D], BF16)
            Wd = T("d_Wd", [128, NF, D], BF16)
            stage = [T("d_stage%d" % i, [128, DFF], F32) for i in range(2)]
            cvt = [T("d_cvt%d" % i, [128, DFF], BF16) for i in range(2)]
            ghead = T("d_ghead", [128, 8], F32)
            gffn = T("d_gffn", [128, 8], F32)
            ident = T("d_ident", [128, 128], BF16)
            h = [T("d_h%d" % i, [128, D], F32) for i in range(4)]
            ot = [T("d_ot%d" % i, [128, D], F32) for i in range(2)]
            osq = T("d_osq", [128, D], F32)
            ssh = T("d_ssh", [128, 16], F32)
            on = [T("d_on%d" % i, [128, D], BF16) for i in range(2)]
            T4 = [dict(junk=T("d_junk%d" % i, [128, D], BF16), ss=T("d_ss%d" % i, [128, 1], F32),
                       rs=T("d_rs%d" % i, [128, 1], F32)) for i in range(2)]
            xT = T("d_xT", [128, 8, CH], BF16)
            actT = T("d_actT", [128, NF, CH], BF16)
            wgu = [T("d_wgu%d" % i, [128, 2, 8, 128], BF16) for i in range(3)]
            sg = [T("d_sg%d" % i, [128, CH], F32) for i in range(2)]
            P.dma("sp", I("dma_start", out=ghead[:], in_=dr["head_norm"][l]), writes=["ghead"])
            P.dma("sp", I("dma_start", out=gffn[:], in_=dr["norm_ffn"][l]), writes=["gffn"])
            P.dma("sp", I("dma_start", out=ident[:], in_=dr["ident"]), writes=["ident"])
            n = 0
            for k in range(8):
                self.load_weight_bf(Wout[:, k, :], dr["w_out"][l, k * 128:(k + 1) * 128, :], stage[n % 2][:, 0:D], ("stage", n % 2),
                                    ("Wout", k), scale_ap=ghead[:, k:k + 1], scalek="ghead", eng=("dve" if n % 2 == 0 else "pool"),
                                    q=("sp" if n % 2 == 0 else "act"))
                n += 1
            for f in range(NF):
                self.load_weight_bf(Wd[:, f, :], dr["w_ffn_down"][l, f * 128:(f + 1) * 128, :], stage[n % 2][:, 0:D], ("stage", n % 2),
                                    ("Wd", f), eng=("dve" if n % 2 == 0 else "pool"), q=("sp" if n % 2 == 0 else "act"))
                n += 1
            for gi, wn in enumerate(("w_ffn_gate", "w_ffn_up")):
                for k in range(8):
                    b = n % 2
                    self.load_weight_bf(cvt[b][:], dr[wn][l, k * 128:(k + 1) * 128, :], stage[b][:], ("stage", b), ("cvt", b),
                                        scale_ap=gffn[:, k:k + 1], scalek="gffn", eng=("dve" if b == 0 else "pool"),
                                        q=("sp" if b == 0 else "act"))
                    for f0, f1 in ((0, 8), (8, 16), (16, NF)):
                        P.dma("sp", I("dma_start", out=dr["WGU"][f0:f1, :, gi, k, :].rearrange("f p c -> p f c"),
                                      in_=cvt[b][:, f0 * 128:f1 * 128].rearrange("p (f c) -> p f c", c=128)),
                              reads=[("cvt", b)], writes=[("WGU", gi, k, f0)])
                    n += 1
            wgu_ready = [("WGU", gi, k, f0) for gi in range(2) for k in range(8) for f0 in (0, 8, 16)]
            Woutk = [("Wout", k) for k in range(8)]
            Wdk = [("Wd", f) for f in range(NF)]
            wl = 0
            for c in range(NCH):
                for i in range(4):
                    ti = c * 4 + i
                    b = ti % 2
                    rows = slice(ti * 128, (ti + 1) * 128)
                    P.dma("sp", I("dma_start", out=h[i][:], in_=hsrc[rows, :]), writes=[("h", i)])
                    P.dma("act", I("dma_start", out=ot[b][:], in_=dr["oS"][rows, :]), writes=[("ot", b)])
                    P.op("pool", I("tensor_tensor", out=osq[:], in0=ot[b][:], in1=ot[b][:], op=ALU.mult), reads=[("ot", b)], writes=["osq"])
                    P.op("dve", I("tensor_reduce", out=ssh[:], in_=osq[:].rearrange("p (h d) -> p h d", d=64), axis=AX.X, op=ALU.add),
                         reads=["osq"], writes=["ssh"])
                    P.op("dve", I("tensor_scalar", out=ssh[:], in0=ssh[:], scalar1=1.0 / 64, scalar2=1e-6, op0=ALU.mult, op1=ALU.add),
                         reads=["ssh"], writes=["ssh"])
                    P.op("act", I("sqrt", out=ssh[:], in_=ssh[:]), reads=["ssh"], writes=["ssh"])
                    P.op("dve", I("reciprocal", out=ssh[:], in_=ssh[:]), reads=["ssh"], writes=["ssh"])
                    P.op("dve", I("tensor_tensor", out=on[b][:].rearrange("p (h d) -> p h d", d=64),
                                  in0=ot[b][:].rearrange("p (h d) -> p h d", d=64),
                                  in1=ssh[:, :].unsqueeze(2).to_broadcast([128, 16, 64]), op=ALU.mult),
                         reads=[("ot", b), "ssh"], writes=[("on", b)])
                    self.transpose8(on[b], ("on", b), xT[:, :, i * 128:(i + 1) * 128], ("xT", i), ident, eng=("dve" if i % 2 == 0 else "act"))
                    for half in range(2):
                        pa = self.psn()
                        for k in range(8):
                            P.op("pe", I("matmul", ps[pa][:, :], lhsT=xT[:, k, i * 128:(i + 1) * 128], rhs=Wout[:, k, half * 512:(half + 1) * 512],
                                         start=(k == 0), stop=(k == 7)), reads=[("xT", i)] + Woutk, writes=[("ps", pa)])
                        P.op("dve", I("tensor_tensor", out=h[i][:, half * 512:(half + 1) * 512], in0=ps[pa][:, :],
                                      in1=h[i][:, half * 512:(half + 1) * 512], op=ALU.add), reads=[("ps", pa), ("h", i)], writes=[("h", i)])
                if "hmix" in self.dr:
                    for i in range(4):
                        rows = slice((c * 4 + i) * 128, (c * 4 + i + 1) * 128)
                        P.dma("sp", I("dma_start", out=dr["hmix"][rows, :], in_=h[i][:]), reads=[("h", i)], writes=[("hmix", c, i)])
                for i in range(4):
                    b = i % 2
                    self.rms_tile(T4[b], b, h[i], ("h", i), ("junk", b), ("ss", b), ("rs", b), on[b], ("on", b))
                    self.transpose8(on[b], ("on", b), xT[:, :, i * 128:(i + 1) * 128], ("xT", i), ident, eng=("dve" if i % 2 == 0 else "act"))
                xk = [("xT", i) for i in range(4)]
                for f in range(NF):
                    wb = wl % 3
                    wl += 1
                    P.dma("sp" if f % 2 == 0 else "act", I("dma_start", out=wgu[wb][:], in_=dr["WGU"][f]), reads=wgu_ready, writes=[("wgu", wb)])
                    pg, pu = self.psn(), self.psn()
                    for gi, pp in ((0, pg), (1, pu)):
                        for k in range(8):
                            P.op("pe", I("matmul", ps[pp][:, :], lhsT=wgu[wb][:, gi, k, :], rhs=xT[:, k, :], start=(k == 0), stop=(k == 7)),
                                 reads=xk + [("wgu", wb)], writes=[("ps", pp)])
                    sb_ = f % 2
                    P.op("act", I("activation", out=sg[sb_][:], in_=ps[pg][:, :], func=AF.Silu), reads=[("ps", pg)], writes=[("sg", sb_)])
                    P.op("dve", I("tensor_tensor", out=actT[:, f, :], in0=ps[pu][:, :], in1=sg[sb_][:], op=ALU.mult),
                         reads=[("ps", pu), ("sg", sb_)], writes=[("actT", f)])
                ak = [("actT", f) for f in range(NF)]
                for i in range(4):
                    for half in range(2):
                        pa = self.psn()
                        for f in range(NF):
                            P.op("pe", I("matmul", ps[pa][:, :], lhsT=actT[:, f, i * 128:(i + 1) * 128], rhs=Wd[:, f, half * 512:(half + 1) * 512],
                                         start=(f == 0), stop=(f == NF - 1)), reads=ak + Wdk, writes=[("ps", pa)])
                        P.op("dve", I("tensor_tensor", out=h[i][:, half * 512:(half + 1) * 512], in0=ps[pa][:, :],
                                      in1=h[i][:, half * 512:(half + 1) * 512], op=ALU.add), reads=[("ps", pa), ("h", i)], writes=[("h", i)])
                    rows = slice((c * 4 + i) * 128, (c * 4 + i + 1) * 128)
                    P.dma("sp", I("dma_start", out=dr["hS"][rows, :], in_=h[i][:]), reads=[("h", i)], writes=[("hS", c, i)])
            P.end_phase()

    def phase_D2(self, l, last):
        nc, P, dr, ps = self.nc, self.P, self.dr, self.ps
        with contextlib.ExitStack() as st:
            T = lambda n, sh, dt: st.enter_context(nc.sbuf_tensor(self.uname(n), sh, dt))
            Wpg = T("e_Wpg", [128, 8, D], BF16)
            Wpp = T("e_Wpp", [128, 2, D], BF16)
            stage = [T("e_stage%d" % i, [128, D], F32) for i in range(2)]
            gple = T("e_gple", [128, 8], F32)
            gfin = T("e_gfin", [128, D], F32)
            ident = T("e_ident", [128, 128], BF16)
            h = [T("e_h%d" % i, [128, D], F32) for i in range(2)]
            p32 = [T("e_p32%d" % i, [128, 256], F32) for i in range(2)]
            pbf = [T("e_pbf%d" % i, [128, 256], BF16) for i in range(2)]
            hn = [T("e_hn%d" % i, [128, D], BF16) for i in range(2)]
            T4 = [dict(junk=T("e_junk%d" % i, [128, D], BF16), ss=T("e_ss%d" % i, [128, 1], F32),
                       rs=T("e_rs%d" % i, [128, 1], F32)) for i in range(2)]
            xT = [T("e_xT%d" % i, [128, 8, 128], BF16) for i in range(2)]
            pT = [T("e_pT%d" % i, [128, 2, 128], BF16) for i in range(2)]
            sig = [T("e_sig%d" % i, [128, CH], F32) for i in range(2)]
            tmp = [T("e_tmp%d" % i, [128, CH], F32) for i in range(2)]
            outt = [T("e_out%d" % i, [128, D], F32) for i in range(2)]
            P.dma("sp", I("dma_start", out=gple[:], in_=dr["norm_ple"][l]), writes=["gple"])
            P.dma("sp", I("dma_start", out=ident[:], in_=dr["ident"]), writes=["ident"])
            if last:
                P.dma("sp", I("dma_start", out=gfin[:], in_=dr["norm_final"].to_broadcast([128, D])), writes=["gfin"])
            n = 0
            for k in range(8):
                self.load_weight_bf(Wpg[:, k, :], dr["w_ple_gate"][l, k * 128:(k + 1) * 128, :], stage[n % 2][:], ("stage", n % 2),
                                    ("Wpg", k), scale_ap=gple[:, k:k + 1], scalek="gple", eng=("dve" if n % 2 == 0 else "pool"),
                                    q=("sp" if n % 2 == 0 else "act"))
                n += 1
            for k in range(2):
                self.load_weight_bf(Wpp[:, k, :], dr["w_ple_proj"][l, k * 128:(k + 1) * 128, :], stage[n % 2][:], ("stage", n % 2),
                                    ("Wpp", k), eng=("dve" if n % 2 == 0 else "pool"), q=("sp" if n % 2 == 0 else "act"))
                n += 1
            Wpgk = [("Wpg", k) for k in range(8)]
            Wppk = [("Wpp", k) for k in range(2)]
            for ti in range(NTILE):
                b = ti % 2
                rows = slice(ti * 128, (ti + 1) * 128)
                P.dma("sp", I("dma_start", out=h[b][:], in_=dr["hS"][rows, :]), writes=[("h", b)])
                P.dma("act", I("dma_start", out=p32[b][:], in_=dr["p"][l, rows, :]), writes=[("p32", b)])
                P.op("pool", I("tensor_copy", out=pbf[b][:], in_=p32[b][:]), reads=[("p32", b)], writes=[("pbf", b)])
                self.rms_tile(T4[b], b, h[b], ("h", b), ("junk", b), ("ss", b), ("rs", b), hn[b], ("hn", b))
                self.transpose8(hn[b], ("hn", b), xT[b][:, :, :], ("xT", b), ident, eng="dve")
                self.transpose8(pbf[b], ("pbf", b), pT[b][:, :, :], ("pT", b), ident, nblk=2, eng="act")
                self.run_deferred()
                self.defer(self._d2_back, l, last, ti, b, rows, (h, hn, xT, pT, Wpg, Wpp, Wpgk, Wppk, sig, tmp, T4, outt, gfin))
            self.run_deferred()
            P.end_phase()

    def _d2_back(self, l, last, ti, b, rows, tl):
        P, dr, ps = self.P, self.dr, self.ps
        h, hn, xT, pT, Wpg, Wpp, Wpgk, Wppk, sig, tmp, T4, outt, gfin = tl
        if True:
            if True:
                for half in range(2):
                    hs = slice(half * 512, (half + 1) * 512)
                    pg, pp = self.psn(), self.psn()
                    for k in range(8):
                        P.op("pe", I("matmul", ps[pg][:, :], lhsT=xT[b][:, k, :], rhs=Wpg[:, k, hs], start=(k == 0), stop=(k == 7)),
                             reads=[("xT", b)] + Wpgk, writes=[("ps", pg)])
                    for k in range(2):
                        P.op("pe", I("matmul", ps[pp][:, :], lhsT=pT[b][:, k, :], rhs=Wpp[:, k, hs], start=(k == 0), stop=(k == 1)),
                             reads=[("pT", b)] + Wppk, writes=[("ps", pp)])
                    P.op("act", I("activation", out=sig[half][:], in_=ps[pg][:, :], func=AF.Sigmoid), reads=[("ps", pg)], writes=[("sig", half)])
                    P.op("dve", I("tensor_tensor", out=tmp[half][:], in0=ps[pp][:, :], in1=sig[half][:], op=ALU.mult),
                         reads=[("ps", pp), ("sig", half)], writes=[("tmp", half)])
                    P.op("pool", I("tensor_tensor", out=h[b][:, hs], in0=h[b][:, hs], in1=tmp[half][:], op=ALU.add),
                         reads=[("tmp", half), ("h", b)], writes=[("h", b)])
                if not last:
                    P.dma("sp", I("dma_start", out=dr["hS"][rows, :], in_=h[b][:]), reads=[("h", b)], writes=[("hS", ti)])
                else:
                    self.rms_tile(T4[b], b, h[b], ("h", b), ("junk", b), ("ss", b), ("rs", b), None, None) if False else None
                    junk, ss, rs = T4[b]["junk"], T4[b]["ss"], T4[b]["rs"]
                    P.op("act", I("activation", out=junk[:], in_=h[b][:], func=AF.Square, accum_out=ss[:]), reads=[("h", b)], writes=[("junk", b), ("ss", b)])
                    P.op("dve", I("tensor_scalar", out=rs[:], in0=ss[:], scalar1=1.0 / D, scalar2=1e-6, op0=ALU.mult, op1=ALU.add),
                         reads=[("ss", b)], writes=[("rs", b)])
                    P.op("act", I("sqrt", out=rs[:], in_=rs[:]), reads=[("rs", b)], writes=[("rs", b)])
                    P.op("dve", I("reciprocal", out=rs[:], in_=rs[:]), reads=[("rs", b)], writes=[("rs", b)])
                    P.op("dve", I("scalar_tensor_tensor", out=outt[b][:], in0=h[b][:], scalar=rs[:, 0:1], in1=gfin[:], op0=ALU.mult, op1=ALU.mult),
                         reads=[("h", b), ("rs", b), "gfin"], writes=[("outt", b)])
                    P.dma("sp", I("dma_start", out=self.out[rows, :], in_=outt[b][:]), reads=[("outt", b)], writes=[("out", ti)])


def make_in_maps(inputs, cores):
    inp = {k: np.asarray(v) for k, v in inputs.items()}
    sh = _prep_shared(inp)
    maps = []
    for b in cores:
        m = dict(sh)
        m["x"] = np.ascontiguousarray(inp["x"][b])
        m["p"] = np.ascontiguousarray(inp["p"][:, b])
        m["pos"] = np.ascontiguousarray(inp["positions"][b].reshape(1, S).astype(np.int32))
        maps.append(m)
    return maps


_NC_CACHE = {}


def kernel(**inputs):
    if "nc" not in _NC_CACHE:
        _NC_CACHE["nc"] = Builder().build()
    nc = _NC_CACHE["nc"]
    maps = make_in_maps(inputs, list(range(8)))
    res = run_bass_kernel_spmd(nc, maps, core_ids=list(range(8)))
    out = np.stack([np.asarray(r["out"]) for r in res.results], axis=0)
    return out.astype(np.float32)
```

```python
import contextlib
import numpy as np
import ml_dtypes
import concourse.bass as bass
import concourse.mybir as mybir
from concourse.bass_utils import run_bass_kernel_spmd

F32 = mybir.dt.float32
BF16 = mybir.dt.bfloat16
I32 = mybir.dt.int32
AF = mybir.ActivationFunctionType
ALU = mybir.AluOpType
AX = mybir.AxisListType

S = 4096
D = 1024
L = 2
NTILE = 32
CH = 512
NCH = 8
DFF = 2816
NF = 22
BIG = 30000.0
WCOLS = 3740
COMPUTE = ("pe", "act", "dve", "pool", "sp")
import os as _os
NSA_STOP = int(_os.environ.get("NSA_STOP", "0"))
DMAQ = ("sp", "pool", "act")


def I(m, *a, **k):
    return lambda e: getattr(e, m)(*a, **k)


class Op:
    __slots__ = ("eng", "fn", "waits", "signal", "cnt", "dma_sem", "dma_val", "is_dma", "idx")

    def __init__(self, eng, fn, is_dma):
        self.eng = eng
        self.fn = fn
        self.waits = []
        self.signal = False
        self.cnt = None
        self.dma_sem = None
        self.dma_val = None
        self.is_dma = is_dma
        self.idx = None


class Prog:
    def __init__(self, nc, st, n_dma_sems=8):
        self.nc = nc
        self.lists = {e: [] for e in COMPUTE}
        self.last_w = {}
        self.readers = {}
        self.n_dma_sems = n_dma_sems
        self.dma_count = {q: 0 for q in DMAQ}
        self.csem = {e: st.enter_context(nc.semaphore("c_" + e)) for e in COMPUTE}
        self.dsem = {(q, j): st.enter_context(nc.semaphore("d_%s%d" % (q, j)))
                     for q in DMAQ for j in range(n_dma_sems)}
        self.cbase = {e: 0 for e in COMPUTE}
        self.gidx = {e: 0 for e in COMPUTE}
        self.barrier = {}
        self.dma_last = {}

    def _deps(self, reads, writes):
        deps = []
        for k in reads:
            w = self.last_w.get(k)
            if w is not None:
                deps.append(w)
        for k in writes:
            w = self.last_w.get(k)
            if w is not None:
                deps.append(w)
            deps.extend(self.readers.get(k, ()))
        return deps

    def _record(self, h, reads, writes):
        for k in reads:
            self.readers.setdefault(k, []).append(h)
        for k in writes:
            self.last_w[k] = h
            self.readers[k] = []

    def _attach(self, h, deps):
        best = {}
        for d in deps:
            if d is h or d.fn is None:
                continue
            if d.is_dma:
                key = ("d",) + d.dma_sem
                cur = best.get(key)
                if cur is None or d.dma_val > cur.dma_val:
                    best[key] = d
            else:
                if d.eng == "pe" and h.eng == "pe" and not h.is_dma:
                    continue
                cur = best.get(d.eng)
                if cur is None or d.idx > cur.idx:
                    best[d.eng] = d
        for d in best.values():
            d.signal = True
            h.waits.append(d)

    def op(self, eng, fn, reads=(), writes=(), extra=()):
        h = Op(eng, fn, False)
        self._attach(h, self._deps(reads, writes) + list(extra))
        h.idx = self.gidx[eng]
        self.gidx[eng] += 1
        self.lists[eng].append(h)
        self._record(h, reads, writes)
        return h

    def dma(self, q, fn, reads=(), writes=(), extra=()):
        h = Op(q, fn, True)
        self._attach(h, self._deps(reads, writes) + list(extra))
        i = self.dma_count[q]
        self.dma_count[q] += 1
        h.dma_sem = (q, i % self.n_dma_sems)
        h.dma_val = 16 * (i // self.n_dma_sems + 1)
        h.idx = self.gidx[q]
        self.gidx[q] += 1
        self.lists[q].append(h)
        self._record(h, reads, writes)
        self.dma_last[h.dma_sem] = h.dma_val
        return h

    def flush(self, final=False):
        nc = self.nc
        for e in COMPUTE:
            c = self.cbase[e]
            for h in self.lists[e]:
                if not h.is_dma and h.signal:
                    c += 1
                    h.cnt = c
        if final:
            pass
        barrier = dict(self.barrier)
        with nc.Block() as block:
            engs = {"pe": block.tensor, "act": block.scalar, "dve": block.vector,
                    "pool": block.gpsimd, "sp": block.sync}

            def make(ename):
                lst = self.lists[ename]

                def body(eng):
                    waited = {}
                    for key, val in barrier.items():
                        if val <= 0:
                            continue
                        sem = self.csem[key[1]] if key[0] == "c" else self.dsem[key[1:]]
                        eng.wait_ge(sem, val)
                        waited[key] = val
                    for h in lst:
                        for d in h.waits:
                            if d.is_dma:
                                key = ("d",) + d.dma_sem
                                sem = self.dsem[d.dma_sem]
                                val = d.dma_val
                            else:
                                key = ("c", d.eng)
                                sem = self.csem[d.eng]
                                val = d.cnt
                            if waited.get(key, 0) >= val:
                                continue
                            waited[key] = val
                            eng.wait_ge(sem, val)
                        if h.is_dma:
                            prev = h.dma_val - 16
                            key = ("d",) + h.dma_sem
                            if prev > 0 and waited.get(key, 0) < prev:
                                eng.wait_ge(self.dsem[h.dma_sem], prev)
                                waited[key] = prev
                            ins = h.fn(eng)
                            ins.then_inc(self.dsem[h.dma_sem], 16)
                        else:
                            ins = h.fn(eng)
                            if h.signal:
                                ins.then_inc(self.csem[ename], 1)
                    if final and ename == "sp":
                        for key, val in self._barrier_now().items():
                            if val > 0 and waited.get(key, 0) < val and key != ("c", "sp"):
                                sem = self.csem[key[1]] if key[0] == "c" else self.dsem[key[1:]]
                                eng.wait_ge(sem, val)
                return body

            for ename in ("sp", "pool", "act", "dve", "pe"):
                if self.lists[ename] or barrier or final:
                    engs[ename](make(ename))
        self.barrier = self._barrier_now()
        for e in COMPUTE:
            for h in self.lists[e]:
                h.fn = None
            self.lists[e] = []
        self.last_w = {}
        self.readers = {}

    def _barrier_now(self):
        b = {}
        for e in COMPUTE:
            c = self.cbase[e]
            for h in self.lists[e]:
                if h.cnt is not None and h.cnt > c:
                    c = h.cnt
            b[("c", e)] = c
        for k, v in self.dma_last.items():
            b[("d",) + k] = v
        return b

    def end_phase(self, final=False):
        for e in COMPUTE:
            for h in reversed(self.lists[e]):
                if not h.is_dma:
                    h.signal = True
                    break
        self.flush(final=final)
        for e in COMPUTE:
            self.cbase[e] = self.barrier[("c", e)]


def _win_cols():
    o = {}
    names = ["nq", "nkc", "nvc", "nks", "nvs", "nkw", "nvw", "ngate", "fq", "fk", "fv", "ff", "sq", "sk", "sv"]
    sizes = [512, 128, 128, 128, 128, 128, 128, 24, 256, 256, 256, 4, 256, 256, 256]
    off = 0
    for n, s in zip(names, sizes):
        o[n] = np.arange(off, off + s)
        off += s
    assert off == 2844

    def rot(c):
        c = c.reshape(-1, 2, 32)
        return c[:, ::-1, :].reshape(-1)

    ft = []
    for j in range(4):
        ft.append(np.concatenate([o["nq"][64 * j:64 * j + 64], o["nq"][64 * (4 + j):64 * (4 + j) + 64]]))
    ft += [o["nkc"], o["nks"], o["nkw"]]
    ft += [rot(c) for c in ft[:7]]
    ft.append(o["nvc"])
    for n in ("fq", "fk", "sq", "sk"):
        ft += [o[n][:128], o[n][128:]]
    cols = np.concatenate(ft + [o["ff"], o["nvs"], o["nvw"], o["fv"], o["sv"], o["ngate"]])
    assert cols.shape[0] == WCOLS
    return cols


def _consts():
    bf = ml_dtypes.bfloat16
    c = {}
    c["ident"] = np.eye(128, dtype=np.float32).astype(bf)
    s = np.arange(128)[:, None, None]
    r = np.arange(4)[None, :, None]
    t = np.arange(512)[None, None, :]
    sa = 128 * r + s
    c["m_gt"] = np.where(sa > t, -BIG, 0.0).astype(bf)
    c["m_ge"] = np.where(sa >= t, -BIG, 0.0).astype(bf)
    c["m_le"] = np.where(sa <= t, -BIG, 0.0).astype(bf)
    n = np.arange(128)[:, None, None] + 128 * np.arange(2)[None, :, None]
    tt = np.arange(S)[None, None, :]
    cm = np.where((16 * n + 31 > tt) | (n >= 255), -BIG, 0.0)
    c["m_cmp"] = cm.astype(bf)
    j = np.arange(64)[:, None, None]
    sb = np.arange(32)[None, :, None]
    ss = np.arange(128)[None, None, :]
    c["eblk"] = (np.arange(64)[:, None] == (np.arange(S)[None, :] // 64)).astype(np.float32).astype(bf)
    nn = np.arange(256)
    cs = nn[:, None] * 16
    bs = np.arange(64)[None, :] * 64
    ov = ((cs < bs + 64) & (cs + 32 > bs) & (nn[:, None] < 255)).astype(np.float32)
    c["ovl"] = ov.reshape(2, 128, 64).transpose(1, 0, 2).astype(bf).copy()
    tq = np.arange(S)[:, None]
    jb = np.arange(64)[None, :]
    cur = tq // 64
    forced = (jb == 0) | (jb == cur) | (jb == cur - 1)
    valid = jb <= cur
    c["selbias"] = np.where(forced, 1e9, np.where(valid, 0.0, -1e9)).astype(np.float32)
    half = 32
    invf = (10000.0 ** (-np.arange(half, dtype=np.float32) / half)).astype(np.float32)
    rr = np.arange(128)
    c["invf"] = invf[rr % 32].reshape(128, 1).astype(np.float32)
    c["sgn"] = np.where((rr % 64) < 32, -1.0, 1.0).reshape(128, 1).astype(np.float32)
    jj = np.arange(128)
    c["ntri"] = np.where(jj[:, None] >= jj[None, :], -1.0, 0.0).astype(np.float32).astype(bf)
    c["nones"] = np.full((1, 128), -1.0, np.float32).astype(bf)
    c["onec"] = np.ones((128, 1), np.float32).astype(bf)
    return c


def _col8(v):
    return np.ascontiguousarray(v.reshape(8, 128).T)


def _prep_shared(inp):
    sh = {}
    cols = _win_cols()
    sh["w_in"] = np.ascontiguousarray(inp["w_in"][:, :, cols])
    for n in ("norm_mix", "norm_ffn", "norm_ple", "head_norm"):
        sh[n] = np.stack([_col8(inp[n][l]) for l in range(L)])
    sh["norm_final"] = inp["norm_final"].reshape(1, D)
    sh["b_gate"] = inp["b_nsa_gate"].reshape(L, 1, 24)
    sh["b_forget"] = inp["b_forget"].reshape(L, 4, 1)
    for kv in ("k", "v"):
        w1 = inp["nsa_cmp_w1_" + kv].reshape(L, 32, 64, 128).transpose(0, 2, 1, 3)
        sh["w1_" + kv] = np.ascontiguousarray(np.concatenate([w1, w1], axis=1).reshape(L, 128, 32 * 128))
        pt = inp["nsa_cmp_pos_" + kv].transpose(0, 2, 1)
        sh["pos_" + kv] = np.ascontiguousarray(np.concatenate([pt, pt], axis=1))
        sh["w2_" + kv] = inp["nsa_cmp_w2_" + kv]
    for n in ("w_out", "w_ffn_gate", "w_ffn_up", "w_ffn_down", "w_ple_proj", "w_ple_gate"):
        sh[n] = inp[n]
    sh.update(_consts())
    return sh


class Builder:
    def __init__(self, debug=(), nlayers=L, phases=None):
        self.debug = set(debug)
        self.nlayers = nlayers
        self.phases = phases
        self.nc = bass.Bass("TRN2", target_bir_lowering=False)
        self.dr = {}

    def din(self, name, shape, dt):
        self.dr[name] = self.nc.dram_tensor(name, list(shape), dt, kind="ExternalInput").ap()
        return self.dr[name]

    def dscr(self, name, shape, dt):
        kind = "ExternalOutput" if name in self.debug else "Internal"
        self.dr[name] = self.nc.dram_tensor(name, list(shape), dt, kind=kind).ap()
        return self.dr[name]

    def want(self, ph):
        return self.phases is None or ph in self.phases

    def build(self):
        nc = self.nc
        din, dscr = self.din, self.dscr
        din("x", [S, D], F32)
        din("p", [L, S, 256], F32)
        din("pos", [1, S], I32)
        din("w_in", [L, D, WCOLS], F32)
        for n in ("norm_mix", "norm_ffn", "norm_ple", "head_norm"):
            din(n, [L, 128, 8], F32)
        din("norm_final", [1, D], F32)
        din("b_gate", [L, 1, 24], F32)
        din("b_forget", [L, 4, 1], F32)
        for kv in ("k", "v"):
            din("w1_" + kv, [L, 128, 4096], F32)
            din("pos_" + kv, [L, 128, 32], F32)
            din("w2_" + kv, [L, 128, 64], F32)
        din("w_out", [L, D, D], F32)
        din("w_ffn_gate", [L, D, DFF], F32)
        din("w_ffn_up", [L, D, DFF], F32)
        din("w_ffn_down", [L, DFF, D], F32)
        din("w_ple_proj", [L, 256, D], F32)
        din("w_ple_gate", [L, D, D], F32)
        din("ident", [128, 128], BF16)
        for n in ("m_gt", "m_ge", "m_le"):
            din(n, [128, 4, 512], BF16)
        din("m_cmp", [128, 2, S], BF16)
        din("eblk", [64, S], BF16)
        din("ovl", [128, 2, 64], BF16)
        din("selbias", [S, 64], F32)
        din("invf", [128, 1], F32)
        din("sgn", [128, 1], F32)
        din("ntri", [128, 128], BF16)
        din("nones", [1, 128], BF16)
        din("onec", [128, 1], BF16)
        self.out = nc.dram_tensor("out", [S, D], F32, kind="ExternalOutput").ap()
        dscr("hS", [S, D], F32)
        dscr("cosS", [128, S], F32)
        dscr("sinS", [128, S], F32)
        dscr("FT", [16, 128, S], BF16)
        dscr("cT", [4, S], F32)
        dscr("VA", [S, 8, 65], BF16)
        dscr("SV", [S, 256], BF16)
        dscr("GT", [S, 24], F32)
        dscr("kcT", [128, 256], BF16)
        dscr("VC", [128, 2, 2, 65], BF16)
        dscr("oS", [S, D], F32)
        dscr("WGU", [NF, 128, 2, 8, 128], BF16)
        if "hmix" in self.debug:
            dscr("hmix", [S, D], F32)

        with contextlib.ExitStack() as st:
            self.P = Prog(nc, st)
            self.ps = [st.enter_context(nc.psum_tensor("ps%d" % i, [128, 512], F32)) for i in range(8)]
            self.pt = self.ps[7][:, :].bitcast(BF16)
            self.ps_i = 0
            if self.want("T"):
                self.phase_tables()
            for l in range(self.nlayers):
                if self.want("A"):
                    self.phase_A(l)
                if self.want("B"):
                    self.phase_B(l)
                if self.want("N"):
                    self.phase_nsa(l)
                if self.want("F"):
                    self.phase_fox(l)
                if self.want("SB"):
                    self.phase_sb(l)
                if self.want("D"):
                    self.phase_D(l, last=(l == self.nlayers - 1))
            self.P.op("sp", I("nop"))
            self.P.end_phase(final=True)
        return nc

    def defer(self, fn, *a):
        if not hasattr(self, "_pend"):
            self._pend = []
        self._pend.append((fn, a))

    def run_deferred(self):
        pend = getattr(self, "_pend", [])
        self._pend = []
        for fn, a in pend:
            fn(*a)

    def uname(self, n):
        self._un = getattr(self, "_un", 0) + 1
        return "%s_u%d" % (n, self._un)

    def psn(self):
        i = self.ps_i
        self.ps_i = (i + 1) % 7
        return i

    def phase_tables(self):
        nc, P, dr = self.nc, self.P, self.dr
        with contextlib.ExitStack() as st:
            T = lambda n, sh, dt: st.enter_context(nc.sbuf_tensor(self.uname(n), sh, dt))
            posi = T("t_posi", [128, S], I32)
            ang = T("t_ang", [128, S], F32)
            kk = T("t_kk", [128, S], F32)
            rr = T("t_r", [128, S], F32)
            oo = T("t_o", [128, S], F32)
            invf = T("t_invf", [128, 1], F32)
            sgn = T("t_sgn", [128, 1], F32)
            hpi = T("t_hpi", [128, 1], F32)
            P.dma("sp", I("dma_start", out=posi[:], in_=dr["pos"].to_broadcast([128, S])), writes=["posi"])
            P.dma("sp", I("dma_start", out=invf[:], in_=dr["invf"]), writes=["invf"])
            P.dma("sp", I("dma_start", out=sgn[:], in_=dr["sgn"]), writes=["sgn"])
            P.op("pool", I("memset", hpi[:], float(np.pi / 2)), writes=["hpi"])
            P.op("dve", I("tensor_copy", out=ang[:], in_=posi[:]), reads=["posi"], writes=["ang"])
            P.op("dve", I("tensor_scalar", out=ang[:], in0=ang[:], scalar1=invf[:, 0:1], scalar2=None, op0=ALU.mult),
                 reads=["ang", "invf"], writes=["ang"])
            MAGIC = 12582912.0
            P.op("dve", I("tensor_scalar", out=kk[:], in0=ang[:], scalar1=float(1.0 / (2 * np.pi)), scalar2=MAGIC,
                                                   op0=ALU.mult, op1=ALU.add), reads=["ang"], writes=["kk"])
            P.op("dve", I("tensor_scalar", out=kk[:], in0=kk[:], scalar1=-MAGIC, scalar2=None, op0=ALU.add),
                 reads=["kk"], writes=["kk"])
            C1 = 6.28125
            C2 = float(np.float32(2 * np.pi - C1))
            P.op("dve", I("scalar_tensor_tensor", out=rr[:], in0=kk[:], scalar=-C1, in1=ang[:], op0=ALU.mult, op1=ALU.add),
                 reads=["kk", "ang"], writes=["rr"])
            P.op("dve", I("scalar_tensor_tensor", out=rr[:], in0=kk[:], scalar=-C2, in1=rr[:], op0=ALU.mult, op1=ALU.add),
                 reads=["kk", "rr"], writes=["rr"])
            PL = 3.1415925
            P.op("dve", I("tensor_scalar", out=rr[:], in0=rr[:], scalar1=-PL, scalar2=PL, op0=ALU.max, op1=ALU.min),
                 reads=["rr"], writes=["rr"])
            P.op("act", I("activation", out=oo[:], in_=rr[:], func=AF.Sin), reads=["rr"], writes=["oo"])
            P.op("dve", I("tensor_scalar", out=oo[:], in0=oo[:], scalar1=sgn[:, 0:1], scalar2=None, op0=ALU.mult),
                 reads=["oo", "sgn"], writes=["oo"])
            P.dma("sp", I("dma_start", out=dr["sinS"], in_=oo[:]), reads=["oo"], writes=["sinS"])
            P.op("dve", I("scalar_tensor_tensor", out=kk[:], in0=rr[:], scalar=-1.0, in1=rr[:], op0=ALU.mult, op1=ALU.max),
                 reads=["rr"], writes=["kk"])
            P.op("act", I("activation", out=ang[:], in_=kk[:], func=AF.Sin, bias=hpi[:, 0:1], scale=-1.0),
                 reads=["kk", "hpi", "ang"], writes=["ang"])
            P.dma("sp", I("dma_start", out=dr["cosS"], in_=ang[:]), reads=["ang"], writes=["cosS"])
            P.end_phase()

    def rms_tile(self, T4, i, hx, hxk, jk, ssk, rsk, hn, hnk):
        P = self.P
        junk, ss, rs = T4["junk"], T4["ss"], T4["rs"]
        P.op("act", I("activation", out=junk[:], in_=hx[:], func=AF.Square, accum_out=ss[:]),
             reads=[hxk], writes=[jk, ssk])
        P.op("dve", I("tensor_scalar", out=rs[:], in0=ss[:], scalar1=1.0 / D, scalar2=1e-6, op0=ALU.mult, op1=ALU.add),
             reads=[ssk], writes=[rsk])
        P.op("act", I("sqrt", out=rs[:], in_=rs[:]), reads=[rsk], writes=[rsk])
        P.op("dve", I("reciprocal", out=rs[:], in_=rs[:]), reads=[rsk], writes=[rsk])
        P.op("dve", I("tensor_scalar", out=hn[:], in0=hx[:], scalar1=rs[:, 0:1], scalar2=None, op0=ALU.mult),
             reads=[hxk, rsk], writes=[hnk])

    def transpose8(self, src, srck, dst_ap, dstk, ident, nblk=8, eng="dve"):
        P = self.P
        pt = self.pt
        for c in range(nblk):
            P.op("pe", I("transpose", out=pt[:, c * 128:(c + 1) * 128], in_=src[:, c * 128:(c + 1) * 128],
                                                  identity=ident[:]), reads=[srck, "ident"], writes=[("ps", 7)])
        view = pt[:, 0:nblk * 128].rearrange("p (c t) -> p c t", t=128)
        if eng == "dve":
            P.op("dve", I("tensor_copy", out=dst_ap, in_=view), reads=[("ps", 7)], writes=[dstk])
        else:
            P.op("act", I("copy", out=dst_ap, in_=view), reads=[("ps", 7)], writes=[dstk])

    def load_weight_bf(self, dst_ap, src_ap, stage, stagek, dstk, scale_ap=None, scalek=None, eng="dve", q="sp"):
        P = self.P
        P.dma(q, I("dma_start", out=stage, in_=src_ap), writes=[stagek])
        en = "dve" if eng == "dve" else "pool"
        if scale_ap is not None:
            P.op(en, I("tensor_scalar", out=dst_ap, in0=stage, scalar1=scale_ap, scalar2=None, op0=ALU.mult),
                 reads=[stagek, scalek], writes=[dstk])
        else:
            P.op(en, I("tensor_copy", out=dst_ap, in_=stage), reads=[stagek], writes=[dstk])

    def phase_A(self, l):
        nc, P, dr = self.nc, self.P, self.dr
        hsrc = dr["x"] if l == 0 else dr["hS"]
        with contextlib.ExitStack() as st:
            T = lambda n, sh, dt: st.enter_context(nc.sbuf_tensor(self.uname(n), sh, dt))
            W = T("a_W", [128, 8, WCOLS], BF16)
            stage = [T("a_stage%d" % i, [128, WCOLS], F32) for i in range(2)]
            cosT = T("a_cos", [128, S], F32)
            sinT = T("a_sin", [128, S], F32)
            gcol = T("a_gcol", [128, 8], F32)
            ident = T("a_ident", [128, 128], BF16)
            bgate = T("a_bgate", [128, 24], F32)
            negb = T("a_negb", [4, 1], F32)
            ones4 = T("a_ones4", [4, CH], F32)
            cc = T("a_cc", [4, S], F32)
            hx = [T("a_hx%d" % i, [128, D], F32) for i in range(2)]
            T4 = [dict(junk=T("a_junk%d" % i, [128, D], BF16), ss=T("a_ss%d" % i, [128, 1], F32),
                       rs=T("a_rs%d" % i, [128, 1], F32)) for i in range(2)]
            hn = [T("a_hn%d" % i, [128, D], BF16) for i in range(2)]
            hnT = [T("a_hnT%d" % i, [128, 8, CH], BF16) for i in range(2)]
            t1 = [T("a_t1%d" % i, [128, CH], F32) for i in range(2)]
            t2 = [T("a_t2%d" % i, [128, CH], F32) for i in range(2)]
            ob = [T("a_ob%d" % i, [128, CH], BF16) for i in range(4)]
            va = [T("a_va%d" % i, [128, 8, 65], BF16) for i in range(2)]
            svt = [T("a_sv%d" % i, [128, 256], BF16) for i in range(2)]
            gt = [T("a_gt%d" % i, [128, 24], F32) for i in range(2)]
            e4 = T("a_e4", [4, CH], F32)
            sp4 = T("a_sp4", [4, CH], F32)

            P.dma("sp", I("dma_start", out=gcol[:], in_=dr["norm_mix"][l]), writes=["gcol"])
            P.dma("sp", I("dma_start", out=ident[:], in_=dr["ident"]), writes=["ident"])
            P.dma("sp", I("dma_start", out=cosT[:], in_=dr["cosS"]), writes=["cosT"])
            P.dma("sp", I("dma_start", out=sinT[:], in_=dr["sinS"]), writes=["sinT"])
            P.dma("sp", I("dma_start", out=bgate[:], in_=dr["b_gate"][l].to_broadcast([128, 24])), writes=["bgate"])
            P.dma("sp", I("dma_start", out=negb[:], in_=dr["b_forget"][l]), writes=["negb"])
            P.op("dve", I("tensor_scalar", out=negb[:], in0=negb[:], scalar1=-1.0, scalar2=None, op0=ALU.mult),
                 reads=["negb"], writes=["negb"])
            P.op("pool", I("memset", ones4[:], 1.0), writes=["ones4"])
            for i in range(2):
                P.op("pool", I("memset", va[i][:], 1.0), writes=[("va", i)])
            for k in range(8):
                self.load_weight_bf(W[:, k, :], dr["w_in"][l, k * 128:(k + 1) * 128, :], stage[k % 2][:], ("stage", k % 2),
                                    ("W", k), scale_ap=gcol[:, k:k + 1], scalek="gcol", eng=("dve" if k % 2 == 0 else "pool"),
                                    q=("sp" if k % 2 == 0 else "act"))
            Wk = [("W", k) for k in range(8)]
            cn_a = {"obi": 0}
            for c in range(NCH):
                hb = c % 2
                csl = slice(c * CH, (c + 1) * CH)
                for i in range(4):
                    ti = c * 4 + i
                    b = ti % 2
                    P.dma("sp", I("dma_start", out=hx[b][:], in_=hsrc[ti * 128:(ti + 1) * 128, :]),
                          writes=[("hx", b)])
                    self.rms_tile(T4[b], b, hx[b], ("hx", b), ("junk", b), ("ss", b), ("rs", b), hn[b], ("hn", b))
                    self.transpose8(hn[b], ("hn", b), hnT[hb][:, :, i * 128:(i + 1) * 128], ("hnT", hb, i), ident,
                                    eng=("dve" if i % 2 == 0 else "act"))
                hk = [("hnT", hb, i) for i in range(4)]

                def back(c, hb, csl, hk):

                    def fm_matmul(pi, col0, ncols=128):
                        for k in range(8):
                            P.op("pe", I("matmul", self.ps[pi][0:ncols, :], lhsT=W[:, k, col0:col0 + ncols],
                                                              rhs=hnT[hb][:, k, :], start=(k == 0), stop=(k == 7)),
                                 reads=hk + Wk, writes=[("ps", pi)])
                    for ft in range(7):
                        pa, pb = self.psn(), self.psn()
                        fm_matmul(pa, ft * 128)
                        fm_matmul(pb, (7 + ft) * 128)
                        tb = ft % 2
                        P.op("dve", I("tensor_tensor", out=t1[tb][:], in0=self.ps[pa][:], in1=cosT[:, csl], op=ALU.mult),
                             reads=[("ps", pa), "cosT"], writes=[("t1", tb)])
                        P.op("dve", I("tensor_tensor", out=t2[tb][:], in0=self.ps[pb][:], in1=sinT[:, csl], op=ALU.mult),
                             reads=[("ps", pb), "sinT"], writes=[("t2", tb)])
                        o = cn_a["obi"] % 4
                        cn_a["obi"] += 1
                        P.op("pool", I("tensor_tensor", out=ob[o][:], in0=t1[tb][:], in1=t2[tb][:], op=ALU.add),
                             reads=[("t1", tb), ("t2", tb)], writes=[("ob", o)])
                        P.dma("sp", I("dma_start", out=dr["FT"][ft, :, csl], in_=ob[o][:]),
                              reads=[("ob", o)], writes=[("FT", ft, c)])
                    for ft in range(14, 23):
                        pa = self.psn()
                        fm_matmul(pa, ft * 128)
                        o = cn_a["obi"] % 4
                        cn_a["obi"] += 1
                        sc = 0.125 if ft in (15, 16, 19, 20) else 1.0
                        P.op("act", I("activation", out=ob[o][:], in_=self.ps[pa][:], func=AF.Copy, scale=sc),
                             reads=[("ps", pa)], writes=[("ob", o)])
                        P.dma("sp", I("dma_start", out=dr["FT"][ft - 7, :, csl], in_=ob[o][:]),
                              reads=[("ob", o)], writes=[("FT", ft, c)])
                    pa = self.psn()
                    fm_matmul(pa, 23 * 128, ncols=4)
                    P.op("act", I("activation", out=e4[:], in_=self.ps[pa][0:4, :], func=AF.Exp, bias=negb[:, 0:1], scale=-1.0),
                         reads=[("ps", pa), "negb"], writes=["e4"])
                    P.op("act", I("activation", out=sp4[:], in_=e4[:], func=AF.Ln, bias=1.0, scale=1.0), reads=["e4"], writes=["sp4"])
                    if c == 0:
                        P.op("dve", I("tensor_tensor_scan", out=cc[:, csl], data0=ones4[:], data1=sp4[:], initial=0.0,
                                                                    op0=ALU.mult, op1=ALU.subtract), reads=["sp4", "ones4"], writes=["cc"])
                    else:
                        P.op("dve", I("tensor_tensor_scan", out=cc[:, csl], data0=ones4[:], data1=sp4[:],
                                                                         initial=cc[:, c * CH - 1:c * CH],
                                                                         op0=ALU.mult, op1=ALU.subtract), reads=["sp4", "ones4", "cc"], writes=["cc"])
                    c0 = 23 * 128 + 4
                    for i in range(4):
                        ti = c * 4 + i
                        b = ti % 2
                        pa, pb = self.psn(), self.psn()
                        for k in range(8):
                            P.op("pe", I("matmul", self.ps[pa][:, 0:512], lhsT=hnT[hb][:, k, i * 128:(i + 1) * 128],
                                                                        rhs=W[:, k, c0:c0 + 512], start=(k == 0), stop=(k == 7)),
                                 reads=hk + Wk, writes=[("ps", pa)])
                        for k in range(8):
                            P.op("pe", I("matmul", self.ps[pb][:, 0:280], lhsT=hnT[hb][:, k, i * 128:(i + 1) * 128],
                                                                        rhs=W[:, k, c0 + 512:c0 + 792], start=(k == 0), stop=(k == 7)),
                                 reads=hk + Wk, writes=[("ps", pb)])
                        P.op("act", I("copy", out=va[b][:, :, 0:64], in_=self.ps[pa][:, 0:512].rearrange("p (g d) -> p g d", d=64)),
                             reads=[("ps", pa)], writes=[("va", b)])
                        P.op("dve", I("tensor_copy", out=svt[b][:], in_=self.ps[pb][:, 0:256]),
                             reads=[("ps", pb)], writes=[("svt", b)])
                        P.op("dve", I("tensor_tensor", out=gt[b][:], in0=self.ps[pb][:, 256:280], in1=bgate[:], op=ALU.add),
                             reads=[("ps", pb), "bgate"], writes=[("gt", b)])
                        P.op("act", I("activation", out=gt[b][:], in_=gt[b][:], func=AF.Sigmoid), reads=[("gt", b)], writes=[("gt", b)])
                        rows = slice(ti * 128, (ti + 1) * 128)
                        P.dma("sp", I("dma_start", out=dr["VA"][rows], in_=va[b][:]), reads=[("va", b)], writes=[("VA", ti)])
                        P.dma("sp", I("dma_start", out=dr["SV"][rows], in_=svt[b][:]), reads=[("svt", b)], writes=[("SV", ti)])
                        P.dma("sp", I("dma_start", out=dr["GT"][rows], in_=gt[b][:]), reads=[("gt", b)], writes=[("GT", ti)])

                self.run_deferred()
                self.defer(back, c, hb, csl, hk)
            self.run_deferred()
            P.dma("sp", I("dma_start", out=dr["cT"], in_=cc[:]), reads=["cc"], writes=["cT"])
            P.end_phase()

    def phase_B(self, l):
        nc, P, dr = self.nc, self.P, self.dr
        with contextlib.ExitStack() as st:
            T = lambda n, sh, dt: st.enter_context(nc.sbuf_tensor(self.uname(n), sh, dt))
            kvT = {"k": T("b_kT", [128, S], BF16), "v": T("b_vT", [128, S], BF16)}
            stage = T("b_stage", [128, 4096], F32)
            W1 = {kv: T("b_w1" + kv, [128, 32, 128], BF16) for kv in "kv"}
            posT = {kv: T("b_pos" + kv, [128, 32], BF16) for kv in "kv"}
            W2 = {kv: T("b_w2" + kv, [128, 64], BF16) for kv in "kv"}
            st32 = T("b_st32", [128, 32], F32)
            st64 = T("b_st64", [128, 64], F32)
            bias = T("b_bias", [128, 1], F32)
            xs = T("b_xs", [128, 255], F32)
            x2 = T("b_x2", [128, 255], F32)
            sg = T("b_sg", [128, 255], F32)
            gl = T("b_gl", [128, 256], BF16)
            kc = T("b_kc", [128, 256], BF16)
            vc = T("b_vc", [128, 2, 2, 65], BF16)
            P.dma("sp", I("dma_start", out=kvT["k"][:], in_=dr["FT"][4]), writes=["kT"])
            P.dma("sp", I("dma_start", out=kvT["v"][:], in_=dr["FT"][7]), writes=["vT"])
            P.op("pool", I("memset", vc[:], 1.0), writes=["vc"])
            P.op("pool", I("memset", gl[:], 0.0), writes=["gl"])
            for kv in "kv":
                self.load_weight_bf(W1[kv][:].rearrange("p l h -> p (l h)"), dr["w1_" + kv][l], stage[:], "stage", "W1" + kv)
                self.load_weight_bf(posT[kv][:], dr["pos_" + kv][l], st32[:], "st32", "pos" + kv)
                self.load_weight_bf(W2[kv][:], dr["w2_" + kv][l], st64[:], "st64", "W2" + kv)
            for kv in "kv":
                pb = self.psn()
                for ll in range(32):
                    P.op("pe", I("matmul", self.ps[pb][:, 0:1], lhsT=W1[kv][0:64, ll, :], rhs=posT[kv][0:64, ll:ll + 1],
                                                                start=(ll == 0), stop=(ll == 31)),
                         reads=["W1" + kv, "pos" + kv], writes=[("ps", pb)])
                P.op("dve", I("tensor_copy", out=bias[:], in_=self.ps[pb][:, 0:1]), reads=[("ps", pb)], writes=["bias"])
                for g in range(2):
                    gs = slice(64 * g, 64 * g + 64)
                    pa = self.psn()
                    for ll in range(32):
                        P.op("pe", I("matmul",
                            self.ps[pa][:, 0:255], lhsT=W1[kv][gs, ll, :], rhs=kvT[kv][gs, ll:ll + 16 * 254 + 1:16],
                            start=(ll == 0), stop=(ll == 31)), reads=["W1" + kv, kv + "T"], writes=[("ps", pa)])
                    P.op("act", I("activation", out=xs[:], in_=self.ps[pa][:, 0:255], func=AF.Identity, bias=bias[:, 0:1], scale=1.0),
                         reads=[("ps", pa), "bias"], writes=["xs"])
                    P.op("dve", I("tensor_tensor", out=x2[:], in0=xs[:], in1=xs[:], op=ALU.mult), reads=["xs"], writes=["x2"])
                    P.op("dve", I("tensor_scalar", out=x2[:], in0=x2[:], scalar1=0.044715, scalar2=1.0, op0=ALU.mult, op1=ALU.add),
                         reads=["x2"], writes=["x2"])
                    P.op("dve", I("tensor_tensor", out=x2[:], in0=x2[:], in1=xs[:], op=ALU.mult), reads=["x2", "xs"], writes=["x2"])
                    P.op("act", I("activation", out=sg[:], in_=x2[:], func=AF.Sigmoid, scale=1.5957691216057308),
                         reads=["x2"], writes=["sg"])
                    P.op("dve", I("tensor_tensor", out=gl[:, 0:255], in0=xs[:], in1=sg[:], op=ALU.mult), reads=["xs", "sg"], writes=["gl"])
                    if kv == "k":
                        po = self.psn()
                        P.op("pe", I("matmul", self.ps[po][gs, 0:256], lhsT=W2["k"][:, :], rhs=gl[:, :], start=True, stop=True),
                             reads=["W2k", "gl"], writes=[("ps", po)])
                        P.op("dve", I("tensor_copy", out=kc[gs, :], in_=self.ps[po][gs, 0:256]),
                             reads=[("ps", po)], writes=[("kc", g)])
                    else:
                        for nb in range(2):
                            po = self.psn()
                            P.op("pe", I("matmul", self.ps[po][:, 0:64], lhsT=gl[:, nb * 128:(nb + 1) * 128], rhs=W2["v"][:, :],
                                                                     start=True, stop=True), reads=["W2v", "gl"], writes=[("ps", po)])
                            P.op("dve", I("tensor_copy", out=vc[:, g, nb, 0:64], in_=self.ps[po][:, 0:64]),
                                 reads=[("ps", po)], writes=["vc"])
            P.dma("sp", I("dma_start", out=dr["kcT"], in_=kc[:]), reads=[("kc", 0), ("kc", 1)], writes=["kcT"])
            P.dma("sp", I("dma_start", out=dr["VC"], in_=vc[:]), reads=["vc"], writes=["VC"])
            P.end_phase()

    def phase_fox(self, l):
        nc, P, dr, ps = self.nc, self.P, self.dr, self.ps
        if "cH" not in dr:
            self.dscr("cH", [4, 3, S], BF16)
        with contextlib.ExitStack() as st:
            T = lambda n, sh, dt: st.enter_context(nc.sbuf_tensor(self.uname(n), sh, dt))
            QaT = T("f_QaT", [70, 4, S], BF16)
            KaT = T("f_KaT", [70, 4, S], BF16)
            V = T("f_V", [128, 32, 4, 65], BF16)
            c4 = T("f_c4", [4, S], F32)
            rr = T("f_rr", [4, S], F32)
            H = T("f_H", [4, 3, S], BF16)
            mgt = T("f_mgt", [128, 4, 512], BF16)
            ident = T("f_ident", [128, 128], BF16)
            Pt = [T("f_P%d" % i, [128, CH], BF16) for i in range(3)]
            rs = [T("f_rs%d" % i, [128, 1], F32) for i in range(4)]
            of = [T("f_of%d" % i, [128, 4, 64], F32) for i in range(8)]
            P.dma("sp", I("dma_start", out=mgt[:], in_=dr["m_gt"]), writes=["mgt"])
            P.dma("sp", I("dma_start", out=ident[:], in_=dr["ident"]), writes=["ident"])
            P.dma("sp", I("dma_start", out=c4[:], in_=dr["cT"]), writes=["c4"])
            for q8 in range(8):
                P.dma("sp", I("dma_start", out=V[:, q8 * 4:(q8 + 1) * 4, :, :],
                              in_=dr["VA"][q8 * 512:(q8 + 1) * 512, 4:8, :].rearrange("(sb p) h c -> p sb h c", p=128)), writes=["V"])
            P.op("pool", I("memset", QaT[64:70, :, :], -1.0), writes=["Qaug"])
            P.op("pool", I("memset", KaT[64:70, :, :], 1.0), writes=["Kaug"])
            for hh in range(4):
                src_q = dr["FT"][8 + hh // 2, (hh % 2) * 64:(hh % 2) * 64 + 64, :]
                src_k = dr["FT"][10 + hh // 2, (hh % 2) * 64:(hh % 2) * 64 + 64, :]
                P.dma("sp", I("dma_start", out=QaT[0:64, hh, :], in_=src_q), writes=[("Q", hh)])
                P.dma("act", I("dma_start", out=KaT[0:64, hh, :], in_=src_k), writes=[("K", hh)])
            P.op("dve", I("tensor_copy", out=H[:, 0, :], in_=c4[:]), reads=["c4"], writes=["H0"])
            P.op("dve", I("tensor_tensor", out=rr[:], in0=c4[:], in1=H[:, 0, :], op=ALU.subtract), reads=["c4", "H0"], writes=["rr"])
            P.op("dve", I("tensor_copy", out=H[:, 1, :], in_=rr[:]), reads=["rr"], writes=["H1"])
            P.op("dve", I("tensor_tensor", out=rr[:], in0=rr[:], in1=H[:, 1, :], op=ALU.subtract), reads=["rr", "H1"], writes=["rr"])
            P.op("dve", I("tensor_copy", out=H[:, 2, :], in_=rr[:]), reads=["rr"], writes=["H2"])
            P.dma("sp", I("dma_start", out=dr["cH"], in_=H[:]), reads=["H0", "H1", "H2"], writes=["cH"])
            for hh in range(4):
                P.dma("sp", I("dma_start", out=QaT[64:67, hh, :], in_=dr["cH"][hh]), reads=["cH", "Qaug"], writes=[("Qa", hh)])
                P.dma("sp", I("dma_start", out=KaT[67:70, hh, :], in_=dr["cH"][hh]), reads=["cH", "Kaug"], writes=[("Ka", hh)])
            sti = 0
            pi = 0
            for qc in range(NCH):
                csl = slice(qc * CH, (qc + 1) * CH)
                for hh in range(4):
                    qk = [("Q", hh), ("Qa", hh), ("K", hh), ("Ka", hh), "Qaug", "Kaug"]
                    for sb in range(4 * qc + 4):
                        diag = sb >= 4 * qc
                        b = sti % 3
                        sti += 1
                        c0 = 128 * (sb - 4 * qc) if diag else 0
                        P.op("pe", I("matmul", ps[b][:, c0:CH], lhsT=KaT[0:70, hh, sb * 128:(sb + 1) * 128],
                                     rhs=QaT[0:70, hh, qc * CH + c0:(qc + 1) * CH],
                                     start=True, stop=not diag), reads=qk, writes=[("ps", b)])
                        if diag:
                            rq = sb - 4 * qc
                            P.op("pe", I("matmul", ps[b][:, rq * 128:(rq + 1) * 128], lhsT=ident[:, :], rhs=mgt[:, rq, rq * 128:(rq + 1) * 128],
                                         start=False, stop=True), reads=["ident", "mgt"], writes=[("ps", b)])
                        pb = pi % 3
                        pi += 1
                        P.op("act", I("activation", out=Pt[pb][:, c0:CH], in_=ps[b][:, c0:CH], func=AF.Exp), reads=[("ps", b)], writes=[("P", pb)])
                        def back(sb, pb, hh, qc):
                            for ti in range(4):
                                if sb <= 4 * qc + ti:
                                    P.op("pe", I("matmul", ps[3 + ti][:, 0:65], lhsT=Pt[pb][:, ti * 128:(ti + 1) * 128], rhs=V[:, sb, hh, :],
                                                 start=(sb == 0), stop=(sb == 4 * qc + ti)), reads=[("P", pb), "V"], writes=[("ps", 3 + ti)])
                        self.run_deferred()
                        self.defer(back, sb, pb, hh, qc)

                    def fin(hh, qc):
                        for ti in range(4):
                            P.op("dve", I("tensor_scalar", out=rs[ti][:], in0=ps[3 + ti][:, 64:65], scalar1=1e-20, scalar2=None, op0=ALU.max),
                                 reads=[("ps", 3 + ti)], writes=[("rs", ti)])
                        for ti in range(4):
                            P.op("dve", I("reciprocal", out=rs[ti][:], in_=rs[ti][:]), reads=[("rs", ti)], writes=[("rs", ti)])
                        for ti in range(4):
                            o = (qc % 2) * 4 + ti
                            P.op("dve", I("tensor_scalar", out=of[o][:, hh, :], in0=ps[3 + ti][:, 0:64], scalar1=rs[ti][:, 0:1], scalar2=None, op0=ALU.mult),
                                 reads=[("ps", 3 + ti), ("rs", ti)], writes=[("of", o, hh)])
                    self.defer(fin, hh, qc)

                def store(qc):
                    for ti in range(4):
                        o = (qc % 2) * 4 + ti
                        rows = slice((qc * 4 + ti) * 128, (qc * 4 + ti + 1) * 128)
                        P.dma("sp", I("dma_start", out=dr["oS"][rows, 512:768], in_=of[o][:].rearrange("p h d -> p (h d)")),
                              reads=[("of", o, hh) for hh in range(4)], writes=[("oS", "f", qc, ti)])
                self.defer(store, qc)
            self.run_deferred()
            P.end_phase()

    def phase_sb(self, l):
        nc, P, dr, ps = self.nc, self.P, self.dr, self.ps
        with contextlib.ExitStack() as st:
            T = lambda n, sh, dt: st.enter_context(nc.sbuf_tensor(self.uname(n), sh, dt))
            QT = T("s_QT", [128, 2, S], BF16)
            KT = T("s_KT", [128, 2, S], BF16)
            V = T("s_V", [128, 32, 256], BF16)
            mge = T("s_mge", [128, 4, 512], BF16)
            ident = T("s_ident", [128, 128], BF16)
            ntri = T("s_ntri", [128, 128], BF16)
            nones = T("s_nones", [1, 128], BF16)
            onec = T("s_onec", [128, 1], BF16)
            et = [T("s_e%d" % i, [128, CH], F32) for i in range(2)]
            SP = [T("s_SP%d" % i, [128, CH], BF16) for i in range(3)]
            at = [T("s_a%d" % i, [128, CH], BF16) for i in range(2)]
            carry = T("s_carry", [1, CH], F32)
            ctmp = T("s_ctmp", [1, CH], F32)
            chi = [T("s_chi%d" % i, [1, CH], BF16) for i in range(3)]
            clo = [T("s_clo%d" % i, [1, CH], BF16) for i in range(3)]
            osb = [T("s_o%d" % i, [128, 4, 64], F32) for i in range(8)]
            for n, t_ in (("m_ge", mge), ("ident", ident), ("ntri", ntri), ("nones", nones), ("onec", onec)):
                P.dma("sp", I("dma_start", out=t_[:], in_=dr[n]), writes=[n])
            for j in range(2):
                P.dma("sp", I("dma_start", out=QT[:, j, :], in_=dr["FT"][12 + j]), writes=[("Q", j)])
                P.dma("act", I("dma_start", out=KT[:, j, :], in_=dr["FT"][14 + j]), writes=[("K", j)])
            for q8 in range(8):
                P.dma("sp", I("dma_start", out=V[:, q8 * 4:(q8 + 1) * 4, :],
                              in_=dr["SV"][q8 * 512:(q8 + 1) * 512, :].rearrange("(sb p) c -> p sb c", p=128)), writes=["V"])
            cnt = {"a": 0, "e": 0, "sp": 0, "at": 0, "c": 0}
            for qc in range(NCH):
                csl = slice(qc * CH, (qc + 1) * CH)
                for h in range(4):
                    hb = slice(64 * (h % 2), 64 * (h % 2) + 64)
                    j = h // 2
                    qk = [("Q", j), ("K", j)]
                    blocks = list(range(4 * qc + 3, -1, -1))
                    nblk = len(blocks)
                    info = {}

                    def S1a(idx):
                        sb = blocks[idx]
                        diag = sb >= 4 * qc
                        P.op("pe", I("matmul", ps[0][:, :], lhsT=KT[hb, j, sb * 128:(sb + 1) * 128], rhs=QT[hb, j, csl], start=True, stop=not diag),
                             reads=qk, writes=[("ps", 0)])
                        if diag:
                            P.op("pe", I("matmul", ps[0][:, :], lhsT=ident[:, :], rhs=mge[:, sb - 4 * qc, :], start=False, stop=True),
                                 reads=["ident", "m_ge"], writes=[("ps", 0)])
                        eb = cnt["e"] % 2
                        cnt["e"] += 1
                        P.op("act", I("activation", out=et[eb][:], in_=ps[0][:, :], func=AF.Exp), reads=[("ps", 0)], writes=[("e", eb)])
                        sb_i = cnt["sp"] % 3
                        cnt["sp"] += 1
                        P.op("act", I("activation", out=SP[sb_i][:], in_=et[eb][:], func=AF.Ln, bias=1.0, scale=1.0),
                             reads=[("e", eb)], writes=[("SP", sb_i)])
                        info[idx] = sb_i

                    def S1b(idx):
                        sb_i = info[idx]
                        P.op("pe", I("matmul", ps[3][0:1, :], lhsT=onec[:, 0:1], rhs=SP[sb_i][:, :], start=(idx == 0), stop=(idx + 2 == nblk)),
                             reads=["onec", ("SP", sb_i)], writes=[("ps", 3)])
                        cb = (idx + 1) % 3
                        P.op("dve", I("tensor_copy", out=chi[cb][:], in_=ps[3][0:1, :]), reads=[("ps", 3)], writes=[("chi", cb)])
                        P.op("dve", I("tensor_tensor", out=clo[cb][:], in0=ps[3][0:1, :], in1=chi[cb][:], op=ALU.subtract),
                             reads=[("ps", 3), ("chi", cb)], writes=[("clo", cb)])

                    def S2a(idx):
                        sb = blocks[idx]
                        diag = sb >= 4 * qc
                        sb_i = info[idx]
                        bb = 1 + (cnt["a"] % 2)
                        cnt["a"] += 1
                        c0 = 128 * (sb - 4 * qc) if diag else 0
                        qsl = slice(qc * CH + c0, (qc + 1) * CH)
                        P.op("pe", I("matmul", ps[bb][:, c0:CH], lhsT=KT[hb, j, sb * 128:(sb + 1) * 128], rhs=QT[hb, j, qsl], start=True, stop=False),
                             reads=qk, writes=[("ps", bb)])
                        if diag:
                            rq = sb - 4 * qc
                            P.op("pe", I("matmul", ps[bb][:, c0:c0 + 128], lhsT=ident[:, :], rhs=mge[:, rq, c0:c0 + 128], start=False, stop=False),
                                 reads=["ident", "m_ge"], writes=[("ps", bb)])
                        last = (idx == 0)
                        P.op("pe", I("matmul", ps[bb][:, c0:CH], lhsT=ntri[:, :], rhs=SP[sb_i][:, c0:CH], start=False, stop=last),
                             reads=["ntri", ("SP", sb_i)], writes=[("ps", bb)])
                        if idx > 0:
                            cb = idx % 3
                            P.op("pe", I("matmul", ps[bb][:, c0:CH], lhsT=nones[0:1, :], rhs=chi[cb][0:1, c0:CH], start=False, stop=False),
                                 reads=["nones", ("chi", cb)], writes=[("ps", bb)])
                            P.op("pe", I("matmul", ps[bb][:, c0:CH], lhsT=nones[0:1, :], rhs=clo[cb][0:1, c0:CH], start=False, stop=True),
                                 reads=["nones", ("clo", cb)], writes=[("ps", bb)])
                        ab = cnt["at"] % 2
                        cnt["at"] += 1
                        P.op("act", I("activation", out=at[ab][:, c0:CH], in_=ps[bb][:, c0:CH], func=AF.Exp), reads=[("ps", bb)], writes=[("at", ab)])
                        info[("ab", idx)] = ab

                    def S2b(idx):
                        sb = blocks[idx]
                        ab = info[("ab", idx)]
                        for ti in range(4):
                            if sb <= 4 * qc + ti:
                                P.op("pe", I("matmul", ps[4 + ti][:, 0:64], lhsT=at[ab][:, ti * 128:(ti + 1) * 128], rhs=V[:, sb, h * 64:(h + 1) * 64],
                                             start=(sb == 4 * qc + ti), stop=(sb == 0)), reads=[("at", ab), "V"], writes=[("ps", 4 + ti)])

                    S1a(0)
                    if nblk > 1:
                        S1a(1)
                        S1b(0)
                    for idx in range(nblk):
                        if idx + 2 < nblk:
                            S1a(idx + 2)
                            S1b(idx + 1)
                        S2a(idx)
                        if idx >= 1:
                            S2b(idx - 1)
                    S2b(nblk - 1)
                    for ti in range(4):
                        o = (qc % 2) * 4 + ti
                        P.op("dve", I("tensor_copy", out=osb[o][:, h, :], in_=ps[4 + ti][:, 0:64]), reads=[("ps", 4 + ti)], writes=[("osb", o, h)])
                for ti in range(4):
                    o = (qc % 2) * 4 + ti
                    rows = slice((qc * 4 + ti) * 128, (qc * 4 + ti + 1) * 128)
                    P.dma("sp", I("dma_start", out=dr["oS"][rows, 768:1024], in_=osb[o][:].rearrange("p h d -> p (h d)")),
                          reads=[("osb", o, h) for h in range(4)], writes=[("oS", "s", qc, ti)])
            P.end_phase()

    def phase_nsa(self, l):
        nc, P, dr, ps = self.nc, self.P, self.dr, self.ps
        pt = self.pt
        with contextlib.ExitStack() as st:
            T = lambda n, sh, dt: st.enter_context(nc.sbuf_tensor(self.uname(n), sh, dt))
            QT = T("n_QT", [128, 4, S], BF16)
            KsE = T("n_KsE", [128, 2, S], BF16)
            Qs = T("n_Qs", [128, 4, CH], BF16)
            KwT = T("n_KwT", [128, S], BF16)
            kcT = T("n_kcT", [128, 256], BF16)
            V4 = T("n_V4", [128, 32, 4, 65], BF16)
            VC = T("n_VC", [128, 2, 2, 65], BF16)
            ovl = T("n_ovl", [128, 2, 64], BF16)
            mgt = T("n_mgt", [128, 4, 512], BF16)
            mle = T("n_mle", [128, 4, 512], BF16)
            ident = T("n_ident", [128, 128], BF16)
            mcmp = [T("n_mcmp%d" % i, [128, 2, CH], BF16) for i in range(2)]
            sbias = [T("n_sbias%d" % i, [128, 64], F32) for i in range(4)]
            gts = [T("n_gt%d" % i, [128, 24], F32) for i in range(8)]
            Pt = [T("n_P%d" % i, [128, CH], BF16) for i in range(3)]
            Pc = [T("n_Pc%d" % i, [128, CH], BF16) for i in range(4)]
            onsa = [T("n_o%d" % i, [128, 8, 64], F32) for i in range(8)]
            impg = [T("n_imp%d" % i, [128, 64], F32) for i in range(4)]
            score = T("n_score", [128, 64], F32)
            sc2 = T("n_sc2", [128, 64], F32)
            sc3 = T("n_sc3", [128, 64], F32)
            m8a = T("n_m8a", [128, 8], F32)
            m8b = T("n_m8b", [128, 8], F32)
            seln = T("n_seln", [128, 64], BF16)
            rc4 = T("n_rc4", [128, 4], F32)
            rg = T("n_rg", [128, 1], F32)
            rs1 = T("n_rs1", [128, 1], F32)
            self._nsa_tmp = ([T("n_r4%d" % i, [128, 1], F32) for i in range(4)], [T("n_g4%d" % i, [128, 1], F32) for i in range(4)],
                             [T("n_s4%d" % i, [128, 64], F32) for i in range(4)])
            for n, t_ in (("m_gt", mgt), ("m_le", mle), ("ident", ident), ("ovl", ovl)):
                P.dma("sp", I("dma_start", out=t_[:], in_=dr[n]), writes=[n])
            for j in range(4):
                P.dma("sp" if j % 2 == 0 else "act", I("dma_start", out=QT[:, j, :], in_=dr["FT"][j]), writes=[("Q", j)])
            for g_ in range(2):
                P.dma("sp", I("dma_start", out=KsE[0:64, g_, :], in_=dr["FT"][5, 64 * g_:64 * g_ + 64, :]), writes=[("KsE", g_, 0)])
                P.dma("act", I("dma_start", out=KsE[64:128, g_, :], in_=dr["eblk"]), writes=[("KsE", g_, 1)])
            P.dma("act", I("dma_start", out=KwT[:], in_=dr["FT"][6]), writes=["KwT"])
            P.dma("sp", I("dma_start", out=kcT[:], in_=dr["kcT"]), writes=["kcT"])
            P.dma("sp", I("dma_start", out=VC[:], in_=dr["VC"]), writes=["VC"])
            for q8 in range(8):
                P.dma("sp", I("dma_start", out=V4[:, q8 * 4:(q8 + 1) * 4, :, :],
                              in_=dr["VA"][q8 * 512:(q8 + 1) * 512, 0:4, :].rearrange("(sb p) h c -> p sb h c", p=128)), writes=["V4"])
            cn = {"st": 0, "p": 0}

            def st_bank():
                b = cn["st"] % 3
                cn["st"] += 1
                return b

            def exp_to(b, dst, dstk, scale=0.125, c0=0, c1=CH):
                P.op("act", I("activation", out=dst[:, c0:c1], in_=ps[b][:, c0:c1], func=AF.Exp, scale=scale), reads=[("ps", b)], writes=[dstk])

            for qc in range(NCH):
                csl = slice(qc * CH, (qc + 1) * CH)
                mb = qc % 2
                P.dma("sp", I("dma_start", out=mcmp[mb][:], in_=dr["m_cmp"][:, :, csl]), writes=[("mcmp", mb)])
                for ti in range(4):
                    rows = slice((qc * 4 + ti) * 128, (qc * 4 + ti + 1) * 128)
                    P.dma("sp", I("dma_start", out=sbias[ti][:], in_=dr["selbias"][rows]), writes=[("sbias", ti)])
                    P.dma("sp", I("dma_start", out=gts[mb * 4 + ti][:], in_=dr["GT"][rows]), writes=[("gts", mb * 4 + ti)])
                nbs = [0] if qc < 4 else [0, 1]
                for g in range(2):
                    gs = slice(64 * g, 64 * g + 64)
                    self.run_deferred()
                    for hh in range(4):
                        head = 4 * g + hh
                        for nb in nbs:
                            b = st_bank()
                            P.op("pe", I("matmul", ps[b][:, :], lhsT=kcT[gs, nb * 128:(nb + 1) * 128], rhs=QT[gs, hh, csl], start=True, stop=False),
                                 reads=["kcT", ("Q", hh)], writes=[("ps", b)])
                            P.op("pe", I("matmul", ps[b][:, :], lhsT=ident[:, :], rhs=mcmp[mb][:, nb, :], start=False, stop=True),
                                 reads=["ident", ("mcmp", mb)], writes=[("ps", b)])
                            pk = (hh % 2) * 2 + nb
                            exp_to(b, Pc[pk], ("Pc", pk))
                        for ti in range(4):
                            for nb in nbs:
                                pk = (hh % 2) * 2 + nb
                                P.op("pe", I("matmul", ps[3 + ti][:, 0:65], lhsT=Pc[pk][:, ti * 128:(ti + 1) * 128], rhs=VC[:, g, nb, :],
                                             start=(nb == nbs[0]), stop=(nb == nbs[-1])), reads=[("Pc", pk), "VC"], writes=[("ps", 3 + ti)])
                            for nb in nbs:
                                pk = (hh % 2) * 2 + nb
                                P.op("pe", I("matmul", ps[3 + ti][:, 128:192], lhsT=Pc[pk][:, ti * 128:(ti + 1) * 128], rhs=ovl[:, nb, :],
                                             start=(nb == nbs[0]), stop=(nb == nbs[-1])), reads=[("Pc", pk), "ovl"], writes=[("ps", 3 + ti)])
                        r4, g4, s4 = self._nsa_tmp
                        for ti in range(4):
                            P.op("dve", I("tensor_scalar", out=r4[ti][:], in0=ps[3 + ti][:, 64:65], scalar1=1e-20, scalar2=None, op0=ALU.max),
                                 reads=[("ps", 3 + ti)], writes=[("r4", ti)])
                        for ti in range(4):
                            P.op("dve", I("reciprocal", out=r4[ti][:], in_=r4[ti][:]), reads=[("r4", ti)], writes=[("r4", ti)])
                        for ti in range(4):
                            o = mb * 4 + ti
                            P.op("dve", I("tensor_tensor", out=g4[ti][:], in0=r4[ti][:], in1=gts[o][:, head * 3:head * 3 + 1], op=ALU.mult),
                                 reads=[("r4", ti), ("gts", o)], writes=[("g4", ti)])
                        for ti in range(4):
                            if hh == 0:
                                P.op("dve", I("tensor_scalar", out=impg[ti][:], in0=ps[3 + ti][:, 128:192], scalar1=r4[ti][:, 0:1], scalar2=None, op0=ALU.mult),
                                     reads=[("ps", 3 + ti), ("r4", ti)], writes=[("impg", ti)])
                            else:
                                P.op("dve", I("tensor_scalar", out=s4[ti][:], in0=ps[3 + ti][:, 128:192], scalar1=r4[ti][:, 0:1], scalar2=None, op0=ALU.mult),
                                     reads=[("ps", 3 + ti), ("r4", ti)], writes=[("s4", ti)])
                        if hh > 0:
                            for ti in range(4):
                                P.op("pool", I("tensor_tensor", out=impg[ti][:], in0=impg[ti][:], in1=s4[ti][:], op=ALU.add),
                                     reads=[("s4", ti), ("impg", ti)], writes=[("impg", ti)])
                        for ti in range(4):
                            o = mb * 4 + ti
                            P.op("dve", I("tensor_scalar", out=onsa[o][:, head, :], in0=ps[3 + ti][:, 0:64], scalar1=g4[ti][:, 0:1], scalar2=None, op0=ALU.mult),
                                 reads=[("ps", 3 + ti), ("g4", ti)], writes=[("onsa", o, head)])
                    if NSA_STOP == 1:
                        continue
                    for ti in range(4):
                        P.op("dve", I("tensor_tensor", out=score[:], in0=impg[ti][:], in1=sbias[ti][:], op=ALU.add),
                             reads=[("impg", ti), ("sbias", ti)], writes=["score"])
                        P.op("dve", I("max", out=m8a[:], in_=score[:]), reads=["score"], writes=["m8a"])
                        P.op("dve", I("match_replace", out=sc3[:], in_to_replace=m8a[:], in_values=score[:], imm_value=-3.0e9),
                             reads=["score", "m8a"], writes=["sc3"])
                        P.op("dve", I("max", out=m8b[:], in_=sc3[:]), reads=["sc3"], writes=["m8b"])
                        P.op("dve", I("tensor_scalar", out=seln[:], in0=score[:], scalar1=m8b[:, 7:8], scalar2=-BIG, op0=ALU.is_lt, op1=ALU.mult),
                             reads=["score", "m8b"], writes=["seln"])
                        P.op("pe", I("transpose", out=pt[0:64, ti * 128:(ti + 1) * 128], in_=seln[:, :], identity=ident[:, :]),
                             reads=["seln", "ident"], writes=[("ps", 7)])
                    for hh in range(4):
                        P.op("pool", I("tensor_copy", out=Qs[0:64, hh, :], in_=QT[gs, hh, csl]), reads=[("Q", hh)], writes=[("Qs", hh, 0)])
                        P.op("dve", I("tensor_copy", out=Qs[64:128, hh, :], in_=pt[0:64, 0:512]), reads=[("ps", 7)], writes=[("Qs", hh, 1)])
                    if NSA_STOP == 2:
                        continue
                    for branch in ((1, 2) if NSA_STOP != 3 else (1,)):
                        for hh in range(4):
                            head = 4 * g + hh
                            if branch == 1:
                                seq = [(sb, None) for sb in range(4 * qc + 4)]
                            else:
                                seq = [(4 * qc - 4 + r, r) for r in range(8) if 4 * qc - 4 + r >= 0]
                            first = {}
                            last = {}
                            for (sb, r) in seq:
                                for ti in range(4):
                                    if branch == 1:
                                        ok = sb <= 4 * qc + ti
                                    else:
                                        ok = (ti <= r) if r < 4 else (r - 4 <= ti)
                                    if ok:
                                        first.setdefault(ti, sb)
                                        last[ti] = sb
                            for (sb, r) in seq:
                                b = st_bank()
                                if branch == 1:
                                    diag = sb >= 4 * qc
                                    c0, c1 = (128 * (sb - 4 * qc) if diag else 0), CH
                                    P.op("pe", I("matmul", ps[b][:, c0:c1], lhsT=KsE[:, g, sb * 128:(sb + 1) * 128], rhs=Qs[:, hh, c0:c1], start=True, stop=not diag),
                                         reads=[("KsE", g, 0), ("KsE", g, 1), ("Qs", hh, 0), ("Qs", hh, 1)], writes=[("ps", b)])
                                    if diag:
                                        rq = sb - 4 * qc
                                        P.op("pe", I("matmul", ps[b][:, rq * 128:(rq + 1) * 128], lhsT=ident[:, :],
                                                     rhs=mgt[:, rq, rq * 128:(rq + 1) * 128], start=False, stop=True),
                                             reads=["ident", "m_gt"], writes=[("ps", b)])
                                else:
                                    c0, c1 = (0, 128 * (r + 1)) if r < 4 else (128 * (r - 4), CH)
                                    P.op("pe", I("matmul", ps[b][:, c0:c1], lhsT=KwT[gs, sb * 128:(sb + 1) * 128],
                                                 rhs=QT[gs, hh, qc * CH + c0:qc * CH + c1], start=True, stop=False),
                                         reads=["KwT", ("Q", hh)], writes=[("ps", b)])
                                    rq = r if r < 4 else r - 4
                                    msk = mle[:, rq, rq * 128:(rq + 1) * 128] if r < 4 else mgt[:, rq, rq * 128:(rq + 1) * 128]
                                    P.op("pe", I("matmul", ps[b][:, rq * 128:(rq + 1) * 128], lhsT=ident[:, :], rhs=msk, start=False, stop=True),
                                         reads=["ident", "m_gt", "m_le"], writes=[("ps", b)])
                                pb = cn["p"] % 3
                                cn["p"] += 1
                                exp_to(b, Pt[pb], ("P", pb), c0=c0, c1=c1)
                                def back(sb, r, pb, branch, g, first, last):
                                    for ti in range(4):
                                        if ti in first and first[ti] <= sb <= last[ti]:
                                            if branch == 2:
                                                ok = (ti <= r) if r < 4 else (r - 4 <= ti)
                                                if not ok:
                                                    continue
                                            vg = g if branch == 1 else 2 + g
                                            P.op("pe", I("matmul", ps[3 + ti][:, 0:65], lhsT=Pt[pb][:, ti * 128:(ti + 1) * 128], rhs=V4[:, sb, vg, :],
                                                         start=(sb == first[ti]), stop=(sb == last[ti])), reads=[("P", pb), "V4"], writes=[("ps", 3 + ti)])
                                self.run_deferred()
                                self.defer(back, sb, r, pb, branch, g, first, last)
                            self.defer(self._nsa_fin, mb, head, branch, gts, rs1, rg, sc2, onsa)
                            for ti in range(0):
                                o = mb * 4 + ti
                                gtile = gts[o]
                                P.op("dve", I("tensor_scalar", out=rs1[:], in0=ps[3 + ti][:, 64:65], scalar1=1e-20, scalar2=None, op0=ALU.max),
                                     reads=[("ps", 3 + ti)], writes=["rs1"])
                                P.op("dve", I("reciprocal", out=rs1[:], in_=rs1[:]), reads=["rs1"], writes=["rs1"])
                                P.op("dve", I("tensor_tensor", out=rg[:], in0=rs1[:], in1=gtile[:, head * 3 + branch:head * 3 + branch + 1], op=ALU.mult),
                                     reads=["rs1", ("gts", o)], writes=["rg"])
                                P.op("dve", I("tensor_scalar", out=sc2[:], in0=ps[3 + ti][:, 0:64], scalar1=rg[:, 0:1], scalar2=None, op0=ALU.mult),
                                     reads=[("ps", 3 + ti), "rg"], writes=["sc2"])
                                P.op("pool", I("tensor_tensor", out=onsa[o][:, head, :], in0=onsa[o][:, head, :], in1=sc2[:], op=ALU.add),
                                     reads=["sc2", ("onsa", o, head)], writes=[("onsa", o, head)])
                def store(qc, mb):
                    for ti in range(4):
                        o = mb * 4 + ti
                        rows = slice((qc * 4 + ti) * 128, (qc * 4 + ti + 1) * 128)
                        P.dma("sp", I("dma_start", out=dr["oS"][rows, 0:512], in_=onsa[o][:].rearrange("p h d -> p (h d)")),
                              reads=[("onsa", o, hd) for hd in range(8)], writes=[("oS", "n", qc, ti)])
                self.defer(store, qc, mb)
            self.run_deferred()
            P.end_phase()

    def _nsa_fin(self, mb, head, branch, gts, rs1, rg, sc2, onsa):
        P, ps = self.P, self.ps
        r4, g4, s4 = self._nsa_tmp
        for ti in range(4):
            P.op("dve", I("tensor_scalar", out=r4[ti][:], in0=ps[3 + ti][:, 64:65], scalar1=1e-20, scalar2=None, op0=ALU.max),
                 reads=[("ps", 3 + ti)], writes=[("r4", ti)])
        for ti in range(4):
            P.op("dve", I("reciprocal", out=r4[ti][:], in_=r4[ti][:]), reads=[("r4", ti)], writes=[("r4", ti)])
        for ti in range(4):
            o = mb * 4 + ti
            P.op("dve", I("tensor_tensor", out=g4[ti][:], in0=r4[ti][:], in1=gts[o][:, head * 3 + branch:head * 3 + branch + 1], op=ALU.mult),
                 reads=[("r4", ti), ("gts", o)], writes=[("g4", ti)])
        for ti in range(4):
            P.op("dve", I("tensor_scalar", out=s4[ti][:], in0=ps[3 + ti][:, 0:64], scalar1=g4[ti][:, 0:1], scalar2=None, op0=ALU.mult),
                 reads=[("ps", 3 + ti), ("g4", ti)], writes=[("s4", ti)])
        for ti in range(4):
            o = mb * 4 + ti
            P.op("pool", I("tensor_tensor", out=onsa[o][:, head, :], in0=onsa[o][:, head, :], in1=s4[ti][:], op=ALU.add),
                 reads=[("s4", ti), ("onsa", o, head)], writes=[("onsa", o, head)])

    def phase_D(self, l, last):
        self.phase_D1(l)
        self.phase_D2(l, last)

    def phase_D1(self, l):
        nc, P, dr, ps = self.nc, self.P, self.dr, self.ps
        hsrc = dr["x"] if l == 0 else dr["hS"]
        with contextlib.ExitStack() as st:
            T = lambda n, sh, dt: st.enter_context(nc.sbuf_tensor(self.uname(n), sh, dt))
            Wout = T("d_Wout", [128, 8, D], BF16)
            Wd = T("d_Wd", [128, NF, D], BF16)
            stage = [T("d_stage%d" % i, [128, DFF], F32) for i in range(2)]
            cvt = [T("d_cvt%d" % i, [128, DFF], BF16) for i in range(2)]
            ghead = T("d_ghead", [128, 8], F32)
            gffn = T("d_gffn", [128, 8], F32)
            ident = T("d_ident", [128, 128], BF16)
            h = [T("d_h%d" % i, [128, D], F32) for i in range(4)]
            ot = [T("d_ot%d" % i, [128, D], F32) for i in range(2)]
            osq = T("d_osq", [128, D], F32)
            ssh = T("d_ssh", [128, 16], F32)
            on = [T("d_on%d" % i, [128, D], BF16) for i in range(2)]
            T4 = [dict(junk=T("d_junk%d" % i, [128, D], BF16), ss=T("d_ss%d" % i, [128, 1], F32),
                       rs=T("d_rs%d" % i, [128, 1], F32)) for i in range(2)]
            xT = T("d_xT", [128, 8, CH], BF16)
            actT = T("d_actT", [128, NF, CH], BF16)
            wgu = [T("d_wgu%d" % i, [128, 2, 8, 128], BF16) for i in range(3)]
            sg = [T("d_sg%d" % i, [128, CH], F32) for i in range(2)]
            P.dma("sp", I("dma_start", out=ghead[:], in_=dr["head_norm"][l]), writes=["ghead"])
            P.dma("sp", I("dma_start", out=gffn[:], in_=dr["norm_ffn"][l]), writes=["gffn"])
            P.dma("sp", I("dma_start", out=ident[:], in_=dr["ident"]), writes=["ident"])
            n = 0
            for k in range(8):
                self.load_weight_bf(Wout[:, k, :], dr["w_out"][l, k * 128:(k + 1) * 128, :], stage[n % 2][:, 0:D], ("stage", n % 2),
                                    ("Wout", k), scale_ap=ghead[:, k:k + 1], scalek="ghead", eng=("dve" if n % 2 == 0 else "pool"),
                                    q=("sp" if n % 2 == 0 else "act"))
                n += 1
            for f in range(NF):
                self.load_weight_bf(Wd[:, f, :], dr["w_ffn_down"][l, f * 128:(f + 1) * 128, :], stage[n % 2][:, 0:D], ("stage", n % 2),
                                    ("Wd", f), eng=("dve" if n % 2 == 0 else "pool"), q=("sp" if n % 2 == 0 else "act"))
                n += 1
            for gi, wn in enumerate(("w_ffn_gate", "w_ffn_up")):
                for k in range(8):
                    b = n % 2
                    self.load_weight_bf(cvt[b][:], dr[wn][l, k * 128:(k + 1) * 128, :], stage[b][:], ("stage", b), ("cvt", b),
                                        scale_ap=gffn[:, k:k + 1], scalek="gffn", eng=("dve" if b == 0 else "pool"),
                                        q=("sp" if b == 0 else "act"))
                    for f0, f1 in ((0, 8), (8, 16), (16, NF)):
                        P.dma("sp", I("dma_start", out=dr["WGU"][f0:f1, :, gi, k, :].rearrange("f p c -> p f c"),
                                      in_=cvt[b][:, f0 * 128:f1 * 128].rearrange("p (f c) -> p f c", c=128)),
                              reads=[("cvt", b)], writes=[("WGU", gi, k, f0)])
                    n += 1
            wgu_ready = [("WGU", gi, k, f0) for gi in range(2) for k in range(8) for f0 in (0, 8, 16)]
            Woutk = [("Wout", k) for k in range(8)]
            Wdk = [("Wd", f) for f in range(NF)]
            wl = 0
            for c in range(NCH):
                for i in range(4):
                    ti = c * 4 + i
                    b = ti % 2
                    rows = slice(ti * 128, (ti + 1) * 128)
                    P.dma("sp", I("dma_start", out=h[i][:], in_=hsrc[rows, :]), writes=[("h", i)])
                    P.dma("act", I("dma_start", out=ot[b][:], in_=dr["oS"][rows, :]), writes=[("ot", b)])
                    P.op("pool", I("tensor_tensor", out=osq[:], in0=ot[b][:], in1=ot[b][:], op=ALU.mult), reads=[("ot", b)], writes=["osq"])
                    P.op("dve", I("tensor_reduce", out=ssh[:], in_=osq[:].rearrange("p (h d) -> p h d", d=64), axis=AX.X, op=ALU.add),
                         reads=["osq"], writes=["ssh"])
                    P.op("dve", I("tensor_scalar", out=ssh[:], in0=ssh[:], scalar1=1.0 / 64, scalar2=1e-6, op0=ALU.mult, op1=ALU.add),
                         reads=["ssh"], writes=["ssh"])
                    P.op("act", I("sqrt", out=ssh[:], in_=ssh[:]), reads=["ssh"], writes=["ssh"])
                    P.op("dve", I("reciprocal", out=ssh[:], in_=ssh[:]), reads=["ssh"], writes=["ssh"])
                    P.op("dve", I("tensor_tensor", out=on[b][:].rearrange("p (h d) -> p h d", d=64),
                                  in0=ot[b][:].rearrange("p (h d) -> p h d", d=64),
                                  in1=ssh[:, :].unsqueeze(2).to_broadcast([128, 16, 64]), op=ALU.mult),
                         reads=[("ot", b), "ssh"], writes=[("on", b)])
                    self.transpose8(on[b], ("on", b), xT[:, :, i * 128:(i + 1) * 128], ("xT", i), ident, eng=("dve" if i % 2 == 0 else "act"))
                    for half in range(2):
                        pa = self.psn()
                        for k in range(8):
                            P.op("pe", I("matmul", ps[pa][:, :], lhsT=xT[:, k, i * 128:(i + 1) * 128], rhs=Wout[:, k, half * 512:(half + 1) * 512],
                                         start=(k == 0), stop=(k == 7)), reads=[("xT", i)] + Woutk, writes=[("ps", pa)])
                        P.op("dve", I("tensor_tensor", out=h[i][:, half * 512:(half + 1) * 512], in0=ps[pa][:, :],
                                      in1=h[i][:, half * 512:(half + 1) * 512], op=ALU.add), reads=[("ps", pa), ("h", i)], writes=[("h", i)])
                if "hmix" in self.dr:
                    for i in range(4):
                        rows = slice((c * 4 + i) * 128, (c * 4 + i + 1) * 128)
                        P.dma("sp", I("dma_start", out=dr["hmix"][rows, :], in_=h[i][:]), reads=[("h", i)], writes=[("hmix", c, i)])
                for i in range(4):
                    b = i % 2
                    self.rms_tile(T4[b], b, h[i], ("h", i), ("junk", b), ("ss", b), ("rs", b), on[b], ("on", b))
                    self.transpose8(on[b], ("on", b), xT[:, :, i * 128:(i + 1) * 128], ("xT", i), ident, eng=("dve" if i % 2 == 0 else "act"))
                xk = [("xT", i) for i in range(4)]
                for f in range(NF):
                    wb = wl % 3
                    wl += 1
                    P.dma("sp" if f % 2 == 0 else "act", I("dma_start", out=wgu[wb][:], in_=dr["WGU"][f]), reads=wgu_ready, writes=[("wgu", wb)])
                    pg, pu = self.psn(), self.psn()
                    for gi, pp in ((0, pg), (1, pu)):
                        for k in range(8):
                            P.op("pe", I("matmul", ps[pp][:, :], lhsT=wgu[wb][:, gi, k, :], rhs=xT[:, k, :], start=(k == 0), stop=(k == 7)),
                                 reads=xk + [("wgu", wb)], writes=[("ps", pp)])
                    sb_ = f % 2
                    P.op("act", I("activation", out=sg[sb_][:], in_=ps[pg][:, :], func=AF.Silu), reads=[("ps", pg)], writes=[("sg", sb_)])
                    P.op("dve", I("tensor_tensor", out=actT[:, f, :], in0=ps[pu][:, :], in1=sg[sb_][:], op=ALU.mult),
                         reads=[("ps", pu), ("sg", sb_)], writes=[("actT", f)])
                ak = [("actT", f) for f in range(NF)]
                for i in range(4):
                    for half in range(2):
                        pa = self.psn()
                        for f in range(NF):
                            P.op("pe", I("matmul", ps[pa][:, :], lhsT=actT[:, f, i * 128:(i + 1) * 128], rhs=Wd[:, f, half * 512:(half + 1) * 512],
                                         start=(f == 0), stop=(f == NF - 1)), reads=ak + Wdk, writes=[("ps", pa)])
                        P.op("dve", I("tensor_tensor", out=h[i][:, half * 512:(half + 1) * 512], in0=ps[pa][:, :],
                                      in1=h[i][:, half * 512:(half + 1) * 512], op=ALU.add), reads=[("ps", pa), ("h", i)], writes=[("h", i)])
                    rows = slice((c * 4 + i) * 128, (c * 4 + i + 1) * 128)
                    P.dma("sp", I("dma_start", out=dr["hS"][rows, :], in_=h[i][:]), reads=[("h", i)], writes=[("hS", c, i)])
            P.end_phase()

    def phase_D2(self, l, last):
        nc, P, dr, ps = self.nc, self.P, self.dr, self.ps
        with contextlib.ExitStack() as st:
            T = lambda n, sh, dt: st.enter_context(nc.sbuf_tensor(self.uname(n), sh, dt))
            Wpg = T("e_Wpg", [128, 8, D], BF16)
            Wpp = T("e_Wpp", [128, 2, D], BF16)
            stage = [T("e_stage%d" % i, [128, D], F32) for i in range(2)]
            gple = T("e_gple", [128, 8], F32)
            gfin = T("e_gfin", [128, D], F32)
            ident = T("e_ident", [128, 128], BF16)
            h = [T("e_h%d" % i, [128, D], F32) for i in range(2)]
            p32 = [T("e_p32%d" % i, [128, 256], F32) for i in range(2)]
            pbf = [T("e_pbf%d" % i, [128, 256], BF16) for i in range(2)]
            hn = [T("e_hn%d" % i, [128, D], BF16) for i in range(2)]
            T4 = [dict(junk=T("e_junk%d" % i, [128, D], BF16), ss=T("e_ss%d" % i, [128, 1], F32),
                       rs=T("e_rs%d" % i, [128, 1], F32)) for i in range(2)]
            xT = [T("e_xT%d" % i, [128, 8, 128], BF16) for i in range(2)]
            pT = [T("e_pT%d" % i, [128, 2, 128], BF16) for i in range(2)]
            sig = [T("e_sig%d" % i, [128, CH], F32) for i in range(2)]
            tmp = [T("e_tmp%d" % i, [128, CH], F32) for i in range(2)]
            outt = [T("e_out%d" % i, [128, D], F32) for i in range(2)]
            P.dma("sp", I("dma_start", out=gple[:], in_=dr["norm_ple"][l]), writes=["gple"])
            P.dma("sp", I("dma_start", out=ident[:], in_=dr["ident"]), writes=["ident"])
            if last:
                P.dma("sp", I("dma_start", out=gfin[:], in_=dr["norm_final"].to_broadcast([128, D])), writes=["gfin"])
            n = 0
            for k in range(8):
                self.load_weight_bf(Wpg[:, k, :], dr["w_ple_gate"][l, k * 128:(k + 1) * 128, :], stage[n % 2][:], ("stage", n % 2),
                                    ("Wpg", k), scale_ap=gple[:, k:k + 1], scalek="gple", eng=("dve" if n % 2 == 0 else "pool"),
                                    q=("sp" if n % 2 == 0 else "act"))
                n += 1
            for k in range(2):
                self.load_weight_bf(Wpp[:, k, :], dr["w_ple_proj"][l, k * 128:(k + 1) * 128, :], stage[n % 2][:], ("stage", n % 2),
                                    ("Wpp", k), eng=("dve" if n % 2 == 0 else "pool"), q=("sp" if n % 2 == 0 else "act"))
                n += 1
            Wpgk = [("Wpg", k) for k in range(8)]
            Wppk = [("Wpp", k) for k in range(2)]
            for ti in range(NTILE):
                b = ti % 2
                rows = slice(ti * 128, (ti + 1) * 128)
                P.dma("sp", I("dma_start", out=h[b][:], in_=dr["hS"][rows, :]), writes=[("h", b)])
                P.dma("act", I("dma_start", out=p32[b][:], in_=dr["p"][l, rows, :]), writes=[("p32", b)])
                P.op("pool", I("tensor_copy", out=pbf[b][:], in_=p32[b][:]), reads=[("p32", b)], writes=[("pbf", b)])
                self.rms_tile(T4[b], b, h[b], ("h", b), ("junk", b), ("ss", b), ("rs", b), hn[b], ("hn", b))
                self.transpose8(hn[b], ("hn", b), xT[b][:, :, :], ("xT", b), ident, eng="dve")
                self.transpose8(pbf[b], ("pbf", b), pT[b][:, :, :], ("pT", b), ident, nblk=2, eng="act")
                self.run_deferred()
                self.defer(self._d2_back, l, last, ti, b, rows, (h, hn, xT, pT, Wpg, Wpp, Wpgk, Wppk, sig, tmp, T4, outt, gfin))
            self.run_deferred()
            P.end_phase()

    def _d2_back(self, l, last, ti, b, rows, tl):
        P, dr, ps = self.P, self.dr, self.ps
        h, hn, xT, pT, Wpg, Wpp, Wpgk, Wppk, sig, tmp, T4, outt, gfin = tl
        if True:
            if True:
                for half in range(2):
                    hs = slice(half * 512, (half + 1) * 512)
                    pg, pp = self.psn(), self.psn()
                    for k in range(8):
                        P.op("pe", I("matmul", ps[pg][:, :], lhsT=xT[b][:, k, :], rhs=Wpg[:, k, hs], start=(k == 0), stop=(k == 7)),
                             reads=[("xT", b)] + Wpgk, writes=[("ps", pg)])
                    for k in range(2):
                        P.op("pe", I("matmul", ps[pp][:, :], lhsT=pT[b][:, k, :], rhs=Wpp[:, k, hs], start=(k == 0), stop=(k == 1)),
                             reads=[("pT", b)] + Wppk, writes=[("ps", pp)])
                    P.op("act", I("activation", out=sig[half][:], in_=ps[pg][:, :], func=AF.Sigmoid), reads=[("ps", pg)], writes=[("sig", half)])
                    P.op("dve", I("tensor_tensor", out=tmp[half][:], in0=ps[pp][:, :], in1=sig[half][:], op=ALU.mult),
                         reads=[("ps", pp), ("sig", half)], writes=[("tmp", half)])
                    P.op("pool", I("tensor_tensor", out=h[b][:, hs], in0=h[b][:, hs], in1=tmp[half][:], op=ALU.add),
                         reads=[("tmp", half), ("h", b)], writes=[("h", b)])
                if not last:
                    P.dma("sp", I("dma_start", out=dr["hS"][rows, :], in_=h[b][:]), reads=[("h", b)], writes=[("hS", ti)])
                else:
                    self.rms_tile(T4[b], b, h[b], ("h", b), ("junk", b), ("ss", b), ("rs", b), None, None) if False else None
                    junk, ss, rs = T4[b]["junk"], T4[b]["ss"], T4[b]["rs"]
                    P.op("act", I("activation", out=junk[:], in_=h[b][:], func=AF.Square, accum_out=ss[:]), reads=[("h", b)], writes=[("junk", b), ("ss", b)])
                    P.op("dve", I("tensor_scalar", out=rs[:], in0=ss[:], scalar1=1.0 / D, scalar2=1e-6, op0=ALU.mult, op1=ALU.add),
                         reads=[("ss", b)], writes=[("rs", b)])
                    P.op("act", I("sqrt", out=rs[:], in_=rs[:]), reads=[("rs", b)], writes=[("rs", b)])
                    P.op("dve", I("reciprocal", out=rs[:], in_=rs[:]), reads=[("rs", b)], writes=[("rs", b)])
                    P.op("dve", I("scalar_tensor_tensor", out=outt[b][:], in0=h[b][:], scalar=rs[:, 0:1], in1=gfin[:], op0=ALU.mult, op1=ALU.mult),
                         reads=[("h", b), ("rs", b), "gfin"], writes=[("outt", b)])
                    P.dma("sp", I("dma_start", out=self.out[rows, :], in_=outt[b][:]), reads=[("outt", b)], writes=[("out", ti)])


def make_in_maps(inputs, cores):
    inp = {k: np.asarray(v) for k, v in inputs.items()}
    sh = _prep_shared(inp)
    maps = []
    for b in cores:
        m = dict(sh)
        m["x"] = np.ascontiguousarray(inp["x"][b])
        m["p"] = np.ascontiguousarray(inp["p"][:, b])
        m["pos"] = np.ascontiguousarray(inp["positions"][b].reshape(1, S).astype(np.int32))
        maps.append(m)
    return maps


_NC_CACHE = {}


def kernel(**inputs):
    if "nc" not in _NC_CACHE:
        _NC_CACHE["nc"] = Builder().build()
    nc = _NC_CACHE["nc"]
    maps = make_in_maps(inputs, list(range(8)))
    res = run_bass_kernel_spmd(nc, maps, core_ids=list(range(8)))
    out = np.stack([np.asarray(r["out"]) for r in res.results], axis=0)
    return out.astype(np.float32)
```
